# Optimizing a Trainium2 kernel written in Bass

```python
import jax, jax.numpy as jnp
from jax import lax
import numpy as np

D_MODEL = 1024
BATCH = 2
SEQ = 8192
DEPTH = 2

HEAD_DIM = 64
MIX_W = D_MODEL // 2
N_BRANCH = 3
POOL_WINDOWS = (2, 4, 8, 16)
POOL_GROUPS = len(POOL_WINDOWS)
POOL_GC = MIX_W // POOL_GROUPS
NSA_HEADS = MIX_W // HEAD_DIM
NSA_KV_GROUPS = 2
NSA_HPG = NSA_HEADS // NSA_KV_GROUPS
CMP_LEN = 32
CMP_STRIDE = 16
CMP_HID = 4 * HEAD_DIM
SEL_LEN = 64
N_SEL = 16
WINDOW = 512
FOX_HEADS = MIX_W // HEAD_DIM
Q_BLOCK = 128
N_EXPERTS = 32
TOP_K = 4
D_FF = D_MODEL
SWIGLU_LIMIT = 7.0
SWIGLU_ALPHA = 1.702
MOE_BLOCK = 256
LN_EPS = 1e-5
NEG_INF = -1e30
SCALE = HEAD_DIM ** -0.5
ALPHA = (2 * DEPTH) ** 0.25
BETA = (8 * DEPTH) ** -0.25
FORGET_BIAS_LO = 1.0
FORGET_BIAS_HI = 6.0

SPLIT_SIZES = (
    MIX_W,
    NSA_HEADS * HEAD_DIM,
    N_BRANCH * 2 * NSA_KV_GROUPS * HEAD_DIM,
    N_BRANCH * NSA_HEADS,
    3 * FOX_HEADS * HEAD_DIM,
    FOX_HEADS,
    N_BRANCH * D_MODEL,
)
SPLIT_IDX = tuple(sum(SPLIT_SIZES[:i + 1]) for i in range(len(SPLIT_SIZES) - 1))
IN_WIDTH = sum(SPLIT_SIZES)
FOX_F_OFFSET = SPLIT_IDX[4]

kernel_name = 'hybrid_pool_nsa_fox_moe_deepnorm'


def layer_norm(x, g, b):
    xf = x.astype(jnp.float32)
    mu = jnp.mean(xf, axis=-1, keepdims=True)
    var = jnp.mean(jnp.square(xf - mu), axis=-1, keepdims=True)
    return ((xf - mu) * lax.rsqrt(var + LN_EPS) * g + b).astype(x.dtype)


def masked_softmax(s, mask):
    s = jnp.where(mask, s.astype(jnp.float32), NEG_INF)
    return jnp.where(mask, jax.nn.softmax(s, axis=-1), 0.0)


def pool_mixer(u, w, b, scale):
    B, T, _ = u.shape
    uf = u.astype(jnp.float32)
    cs = jnp.concatenate([jnp.zeros((B, 1, MIX_W), jnp.float32), jnp.cumsum(uf, axis=1)], axis=1)
    t_idx = jnp.arange(T)
    outs = []
    for gi, win in enumerate(POOL_WINDOWS):
        sl = slice(gi * POOL_GC, (gi + 1) * POOL_GC)
        c = cs[:, :, sl]
        lag = jnp.pad(c, ((0, 0), (win, 0), (0, 0)))[:, :T + 1]
        wsum = c[:, 1:] - lag[:, 1:]
        cnt = jnp.minimum(t_idx + 1, win).astype(jnp.float32)
        outs.append(wsum / cnt[:, None] - uf[:, :, sl])
    d = jnp.stack(outs, axis=2)
    y = jnp.einsum('btgc,gcd->btgd', d, w.astype(jnp.float32)) + b
    return (y.reshape(B, T, MIX_W) * scale).astype(u.dtype)


def compress_blocks(k, pos, w1, b1, w2, b2):
    B, T, G, HD = k.shape
    n_chunk = T // CMP_STRIDE
    r = CMP_LEN // CMP_STRIDE
    nc = n_chunk - r + 1
    ch = k.reshape(B, n_chunk, CMP_STRIDE, G, HD)
    blocks = jnp.concatenate([ch[:, j:j + nc] for j in range(r)], axis=2)
    blocks = blocks + pos[:, None, :]
    flat = blocks.transpose(0, 1, 3, 2, 4).reshape(B, nc, G, CMP_LEN * HD)
    h = jax.nn.gelu(flat @ w1 + b1)
    return h @ w2 + b2


def nsa_attention(q, kv, g, cmp_pos, cmp_w1, cmp_b1, cmp_w2, cmp_b2):
    B, T, _ = q.shape
    G, HPG, HD = NSA_KV_GROUPS, NSA_HPG, HEAD_DIM
    q = q.reshape(B, T, G, HPG, HD)
    kv = kv.reshape(B, T, N_BRANCH, 2, G, HD)
    gates = jax.nn.sigmoid(g.astype(jnp.float32)).astype(q.dtype).reshape(B, T, G, HPG, N_BRANCH)
    kc = compress_blocks(kv[:, :, 0, 0], cmp_pos[0], cmp_w1[0], cmp_b1[0], cmp_w2[0], cmp_b2[0])
    vc = compress_blocks(kv[:, :, 0, 1], cmp_pos[1], cmp_w1[1], cmp_b1[1], cmp_w2[1], cmp_b2[1])
    NC = kc.shape[1]
    NS = T // SEL_LEN
    n_sel = min(N_SEL, NS)
    k_sel = kv[:, :, 1, 0].reshape(B, NS, SEL_LEN, G, HD).transpose(0, 3, 1, 2, 4)
    v_sel = kv[:, :, 1, 1].reshape(B, NS, SEL_LEN, G, HD).transpose(0, 3, 1, 2, 4)
    pad = ((0, 0), (WINDOW, 0), (0, 0), (0, 0))
    k_win = jnp.pad(kv[:, :, 2, 0], pad)
    v_win = jnp.pad(kv[:, :, 2, 1], pad)
    cmp_start = jnp.arange(NC) * CMP_STRIDE
    cmp_end = cmp_start + CMP_LEN - 1
    sel_blk = jnp.arange(NS)
    sel_start = sel_blk * SEL_LEN
    overlap = ((cmp_start[:, None] < sel_start[None, :] + SEL_LEN)
               & (cmp_start[:, None] + CMP_LEN > sel_start[None, :])).astype(jnp.float32)
    bi = jnp.arange(B)[:, None, None, None]
    gi = jnp.arange(G)[None, :, None, None]
    in_blk = jnp.arange(SEL_LEN)
    win_off = jnp.arange(WINDOW + Q_BLOCK) - WINDOW

    def block(i):
        t0 = i * Q_BLOCK
        tq = t0 + jnp.arange(Q_BLOCK)
        qb = lax.dynamic_slice_in_dim(q, t0, Q_BLOCK, axis=1)
        gb = lax.dynamic_slice_in_dim(gates, t0, Q_BLOCK, axis=1)
        s = jnp.einsum('bqghd,bcgd->bgqhc', qb, kc) * SCALE
        m = (cmp_end[None, :] <= tq[:, None])[None, None, :, None, :]
        p_cmp = masked_softmax(s, m)
        o_cmp = jnp.einsum('bgqhc,bcgd->bqghd', p_cmp.astype(vc.dtype), vc)
        imp = jnp.einsum('bgqhc,cs->bgqs', p_cmp, overlap)
        cur = tq // SEL_LEN
        forced = (sel_blk[None, :] == 0) | (sel_blk[None, :] == cur[:, None]) | (sel_blk[None, :] == cur[:, None] - 1)
        future = sel_start[None, :] > tq[:, None]
        score = jnp.where(future, -1.0, jnp.where(forced, 1e6, imp))
        _, idx = lax.top_k(score, n_sel)
        ks = k_sel[bi, gi, idx].reshape(B, G, Q_BLOCK, n_sel * SEL_LEN, HD)
        vs = v_sel[bi, gi, idx].reshape(B, G, Q_BLOCK, n_sel * SEL_LEN, HD)
        kpos = (idx[..., None] * SEL_LEN + in_blk).reshape(B, G, Q_BLOCK, n_sel * SEL_LEN)
        s = jnp.einsum('bqghd,bgqkd->bgqhk', qb, ks) * SCALE
        m = (kpos <= tq[None, None, :, None])[:, :, :, None, :]
        p = masked_softmax(s, m)
        o_slc = jnp.einsum('bgqhk,bgqkd->bqghd', p.astype(vs.dtype), vs)
        kw = lax.dynamic_slice_in_dim(k_win, t0, WINDOW + Q_BLOCK, axis=1)
        vw = lax.dynamic_slice_in_dim(v_win, t0, WINDOW + Q_BLOCK, axis=1)
        wpos = t0 + win_off
        dist = tq[:, None] - wpos[None, :]
        m = ((dist >= 0) & (dist < WINDOW) & (wpos[None, :] >= 0))[None, None, :, None, :]
        s = jnp.einsum('bqghd,bkgd->bgqhk', qb, kw) * SCALE
        p = masked_softmax(s, m)
        o_win = jnp.einsum('bgqhk,bkgd->bqghd', p.astype(vw.dtype), vw)
        return gb[..., 0:1] * o_cmp + gb[..., 1:2] * o_slc + gb[..., 2:3] * o_win

    o = lax.map(block, jnp.arange(T // Q_BLOCK))
    return o.transpose(1, 0, 2, 3, 4, 5).reshape(B, T, NSA_HEADS * HD)


def forgetting_attention(qkv, f_logit):
    B, T, _ = qkv.shape
    qkv = qkv.reshape(B, T, 3, FOX_HEADS, HEAD_DIM)
    q, k, v = qkv[:, :, 0], qkv[:, :, 1], qkv[:, :, 2]
    log_f = jax.nn.log_sigmoid(f_logit.astype(jnp.float32))
    cum = jnp.cumsum(log_f, axis=1).transpose(0, 2, 1)
    kpos = jnp.arange(T)

    def block(i):
        t0 = i * Q_BLOCK
        tq = t0 + jnp.arange(Q_BLOCK)
        qb = lax.dynamic_slice_in_dim(q, t0, Q_BLOCK, axis=1)
        cq = lax.dynamic_slice_in_dim(cum, t0, Q_BLOCK, axis=2)
        s = (jnp.einsum('bqhd,bkhd->bhqk', qb, k).astype(jnp.float32) * SCALE
             + cq[..., None] - cum[:, :, None, :])
        m = (kpos[None, :] <= tq[:, None])[None, None]
        p = masked_softmax(s, m)
        return jnp.einsum('bhqk,bkhd->bqhd', p.astype(v.dtype), v)

    o = lax.map(block, jnp.arange(T // Q_BLOCK))
    return o.transpose(1, 0, 2, 3, 4).reshape(B, T, FOX_HEADS * HEAD_DIM)


def hybrid_mixer(x, w_in, b_in, pool_w, pool_b, pool_scale, cmp_pos, cmp_w1, cmp_b1, cmp_w2, cmp_b2, w_up, w_o):
    B, T, D = x.shape
    z = x @ w_in + b_in
    u_pool, q_nsa, kv_nsa, g_nsa, qkv_fox, f_fox, g_mrg = jnp.split(z, list(SPLIT_IDX), axis=-1)
    y_pool = pool_mixer(u_pool, pool_w, pool_b, pool_scale)
    y_nsa = nsa_attention(q_nsa, kv_nsa, g_nsa, cmp_pos, cmp_w1, cmp_b1, cmp_w2, cmp_b2)
    y_fox = forgetting_attention(qkv_fox, f_fox)
    ys = jnp.stack([y_pool, y_nsa, y_fox], axis=2)
    up = jnp.einsum('btnc,ncd->btnd', ys, w_up)
    gate = jax.nn.sigmoid(g_mrg.astype(jnp.float32)).astype(x.dtype).reshape(B, T, N_BRANCH, D)
    return jnp.sum(gate * up, axis=2) @ w_o


def moe_ffn(x, router_w, router_b, w1, b1, w2, b2):
    B, T, D = x.shape
    N = B * T
    NK = N * TOP_K
    xf = x.reshape(N, D)
    logits = (xf @ router_w + router_b).astype(jnp.float32)
    top_v, top_i = lax.top_k(logits, TOP_K)
    gates = jax.nn.softmax(top_v, axis=-1)
    e_flat = top_i.reshape(NK)
    g_flat = gates.reshape(NK)
    tok = jnp.arange(NK) // TOP_K
    order = jnp.argsort(e_flat)
    e_sorted = e_flat[order]
    counts = jnp.bincount(e_flat, length=N_EXPERTS)
    start = jnp.cumsum(counts) - counts
    padded = (counts + MOE_BLOCK - 1) // MOE_BLOCK * MOE_BLOCK
    pend = jnp.cumsum(padded)
    pstart = pend - padded
    dest = pstart[e_sorted] + (jnp.arange(NK) - start[e_sorted])
    P = ((NK + MOE_BLOCK - 1) // MOE_BLOCK + N_EXPERTS) * MOE_BLOCK
    n_blk = P // MOE_BLOCK
    buf_tok = jnp.full((P,), N, jnp.int32).at[dest].set(tok[order])
    buf_gate = jnp.zeros((P,), jnp.float32).at[dest].set(g_flat[order])
    blk_e = jnp.minimum(jnp.sum(jnp.arange(n_blk)[:, None] * MOE_BLOCK >= pend[None, :], axis=1), N_EXPERTS - 1)
    xpad = jnp.concatenate([xf, jnp.zeros((1, D), xf.dtype)], axis=0)
    xb = xpad[buf_tok].reshape(n_blk, MOE_BLOCK, D)

    def expert_block(args):
        xblk, e = args
        h = xblk @ w1[e] + b1[e]
        gate, upv = jnp.split(h, 2, axis=-1)
        gate = jnp.minimum(gate, SWIGLU_LIMIT)
        upv = jnp.clip(upv, -SWIGLU_LIMIT, SWIGLU_LIMIT)
        act = (upv + 1.0) * (gate * jax.nn.sigmoid(SWIGLU_ALPHA * gate))
        return act @ w2[e] + b2[e]

    yb = lax.map(expert_block, (xb, blk_e)).reshape(P, D)
    y = jax.ops.segment_sum(yb * buf_gate[:, None].astype(yb.dtype), buf_tok, num_segments=N + 1)[:N]
    return y.reshape(B, T, D)


def setup_inputs(seed: int = 0) -> dict:
    key = jax.random.key(seed)
    ks = jax.random.split(key, 24)
    L, D = DEPTH, D_MODEL

    def nrm(k, shape, scale):
        return jax.random.normal(k, shape, jnp.float32) * scale

    b_in = nrm(ks[2], (L, IN_WIDTH), 0.02)
    b_in = b_in.at[:, FOX_F_OFFSET:FOX_F_OFFSET + FOX_HEADS].add(
        jnp.linspace(FORGET_BIAS_LO, FORGET_BIAS_HI, FOX_HEADS))
    return {
        'x': nrm(ks[0], (BATCH, SEQ, D), 1.0),
        'w_in': nrm(ks[1], (L, D, IN_WIDTH), D ** -0.5),
        'b_in': b_in,
        'pool_w': nrm(ks[3], (L, POOL_GROUPS, POOL_GC, POOL_GC), POOL_GC ** -0.5),
        'pool_b': nrm(ks[4], (L, POOL_GROUPS, POOL_GC), 0.02),
        'pool_scale': 1.0 + nrm(ks[5], (L, MIX_W), 0.02),
        'cmp_pos': nrm(ks[6], (L, 2, CMP_LEN, HEAD_DIM), 0.02),
        'cmp_w1': nrm(ks[7], (L, 2, CMP_LEN * HEAD_DIM, CMP_HID), (CMP_LEN * HEAD_DIM) ** -0.5),
        'cmp_b1': nrm(ks[8], (L, 2, CMP_HID), 0.02),
        'cmp_w2': nrm(ks[9], (L, 2, CMP_HID, HEAD_DIM), CMP_HID ** -0.5),
        'cmp_b2': nrm(ks[10], (L, 2, HEAD_DIM), 0.02),
        'w_up': nrm(ks[11], (L, N_BRANCH, MIX_W, D), MIX_W ** -0.5),
        'w_o': nrm(ks[12], (L, D, D), BETA * D ** -0.5),
        'ln1_g': 1.0 + nrm(ks[13], (L, D), 0.02),
        'ln1_b': nrm(ks[14], (L, D), 0.02),
        'router_w': nrm(ks[15], (L, D, N_EXPERTS), D ** -0.5),
        'router_b': nrm(ks[16], (L, N_EXPERTS), 0.01),
        'moe_w1': nrm(ks[17], (L, N_EXPERTS, D, 2 * D_FF), D ** -0.5),
        'moe_b1': nrm(ks[18], (L, N_EXPERTS, 2 * D_FF), 0.02),
        'moe_w2': nrm(ks[19], (L, N_EXPERTS, D_FF, D), BETA * D_FF ** -0.5),
        'moe_b2': nrm(ks[20], (L, N_EXPERTS, D), 0.02),
        'ln2_g': 1.0 + nrm(ks[21], (L, D), 0.02),
        'ln2_b': nrm(ks[22], (L, D), 0.02),
    }


def reference(x, w_in, b_in, pool_w, pool_b, pool_scale, cmp_pos, cmp_w1, cmp_b1, cmp_w2, cmp_b2,
              w_up, w_o, ln1_g, ln1_b, router_w, router_b, moe_w1, moe_b1, moe_w2, moe_b2, ln2_g, ln2_b):
    for l in range(DEPTH):
        h = hybrid_mixer(x, w_in[l], b_in[l], pool_w[l], pool_b[l], pool_scale[l], cmp_pos[l],
                         cmp_w1[l], cmp_b1[l], cmp_w2[l], cmp_b2[l], w_up[l], w_o[l])
        x = layer_norm(ALPHA * x + h, ln1_g[l], ln1_b[l])
        h = moe_ffn(x, router_w[l], router_b[l], moe_w1[l], moe_b1[l], moe_w2[l], moe_b2[l])
        x = layer_norm(ALPHA * x + h, ln2_g[l], ln2_b[l])
    return x
```

```python
import numpy as np
from contextlib import ExitStack
import concourse.bass as bass
import concourse.mybir as mybir
from concourse.bass_utils import run_bass_kernel_spmd

F32 = mybir.dt.float32
BF16 = mybir.dt.bfloat16
AF = mybir.ActivationFunctionType
OP = mybir.AluOpType
AX = mybir.AxisListType

T = 8192
D = 1024
NT = T // 128
SCALE = 0.125
ALPHA = 4.0 ** 0.25
POOL_WINDOWS = (2, 4, 8, 16)
LN_EPS = 1e-5


class Prog:
    NDMA = 12

    def __init__(self, nc, es):
        self.nc = nc
        self.eng = {'pe': nc.tensor, 'act': nc.scalar, 'dve': nc.vector, 'pool': nc.gpsimd, 'sp': nc.sync}
        self.sem = {n: es.enter_context(nc.semaphore("s_" + n)) for n in self.eng}
        self.cnt = {n: 0 for n in self.eng}
        self.dsem = [es.enter_context(nc.semaphore(f"d{i}")) for i in range(self.NDMA)]
        self.dval = [0] * self.NDMA
        self.dnext = 0
        self.seen = {n: {} for n in self.eng}
        self.lastw = {}
        self.readers = {}

    def _wait(self, en, tok):
        if tok is None:
            return
        kind, a, v = tok
        if kind == 'e' and a == en and en == 'pe':
            return
        src = (kind, a)
        if self.seen[en].get(src, 0) >= v:
            return
        s = self.sem[a] if kind == 'e' else self.dsem[a]
        self.eng[en].wait_ge(s, v)
        self.seen[en][src] = v

    def _deps(self, en, r, w):
        for k in r:
            self._wait(en, self.lastw.get(k))
        for k in w:
            self._wait(en, self.lastw.get(k))
            for t in self.readers.get(k, ()):
                self._wait(en, t)

    def _commit(self, tok, r, w):
        for k in r:
            self.readers.setdefault(k, []).append(tok)
        for k in w:
            self.lastw[k] = tok
            self.readers[k] = []

    @staticmethod
    def _psum_excl(r, w):
        pr = [k for k in r if k.startswith(('ps', 'pa', 'ph', 'pr'))]
        if pr:
            r = [k for k in r if k not in pr]
            w = list(w) + pr
        w = ['psb' if k in ('psb0', 'psb1') else k for k in w]
        return r, w

    def I(self, en, fn, r=(), w=()):
        r, w = self._psum_excl(r, w)
        self._deps(en, r, w)
        ins = fn(self.eng[en])
        self.cnt[en] += 1
        ins.then_inc(self.sem[en], 1)
        self._commit(('e', en, self.cnt[en]), r, w)
        return ins

    def D(self, en, fn, r=(), w=()):
        i = self.dnext
        self.dnext = (self.dnext + 1) % self.NDMA
        if self.dval[i] > 0:
            self._wait(en, ('d', i, self.dval[i]))
        self._deps(en, r, w)
        ins = fn(self.eng[en])
        self.dval[i] += 16
        ins.then_inc(self.dsem[i], 16)
        self._commit(('d', i, self.dval[i]), r, w)
        return ins

    def barrier(self):
        for en in self.eng:
            for o in self.eng:
                if o != en and self.cnt[o] > 0:
                    self._wait(en, ('e', o, self.cnt[o]))
            for i in range(self.NDMA):
                if self.dval[i] > 0:
                    self._wait(en, ('d', i, self.dval[i]))
        self.lastw.clear()
        self.readers.clear()

    def finish(self):
        for i in range(self.NDMA):
            if self.dval[i] > 0:
                self._wait('sp', ('d', i, self.dval[i]))
        for o in self.eng:
            if o != 'sp' and self.cnt[o] > 0:
                self._wait('sp', ('e', o, self.cnt[o]))


def _mm(P, out, lhsT, rhs, start, stop, r, w):
    P.I('pe', lambda e: e.matmul(out, lhsT=lhsT, rhs=rhs, start=start, stop=stop), r=r, w=w)


def _consts(P, nc, sb):
    c = {}
    tf = sb("c_tf", [128, 128], F32)
    P.I('pool', lambda e: e.iota(tf[:], pattern=[[1, 128]], base=0, channel_multiplier=-1,
                                 allow_small_or_imprecise_dtypes=True), w=['c_tf'])
    c['ident'] = sb("c_ident", [128, 128], BF16)
    c['tri'] = sb("c_tri", [128, 128], BF16)
    c['atri'] = sb("c_atri", [128, 128], BF16)
    c['trif'] = sb("c_trif", [128, 128], F32)
    c['onesf'] = sb("c_onesf", [128, 128], F32)
    P.I('dve', lambda e: e.tensor_scalar(out=c['ident'][:], in0=tf[:], scalar1=0.0, scalar2=None, op0=OP.is_equal), r=['c_tf'], w=['c_ident'])
    P.I('dve', lambda e: e.tensor_scalar(out=c['tri'][:], in0=tf[:], scalar1=0.0, scalar2=None, op0=OP.is_ge), r=['c_tf'], w=['c_tri'])
    P.I('dve', lambda e: e.tensor_scalar(out=c['atri'][:], in0=tf[:], scalar1=0.0, scalar2=None, op0=OP.is_lt), r=['c_tf'], w=['c_atri'])
    P.I('dve', lambda e: e.tensor_scalar(out=c['trif'][:], in0=tf[:], scalar1=0.0, scalar2=None, op0=OP.is_ge), r=['c_tf'], w=['c_trif'])
    P.I('dve', lambda e: e.memset(c['onesf'][:], 1.0), w=['c_onesf'])
    return c


def _inproj(P, nc, es_outer, xT, wfm_d, bfm, fm_list, wtm_d, ntm, tm_evac, pss, tag, n_tok=T):
    with ExitStack() as es:
        sb = lambda n, s, d: es.enter_context(nc.sbuf_tensor(tag + n, s, d))
        nfm = int(wfm_d.shape[1])
        W = sb("W", [128, 8, nfm], BF16)
        for k in range(8):
            P.D('pool', lambda e, k=k: e.dma_start(out=W[:, k, :], in_=wfm_d[k * 128:(k + 1) * 128, :]), w=[tag + 'W'])
        if ntm:
            WT = sb("WT", [128, 8, ntm], BF16)
            for k in range(8):
                P.D('pool', lambda e, k=k: e.dma_start(out=WT[:, k, :], in_=wtm_d[k * 128:(k + 1) * 128, :]), w=[tag + 'WT'])
        xb = [sb(f"xb{i}", [128, 8, 512], BF16) for i in range(2)]
        xv = xT.rearrange("(k p) t -> p k t", p=128)
        nch = n_tok // 512
        pi = 0
        for tc in range(nch):
            X = xb[tc % 2]
            xk = f"{tag}xb{tc % 2}"
            for k2 in range(2):
                P.D('pool', lambda e, X=X, tc=tc, k2=k2: e.dma_start(out=X[:, 4 * k2:4 * k2 + 4, :], in_=xv[:, 4 * k2:4 * k2 + 4, tc * 512:(tc + 1) * 512]), w=[xk])
            for (c0, M, evac) in fm_list:
                ps, pk = pss[pi % len(pss)]
                pi += 1
                for k in range(8):
                    _mm(P, ps[0:M, 0:512], W[:, k, c0:c0 + M], X[:, k, :], k == 0, k == 7, [tag + 'W', xk], [pk])
                evac(tc, ps, pk)
            if ntm:
                for tt in range(4):
                    ps, pk = pss[pi % len(pss)]
                    pi += 1
                    for k in range(8):
                        _mm(P, ps[:, 0:ntm], X[:, k, tt * 128:(tt + 1) * 128], WT[:, k, :], k == 0, k == 7, [tag + 'WT', xk], [pk])
                    tm_evac(tc * 4 + tt, ps, pk)
    P.barrier()


def build_A():
    nc = bass.Bass("TRN2", target_bir_lowering=False)
    dt = lambda n, s, k="ExternalInput": nc.dram_tensor(n, s, F32, kind=k).ap()
    xT = dt("xT", [D, T])
    wfm1 = dt("wfm1", [D, 256]); bfm1 = dt("bfm1", [128, 2])
    wfm2 = dt("wfm2", [D, 516]); bfm2 = dt("bfm2", [128, 5])
    wtm2 = dt("wtm2", [D, 130]); btm2 = dt("btm2", [128, 130])
    wfm3 = dt("wfm3", [D, 256]); bfm3 = dt("bfm3", [128, 2])
    wtm3 = dt("wtm3", [D, 130]); btm3 = dt("btm3", [128, 130])
    pw_d = dt("pw", [128, 128]); pbs_d = dt("pbs", [128, 2]); cw_d = dt("cw", [128, 4]); fix_d = dt("fix", [128, 16])
    cw1_d = dt("cw1", [128, 32 * 256]); posT_d = dt("posT", [128, 32]); cb1_d = dt("cb1", [128, 4])
    cw2k_d = dt("cw2k", [128, 256]); cb2k_d = dt("cb2k", [128, 1]); cw2v_d = dt("cw2v", [128, 128]); cb2v_d = dt("cb2v", [128, 64])
    yT = dt("yT", [384, T], "ExternalOutput")

    with ExitStack() as es:
        P = Prog(nc, es)
        sbg = lambda n, s, d: es.enter_context(nc.sbuf_tensor(n, s, d))
        PS = [es.enter_context(nc.psum_tensor(f"ps{i}", [128, 512], F32)) for i in range(7)]
        PSB = es.enter_context(nc.psum_tensor("psb", [128, 1024], BF16))
        pk = [f"ps{i}" for i in range(7)]
        C = _consts(P, nc, sbg)
        KCT = sbg("KCT", [128, 512], BF16)
        VC = sbg("VC", [128, 4, 64], BF16)

        with ExitStack() as e1:
            sb = lambda n, s, d: e1.enter_context(nc.sbuf_tensor(n, s, d))
            bf1 = sb("bf1", [128, 2], F32); pwf = sb("pwf", [128, 128], BF16); pbs = sb("pbs_s", [128, 2], F32)
            cw = sb("cw_s", [128, 4], F32); fix = sb("fix_s", [128, 16], F32)
            P.D('sp', lambda e: e.dma_start(out=bf1[:], in_=bfm1), w=['bf1'])
            P.D('pool', lambda e: e.dma_start(out=pwf[:], in_=pw_d), w=['pwf'])
            P.D('sp', lambda e: e.dma_start(out=pbs[:], in_=pbs_d), w=['pbs'])
            P.D('sp', lambda e: e.dma_start(out=cw[:], in_=cw_d), w=['cw'])
            P.D('sp', lambda e: e.dma_start(out=fix[:], in_=fix_d), w=['fix'])
            KVC = sb("KVC", [128, T], BF16)
            UC = [sb(f"UC{i}", [128, 528], F32) for i in range(2)]
            S2 = sb("S2", [128, 528], F32); S4 = sb("S4", [128, 528], F32); S8 = sb("S8", [128, 528], F32); S16 = sb("S16", [128, 528], F32)
            ACC = sb("ACC", [128, 512], F32); DT_ = [sb(f"DTb{i}", [128, 512], BF16) for i in range(2)]
            YP = [sb(f"YP{i}", [128, 512], F32) for i in range(2)]
            P.I('dve', lambda e: e.memset(UC[1][:], 0.0), w=['UC1'])

            def evac_u(tc, ps, pkk):
                U = UC[tc % 2]; Up = UC[(tc + 1) % 2]
                uk, upk = f"UC{tc % 2}", f"UC{(tc + 1) % 2}"
                P.I('act', lambda e: e.activation(out=U[:, 16:528], in_=ps[:, 0:512], func=AF.Identity, bias=bf1[:, 0:1], scale=1.0), r=[pkk, 'bf1'], w=[uk])
                P.I('dve', lambda e: e.tensor_copy(out=U[:, 0:16], in_=Up[:, 512:528]), r=[upk], w=[uk])
                P.I('dve', lambda e: e.tensor_tensor(out=S2[:, 1:528], in0=U[:, 1:528], in1=U[:, 0:527], op=OP.add), r=[uk], w=['S2'])
                P.I('dve', lambda e: e.tensor_tensor(out=S4[:, 3:528], in0=S2[:, 3:528], in1=S2[:, 1:526], op=OP.add), r=['S2'], w=['S4'])
                P.I('dve', lambda e: e.tensor_tensor(out=S8[:, 7:528], in0=S4[:, 7:528], in1=S4[:, 3:524], op=OP.add), r=['S4'], w=['S8'])
                P.I('dve', lambda e: e.tensor_tensor(out=S16[:, 15:528], in0=S8[:, 15:528], in1=S8[:, 7:520], op=OP.add), r=['S8'], w=['S16'])
                P.I('dve', lambda e: e.tensor_scalar(out=ACC[:], in0=S2[:, 16:528], scalar1=cw[:, 0:1], scalar2=None, op0=OP.mult), r=['S2', 'cw'], w=['ACC'])
                for wi, S in enumerate((S4, S8, S16)):
                    P.I('dve', lambda e, S=S, wi=wi: e.scalar_tensor_tensor(out=ACC[:], in0=S[:, 16:528], scalar=cw[:, wi + 1:wi + 2], in1=ACC[:], op0=OP.mult, op1=OP.add),
                        r=['S4', 'S8', 'S16', 'cw', 'ACC'], w=['ACC'])
                if tc == 0:
                    P.I('dve', lambda e: e.tensor_tensor(out=ACC[:, 0:16], in0=ACC[:, 0:16], in1=fix[:], op=OP.mult), r=['ACC', 'fix'], w=['ACC'])
                Dt = DT_[tc % 2]; dk = f"DT{tc % 2}"
                P.I('dve', lambda e: e.tensor_tensor(out=Dt[:], in0=ACC[:], in1=U[:, 16:528], op=OP.subtract), r=['ACC', uk], w=[dk])
                ps2, pk2 = PS[4 + tc % 2], pk[4 + tc % 2]
                _mm(P, ps2[:, 0:512], pwf[:], Dt[:], True, True, ['pwf', dk], [pk2])
                Y = YP[tc % 2]; yk = f"YP{tc % 2}"
                P.I('dve', lambda e: e.tensor_scalar(out=Y[:], in0=ps2[:, 0:512], scalar1=pbs[:, 0:1], scalar2=pbs[:, 1:2], op0=OP.add, op1=OP.mult), r=[pk2, 'pbs'], w=[yk])
                P.D('sp', lambda e: e.dma_start(out=yT[0:128, tc * 512:(tc + 1) * 512], in_=Y[:]), r=[yk])

            def evac_kvc(tc, ps, pkk):
                P.I('act', lambda e: e.activation(out=KVC[:, tc * 512:(tc + 1) * 512], in_=ps[:, 0:512], func=AF.Identity, bias=bf1[:, 1:2], scale=1.0), r=[pkk, 'bf1'], w=['KVC'])

            _inproj(P, nc, e1, xT, wfm1, bf1, [(0, 128, evac_u), (128, 128, evac_kvc)], None, 0, None,
                    [(PS[0], pk[0]), (PS[1], pk[1]), (PS[2], pk[2]), (PS[3], pk[3])], "p1")

            W1 = sb("W1", [128, 32 * 256], BF16)
            for q4 in range(4):
                P.D('pool', lambda e, q4=q4: e.dma_start(out=W1[:, q4 * 2048:(q4 + 1) * 2048], in_=cw1_d[:, q4 * 2048:(q4 + 1) * 2048]), w=['W1'])
            posT = sb("posT_s", [128, 32], BF16); cb1 = sb("cb1_s", [128, 4], F32)
            w2k = sb("w2k", [128, 256], BF16); b2k = sb("b2k", [128, 1], F32); w2v = sb("w2v", [128, 128], BF16); b2v = sb("b2v", [128, 64], F32)
            P.D('pool', lambda e: e.dma_start(out=posT[:], in_=posT_d), w=['posT'])
            P.D('sp', lambda e: e.dma_start(out=cb1[:], in_=cb1_d), w=['cb1'])
            P.D('pool', lambda e: e.dma_start(out=w2k[:], in_=cw2k_d), w=['w2k'])
            P.D('sp', lambda e: e.dma_start(out=b2k[:], in_=cb2k_d), w=['b2k'])
            P.D('pool', lambda e: e.dma_start(out=w2v[:], in_=cw2v_d), w=['w2v'])
            P.D('sp', lambda e: e.dma_start(out=b2v[:], in_=cb2v_d), w=['b2v'])
            HT = [[sb(f"HT{kv}{hf}", [128, 512], BF16) for hf in range(2)] for kv in range(2)]
            XH = sb("XH", [128, 512], F32); T1 = sb("T1c", [128, 512], F32); T2 = sb("T2c", [128, 512], F32); cbt = sb("cbt", [128, 4], F32)
            P.I('dve', lambda e: e.memset(VC[:], 0.0), w=['VC'])
            for kv in range(2):
                po = 64 * kv
                for hf in range(2):
                    ci = kv * 2 + hf
                    ps, pkk = PS[ci % 4], pk[ci % 4]
                    psc, pkc = PS[4 + ci % 2], pk[4 + ci % 2]
                    for l in range(32):
                        wsl = W1[po:po + 64, l * 256 + hf * 128: l * 256 + hf * 128 + 128]
                        _mm(P, psc[:, 0:1], wsl, posT[po:po + 64, l:l + 1], l == 0, l == 31, ['W1', 'posT'], [pkc])
                    P.I('dve', lambda e, ci=ci, psc=psc: e.tensor_tensor(out=cbt[:, ci:ci + 1], in0=psc[:, 0:1], in1=cb1[:, ci:ci + 1], op=OP.add), r=[pkc, 'cb1'], w=['cbt'])
                    for l in range(32):
                        wsl = W1[po:po + 64, l * 256 + hf * 128: l * 256 + hf * 128 + 128]
                        _mm(P, ps[:, 0:511], wsl, KVC[po:po + 64, l:l + 16 * 510 + 1:16], l == 0, l == 31, ['W1', 'KVC'], [pkk])
                    P.I('act', lambda e, ci=ci, ps=ps: e.activation(out=XH[:, 0:511], in_=ps[:, 0:511], func=AF.Identity, bias=cbt[:, ci:ci + 1], scale=1.0), r=[pkk, 'cbt'], w=['XH'])
                    P.I('dve', lambda e: e.tensor_tensor(out=T1[:, 0:511], in0=XH[:, 0:511], in1=XH[:, 0:511], op=OP.mult), r=['XH'], w=['T1'])
                    P.I('dve', lambda e: e.tensor_scalar(out=T1[:, 0:511], in0=T1[:, 0:511], scalar1=0.044715, scalar2=1.0, op0=OP.mult, op1=OP.add), r=['T1'], w=['T1'])
                    P.I('dve', lambda e: e.tensor_tensor(out=T1[:, 0:511], in0=T1[:, 0:511], in1=XH[:, 0:511], op=OP.mult), r=['T1', 'XH'], w=['T1'])
                    P.I('act', lambda e: e.activation(out=T2[:, 0:511], in_=T1[:, 0:511], func=AF.Sigmoid, scale=1.5957691216057308), r=['T1'], w=['T2'])
                    H = HT[kv][hf]
                    P.I('dve', lambda e, H=H: e.memset(H[:, 511:512], 0.0), w=[f'HT{kv}{hf}'])
                    P.I('dve', lambda e, H=H: e.tensor_tensor(out=H[:, 0:511], in0=T2[:, 0:511], in1=XH[:, 0:511], op=OP.mult), r=['T2', 'XH'], w=[f'HT{kv}{hf}'])
            for hf in range(2):
                _mm(P, PS[0][:, 0:512], w2k[:, hf * 128:(hf + 1) * 128], HT[0][hf][:], hf == 0, hf == 1, ['w2k', f'HT0{hf}'], [pk[0]])
            P.I('act', lambda e: e.activation(out=KCT[:], in_=PS[0][:, 0:512], func=AF.Identity, bias=b2k[:, 0:1], scale=1.0), r=[pk[0], 'b2k'], w=['KCT'])
            for ct in range(4):
                m = 128 if ct < 3 else 127
                for hf in range(2):
                    _mm(P, PS[1 + ct % 2][0:m, 0:64], HT[1][hf][:, ct * 128:ct * 128 + m], w2v[:, hf * 64:(hf + 1) * 64], hf == 0, hf == 1, ['w2v', f'HT1{hf}'], [pk[1 + ct % 2]])
                P.I('dve', lambda e, ct=ct, m=m: e.tensor_tensor(out=VC[0:m, ct, :], in0=PS[1 + ct % 2][0:m, 0:64], in1=b2v[0:m, :], op=OP.add), r=[pk[1 + ct % 2], 'b2v'], w=['VC'])
            P.barrier()

        with ExitStack() as e2:
            sb = lambda n, s, d: e2.enter_context(nc.sbuf_tensor(n, s, d))
            bf2 = sb("bf2", [128, 5], F32); bt2 = sb("bt2", [128, 130], F32)
            P.D('sp', lambda e: e.dma_start(out=bf2[:], in_=bfm2), w=['bf2'])
            P.D('sp', lambda e: e.dma_start(out=bt2[:], in_=btm2), w=['bt2'])
            NQ = sb("NQ", [128, T], BF16); NQo = sb("NQo", [128, T], BF16); KS = sb("KS", [128, T], BF16); KW = sb("KW", [128, T], BF16)
            G4 = sb("G4", [4, T], BF16)
            VS = sb("VS", [128, NT, 65], BF16); VW = sb("VW", [128, NT, 65], BF16); GC = sb("GC", [128, NT, 2], F32)
            P.I('pool', lambda e: e.memset(VS[:, :, 64:65], 1.0), w=['VS'])
            P.I('pool', lambda e: e.memset(VW[:, :, 64:65], 1.0), w=['VW'])

            def ev(dst, key, col, func=AF.Identity, M=128):
                def f(tc, ps, pkk):
                    P.I('act', lambda e: e.activation(out=dst[0:M, tc * 512:(tc + 1) * 512], in_=ps[0:M, 0:512], func=func, bias=bf2[0:M, col:col + 1], scale=1.0), r=[pkk, 'bf2'], w=[key])
                return f

            TMB = sb("TMB", [128, 130], F32)

            def tm2(ti, ps, pkk):
                P.I('dve', lambda e: e.tensor_tensor(out=VS[:, ti, 0:64], in0=ps[:, 0:64], in1=bt2[:, 0:64], op=OP.add), r=[pkk, 'bt2'], w=['VS'])
                P.I('dve', lambda e: e.tensor_tensor(out=VW[:, ti, 0:64], in0=ps[:, 64:128], in1=bt2[:, 64:128], op=OP.add), r=[pkk, 'bt2'], w=['VW'])
                P.I('dve', lambda e: e.tensor_tensor(out=TMB[:, 128:130], in0=ps[:, 128:130], in1=bt2[:, 128:130], op=OP.add), r=[pkk, 'bt2'], w=['TMB'])
                P.I('act', lambda e: e.activation(out=GC[:, ti, :], in_=TMB[:, 128:130], func=AF.Sigmoid), r=['TMB'], w=['GC'])

            _inproj(P, nc, e2, xT, wfm2, bf2,
                    [(0, 128, ev(NQ, 'NQ', 0)), (128, 128, ev(NQo, 'NQo', 1)), (256, 128, ev(KS, 'KS', 2)), (384, 128, ev(KW, 'KW', 3)),
                     (512, 4, ev(G4, 'G4', 4, AF.Sigmoid, 4))],
                    wtm2, 130, tm2, [(PS[0], pk[0]), (PS[1], pk[1]), (PS[2], pk[2]), (PS[3], pk[3])], "p2")

            EJ = sb("EJ", [128, NT * 128], BF16); ejf = sb("ejf", [128, 2048], F32)
            for q4 in range(4):
                P.I('pool', lambda e, q4=q4: e.iota(ejf[:], pattern=[[-2, 16], [-1, 2], [0, 64]], base=-32 * q4, channel_multiplier=1,
                                             allow_small_or_imprecise_dtypes=True), w=['ejf'])
                P.I('dve', lambda e, q4=q4: e.tensor_scalar(out=EJ[:, q4 * 2048:(q4 + 1) * 2048], in0=ejf[:], scalar1=0.0, scalar2=None, op0=OP.is_equal), r=['ejf'], w=['EJ'])
            REL = sb("REL", [128, 512], F32)
            P.I('pool', lambda e: e.iota(REL[:], pattern=[[16, 512]], base=31, channel_multiplier=-1, allow_small_or_imprecise_dtypes=True), w=['REL'])
            VV = sb("VV", [128, 254], F32); HP = sb("HP", [128, 1], F32)
            P.I('pool', lambda e: e.iota(VV[:], pattern=[[1, 254]], base=-126, channel_multiplier=0, allow_small_or_imprecise_dtypes=True), w=['VV'])
            P.I('pool', lambda e: e.iota(HP[:], pattern=[[0, 1]], base=0, channel_multiplier=1, allow_small_or_imprecise_dtypes=True), w=['HP'])
            P.I('dve', lambda e: e.tensor_scalar(out=HP[:], in0=HP[:], scalar1=64.0, scalar2=None, op0=OP.is_ge), r=['HP'], w=['HP'])
            P.I('dve', lambda e: e.tensor_scalar(out=VV[:], in0=VV[:], scalar1=HP[:, 0:1], scalar2=None, op0=OP.subtract), r=['VV', 'HP'], w=['VV'])
            KEEP = sb("KEEP", [128, 254], F32); NF = sb("NF", [128, 254], F32); ADD = sb("ADD", [128, 254], F32); TA = sb("TA", [128, 254], F32)
            P.I('dve', lambda e: e.tensor_scalar(out=KEEP[:], in0=VV[:], scalar1=-2.0, scalar2=None, op0=OP.is_le), r=['VV'], w=['KEEP'])
            P.I('dve', lambda e: e.tensor_scalar(out=NF[:], in0=VV[:], scalar1=0.0, scalar2=None, op0=OP.is_le), r=['VV'], w=['NF'])
            P.I('dve', lambda e: e.tensor_scalar(out=ADD[:], in0=VV[:], scalar1=-1.0, scalar2=1.0e6, op0=OP.is_ge, op1=OP.mult), r=['VV'], w=['ADD'])
            P.I('dve', lambda e: e.tensor_scalar(out=TA[:], in0=VV[:], scalar1=0.0, scalar2=-1000001.0, op0=OP.is_gt, op1=OP.mult), r=['VV'], w=['TA'])
            P.I('dve', lambda e: e.tensor_tensor(out=ADD[:], in0=ADD[:], in1=TA[:], op=OP.add), r=['ADD', 'TA'], w=['ADD'])
            SEL = sb("SEL", [4, 256], BF16); self_ = sb("self_", [4, 256], F32)
            P.I('pool', lambda e: e.iota(self_[:], pattern=[[1, 4], [0, 64]], base=0, channel_multiplier=-1, allow_small_or_imprecise_dtypes=True), w=['self_'])
            P.I('dve', lambda e: e.tensor_scalar(out=SEL[:], in0=self_[:], scalar1=0.0, scalar2=None, op0=OP.is_equal), r=['self_'], w=['SEL'])

            WMASK = sb("WMASK", [128, 8 * 512], BF16)
            P.I('dve', lambda e: e.memset(WMASK[:], 0.0), w=['WMASK'])
            for a in range(-4, 4):
                for b in range(4):
                    dst = WMASK[:, (a + 4) * 512 + b * 128:(a + 4) * 512 + (b + 1) * 128]
                    if b == a:
                        P.I('dve', lambda e, dst=dst: e.tensor_copy(out=dst, in_=C['tri'][:]), r=['c_tri'], w=['WMASK'])
                    elif b == a + 4:
                        P.I('dve', lambda e, dst=dst: e.tensor_copy(out=dst, in_=C['atri'][:]), r=['c_atri'], w=['WMASK'])
                    elif a < b < a + 4:
                        P.I('dve', lambda e, dst=dst: e.memset(dst, 1.0), w=['WMASK'])
            EX = [sb(f"EX{i}", [128, 512], F32) for i in range(2)]
            EM = [sb(f"EM{i}", [128, 512], F32) for i in range(2)]
            RS = sb("RS", [128, 8], F32)
            PSP = sb("PSP", [128, 520], F32)
            P.I('dve', lambda e: e.memset(PSP[:], 0.0), w=['PSP'])
            PN = [sb(f"PN{i}", [128, 512], BF16) for i in range(2)]
            PNT = [sb(f"PNT{i}", [128, 512], BF16) for i in range(2)]
            IMP = sb("IMP", [128, 128], F32); SC = sb("SC", [128, 128], F32); SC2 = sb("SC2", [128, 128], F32); M8 = sb("M8", [128, 16], F32)
            SELM = sb("SELM", [128, 128], F32); MB = sb("MB", [128, 128], BF16)
            MBT = [sb(f"MBT{i}", [128, 512], BF16) for i in range(2)]
            OCc = [[sb(f"OC{i}{h}", [64, 512], F32) for h in range(2)] for i in range(2)]
            PT = [sb(f"PT{i}", [128, 512], BF16) for i in range(3)]
            RR = sb("RR", [65, 512], F32); BCS = sb("BCS", [64, 512], F32); BGS = sb("BGS", [64, 512], F32)
            TY = sb("TY", [64, 512], F32); YN = [sb(f"YN{i}", [64, 512], F32) for i in range(2)]
            ptc = 0

            for qc in range(T // 512):
                par = qc % 2
                mbk = f"MBT{par}"
                for b in range(4):
                    i = 4 * qc + b
                    for hh in range(4):
                        Q = NQ if hh < 2 else NQo
                        qk_ = 'NQ' if hh < 2 else 'NQo'
                        po = 64 * (hh % 2)
                        ps, pkk = PS[hh % 2], pk[hh % 2]
                        _mm(P, ps[:, 0:512], Q[po:po + 64, i * 128:(i + 1) * 128], KCT[po:po + 64, :], True, True, [qk_, 'KCT'], [pkk])
                        ex, exk = EX[hh % 2], f"EX{hh % 2}"
                        em, emk = EM[hh % 2], f"EM{hh % 2}"
                        P.I('act', lambda e, ex=ex, ps=ps: e.activation(out=ex[:], in_=ps[:, 0:512], func=AF.Exp, scale=SCALE), r=[pkk], w=[exk])
                        P.I('dve', lambda e, ex=ex, em=em, i=i, hh=hh: e.scalar_tensor_tensor(out=em[:], in0=REL[:], scalar=float(128 * i), in1=ex[:], op0=OP.is_le, op1=OP.mult),
                            r=['REL', exk], w=[emk])
                        P.I('dve', lambda e, em=em, hh=hh: e.reduce_sum(out=RS[:, hh:hh + 1], in_=em[:], axis=AX.X), r=[emk], w=['RS'])
                        P.I('dve', lambda e, hh=hh: e.tensor_scalar(out=RS[:, hh:hh + 1], in0=RS[:, hh:hh + 1], scalar1=1e-30, scalar2=None, op0=OP.max), r=['RS'], w=['RS'])
                        P.I('dve', lambda e, hh=hh: e.reciprocal(out=RS[:, hh:hh + 1], in_=RS[:, hh:hh + 1]), r=['RS'], w=['RS'])
                        if hh == 0:
                            P.I('dve', lambda e, em=em, hh=hh: e.tensor_scalar(out=PSP[:, 1:513], in0=em[:], scalar1=RS[:, hh:hh + 1], scalar2=None, op0=OP.mult), r=[emk, 'RS'], w=['PSP'])
                        else:
                            P.I('dve', lambda e, em=em, hh=hh: e.scalar_tensor_tensor(out=PSP[:, 1:513], in0=em[:], scalar=RS[:, hh:hh + 1], in1=PSP[:, 1:513], op0=OP.mult, op1=OP.add),
                                r=[emk, 'RS', 'PSP'], w=['PSP'])
                        if hh < 2:
                            P.I('dve', lambda e, hh=hh, i=i: e.tensor_tensor(out=RS[:, 4 + hh:5 + hh], in0=RS[:, hh:hh + 1], in1=GC[:, i, hh:hh + 1], op=OP.mult), r=['RS', 'GC'], w=['RS'])
                            pn, pnk = PN[hh], f"PN{hh}"
                            P.I('pool', lambda e, pn=pn, em=em, hh=hh: e.tensor_scalar(out=pn[:], in0=em[:], scalar1=RS[:, 4 + hh:5 + hh], scalar2=None, op0=OP.mult), r=[emk, 'RS'], w=[pnk])
                            for ct in range(4):
                                P.I('pe', lambda e, pn=pn, ct=ct, hh=hh: e.transpose(out=PSB[:, hh * 512 + ct * 128: hh * 512 + (ct + 1) * 128], in_=pn[:, ct * 128:(ct + 1) * 128], identity=C['ident'][:]),
                                    r=[pnk, 'c_ident'], w=[f'psb{hh}'])
                            pnt, pntk = PNT[hh], f"PNT{hh}"
                            P.I('act', lambda e, pnt=pnt, hh=hh: e.copy(out=pnt[:], in_=PSB[:, hh * 512:(hh + 1) * 512]), r=[f'psb{hh}'], w=[pntk])
                            po_, pok = PS[2 + hh], pk[2 + hh]
                            for ct in range(4):
                                _mm(P, po_[0:64, b * 128:(b + 1) * 128], VC[:, ct, :], pnt[:, ct * 128:(ct + 1) * 128], ct == 0, ct == 3, ['VC', pntk], [pok])
                    P.I('dve', lambda e: e.tensor_reduce(out=IMP[:], in_=PSP[:, 0:512].rearrange("p (s m) -> p s m", m=4), axis=AX.X, op=OP.add), r=['PSP'], w=['IMP'])
                    P.I('dve', lambda e: e.tensor_tensor(out=IMP[:], in0=IMP[:], in1=PSP[:, 4:516:4], op=OP.add), r=['PSP', 'IMP'], w=['IMP'])
                    x0 = 126 - 2 * i
                    P.I('dve', lambda e, x0=x0: e.tensor_tensor(out=SC[:], in0=IMP[:], in1=KEEP[:, x0:x0 + 128], op=OP.mult), r=['IMP', 'KEEP'], w=['SC'])
                    P.I('dve', lambda e, x0=x0: e.tensor_tensor(out=SC[:], in0=SC[:], in1=ADD[:, x0:x0 + 128], op=OP.add), r=['SC', 'ADD'], w=['SC'])
                    P.I('dve', lambda e: e.memset(SC[:, 0:1], 1.0e6), r=['SC'], w=['SC'])
                    P.I('dve', lambda e: e.max(out=M8[:, 0:8], in_=SC[:]), r=['SC'], w=['M8'])
                    P.I('dve', lambda e: e.match_replace(out=SC2[:], in_to_replace=M8[:, 0:8], in_values=SC[:], imm_value=-2.0), r=['SC', 'M8'], w=['SC2'])
                    P.I('dve', lambda e: e.max(out=M8[:, 8:16], in_=SC2[:]), r=['SC2'], w=['M8'])
                    P.I('dve', lambda e, x0=x0: e.scalar_tensor_tensor(out=SELM[:], in0=SC[:], scalar=M8[:, 15:16], in1=NF[:, x0:x0 + 128], op0=OP.is_ge, op1=OP.mult), r=['SC', 'M8', 'NF'], w=['SELM'])
                    P.I('dve', lambda e: e.tensor_scalar(out=MB[:], in0=SELM[:], scalar1=-1.0, scalar2=30000.0, op0=OP.add, op1=OP.mult), r=['SELM'], w=['MB'])
                    P.I('pe', lambda e: e.transpose(out=PSB[:, 0:128], in_=MB[:], identity=C['ident'][:]), r=['MB', 'c_ident'], w=['psb0'])
                    P.I('act', lambda e, b=b, par=par: e.copy(out=MBT[par][:, b * 128:(b + 1) * 128], in_=PSB[:, 0:128]), r=['psb0'], w=[mbk])
                for h in range(2):
                    P.I('act', lambda e, h=h, par=par: e.copy(out=OCc[par][h][:], in_=PS[2 + h][0:64, 0:512]), r=[pk[2 + h]], w=[f'OC{par}{h}'])

                for h in range(2):
                    po = 64 * h
                    pso, psok = PS[4], pk[4]
                    psw, pswk = PS[5], pk[5]
                    nj = 4 * qc + 4
                    for j in range(nj):
                        a = j - 4 * qc
                        c0 = 128 * max(a, 0)
                        pst, pstk = PS[j % 2], pk[j % 2]
                        _mm(P, pst[:, c0:512], KS[po:po + 64, j * 128:(j + 1) * 128], NQ[po:po + 64, qc * 512 + c0:(qc + 1) * 512], True, False, ['KS', 'NQ'], [pstk])
                        _mm(P, pst[:, c0:512], EJ[:, j * 128:(j + 1) * 128], MBT[par][:, c0:512], False, True, ['EJ', mbk], [pstk])
                        pt, ptk = PT[ptc % 3], f"PT{ptc % 3}"
                        ptc += 1
                        P.I('act', lambda e, pt=pt, pst=pst, c0=c0: e.activation(out=pt[:, c0:512], in_=pst[:, c0:512], func=AF.Exp, scale=SCALE), r=[pstk], w=[ptk])
                        if a >= 0:
                            P.I('dve', lambda e, pt=pt, c0=c0: e.tensor_tensor(out=pt[:, c0:c0 + 128], in0=pt[:, c0:c0 + 128], in1=C['tri'][:], op=OP.mult), r=[ptk, 'c_tri'], w=[ptk])
                        _mm(P, pso[0:65, c0:512], VS[:, j, :], pt[:, c0:512], j == 0, j == nj - 1, ['VS', ptk], [psok])
                    for a in range(-4, 4):
                        j = 4 * qc + a
                        if j < 0:
                            continue
                        pst, pstk = PS[j % 2], pk[j % 2]
                        _mm(P, pst[:, 0:512], KW[po:po + 64, j * 128:(j + 1) * 128], NQ[po:po + 64, qc * 512:(qc + 1) * 512], True, True, ['KW', 'NQ'], [pstk])
                        pt, ptk = PT[ptc % 3], f"PT{ptc % 3}"
                        ptc += 1
                        P.I('act', lambda e, pt=pt, pst=pst: e.activation(out=pt[:, 0:512], in_=pst[:, 0:512], func=AF.Exp, scale=SCALE), r=[pstk], w=[ptk])
                        P.I('dve', lambda e, pt=pt, a=a: e.tensor_tensor(out=pt[:, 0:512], in0=pt[:, 0:512], in1=WMASK[:, (a + 4) * 512:(a + 5) * 512], op=OP.mult), r=[ptk, 'WMASK'], w=[ptk])
                        jfirst = max(4 * qc - 4, 0)
                        _mm(P, psw[0:65, 0:512], VW[:, j, :], pt[:, 0:512], j == jfirst, a == 3, ['VW', ptk], [pswk])
                    Y = YN[h]; yk = f"YN{h}"
                    for br, (pacc, pacck) in enumerate(((pso, psok), (psw, pswk))):
                        P.I('dve', lambda e, pacc=pacc: e.reciprocal(out=RR[64:65, :], in_=pacc[64:65, 0:512]), r=[pacck], w=['RR'])
                        _mm(P, PS[6][0:64, 0:512], C['onesf'][64:65, 0:64], RR[64:65, :], True, True, ['c_onesf', 'RR'], [pk[6]])
                        P.I('act', lambda e: e.copy(out=BCS[:], in_=PS[6][0:64, 0:512]), r=[pk[6]], w=['BCS'])
                        gi = 2 * h + br
                        _mm(P, PS[6][0:64, 0:512], SEL[0:4, gi * 64:(gi + 1) * 64], G4[0:4, qc * 512:(qc + 1) * 512], True, True, ['SEL', 'G4'], [pk[6]])
                        P.I('act', lambda e: e.copy(out=BGS[:], in_=PS[6][0:64, 0:512]), r=[pk[6]], w=['BGS'])
                        P.I('dve', lambda e, pacc=pacc: e.tensor_tensor(out=TY[:], in0=pacc[0:64, 0:512], in1=BCS[:], op=OP.mult), r=[pacck, 'BCS'], w=['TY'])
                        P.I('dve', lambda e: e.tensor_tensor(out=TY[:], in0=TY[:], in1=BGS[:], op=OP.mult), r=['TY', 'BGS'], w=['TY'])
                        src = OCc[par][h] if br == 0 else Y
                        srck = f'OC{par}{h}' if br == 0 else yk
                        P.I('dve', lambda e, src=src, Y=Y: e.tensor_tensor(out=Y[:], in0=TY[:], in1=src[:], op=OP.add), r=['TY', srck], w=[yk])
                    P.D('sp', lambda e, Y=Y, h=h, qc=qc: e.dma_start(out=yT[128 + 64 * h:192 + 64 * h, qc * 512:(qc + 1) * 512], in_=Y[:]), r=[yk])
            P.barrier()

        with ExitStack() as e3:
            sb = lambda n, s, d: e3.enter_context(nc.sbuf_tensor(n, s, d))
            bf3 = sb("bf3", [128, 2], F32); bt3 = sb("bt3", [128, 130], F32)
            P.D('sp', lambda e: e.dma_start(out=bf3[:], in_=bfm3), w=['bf3'])
            P.D('sp', lambda e: e.dma_start(out=bt3[:], in_=btm3), w=['bt3'])
            FQ = sb("FQ", [128, T], BF16); FK = sb("FK", [128, T], BF16)
            FV = sb("FV", [128, NT, 2, 65], BF16); LF = sb("LF", [128, NT, 2], F32)
            P.I('pool', lambda e: e.memset(FV[:, :, :, 64:65], 1.0), w=['FV'])

            def ev3(dst, key, col):
                def f(tc, ps, pkk):
                    P.I('act', lambda e: e.activation(out=dst[:, tc * 512:(tc + 1) * 512], in_=ps[:, 0:512], func=AF.Identity, bias=bf3[:, col:col + 1], scale=1.0), r=[pkk, 'bf3'], w=[key])
                return f

            def tm3(ti, ps, pkk):
                P.I('dve', lambda e: e.tensor_tensor(out=FV[:, ti, :, 0:64], in0=ps[:, 0:128].rearrange("p (h d) -> p h d", d=64), in1=bt3[:, 0:128].rearrange("p (h d) -> p h d", d=64), op=OP.add),
                    r=[pkk, 'bt3'], w=['FV'])
                P.I('dve', lambda e: e.tensor_tensor(out=LF[:, ti, :], in0=ps[:, 128:130], in1=bt3[:, 128:130], op=OP.add), r=[pkk, 'bt3'], w=['LF'])

            _inproj(P, nc, e3, xT, wfm3, bf3, [(0, 128, ev3(FQ, 'FQ', 0)), (128, 128, ev3(FK, 'FK', 1))],
                    wtm3, 130, tm3, [(PS[0], pk[0]), (PS[1], pk[1]), (PS[2], pk[2]), (PS[3], pk[3])], "p3")
            LFv = LF[:].rearrange("p j h -> p (j h)")
            P.I('act', lambda e: e.activation(out=LFv, in_=LFv, func=AF.Exp, scale=-1.0), r=['LF'], w=['LF'])
            P.I('dve', lambda e: e.tensor_scalar(out=LFv, in0=LFv, scalar1=1.0, scalar2=None, op0=OP.add), r=['LF'], w=['LF'])
            P.I('act', lambda e: e.activation(out=LFv, in_=LFv, func=AF.Ln), r=['LF'], w=['LF'])
            P.I('dve', lambda e: e.tensor_scalar(out=LFv, in0=LFv, scalar1=-1.0, scalar2=None, op0=OP.mult), r=['LF'], w=['LF'])
            _mm(P, PS[0][:, 0:128], C['trif'][:], LFv, True, True, ['c_trif', 'LF'], [pk[0]])
            _mm(P, PS[1][:, 0:128], C['onesf'][:], LFv, True, True, ['c_onesf', 'LF'], [pk[1]])
            TOT = sb("TOT", [128, NT, 2], F32); INCL = sb("INCL", [128, NT, 2], F32); CUM = sb("CUM", [128, NT, 2], F32); ONE64 = sb("ONE64", [128, NT], F32)
            P.I('dve', lambda e: e.memset(ONE64[:], 1.0), w=['ONE64'])
            P.I('act', lambda e: e.copy(out=TOT[:].rearrange("p j h -> p (j h)"), in_=PS[1][:, 0:128]), r=[pk[1]], w=['TOT'])
            for h in range(2):
                P.I('dve', lambda e, h=h: e.tensor_tensor_scan(out=INCL[:, :, h], data0=ONE64[:], data1=TOT[:, :, h], initial=0.0, op0=OP.mult, op1=OP.add), r=['TOT', 'ONE64'], w=['INCL'])
            P.I('dve', lambda e: e.tensor_tensor(out=CUM[:].rearrange("p j h -> p (j h)"), in0=PS[0][:, 0:128], in1=INCL[:].rearrange("p j h -> p (j h)"), op=OP.add), r=[pk[0], 'INCL'], w=['CUM'])
            P.I('dve', lambda e: e.tensor_tensor(out=CUM[:], in0=CUM[:], in1=TOT[:], op=OP.subtract), r=['CUM', 'TOT'], w=['CUM'])

            BI = [sb(f"BI{i}", [128, 4, NT], F32) for i in range(2)]
            PT = [sb(f"FPT{i}", [128, 512], BF16) for i in range(3)]
            RR = sb("FRR", [65, 512], F32); BCS = sb("FBCS", [64, 512], F32); YF = [sb(f"YF{i}", [64, 512], F32) for i in range(2)]
            ptc = 0
            it = 0
            for h in range(2):
                po = 64 * h
                for qc in range(T // 512):
                    bi, bik = BI[it % 2], f"BI{it % 2}"
                    pso, psok = PS[4 + it % 2], pk[4 + it % 2]
                    for b in range(4):
                        i = 4 * qc + b
                        P.I('dve', lambda e, bi=bi, b=b, i=i, h=h: e.tensor_scalar(out=bi[:, b, 0:i + 1], in0=CUM[:, 0:i + 1, h], scalar1=-1.0, scalar2=INCL[:, i, h:h + 1], op0=OP.mult, op1=OP.add),
                            r=['CUM', 'INCL'], w=[bik])
                    nj = 4 * qc + 4
                    for j in range(nj):
                        a = j - 4 * qc
                        b0 = max(a, 0)
                        c0 = 128 * b0
                        pst, pstk = PS[j % 4], pk[j % 4]
                        _mm(P, pst[:, c0:512], FK[po:po + 64, j * 128:(j + 1) * 128], FQ[po:po + 64, qc * 512 + c0:(qc + 1) * 512], True, True, ['FK', 'FQ'], [pstk])
                        pt, ptk = PT[ptc % 3], f"FPT{ptc % 3}"
                        ptc += 1
                        for b in range(b0, 4):
                            P.I('act', lambda e, pt=pt, pst=pst, b=b, bi=bi, j=j: e.activation(out=pt[:, b * 128:(b + 1) * 128], in_=pst[:, b * 128:(b + 1) * 128], func=AF.Exp, bias=bi[:, b, j:j + 1], scale=SCALE),
                                r=[pstk, bik], w=[ptk])
                        if a >= 0:
                            P.I('dve', lambda e, pt=pt, c0=c0: e.tensor_tensor(out=pt[:, c0:c0 + 128], in0=pt[:, c0:c0 + 128], in1=C['tri'][:], op=OP.mult), r=[ptk, 'c_tri'], w=[ptk])
                        _mm(P, pso[0:65, c0:512], FV[:, j, h, :], pt[:, c0:512], j == 0, j == nj - 1, ['FV', ptk], [psok])
                    P.I('dve', lambda e, pso=pso: e.reciprocal(out=RR[64:65, :], in_=pso[64:65, 0:512]), r=[psok], w=['FRR'])
                    _mm(P, PS[6][0:64, 0:512], C['onesf'][64:65, 0:64], RR[64:65, :], True, True, ['c_onesf', 'FRR'], [pk[6]])
                    P.I('act', lambda e: e.copy(out=BCS[:], in_=PS[6][0:64, 0:512]), r=[pk[6]], w=['FBCS'])
                    Y = YF[it % 2]; yk = f"YF{it % 2}"
                    P.I('dve', lambda e, pso=pso, Y=Y: e.tensor_tensor(out=Y[:], in0=pso[0:64, 0:512], in1=BCS[:], op=OP.mult), r=[psok, 'FBCS'], w=[yk])
                    P.D('sp', lambda e, Y=Y, h=h, qc=qc: e.dma_start(out=yT[256 + 64 * h:320 + 64 * h, qc * 512:(qc + 1) * 512], in_=Y[:]), r=[yk])
                    it += 1
        P.finish()
    return nc


def _prep_A(inp, l, b, j, x_b):
    g = j // 2
    own = [2 * j, 2 * j + 1]
    oth = [2 * j + 2, 2 * j + 3] if j % 2 == 0 else [2 * j - 2, 2 * j - 1]
    w_in = inp['w_in'][l]; b_in = inp['b_in'][l]
    OQ, OKV, OG, OFX, OF = 512, 1024, 1792, 1816, 3352
    r64 = np.arange(64)
    kvc = lambda n, kvi: OKV + ((n * 2 + kvi) * 2 + g) * 64 + r64
    fx = lambda q, h: OFX + (q * 8 + h) * 64 + r64
    fm1 = np.concatenate([128 * j + np.arange(128), kvc(0, 0), kvc(0, 1)])
    fm2 = np.concatenate([OQ + 64 * own[0] + r64, OQ + 64 * own[1] + r64, OQ + 64 * oth[0] + r64, OQ + 64 * oth[1] + r64,
                          kvc(1, 0), kvc(1, 0), kvc(2, 0), kvc(2, 0),
                          [OG + own[0] * 3 + 1, OG + own[0] * 3 + 2, OG + own[1] * 3 + 1, OG + own[1] * 3 + 2]]).astype(np.int64)
    tm2 = np.concatenate([kvc(1, 1), kvc(2, 1), [OG + own[0] * 3, OG + own[1] * 3]]).astype(np.int64)
    fm3 = np.concatenate([fx(0, own[0]), fx(0, own[1]), fx(1, own[0]), fx(1, own[1])])
    tm3 = np.concatenate([fx(2, own[0]), fx(2, own[1]), [OF + own[0], OF + own[1]]]).astype(np.int64)

    def fmb(cols, nch):
        bb = np.zeros((128, nch), np.float32)
        v = b_in[cols]
        for c in range(nch):
            seg = v[c * 128:(c + 1) * 128]
            bb[:len(seg), c] = seg
        return bb
    c32 = np.ascontiguousarray
    win = POOL_WINDOWS[j]
    cwv = np.zeros((128, 4), np.float32); cwv[:, j] = 1.0 / win
    fixv = np.tile((win / np.minimum(np.arange(16) + 1, win)).astype(np.float32)[None, :], (128, 1))
    cw1 = inp['cmp_w1'][l]
    cw1r = np.concatenate([cw1[kv].reshape(32, 64, 256).transpose(1, 0, 2).reshape(64, 32 * 256) for kv in range(2)], axis=0)
    posT = np.concatenate([inp['cmp_pos'][l][kv].T for kv in range(2)], axis=0)
    cb1 = inp['cmp_b1'][l].reshape(2, 2, 128).transpose(2, 0, 1).reshape(128, 4)
    w2 = inp['cmp_w2'][l]
    cw2k = np.concatenate([np.concatenate([w2[0][hf * 128:(hf + 1) * 128], w2[0][hf * 128:(hf + 1) * 128]], axis=1) for hf in range(2)], axis=1)
    cb2k = np.concatenate([inp['cmp_b2'][l][0], inp['cmp_b2'][l][0]])[:, None]
    cw2v = np.concatenate([w2[1][hf * 128:(hf + 1) * 128] for hf in range(2)], axis=1)
    cb2v = np.tile(inp['cmp_b2'][l][1][None, :], (128, 1))
    return {
        "xT": x_b,
        "wfm1": c32(w_in[:, fm1]), "bfm1": fmb(fm1, 2),
        "wfm2": c32(w_in[:, fm2]), "bfm2": fmb(fm2, 5),
        "wtm2": c32(w_in[:, tm2]), "btm2": c32(np.tile(b_in[tm2][None, :], (128, 1))),
        "wfm3": c32(w_in[:, fm3]), "bfm3": fmb(fm3, 2),
        "wtm3": c32(w_in[:, tm3]), "btm3": c32(np.tile(b_in[tm3][None, :], (128, 1))),
        "pw": c32(inp['pool_w'][l][j]), "pbs": c32(np.stack([inp['pool_b'][l][j], inp['pool_scale'][l][128 * j:128 * j + 128]], axis=1)),
        "cw": cwv, "fix": c32(fixv),
        "cw1": c32(cw1r), "posT": c32(posT), "cb1": c32(cb1), "cw2k": c32(cw2k), "cb2k": c32(cb2k.astype(np.float32)),
        "cw2v": c32(cw2v), "cb2v": c32(cb2v),
    }


_NC = {}


def run_A(inp, l, x):
    if 'A' not in _NC:
        _NC['A'] = build_A()
    xTs = [np.ascontiguousarray(x[b].T) for b in range(2)]
    maps = [_prep_A(inp, l, c // 4, c % 4, xTs[c // 4]) for c in range(8)]
    res = run_bass_kernel_spmd(_NC['A'], maps, core_ids=list(range(8)))
    ys = np.empty((2, T, 1536), np.float32)
    for c in range(8):
        b, j = c // 4, c % 4
        yt = res.results[c]["yT"]
        for n in range(3):
            ys[b, :, n * 512 + 128 * j: n * 512 + 128 * j + 128] = yt[n * 128:(n + 1) * 128, :].T
    return ys


NTB = 2048
NE = 32


def _layernorm(P, nc, R, rk, g_t, b_t, out, outk, ST, MV, tag):
    for c in range(2):
        P.I('dve', lambda e, c=c: e.bn_stats(out=ST[:, c, :], in_=R[:, c * 512:(c + 1) * 512]), r=[rk], w=['ST' + tag])
    P.I('dve', lambda e: e.bn_aggr(out=MV[:, 0:2], in_=ST[:].rearrange("p c s -> p (c s)")), r=['ST' + tag], w=['MV' + tag])
    P.I('dve', lambda e: e.tensor_scalar(out=MV[:, 2:3], in0=MV[:, 1:2], scalar1=LN_EPS, scalar2=None, op0=OP.add), r=['MV' + tag], w=['MV' + tag])
    P.I('act', lambda e: e.activation(out=MV[:, 2:3], in_=MV[:, 2:3], func=AF.Sqrt), r=['MV' + tag], w=['MV' + tag])
    P.I('dve', lambda e: e.reciprocal(out=MV[:, 2:3], in_=MV[:, 2:3]), r=['MV' + tag], w=['MV' + tag])
    P.I('dve', lambda e: e.tensor_scalar(out=out, in0=R[:], scalar1=MV[:, 0:1], scalar2=MV[:, 2:3], op0=OP.subtract, op1=OP.mult), r=[rk, 'MV' + tag], w=[outk])
    P.I('dve', lambda e: e.tensor_tensor(out=out, in0=out, in1=g_t[:], op=OP.mult), r=[outk, 'lng' + tag], w=[outk])
    P.I('dve', lambda e: e.tensor_tensor(out=out, in0=out, in1=b_t[:], op=OP.add), r=[outk, 'lnb' + tag], w=[outk])


def build_B():
    nc = bass.Bass("TRN2", target_bir_lowering=False)
    dt = lambda n, s, k="ExternalInput": nc.dram_tensor(n, s, F32, kind=k).ap()
    xT = dt("xT", [D, NTB]); xtok = dt("xtok", [NTB, D]); yT = dt("yT", [1536, NTB])
    wg_d = dt("wg", [D, 3072]); bg_d = dt("bg", [128, 24]); wup_d = dt("wup", [1536, D]); wo_d = dt("wo", [D, D])
    l1g_d = dt("l1g", [128, D]); l1b_d = dt("l1b", [128, D]); l2g_d = dt("l2g", [128, D]); l2b_d = dt("l2b", [128, D])
    rw_d = dt("rw", [D, NE]); rb_d = dt("rb", [128, NE])
    w1_d = dt("w1", [NE, D, 2048]); b1_d = dt("b1", [128, NE * 16]); w2_d = dt("w2", [NE, D, D]); b2_d = dt("b2", [NE, D])
    xo = dt("xo", [NTB, D], "ExternalOutput")
    x1s = dt("x1s", [NTB, D], "Internal")
    NTT = NTB // 128

    with ExitStack() as es:
        P = Prog(nc, es)
        sbg = lambda n, s, d: es.enter_context(nc.sbuf_tensor(n, s, d))
        PA = [es.enter_context(nc.psum_tensor(f"pa{i}", [128, 512], F32)) for i in range(4)]
        pak = [f"pa{i}" for i in range(4)]
        PH = es.enter_context(nc.psum_tensor("ph", [128, 1024], F32))
        PSB = es.enter_context(nc.psum_tensor("psb", [128, 1024], BF16))
        PR = es.enter_context(nc.psum_tensor("pr", [128, 512], F32))
        C = _consts(P, nc, sbg)
        identf = sbg("identf", [128, 128], F32)
        P.I('dve', lambda e: e.tensor_copy(out=identf[:], in_=C['ident'][:]), r=['c_ident'], w=['identf'])
        X1T = sbg("X1T", [128, 8, NTB], BF16)
        GT = sbg("GT", [128, NTT, NE], F32)
        ST = sbg("ST", [128, 2, 6], F32); MV = sbg("MV", [128, 4], F32)

        with ExitStack() as e1:
            sb = lambda n, s, d: e1.enter_context(nc.sbuf_tensor(n, s, d))
            WG = sb("WG", [128, 8, 3072], BF16); WU = sb("WU", [128, 12, D], BF16); WO = sb("WO", [128, 8, D], BF16)
            for k in range(8):
                P.D('pool', lambda e, k=k: e.dma_start(out=WG[:, k, :], in_=wg_d[k * 128:(k + 1) * 128, :]), w=['WG'])
                P.D('pool', lambda e, k=k: e.dma_start(out=WO[:, k, :], in_=wo_d[k * 128:(k + 1) * 128, :]), w=['WO'])
            for k in range(12):
                P.D('pool', lambda e, k=k: e.dma_start(out=WU[:, k, :], in_=wup_d[k * 128:(k + 1) * 128, :]), w=['WU'])
            bg = sb("bg_s", [128, 24], F32); l1g = sb("l1g_s", [128, D], F32); l1b = sb("l1b_s", [128, D], F32)
            RW = sb("RW", [128, 8, NE], F32); rb = sb("rb_s", [128, NE], F32)
            P.D('sp', lambda e: e.dma_start(out=bg[:], in_=bg_d), w=['bg'])
            P.D('sp', lambda e: e.dma_start(out=l1g[:], in_=l1g_d), w=['lng1'])
            P.D('sp', lambda e: e.dma_start(out=l1b[:], in_=l1b_d), w=['lnb1'])
            P.D('sp', lambda e: e.dma_start(out=RW[:], in_=rw_d.rearrange("(k p) n -> p k n", p=128)), w=['RW'])
            P.D('sp', lambda e: e.dma_start(out=rb[:], in_=rb_d), w=['rb'])
            XB = [sb(f"XB{i}", [128, 8, 512], BF16) for i in range(2)]
            YB = [sb(f"YB{i}", [128, 12, 512], BF16) for i in range(1)]
            GS = sb("GS", [128, 512], F32); TM = sb("TM", [128, 512], F32); MA = sb("MA", [128, 512], F32)
            MT = sb("MT", [128, 8, 512], BF16)
            XT_ = [sb(f"XTK{i}", [128, D], F32) for i in range(2)]
            RR_ = sb("RRb", [128, D], F32); X1 = sb("X1", [128, D], F32); X1B = sb("X1B", [128, D], BF16)
            X1T32 = sb("X1T32", [128, 8, 128], F32); LG = sb("LG", [128, NE], F32); M8 = sb("M8b", [128, 8], F32)
            EXg = sb("EXg", [128, NE], F32); MK = sb("MK", [128, NE], F32); SM = sb("SM", [128, 2], F32)
            xv = xT.rearrange("(k p) t -> p k t", p=128)
            yv = yT.rearrange("(k p) t -> p k t", p=128)
            pi = 0
            for tc in range(NTB // 512):
                X = XB[tc % 2]; Y = YB[0]; xk = f"XB{tc % 2}"; yk = "YB0"
                for k2 in range(2):
                    P.D('pool', lambda e, X=X, tc=tc, k2=k2: e.dma_start(out=X[:, 4 * k2:4 * k2 + 4, :], in_=xv[:, 4 * k2:4 * k2 + 4, tc * 512:(tc + 1) * 512]), w=[xk])
                for k3 in range(3):
                    P.D('pool', lambda e, Y=Y, tc=tc, k3=k3: e.dma_start(out=Y[:, 4 * k3:4 * k3 + 4, :], in_=yv[:, 4 * k3:4 * k3 + 4, tc * 512:(tc + 1) * 512]), w=[yk])
                for dc in range(8):
                    for n in range(3):
                        pg, pgk = PA[pi % 4], pak[pi % 4]; pi += 1
                        pu, puk = PA[pi % 4], pak[pi % 4]; pi += 1
                        col = n * 1024 + dc * 128
                        for k in range(8):
                            _mm(P, pg[:, 0:512], WG[:, k, col:col + 128], X[:, k, :], k == 0, k == 7, ['WG', xk], [pgk])
                        for k in range(4):
                            _mm(P, pu[:, 0:512], WU[:, n * 4 + k, dc * 128:(dc + 1) * 128], Y[:, n * 4 + k, :], k == 0, k == 3, ['WU', yk], [puk])
                        bc_ = n * 8 + dc
                        P.I('act', lambda e, pg=pg, bc_=bc_: e.activation(out=GS[:], in_=pg[:, 0:512], func=AF.Sigmoid, bias=bg[:, bc_:bc_ + 1], scale=1.0), r=[pgk, 'bg'], w=['GS'])
                        if n == 0:
                            P.I('dve', lambda e, pu=pu: e.tensor_tensor(out=MA[:], in0=pu[:, 0:512], in1=GS[:], op=OP.mult), r=[puk, 'GS'], w=['MA'])
                        else:
                            P.I('dve', lambda e, pu=pu: e.tensor_tensor(out=TM[:], in0=pu[:, 0:512], in1=GS[:], op=OP.mult), r=[puk, 'GS'], w=['TM'])
                            if n == 1:
                                P.I('dve', lambda e: e.tensor_tensor(out=MA[:], in0=MA[:], in1=TM[:], op=OP.add), r=['MA', 'TM'], w=['MA'])
                            else:
                                P.I('dve', lambda e, dc=dc: e.tensor_tensor(out=MT[:, dc, :], in0=MA[:], in1=TM[:], op=OP.add), r=['MA', 'TM'], w=['MT'])
                for tt in range(4):
                    ti = tc * 4 + tt
                    xt = XT_[ti % 2]; xtk = f"XTK{ti % 2}"
                    P.D('sp', lambda e, xt=xt, ti=ti: e.dma_start(out=xt[:], in_=xtok[ti * 128:(ti + 1) * 128, :]), w=[xtk])
                    for hf in range(2):
                        for k in range(8):
                            _mm(P, PH[:, hf * 512:(hf + 1) * 512], MT[:, k, tt * 128:(tt + 1) * 128], WO[:, k, hf * 512:(hf + 1) * 512], k == 0, k == 7, ['MT', 'WO'], ['ph'])
                    P.I('dve', lambda e, xt=xt: e.scalar_tensor_tensor(out=RR_[:], in0=xt[:], scalar=ALPHA, in1=PH[:, :], op0=OP.mult, op1=OP.add), r=[xtk, 'ph'], w=['RRb'])
                    _layernorm(P, nc, RR_, 'RRb', l1g, l1b, X1[:], 'X1', ST, MV, '1')
                    P.D('sp', lambda e, ti=ti: e.dma_start(out=x1s[ti * 128:(ti + 1) * 128, :], in_=X1[:]), r=['X1'], w=['x1s'])
                    P.I('act', lambda e: e.copy(out=X1B[:], in_=X1[:]), r=['X1'], w=['X1B'])
                    for k in range(8):
                        P.I('pe', lambda e, k=k: e.transpose(out=PSB[:, k * 128:(k + 1) * 128], in_=X1B[:, k * 128:(k + 1) * 128], identity=C['ident'][:]), r=['X1B', 'c_ident'], w=['psb'])
                    P.I('act', lambda e, ti=ti: e.copy(out=X1T[:, :, ti * 128:(ti + 1) * 128], in_=PSB[:, :].rearrange("p (k t) -> p k t", t=128)), r=['psb'], w=['X1T'])
                    for k in range(8):
                        P.I('pe', lambda e, k=k: e.transpose(out=PH[:, k * 128:(k + 1) * 128], in_=X1[:, k * 128:(k + 1) * 128], identity=identf[:]), r=['X1', 'identf'], w=['ph'])
                    P.I('act', lambda e: e.copy(out=X1T32[:].rearrange("p k t -> p (k t)"), in_=PH[:, :]), r=['ph'], w=['X1T32'])
                    for k in range(8):
                        _mm(P, PR[:, 0:NE], X1T32[:, k, :], RW[:, k, :], k == 0, k == 7, ['X1T32', 'RW'], ['pr'])
                    P.I('dve', lambda e: e.tensor_tensor(out=LG[:], in0=PR[:, 0:NE], in1=rb[:], op=OP.add), r=['pr', 'rb'], w=['LG'])
                    P.I('dve', lambda e: e.max(out=M8[:], in_=LG[:]), r=['LG'], w=['M8b'])
                    P.I('dve', lambda e: e.tensor_scalar(out=SM[:, 0:1], in0=M8[:, 0:1], scalar1=-1.0, scalar2=None, op0=OP.mult), r=['M8b'], w=['SM'])
                    P.I('act', lambda e: e.activation(out=EXg[:], in_=LG[:], func=AF.Exp, bias=SM[:, 0:1], scale=1.0), r=['LG', 'SM'], w=['EXg'])
                    P.I('dve', lambda e: e.scalar_tensor_tensor(out=MK[:], in0=LG[:], scalar=M8[:, 3:4], in1=EXg[:], op0=OP.is_ge, op1=OP.mult), r=['LG', 'M8b', 'EXg'], w=['MK'])
                    P.I('dve', lambda e: e.reduce_sum(out=SM[:, 1:2], in_=MK[:], axis=AX.X), r=['MK'], w=['SM'])
                    P.I('dve', lambda e: e.reciprocal(out=SM[:, 1:2], in_=SM[:, 1:2]), r=['SM'], w=['SM'])
                    P.I('dve', lambda e, ti=ti: e.tensor_scalar(out=GT[:, ti, :], in0=MK[:], scalar1=SM[:, 1:2], scalar2=None, op0=OP.mult), r=['MK', 'SM'], w=['GT'])
            P.barrier()

        with ExitStack() as e2:
            sb = lambda n, s, d: e2.enter_context(nc.sbuf_tensor(n, s, d))
            W1 = [sb(f"W1_{i}", [128, 8, 2048], BF16) for i in range(2)]
            W2 = [sb(f"W2_{i}", [128, 8, D], BF16) for i in range(1)]
            B1 = sb("B1", [128, NE * 16], F32)
            P.D('sp', lambda e: e.dma_start(out=B1[:], in_=b1_d), w=['B1'])
            ACC = sb("ACC", [128, NTT, D], F32)
            B2 = sb("B2", [NE, D], F32); GTT = sb("GTT", [NE, 128], F32)
            P.D('sp', lambda e: e.dma_start(out=B2[:], in_=b2_d), w=['B2'])
            for ti in range(NTT):
                P.D('sp', lambda e, ti=ti: e.dma_start(out=ACC[:, ti, :], in_=x1s[ti * 128:(ti + 1) * 128, :]), w=[f'ACC{ti}'])
                P.I('act', lambda e, ti=ti: e.activation(out=ACC[:, ti, :], in_=ACC[:, ti, :], func=AF.Copy, scale=ALPHA), r=[f'ACC{ti}'], w=[f'ACC{ti}'])
                P.I('pe', lambda e, ti=ti: e.transpose(out=PR[0:NE, 128:256], in_=GT[:, ti, :], identity=identf[:]), r=['GT', 'identf'], w=['pr'])
                P.I('act', lambda e: e.copy(out=GTT[:], in_=PR[0:NE, 128:256]), r=['pr'], w=['GTT'])
                for hf in range(2):
                    _mm(P, PH[:, hf * 512:(hf + 1) * 512], GTT[:], B2[:, hf * 512:(hf + 1) * 512], True, True, ['GTT', 'B2'], ['ph'])
                P.I('dve', lambda e, ti=ti: e.tensor_tensor(out=ACC[:, ti, :], in0=ACC[:, ti, :], in1=PH[:, :], op=OP.add), r=[f'ACC{ti}', 'ph'], w=[f'ACC{ti}'])
            SG_l2 = [sb(f"GEN{i}", [128, D], F32) for i in range(3)]
            GG = SG_l2[0][:, 0:512]; SG = SG_l2[0][:, 512:1024]; UU = SG_l2[1][:, 0:512]
            AT = sb("AT", [128, 8, 512], BF16)

            def load_w1(ex):
                s = ex % 2
                for k in range(8):
                    P.D('pool', lambda e, k=k, s=s, ex=ex: e.dma_start(out=W1[s][:, k, :], in_=w1_d[ex, k * 128:(k + 1) * 128, :]), w=[f'W1_{s}'])

            def load_w2(ex):
                for k in range(8):
                    P.D('pool', lambda e, k=k, ex=ex: e.dma_start(out=W2[0][:, k, :], in_=w2_d[ex, k * 128:(k + 1) * 128, :]), w=['W2_0'])

            load_w1(0)
            load_w2(0)
            pi = 0
            for ex in range(NE):
                if ex + 1 < NE:
                    load_w1(ex + 1)
                s = ex % 2
                w1k, w2k = f'W1_{s}', 'W2_0'
                for tc in range(NTB // 512):
                    for f in range(8):
                        pg, pgk = PA[pi % 4], pak[pi % 4]; pi += 1
                        pu, puk = PA[pi % 4], pak[pi % 4]; pi += 1
                        for k in range(8):
                            _mm(P, pg[:, 0:512], W1[s][:, k, f * 128:(f + 1) * 128], X1T[:, k, tc * 512:(tc + 1) * 512], k == 0, k == 7, [w1k, 'X1T'], [pgk])
                        for k in range(8):
                            _mm(P, pu[:, 0:512], W1[s][:, k, 1024 + f * 128:1024 + (f + 1) * 128], X1T[:, k, tc * 512:(tc + 1) * 512], k == 0, k == 7, [w1k, 'X1T'], [puk])
                        cg = ex * 16 + f; cu = ex * 16 + 8 + f
                        P.I('dve', lambda e, pg=pg, cg=cg: e.tensor_scalar(out=GG, in0=pg[:, 0:512], scalar1=B1[:, cg:cg + 1], scalar2=7.0, op0=OP.add, op1=OP.min), r=[pgk, 'B1'], w=['GG'])
                        P.I('act', lambda e: e.activation(out=SG, in_=GG, func=AF.Sigmoid, scale=1.702), r=['GG'], w=['SG'])
                        P.I('dve', lambda e, pu=pu, cu=cu: e.tensor_scalar(out=UU, in0=pu[:, 0:512], scalar1=B1[:, cu:cu + 1], scalar2=7.0, op0=OP.add, op1=OP.min), r=[puk, 'B1'], w=['UU'])
                        P.I('pool', lambda e: e.tensor_scalar(out=UU, in0=UU, scalar1=-7.0, scalar2=1.0, op0=OP.max, op1=OP.add), r=['UU'], w=['UU'])
                        P.I('pool', lambda e: e.tensor_tensor(out=GG, in0=GG, in1=UU, op=OP.mult), r=['GG', 'UU'], w=['GG'])
                        P.I('dve', lambda e, f=f: e.tensor_tensor(out=AT[:, f, :], in0=GG, in1=SG, op=OP.mult), r=['GG', 'SG'], w=['AT'])
                    for tt in range(4):
                        ti = tc * 4 + tt
                        for hf in range(2):
                            for f in range(8):
                                _mm(P, PH[:, hf * 512:(hf + 1) * 512], AT[:, f, tt * 128:(tt + 1) * 128], W2[0][:, f, hf * 512:(hf + 1) * 512], f == 0, f == 7, ['AT', w2k], ['ph'])
                        P.I('dve', lambda e, ti=ti, ex=ex: e.scalar_tensor_tensor(out=ACC[:, ti, :], in0=PH[:, :], scalar=GT[:, ti, ex:ex + 1], in1=ACC[:, ti, :], op0=OP.mult, op1=OP.add),
                            r=['ph', 'GT', f'ACC{ti}'], w=[f'ACC{ti}'])
                if ex + 1 < NE:
                    load_w2(ex + 1)
            P.barrier()
            l2g = SG_l2[0]; l2b = SG_l2[1]
            P.D('sp', lambda e: e.dma_start(out=l2g[:], in_=l2g_d), w=['lng2'])
            P.D('sp', lambda e: e.dma_start(out=l2b[:], in_=l2b_d), w=['lnb2'])
            OUT = [SG_l2[2], SG_l2[2]]
            for ti in range(NTT):
                o = OUT[0]; ok = "OUT0"
                _layernorm(P, nc, ACC[:, ti, :], f'ACC{ti}', l2g, l2b, o[:], ok, ST, MV, '2')
                P.D('sp', lambda e, o=o, ti=ti: e.dma_start(out=xo[ti * 128:(ti + 1) * 128, :], in_=o[:]), r=[ok])
        P.finish()
    return nc


def _prep_B(inp, l, b, r, x, ys):
    tok = slice(NTB * r, NTB * (r + 1))
    c32 = lambda a: np.ascontiguousarray(a, dtype=np.float32)
    w_in = inp['w_in'][l]; b_in = inp['b_in'][l]
    bc = lambda v: c32(np.tile(v[None, :], (128, 1)))
    return {
        "xT": c32(x[b, tok].T), "xtok": c32(x[b, tok]), "yT": c32(ys[b, tok].T),
        "wg": c32(w_in[:, 3360:6432]), "bg": c32(b_in[3360:6432].reshape(24, 128).T),
        "wup": c32(inp['w_up'][l].reshape(1536, D)), "wo": c32(inp['w_o'][l]),
        "l1g": bc(inp['ln1_g'][l]), "l1b": bc(inp['ln1_b'][l]), "l2g": bc(inp['ln2_g'][l]), "l2b": bc(inp['ln2_b'][l]),
        "rw": c32(inp['router_w'][l]), "rb": bc(inp['router_b'][l]),
        "w1": inp['moe_w1'][l], "b1": c32(inp['moe_b1'][l].reshape(NE * 16, 128).T), "w2": inp['moe_w2'][l], "b2": c32(inp['moe_b2'][l]),
    }


def run_B(inp, l, x, ys):
    if 'B' not in _NC:
        _NC['B'] = build_B()
    maps = [_prep_B(inp, l, c // 4, c % 4, x, ys) for c in range(8)]
    res = run_bass_kernel_spmd(_NC['B'], maps, core_ids=list(range(8)))
    out = np.empty((2, T, D), np.float32)
    for c in range(8):
        out[c // 4, NTB * (c % 4):NTB * (c % 4 + 1)] = res.results[c]["xo"]
    return out


def kernel(**inputs):
    inp = {k: np.asarray(v) for k, v in inputs.items()}
    x = np.ascontiguousarray(inp['x'], dtype=np.float32)
    for l in range(2):
        ys = run_A(inp, l, x)
        x = run_B(inp, l, x, ys)
    return x
```

```python
import numpy as np
from contextlib import ExitStack
import concourse.bass as bass
import concourse.mybir as mybir
from concourse.bass_utils import run_bass_kernel_spmd

F32 = mybir.dt.float32
BF16 = mybir.dt.bfloat16
AF = mybir.ActivationFunctionType
OP = mybir.AluOpType
AX = mybir.AxisListType

T = 8192
D = 1024
NT = T // 128
SCALE = 0.125
ALPHA = 4.0 ** 0.25
POOL_WINDOWS = (2, 4, 8, 16)
LN_EPS = 1e-5


class Prog:
    NDMA = 12

    def __init__(self, nc, es):
        self.nc = nc
        self.eng = {'pe': nc.tensor, 'act': nc.scalar, 'dve': nc.vector, 'pool': nc.gpsimd, 'sp': nc.sync}
        self.sem = {n: es.enter_context(nc.semaphore("s_" + n)) for n in self.eng}
        self.cnt = {n: 0 for n in self.eng}
        self.dsem = [es.enter_context(nc.semaphore(f"d{i}")) for i in range(self.NDMA)]
        self.dval = [0] * self.NDMA
        self.dnext = 0
        self.seen = {n: {} for n in self.eng}
        self.lastw = {}
        self.readers = {}

    def _wait(self, en, tok):
        if tok is None:
            return
        kind, a, v = tok
        if kind == 'e' and a == en and en == 'pe':
            return
        src = (kind, a)
        if self.seen[en].get(src, 0) >= v:
            return
        s = self.sem[a] if kind == 'e' else self.dsem[a]
        self.eng[en].wait_ge(s, v)
        self.seen[en][src] = v

    def _deps(self, en, r, w):
        for k in r:
            self._wait(en, self.lastw.get(k))
        for k in w:
            self._wait(en, self.lastw.get(k))
            for t in self.readers.get(k, ()):
                self._wait(en, t)

    def _commit(self, tok, r, w):
        for k in r:
            self.readers.setdefault(k, []).append(tok)
        for k in w:
            self.lastw[k] = tok
            self.readers[k] = []

    @staticmethod
    def _psum_excl(r, w):
        pr = [k for k in r if k.startswith(('ps', 'pa', 'ph', 'pr'))]
        if pr:
            r = [k for k in r if k not in pr]
            w = list(w) + pr
        w = ['psb' if k in ('psb0', 'psb1') else k for k in w]
        return r, w

    def I(self, en, fn, r=(), w=()):
        r, w = self._psum_excl(r, w)
        self._deps(en, r, w)
        ins = fn(self.eng[en])
        self.cnt[en] += 1
        ins.then_inc(self.sem[en], 1)
        self._commit(('e', en, self.cnt[en]), r, w)
        return ins

    def D(self, en, fn, r=(), w=()):
        i = self.dnext
        self.dnext = (self.dnext + 1) % self.NDMA
        if self.dval[i] > 0:
            self._wait(en, ('d', i, self.dval[i]))
        self._deps(en, r, w)
        ins = fn(self.eng[en])
        self.dval[i] += 16
        ins.then_inc(self.dsem[i], 16)
        self._commit(('d', i, self.dval[i]), r, w)
        return ins

    def barrier(self):
        for en in self.eng:
            for o in self.eng:
                if o != en and self.cnt[o] > 0:
                    self._wait(en, ('e', o, self.cnt[o]))
            for i in range(self.NDMA):
                if self.dval[i] > 0:
                    self._wait(en, ('d', i, self.dval[i]))
        self.lastw.clear()
        self.readers.clear()

    def finish(self):
        for i in range(self.NDMA):
            if self.dval[i] > 0:
                self._wait('sp', ('d', i, self.dval[i]))
        for o in self.eng:
            if o != 'sp' and self.cnt[o] > 0:
                self._wait('sp', ('e', o, self.cnt[o]))


def _mm(P, out, lhsT, rhs, start, stop, r, w):
    P.I('pe', lambda e: e.matmul(out, lhsT=lhsT, rhs=rhs, start=start, stop=stop), r=r, w=w)


def _consts(P, nc, sb):
    c = {}
    tf = sb("c_tf", [128, 128], F32)
    P.I('pool', lambda e: e.iota(tf[:], pattern=[[1, 128]], base=0, channel_multiplier=-1,
                                 allow_small_or_imprecise_dtypes=True), w=['c_tf'])
    c['ident'] = sb("c_ident", [128, 128], BF16)
    c['tri'] = sb("c_tri", [128, 128], BF16)
    c['atri'] = sb("c_atri", [128, 128], BF16)
    c['trif'] = sb("c_trif", [128, 128], F32)
    c['onesf'] = sb("c_onesf", [128, 128], F32)
    P.I('dve', lambda e: e.tensor_scalar(out=c['ident'][:], in0=tf[:], scalar1=0.0, scalar2=None, op0=OP.is_equal), r=['c_tf'], w=['c_ident'])
    P.I('dve', lambda e: e.tensor_scalar(out=c['tri'][:], in0=tf[:], scalar1=0.0, scalar2=None, op0=OP.is_ge), r=['c_tf'], w=['c_tri'])
    P.I('dve', lambda e: e.tensor_scalar(out=c['atri'][:], in0=tf[:], scalar1=0.0, scalar2=None, op0=OP.is_lt), r=['c_tf'], w=['c_atri'])
    P.I('dve', lambda e: e.tensor_scalar(out=c['trif'][:], in0=tf[:], scalar1=0.0, scalar2=None, op0=OP.is_ge), r=['c_tf'], w=['c_trif'])
    P.I('dve', lambda e: e.memset(c['onesf'][:], 1.0), w=['c_onesf'])
    return c


def _inproj(P, nc, es_outer, xT, wfm_d, bfm, fm_list, wtm_d, ntm, tm_evac, pss, tag, n_tok=T):
    with ExitStack() as es:
        sb = lambda n, s, d: es.enter_context(nc.sbuf_tensor(tag + n, s, d))
        nfm = int(wfm_d.shape[1])
        W = sb("W", [128, 8, nfm], BF16)
        for k in range(8):
            P.D('pool', lambda e, k=k: e.dma_start(out=W[:, k, :], in_=wfm_d[k * 128:(k + 1) * 128, :]), w=[tag + 'W'])
        if ntm:
            WT = sb("WT", [128, 8, ntm], BF16)
            for k in range(8):
                P.D('pool', lambda e, k=k: e.dma_start(out=WT[:, k, :], in_=wtm_d[k * 128:(k + 1) * 128, :]), w=[tag + 'WT'])
        xb = [sb(f"xb{i}", [128, 8, 512], BF16) for i in range(2)]
        xv = xT.rearrange("(k p) t -> p k t", p=128)
        nch = n_tok // 512
        pi = 0
        for tc in range(nch):
            X = xb[tc % 2]
            xk = f"{tag}xb{tc % 2}"
            for k2 in range(2):
                P.D('pool', lambda e, X=X, tc=tc, k2=k2: e.dma_start(out=X[:, 4 * k2:4 * k2 + 4, :], in_=xv[:, 4 * k2:4 * k2 + 4, tc * 512:(tc + 1) * 512]), w=[xk])
            for (c0, M, evac) in fm_list:
                ps, pk = pss[pi % len(pss)]
                pi += 1
                for k in range(8):
                    _mm(P, ps[0:M, 0:512], W[:, k, c0:c0 + M], X[:, k, :], k == 0, k == 7, [tag + 'W', xk], [pk])
                evac(tc, ps, pk)
            if ntm:
                for tt in range(4):
                    ps, pk = pss[pi % len(pss)]
                    pi += 1
                    for k in range(8):
                        _mm(P, ps[:, 0:ntm], X[:, k, tt * 128:(tt + 1) * 128], WT[:, k, :], k == 0, k == 7, [tag + 'WT', xk], [pk])
                    tm_evac(tc * 4 + tt, ps, pk)
    P.barrier()


def build_A():
    nc = bass.Bass("TRN2", target_bir_lowering=False)
    dt = lambda n, s, k="ExternalInput": nc.dram_tensor(n, s, F32, kind=k).ap()
    xT = dt("xT", [D, T])
    wfm1 = dt("wfm1", [D, 256]); bfm1 = dt("bfm1", [128, 2])
    wfm2 = dt("wfm2", [D, 516]); bfm2 = dt("bfm2", [128, 5])
    wtm2 = dt("wtm2", [D, 130]); btm2 = dt("btm2", [128, 130])
    wfm3 = dt("wfm3", [D, 256]); bfm3 = dt("bfm3", [128, 2])
    wtm3 = dt("wtm3", [D, 130]); btm3 = dt("btm3", [128, 130])
    pw_d = dt("pw", [128, 128]); pbs_d = dt("pbs", [128, 2]); cw_d = dt("cw", [128, 4]); fix_d = dt("fix", [128, 16])
    cw1_d = dt("cw1", [128, 32 * 256]); posT_d = dt("posT", [128, 32]); cb1_d = dt("cb1", [128, 4])
    cw2k_d = dt("cw2k", [128, 256]); cb2k_d = dt("cb2k", [128, 1]); cw2v_d = dt("cw2v", [128, 128]); cb2v_d = dt("cb2v", [128, 64])
    yT = dt("yT", [384, T], "ExternalOutput")

    with ExitStack() as es:
        P = Prog(nc, es)
        sbg = lambda n, s, d: es.enter_context(nc.sbuf_tensor(n, s, d))
        PS = [es.enter_context(nc.psum_tensor(f"ps{i}", [128, 512], F32)) for i in range(7)]
        PSB = es.enter_context(nc.psum_tensor("psb", [128, 1024], BF16))
        pk = [f"ps{i}" for i in range(7)]
        C = _consts(P, nc, sbg)
        KCT = sbg("KCT", [128, 512], BF16)
        VC = sbg("VC", [128, 4, 64], BF16)

        with ExitStack() as e1:
            sb = lambda n, s, d: e1.enter_context(nc.sbuf_tensor(n, s, d))
            bf1 = sb("bf1", [128, 2], F32); pwf = sb("pwf", [128, 128], BF16); pbs = sb("pbs_s", [128, 2], F32)
            cw = sb("cw_s", [128, 4], F32); fix = sb("fix_s", [128, 16], F32)
            P.D('sp', lambda e: e.dma_start(out=bf1[:], in_=bfm1), w=['bf1'])
            P.D('pool', lambda e: e.dma_start(out=pwf[:], in_=pw_d), w=['pwf'])
            P.D('sp', lambda e: e.dma_start(out=pbs[:], in_=pbs_d), w=['pbs'])
            P.D('sp', lambda e: e.dma_start(out=cw[:], in_=cw_d), w=['cw'])
            P.D('sp', lambda e: e.dma_start(out=fix[:], in_=fix_d), w=['fix'])
            KVC = sb("KVC", [128, T], BF16)
            UC = [sb(f"UC{i}", [128, 528], F32) for i in range(2)]
            S2 = sb("S2", [128, 528], F32); S4 = sb("S4", [128, 528], F32); S8 = sb("S8", [128, 528], F32); S16 = sb("S16", [128, 528], F32)
            ACC = sb("ACC", [128, 512], F32); DT_ = [sb(f"DTb{i}", [128, 512], BF16) for i in range(2)]
            YP = [sb(f"YP{i}", [128, 512], F32) for i in range(2)]
            P.I('dve', lambda e: e.memset(UC[1][:], 0.0), w=['UC1'])

            def evac_u(tc, ps, pkk):
                U = UC[tc % 2]; Up = UC[(tc + 1) % 2]
                uk, upk = f"UC{tc % 2}", f"UC{(tc + 1) % 2}"
                P.I('act', lambda e: e.activation(out=U[:, 16:528], in_=ps[:, 0:512], func=AF.Identity, bias=bf1[:, 0:1], scale=1.0), r=[pkk, 'bf1'], w=[uk])
                P.I('dve', lambda e: e.tensor_copy(out=U[:, 0:16], in_=Up[:, 512:528]), r=[upk], w=[uk])
                P.I('dve', lambda e: e.tensor_tensor(out=S2[:, 1:528], in0=U[:, 1:528], in1=U[:, 0:527], op=OP.add), r=[uk], w=['S2'])
                P.I('dve', lambda e: e.tensor_tensor(out=S4[:, 3:528], in0=S2[:, 3:528], in1=S2[:, 1:526], op=OP.add), r=['S2'], w=['S4'])
                P.I('dve', lambda e: e.tensor_tensor(out=S8[:, 7:528], in0=S4[:, 7:528], in1=S4[:, 3:524], op=OP.add), r=['S4'], w=['S8'])
                P.I('dve', lambda e: e.tensor_tensor(out=S16[:, 15:528], in0=S8[:, 15:528], in1=S8[:, 7:520], op=OP.add), r=['S8'], w=['S16'])
                P.I('dve', lambda e: e.tensor_scalar(out=ACC[:], in0=S2[:, 16:528], scalar1=cw[:, 0:1], scalar2=None, op0=OP.mult), r=['S2', 'cw'], w=['ACC'])
                for wi, S in enumerate((S4, S8, S16)):
                    P.I('dve', lambda e, S=S, wi=wi: e.scalar_tensor_tensor(out=ACC[:], in0=S[:, 16:528], scalar=cw[:, wi + 1:wi + 2], in1=ACC[:], op0=OP.mult, op1=OP.add),
                        r=['S4', 'S8', 'S16', 'cw', 'ACC'], w=['ACC'])
                if tc == 0:
                    P.I('dve', lambda e: e.tensor_tensor(out=ACC[:, 0:16], in0=ACC[:, 0:16], in1=fix[:], op=OP.mult), r=['ACC', 'fix'], w=['ACC'])
                Dt = DT_[tc % 2]; dk = f"DT{tc % 2}"
                P.I('dve', lambda e: e.tensor_tensor(out=Dt[:], in0=ACC[:], in1=U[:, 16:528], op=OP.subtract), r=['ACC', uk], w=[dk])
                ps2, pk2 = PS[4 + tc % 2], pk[4 + tc % 2]
                _mm(P, ps2[:, 0:512], pwf[:], Dt[:], True, True, ['pwf', dk], [pk2])
                Y = YP[tc % 2]; yk = f"YP{tc % 2}"
                P.I('dve', lambda e: e.tensor_scalar(out=Y[:], in0=ps2[:, 0:512], scalar1=pbs[:, 0:1], scalar2=pbs[:, 1:2], op0=OP.add, op1=OP.mult), r=[pk2, 'pbs'], w=[yk])
                P.D('sp', lambda e: e.dma_start(out=yT[0:128, tc * 512:(tc + 1) * 512], in_=Y[:]), r=[yk])

            def evac_kvc(tc, ps, pkk):
                P.I('act', lambda e: e.activation(out=KVC[:, tc * 512:(tc + 1) * 512], in_=ps[:, 0:512], func=AF.Identity, bias=bf1[:, 1:2], scale=1.0), r=[pkk, 'bf1'], w=['KVC'])

            _inproj(P, nc, e1, xT, wfm1, bf1, [(0, 128, evac_u), (128, 128, evac_kvc)], None, 0, None,
                    [(PS[0], pk[0]), (PS[1], pk[1]), (PS[2], pk[2]), (PS[3], pk[3])], "p1")

            W1 = sb("W1", [128, 32 * 256], BF16)
            for q4 in range(4):
                P.D('pool', lambda e, q4=q4: e.dma_start(out=W1[:, q4 * 2048:(q4 + 1) * 2048], in_=cw1_d[:, q4 * 2048:(q4 + 1) * 2048]), w=['W1'])
            posT = sb("posT_s", [128, 32], BF16); cb1 = sb("cb1_s", [128, 4], F32)
            w2k = sb("w2k", [128, 256], BF16); b2k = sb("b2k", [128, 1], F32); w2v = sb("w2v", [128, 128], BF16); b2v = sb("b2v", [128, 64], F32)
            P.D('pool', lambda e: e.dma_start(out=posT[:], in_=posT_d), w=['posT'])
            P.D('sp', lambda e: e.dma_start(out=cb1[:], in_=cb1_d), w=['cb1'])
            P.D('pool', lambda e: e.dma_start(out=w2k[:], in_=cw2k_d), w=['w2k'])
            P.D('sp', lambda e: e.dma_start(out=b2k[:], in_=cb2k_d), w=['b2k'])
            P.D('pool', lambda e: e.dma_start(out=w2v[:], in_=cw2v_d), w=['w2v'])
            P.D('sp', lambda e: e.dma_start(out=b2v[:], in_=cb2v_d), w=['b2v'])
            HT = [[sb(f"HT{kv}{hf}", [128, 512], BF16) for hf in range(2)] for kv in range(2)]
            XH = sb("XH", [128, 512], F32); T1 = sb("T1c", [128, 512], F32); T2 = sb("T2c", [128, 512], F32); cbt = sb("cbt", [128, 4], F32)
            P.I('dve', lambda e: e.memset(VC[:], 0.0), w=['VC'])
            for kv in range(2):
                po = 64 * kv
                for hf in range(2):
                    ci = kv * 2 + hf
                    ps, pkk = PS[ci % 4], pk[ci % 4]
                    psc, pkc = PS[4 + ci % 2], pk[4 + ci % 2]
                    for l in range(32):
                        wsl = W1[po:po + 64, l * 256 + hf * 128: l * 256 + hf * 128 + 128]
                        _mm(P, psc[:, 0:1], wsl, posT[po:po + 64, l:l + 1], l == 0, l == 31, ['W1', 'posT'], [pkc])
                    P.I('dve', lambda e, ci=ci, psc=psc: e.tensor_tensor(out=cbt[:, ci:ci + 1], in0=psc[:, 0:1], in1=cb1[:, ci:ci + 1], op=OP.add), r=[pkc, 'cb1'], w=['cbt'])
                    for l in range(32):
                        wsl = W1[po:po + 64, l * 256 + hf * 128: l * 256 + hf * 128 + 128]
                        _mm(P, ps[:, 0:511], wsl, KVC[po:po + 64, l:l + 16 * 510 + 1:16], l == 0, l == 31, ['W1', 'KVC'], [pkk])
                    P.I('act', lambda e, ci=ci, ps=ps: e.activation(out=XH[:, 0:511], in_=ps[:, 0:511], func=AF.Identity, bias=cbt[:, ci:ci + 1], scale=1.0), r=[pkk, 'cbt'], w=['XH'])
                    P.I('dve', lambda e: e.tensor_tensor(out=T1[:, 0:511], in0=XH[:, 0:511], in1=XH[:, 0:511], op=OP.mult), r=['XH'], w=['T1'])
                    P.I('dve', lambda e: e.tensor_scalar(out=T1[:, 0:511], in0=T1[:, 0:511], scalar1=0.044715, scalar2=1.0, op0=OP.mult, op1=OP.add), r=['T1'], w=['T1'])
                    P.I('dve', lambda e: e.tensor_tensor(out=T1[:, 0:511], in0=T1[:, 0:511], in1=XH[:, 0:511], op=OP.mult), r=['T1', 'XH'], w=['T1'])
                    P.I('act', lambda e: e.activation(out=T2[:, 0:511], in_=T1[:, 0:511], func=AF.Sigmoid, scale=1.5957691216057308), r=['T1'], w=['T2'])
                    H = HT[kv][hf]
                    P.I('dve', lambda e, H=H: e.memset(H[:, 511:512], 0.0), w=[f'HT{kv}{hf}'])
                    P.I('dve', lambda e, H=H: e.tensor_tensor(out=H[:, 0:511], in0=T2[:, 0:511], in1=XH[:, 0:511], op=OP.mult), r=['T2', 'XH'], w=[f'HT{kv}{hf}'])
            for hf in range(2):
                _mm(P, PS[0][:, 0:512], w2k[:, hf * 128:(hf + 1) * 128], HT[0][hf][:], hf == 0, hf == 1, ['w2k', f'HT0{hf}'], [pk[0]])
            P.I('act', lambda e: e.activation(out=KCT[:], in_=PS[0][:, 0:512], func=AF.Identity, bias=b2k[:, 0:1], scale=1.0), r=[pk[0], 'b2k'], w=['KCT'])
            for ct in range(4):
                m = 128 if ct < 3 else 127
                for hf in range(2):
                    _mm(P, PS[1 + ct % 2][0:m, 0:64], HT[1][hf][:, ct * 128:ct * 128 + m], w2v[:, hf * 64:(hf + 1) * 64], hf == 0, hf == 1, ['w2v', f'HT1{hf}'], [pk[1 + ct % 2]])
                P.I('dve', lambda e, ct=ct, m=m: e.tensor_tensor(out=VC[0:m, ct, :], in0=PS[1 + ct % 2][0:m, 0:64], in1=b2v[0:m, :], op=OP.add), r=[pk[1 + ct % 2], 'b2v'], w=['VC'])
            P.barrier()

        with ExitStack() as e2:
            sb = lambda n, s, d: e2.enter_context(nc.sbuf_tensor(n, s, d))
            bf2 = sb("bf2", [128, 5], F32); bt2 = sb("bt2", [128, 130], F32)
            P.D('sp', lambda e: e.dma_start(out=bf2[:], in_=bfm2), w=['bf2'])
            P.D('sp', lambda e: e.dma_start(out=bt2[:], in_=btm2), w=['bt2'])
            NQ = sb("NQ", [128, T], BF16); NQo = sb("NQo", [128, T], BF16); KS = sb("KS", [128, T], BF16); KW = sb("KW", [128, T], BF16)
            G4 = sb("G4", [4, T], BF16)
            VS = sb("VS", [128, NT, 65], BF16); VW = sb("VW", [128, NT, 65], BF16); GC = sb("GC", [128, NT, 2], F32)
            P.I('pool', lambda e: e.memset(VS[:, :, 64:65], 1.0), w=['VS'])
            P.I('pool', lambda e: e.memset(VW[:, :, 64:65], 1.0), w=['VW'])

            def ev(dst, key, col, func=AF.Identity, M=128):
                def f(tc, ps, pkk):
                    P.I('act', lambda e: e.activation(out=dst[0:M, tc * 512:(tc + 1) * 512], in_=ps[0:M, 0:512], func=func, bias=bf2[0:M, col:col + 1], scale=1.0), r=[pkk, 'bf2'], w=[key])
                return f

            TMB = sb("TMB", [128, 130], F32)

            def tm2(ti, ps, pkk):
                P.I('dve', lambda e: e.tensor_tensor(out=VS[:, ti, 0:64], in0=ps[:, 0:64], in1=bt2[:, 0:64], op=OP.add), r=[pkk, 'bt2'], w=['VS'])
                P.I('dve', lambda e: e.tensor_tensor(out=VW[:, ti, 0:64], in0=ps[:, 64:128], in1=bt2[:, 64:128], op=OP.add), r=[pkk, 'bt2'], w=['VW'])
                P.I('dve', lambda e: e.tensor_tensor(out=TMB[:, 128:130], in0=ps[:, 128:130], in1=bt2[:, 128:130], op=OP.add), r=[pkk, 'bt2'], w=['TMB'])
                P.I('act', lambda e: e.activation(out=GC[:, ti, :], in_=TMB[:, 128:130], func=AF.Sigmoid), r=['TMB'], w=['GC'])

            _inproj(P, nc, e2, xT, wfm2, bf2,
                    [(0, 128, ev(NQ, 'NQ', 0)), (128, 128, ev(NQo, 'NQo', 1)), (256, 128, ev(KS, 'KS', 2)), (384, 128, ev(KW, 'KW', 3)),
                     (512, 4, ev(G4, 'G4', 4, AF.Sigmoid, 4))],
                    wtm2, 130, tm2, [(PS[0], pk[0]), (PS[1], pk[1]), (PS[2], pk[2]), (PS[3], pk[3])], "p2")

            EJ = sb("EJ", [128, NT * 128], BF16); ejf = sb("ejf", [128, 2048], F32)
            for q4 in range(4):
                P.I('pool', lambda e, q4=q4: e.iota(ejf[:], pattern=[[-2, 16], [-1, 2], [0, 64]], base=-32 * q4, channel_multiplier=1,
                                             allow_small_or_imprecise_dtypes=True), w=['ejf'])
                P.I('dve', lambda e, q4=q4: e.tensor_scalar(out=EJ[:, q4 * 2048:(q4 + 1) * 2048], in0=ejf[:], scalar1=0.0, scalar2=None, op0=OP.is_equal), r=['ejf'], w=['EJ'])
            REL = sb("REL", [128, 512], F32)
            P.I('pool', lambda e: e.iota(REL[:], pattern=[[16, 512]], base=31, channel_multiplier=-1, allow_small_or_imprecise_dtypes=True), w=['REL'])
            VV = sb("VV", [128, 254], F32); HP = sb("HP", [128, 1], F32)
            P.I('pool', lambda e: e.iota(VV[:], pattern=[[1, 254]], base=-126, channel_multiplier=0, allow_small_or_imprecise_dtypes=True), w=['VV'])
            P.I('pool', lambda e: e.iota(HP[:], pattern=[[0, 1]], base=0, channel_multiplier=1, allow_small_or_imprecise_dtypes=True), w=['HP'])
            P.I('dve', lambda e: e.tensor_scalar(out=HP[:], in0=HP[:], scalar1=64.0, scalar2=None, op0=OP.is_ge), r=['HP'], w=['HP'])
            P.I('dve', lambda e: e.tensor_scalar(out=VV[:], in0=VV[:], scalar1=HP[:, 0:1], scalar2=None, op0=OP.subtract), r=['VV', 'HP'], w=['VV'])
            KEEP = sb("KEEP", [128, 254], F32); NF = sb("NF", [128, 254], F32); ADD = sb("ADD", [128, 254], F32); TA = sb("TA", [128, 254], F32)
            P.I('dve', lambda e: e.tensor_scalar(out=KEEP[:], in0=VV[:], scalar1=-2.0, scalar2=None, op0=OP.is_le), r=['VV'], w=['KEEP'])
            P.I('dve', lambda e: e.tensor_scalar(out=NF[:], in0=VV[:], scalar1=0.0, scalar2=None, op0=OP.is_le), r=['VV'], w=['NF'])
            P.I('dve', lambda e: e.tensor_scalar(out=ADD[:], in0=VV[:], scalar1=-1.0, scalar2=1.0e6, op0=OP.is_ge, op1=OP.mult), r=['VV'], w=['ADD'])
            P.I('dve', lambda e: e.tensor_scalar(out=TA[:], in0=VV[:], scalar1=0.0, scalar2=-1000001.0, op0=OP.is_gt, op1=OP.mult), r=['VV'], w=['TA'])
            P.I('dve', lambda e: e.tensor_tensor(out=ADD[:], in0=ADD[:], in1=TA[:], op=OP.add), r=['ADD', 'TA'], w=['ADD'])
            SEL = sb("SEL", [4, 256], BF16); self_ = sb("self_", [4, 256], F32)
            P.I('pool', lambda e: e.iota(self_[:], pattern=[[1, 4], [0, 64]], base=0, channel_multiplier=-1, allow_small_or_imprecise_dtypes=True), w=['self_'])
            P.I('dve', lambda e: e.tensor_scalar(out=SEL[:], in0=self_[:], scalar1=0.0, scalar2=None, op0=OP.is_equal), r=['self_'], w=['SEL'])

            WMASK = sb("WMASK", [128, 8 * 512], BF16)
            P.I('dve', lambda e: e.memset(WMASK[:], 0.0), w=['WMASK'])
            for a in range(-4, 4):
                for b in range(4):
                    dst = WMASK[:, (a + 4) * 512 + b * 128:(a + 4) * 512 + (b + 1) * 128]
                    if b == a:
                        P.I('dve', lambda e, dst=dst: e.tensor_copy(out=dst, in_=C['tri'][:]), r=['c_tri'], w=['WMASK'])
                    elif b == a + 4:
                        P.I('dve', lambda e, dst=dst: e.tensor_copy(out=dst, in_=C['atri'][:]), r=['c_atri'], w=['WMASK'])
                    elif a < b < a + 4:
                        P.I('dve', lambda e, dst=dst: e.memset(dst, 1.0), w=['WMASK'])
            EX = [sb(f"EX{i}", [128, 512], F32) for i in range(2)]
            EM = [sb(f"EM{i}", [128, 512], F32) for i in range(2)]
            RS = sb("RS", [128, 8], F32)
            PSP = sb("PSP", [128, 520], F32)
            P.I('dve', lambda e: e.memset(PSP[:], 0.0), w=['PSP'])
            PN = [sb(f"PN{i}", [128, 512], BF16) for i in range(2)]
            PNT = [sb(f"PNT{i}", [128, 512], BF16) for i in range(2)]
            IMP = sb("IMP", [128, 128], F32); SC = sb("SC", [128, 128], F32); SC2 = sb("SC2", [128, 128], F32); M8 = sb("M8", [128, 16], F32)
            SELM = sb("SELM", [128, 128], F32); MB = sb("MB", [128, 128], BF16)
            MBT = [sb(f"MBT{i}", [128, 512], BF16) for i in range(2)]
            OCc = [[sb(f"OC{i}{h}", [64, 512], F32) for h in range(2)] for i in range(2)]
            PT = [sb(f"PT{i}", [128, 512], BF16) for i in range(3)]
            RR = sb("RR", [65, 512], F32); BCS = sb("BCS", [64, 512], F32); BGS = sb("BGS", [64, 512], F32)
            TY = sb("TY", [64, 512], F32); YN = [sb(f"YN{i}", [64, 512], F32) for i in range(2)]
            ptc = 0

            for qc in range(T // 512):
                par = qc % 2
                mbk = f"MBT{par}"
                for b in range(4):
                    i = 4 * qc + b
                    for hh in range(4):
                        Q = NQ if hh < 2 else NQo
                        qk_ = 'NQ' if hh < 2 else 'NQo'
                        po = 64 * (hh % 2)
                        ps, pkk = PS[hh % 2], pk[hh % 2]
                        _mm(P, ps[:, 0:512], Q[po:po + 64, i * 128:(i + 1) * 128], KCT[po:po + 64, :], True, True, [qk_, 'KCT'], [pkk])
                        ex, exk = EX[hh % 2], f"EX{hh % 2}"
                        em, emk = EM[hh % 2], f"EM{hh % 2}"
                        P.I('act', lambda e, ex=ex, ps=ps: e.activation(out=ex[:], in_=ps[:, 0:512], func=AF.Exp, scale=SCALE), r=[pkk], w=[exk])
                        P.I('dve', lambda e, ex=ex, em=em, i=i, hh=hh: e.scalar_tensor_tensor(out=em[:], in0=REL[:], scalar=float(128 * i), in1=ex[:], op0=OP.is_le, op1=OP.mult),
                            r=['REL', exk], w=[emk])
                        P.I('dve', lambda e, em=em, hh=hh: e.reduce_sum(out=RS[:, hh:hh + 1], in_=em[:], axis=AX.X), r=[emk], w=['RS'])
                        P.I('dve', lambda e, hh=hh: e.tensor_scalar(out=RS[:, hh:hh + 1], in0=RS[:, hh:hh + 1], scalar1=1e-30, scalar2=None, op0=OP.max), r=['RS'], w=['RS'])
                        P.I('dve', lambda e, hh=hh: e.reciprocal(out=RS[:, hh:hh + 1], in_=RS[:, hh:hh + 1]), r=['RS'], w=['RS'])
                        if hh == 0:
                            P.I('dve', lambda e, em=em, hh=hh: e.tensor_scalar(out=PSP[:, 1:513], in0=em[:], scalar1=RS[:, hh:hh + 1], scalar2=None, op0=OP.mult), r=[emk, 'RS'], w=['PSP'])
                        else:
                            P.I('dve', lambda e, em=em, hh=hh: e.scalar_tensor_tensor(out=PSP[:, 1:513], in0=em[:], scalar=RS[:, hh:hh + 1], in1=PSP[:, 1:513], op0=OP.mult, op1=OP.add),
                                r=[emk, 'RS', 'PSP'], w=['PSP'])
                        if hh < 2:
                            P.I('dve', lambda e, hh=hh, i=i: e.tensor_tensor(out=RS[:, 4 + hh:5 + hh], in0=RS[:, hh:hh + 1], in1=GC[:, i, hh:hh + 1], op=OP.mult), r=['RS', 'GC'], w=['RS'])
                            pn, pnk = PN[hh], f"PN{hh}"
                            P.I('pool', lambda e, pn=pn, em=em, hh=hh: e.tensor_scalar(out=pn[:], in0=em[:], scalar1=RS[:, 4 + hh:5 + hh], scalar2=None, op0=OP.mult), r=[emk, 'RS'], w=[pnk])
                            for ct in range(4):
                                P.I('pe', lambda e, pn=pn, ct=ct, hh=hh: e.transpose(out=PSB[:, hh * 512 + ct * 128: hh * 512 + (ct + 1) * 128], in_=pn[:, ct * 128:(ct + 1) * 128], identity=C['ident'][:]),
                                    r=[pnk, 'c_ident'], w=[f'psb{hh}'])
                            pnt, pntk = PNT[hh], f"PNT{hh}"
                            P.I('act', lambda e, pnt=pnt, hh=hh: e.copy(out=pnt[:], in_=PSB[:, hh * 512:(hh + 1) * 512]), r=[f'psb{hh}'], w=[pntk])
                            po_, pok = PS[2 + hh], pk[2 + hh]
                            for ct in range(4):
                                _mm(P, po_[0:64, b * 128:(b + 1) * 128], VC[:, ct, :], pnt[:, ct * 128:(ct + 1) * 128], ct == 0, ct == 3, ['VC', pntk], [pok])
                    P.I('dve', lambda e: e.tensor_reduce(out=IMP[:], in_=PSP[:, 0:512].rearrange("p (s m) -> p s m", m=4), axis=AX.X, op=OP.add), r=['PSP'], w=['IMP'])
                    P.I('dve', lambda e: e.tensor_tensor(out=IMP[:], in0=IMP[:], in1=PSP[:, 4:516:4], op=OP.add), r=['PSP', 'IMP'], w=['IMP'])
                    x0 = 126 - 2 * i
                    P.I('dve', lambda e, x0=x0: e.tensor_tensor(out=SC[:], in0=IMP[:], in1=KEEP[:, x0:x0 + 128], op=OP.mult), r=['IMP', 'KEEP'], w=['SC'])
                    P.I('dve', lambda e, x0=x0: e.tensor_tensor(out=SC[:], in0=SC[:], in1=ADD[:, x0:x0 + 128], op=OP.add), r=['SC', 'ADD'], w=['SC'])
                    P.I('dve', lambda e: e.memset(SC[:, 0:1], 1.0e6), r=['SC'], w=['SC'])
                    P.I('dve', lambda e: e.max(out=M8[:, 0:8], in_=SC[:]), r=['SC'], w=['M8'])
                    P.I('dve', lambda e: e.match_replace(out=SC2[:], in_to_replace=M8[:, 0:8], in_values=SC[:], imm_value=-2.0), r=['SC', 'M8'], w=['SC2'])
                    P.I('dve', lambda e: e.max(out=M8[:, 8:16], in_=SC2[:]), r=['SC2'], w=['M8'])
                    P.I('dve', lambda e, x0=x0: e.scalar_tensor_tensor(out=SELM[:], in0=SC[:], scalar=M8[:, 15:16], in1=NF[:, x0:x0 + 128], op0=OP.is_ge, op1=OP.mult), r=['SC', 'M8', 'NF'], w=['SELM'])
                    P.I('dve', lambda e: e.tensor_scalar(out=MB[:], in0=SELM[:], scalar1=-1.0, scalar2=30000.0, op0=OP.add, op1=OP.mult), r=['SELM'], w=['MB'])
                    P.I('pe', lambda e: e.transpose(out=PSB[:, 0:128], in_=MB[:], identity=C['ident'][:]), r=['MB', 'c_ident'], w=['psb0'])
                    P.I('act', lambda e, b=b, par=par: e.copy(out=MBT[par][:, b * 128:(b + 1) * 128], in_=PSB[:, 0:128]), r=['psb0'], w=[mbk])
                for h in range(2):
                    P.I('act', lambda e, h=h, par=par: e.copy(out=OCc[par][h][:], in_=PS[2 + h][0:64, 0:512]), r=[pk[2 + h]], w=[f'OC{par}{h}'])

                for h in range(2):
                    po = 64 * h
                    pso, psok = PS[4], pk[4]
                    psw, pswk = PS[5], pk[5]
                    nj = 4 * qc + 4
                    for j in range(nj):
                        a = j - 4 * qc
                        c0 = 128 * max(a, 0)
                        pst, pstk = PS[j % 2], pk[j % 2]
                        _mm(P, pst[:, c0:512], KS[po:po + 64, j * 128:(j + 1) * 128], NQ[po:po + 64, qc * 512 + c0:(qc + 1) * 512], True, False, ['KS', 'NQ'], [pstk])
                        _mm(P, pst[:, c0:512], EJ[:, j * 128:(j + 1) * 128], MBT[par][:, c0:512], False, True, ['EJ', mbk], [pstk])
                        pt, ptk = PT[ptc % 3], f"PT{ptc % 3}"
                        ptc += 1
                        P.I('act', lambda e, pt=pt, pst=pst, c0=c0: e.activation(out=pt[:, c0:512], in_=pst[:, c0:512], func=AF.Exp, scale=SCALE), r=[pstk], w=[ptk])
                        if a >= 0:
                            P.I('dve', lambda e, pt=pt, c0=c0: e.tensor_tensor(out=pt[:, c0:c0 + 128], in0=pt[:, c0:c0 + 128], in1=C['tri'][:], op=OP.mult), r=[ptk, 'c_tri'], w=[ptk])
                        _mm(P, pso[0:65, c0:512], VS[:, j, :], pt[:, c0:512], j == 0, j == nj - 1, ['VS', ptk], [psok])
                    for a in range(-4, 4):
                        j = 4 * qc + a
                        if j < 0:
                            continue
                        pst, pstk = PS[j % 2], pk[j % 2]
                        _mm(P, pst[:, 0:512], KW[po:po + 64, j * 128:(j + 1) * 128], NQ[po:po + 64, qc * 512:(qc + 1) * 512], True, True, ['KW', 'NQ'], [pstk])
                        pt, ptk = PT[ptc % 3], f"PT{ptc % 3}"
                        ptc += 1
                        P.I('act', lambda e, pt=pt, pst=pst: e.activation(out=pt[:, 0:512], in_=pst[:, 0:512], func=AF.Exp, scale=SCALE), r=[pstk], w=[ptk])
                        P.I('dve', lambda e, pt=pt, a=a: e.tensor_tensor(out=pt[:, 0:512], in0=pt[:, 0:512], in1=WMASK[:, (a + 4) * 512:(a + 5) * 512], op=OP.mult), r=[ptk, 'WMASK'], w=[ptk])
                        jfirst = max(4 * qc - 4, 0)
                        _mm(P, psw[0:65, 0:512], VW[:, j, :], pt[:, 0:512], j == jfirst, a == 3, ['VW', ptk], [pswk])
                    Y = YN[h]; yk = f"YN{h}"
                    for br, (pacc, pacck) in enumerate(((pso, psok), (psw, pswk))):
                        P.I('dve', lambda e, pacc=pacc: e.reciprocal(out=RR[64:65, :], in_=pacc[64:65, 0:512]), r=[pacck], w=['RR'])
                        _mm(P, PS[6][0:64, 0:512], C['onesf'][64:65, 0:64], RR[64:65, :], True, True, ['c_onesf', 'RR'], [pk[6]])
                        P.I('act', lambda e: e.copy(out=BCS[:], in_=PS[6][0:64, 0:512]), r=[pk[6]], w=['BCS'])
                        gi = 2 * h + br
                        _mm(P, PS[6][0:64, 0:512], SEL[0:4, gi * 64:(gi + 1) * 64], G4[0:4, qc * 512:(qc + 1) * 512], True, True, ['SEL', 'G4'], [pk[6]])
                        P.I('act', lambda e: e.copy(out=BGS[:], in_=PS[6][0:64, 0:512]), r=[pk[6]], w=['BGS'])
                        P.I('dve', lambda e, pacc=pacc: e.tensor_tensor(out=TY[:], in0=pacc[0:64, 0:512], in1=BCS[:], op=OP.mult), r=[pacck, 'BCS'], w=['TY'])
                        P.I('dve', lambda e: e.tensor_tensor(out=TY[:], in0=TY[:], in1=BGS[:], op=OP.mult), r=['TY', 'BGS'], w=['TY'])
                        src = OCc[par][h] if br == 0 else Y
                        srck = f'OC{par}{h}' if br == 0 else yk
                        P.I('dve', lambda e, src=src, Y=Y: e.tensor_tensor(out=Y[:], in0=TY[:], in1=src[:], op=OP.add), r=['TY', srck], w=[yk])
                    P.D('sp', lambda e, Y=Y, h=h, qc=qc: e.dma_start(out=yT[128 + 64 * h:192 + 64 * h, qc * 512:(qc + 1) * 512], in_=Y[:]), r=[yk])
            P.barrier()

        with ExitStack() as e3:
            sb = lambda n, s, d: e3.enter_context(nc.sbuf_tensor(n, s, d))
            bf3 = sb("bf3", [128, 2], F32); bt3 = sb("bt3", [128, 130], F32)
            P.D('sp', lambda e: e.dma_start(out=bf3[:], in_=bfm3), w=['bf3'])
            P.D('sp', lambda e: e.dma_start(out=bt3[:], in_=btm3), w=['bt3'])
            FQ = sb("FQ", [128, T], BF16); FK = sb("FK", [128, T], BF16)
            FV = sb("FV", [128, NT, 2, 65], BF16); LF = sb("LF", [128, NT, 2], F32)
            P.I('pool', lambda e: e.memset(FV[:, :, :, 64:65], 1.0), w=['FV'])

            def ev3(dst, key, col):
                def f(tc, ps, pkk):
                    P.I('act', lambda e: e.activation(out=dst[:, tc * 512:(tc + 1) * 512], in_=ps[:, 0:512], func=AF.Identity, bias=bf3[:, col:col + 1], scale=1.0), r=[pkk, 'bf3'], w=[key])
                return f

            def tm3(ti, ps, pkk):
                P.I('dve', lambda e: e.tensor_tensor(out=FV[:, ti, :, 0:64], in0=ps[:, 0:128].rearrange("p (h d) -> p h d", d=64), in1=bt3[:, 0:128].rearrange("p (h d) -> p h d", d=64), op=OP.add),
                    r=[pkk, 'bt3'], w=['FV'])
                P.I('dve', lambda e: e.tensor_tensor(out=LF[:, ti, :], in0=ps[:, 128:130], in1=bt3[:, 128:130], op=OP.add), r=[pkk, 'bt3'], w=['LF'])

            _inproj(P, nc, e3, xT, wfm3, bf3, [(0, 128, ev3(FQ, 'FQ', 0)), (128, 128, ev3(FK, 'FK', 1))],
                    wtm3, 130, tm3, [(PS[0], pk[0]), (PS[1], pk[1]), (PS[2], pk[2]), (PS[3], pk[3])], "p3")
            LFv = LF[:].rearrange("p j h -> p (j h)")
            P.I('act', lambda e: e.activation(out=LFv, in_=LFv, func=AF.Exp, scale=-1.0), r=['LF'], w=['LF'])
            P.I('dve', lambda e: e.tensor_scalar(out=LFv, in0=LFv, scalar1=1.0, scalar2=None, op0=OP.add), r=['LF'], w=['LF'])
            P.I('act', lambda e: e.activation(out=LFv, in_=LFv, func=AF.Ln), r=['LF'], w=['LF'])
            P.I('dve', lambda e: e.tensor_scalar(out=LFv, in0=LFv, scalar1=-1.0, scalar2=None, op0=OP.mult), r=['LF'], w=['LF'])
            _mm(P, PS[0][:, 0:128], C['trif'][:], LFv, True, True, ['c_trif', 'LF'], [pk[0]])
            _mm(P, PS[1][:, 0:128], C['onesf'][:], LFv, True, True, ['c_onesf', 'LF'], [pk[1]])
            TOT = sb("TOT", [128, NT, 2], F32); INCL = sb("INCL", [128, NT, 2], F32); CUM = sb("CUM", [128, NT, 2], F32); ONE64 = sb("ONE64", [128, NT], F32)
            P.I('dve', lambda e: e.memset(ONE64[:], 1.0), w=['ONE64'])
            P.I('act', lambda e: e.copy(out=TOT[:].rearrange("p j h -> p (j h)"), in_=PS[1][:, 0:128]), r=[pk[1]], w=['TOT'])
            for h in range(2):
                P.I('dve', lambda e, h=h: e.tensor_tensor_scan(out=INCL[:, :, h], data0=ONE64[:], data1=TOT[:, :, h], initial=0.0, op0=OP.mult, op1=OP.add), r=['TOT', 'ONE64'], w=['INCL'])
            P.I('dve', lambda e: e.tensor_tensor(out=CUM[:].rearrange("p j h -> p (j h)"), in0=PS[0][:, 0:128], in1=INCL[:].rearrange("p j h -> p (j h)"), op=OP.add), r=[pk[0], 'INCL'], w=['CUM'])
            P.I('dve', lambda e: e.tensor_tensor(out=CUM[:], in0=CUM[:], in1=TOT[:], op=OP.subtract), r=['CUM', 'TOT'], w=['CUM'])

            BI = [sb(f"BI{i}", [128, 4, NT], F32) for i in range(2)]
            PT = [sb(f"FPT{i}", [128, 512], BF16) for i in range(3)]
            RR = sb("FRR", [65, 512], F32); BCS = sb("FBCS", [64, 512], F32); YF = [sb(f"YF{i}", [64, 512], F32) for i in range(2)]
            ptc = 0
            it = 0
            for h in range(2):
                po = 64 * h
                for qc in range(T // 512):
                    bi, bik = BI[it % 2], f"BI{it % 2}"
                    pso, psok = PS[4 + it % 2], pk[4 + it % 2]
                    for b in range(4):
                        i = 4 * qc + b
                        P.I('dve', lambda e, bi=bi, b=b, i=i, h=h: e.tensor_scalar(out=bi[:, b, 0:i + 1], in0=CUM[:, 0:i + 1, h], scalar1=-1.0, scalar2=INCL[:, i, h:h + 1], op0=OP.mult, op1=OP.add),
                            r=['CUM', 'INCL'], w=[bik])
                    nj = 4 * qc + 4
                    for j in range(nj):
                        a = j - 4 * qc
                        b0 = max(a, 0)
                        c0 = 128 * b0
                        pst, pstk = PS[j % 4], pk[j % 4]
                        _mm(P, pst[:, c0:512], FK[po:po + 64, j * 128:(j + 1) * 128], FQ[po:po + 64, qc * 512 + c0:(qc + 1) * 512], True, True, ['FK', 'FQ'], [pstk])
                        pt, ptk = PT[ptc % 3], f"FPT{ptc % 3}"
                        ptc += 1
                        for b in range(b0, 4):
                            P.I('act', lambda e, pt=pt, pst=pst, b=b, bi=bi, j=j: e.activation(out=pt[:, b * 128:(b + 1) * 128], in_=pst[:, b * 128:(b + 1) * 128], func=AF.Exp, bias=bi[:, b, j:j + 1], scale=SCALE),
                                r=[pstk, bik], w=[ptk])
                        if a >= 0:
                            P.I('dve', lambda e, pt=pt, c0=c0: e.tensor_tensor(out=pt[:, c0:c0 + 128], in0=pt[:, c0:c0 + 128], in1=C['tri'][:], op=OP.mult), r=[ptk, 'c_tri'], w=[ptk])
                        _mm(P, pso[0:65, c0:512], FV[:, j, h, :], pt[:, c0:512], j == 0, j == nj - 1, ['FV', ptk], [psok])
                    P.I('dve', lambda e, pso=pso: e.reciprocal(out=RR[64:65, :], in_=pso[64:65, 0:512]), r=[psok], w=['FRR'])
                    _mm(P, PS[6][0:64, 0:512], C['onesf'][64:65, 0:64], RR[64:65, :], True, True, ['c_onesf', 'FRR'], [pk[6]])
                    P.I('act', lambda e: e.copy(out=BCS[:], in_=PS[6][0:64, 0:512]), r=[pk[6]], w=['FBCS'])
                    Y = YF[it % 2]; yk = f"YF{it % 2}"
                    P.I('dve', lambda e, pso=pso, Y=Y: e.tensor_tensor(out=Y[:], in0=pso[0:64, 0:512], in1=BCS[:], op=OP.mult), r=[psok, 'FBCS'], w=[yk])
                    P.D('sp', lambda e, Y=Y, h=h, qc=qc: e.dma_start(out=yT[256 + 64 * h:320 + 64 * h, qc * 512:(qc + 1) * 512], in_=Y[:]), r=[yk])
                    it += 1
        P.finish()
    return nc


def _prep_A(inp, l, b, j, x_b):
    g = j // 2
    own = [2 * j, 2 * j + 1]
    oth = [2 * j + 2, 2 * j + 3] if j % 2 == 0 else [2 * j - 2, 2 * j - 1]
    w_in = inp['w_in'][l]; b_in = inp['b_in'][l]
    OQ, OKV, OG, OFX, OF = 512, 1024, 1792, 1816, 3352
    r64 = np.arange(64)
    kvc = lambda n, kvi: OKV + ((n * 2 + kvi) * 2 + g) * 64 + r64
    fx = lambda q, h: OFX + (q * 8 + h) * 64 + r64
    fm1 = np.concatenate([128 * j + np.arange(128), kvc(0, 0), kvc(0, 1)])
    fm2 = np.concatenate([OQ + 64 * own[0] + r64, OQ + 64 * own[1] + r64, OQ + 64 * oth[0] + r64, OQ + 64 * oth[1] + r64,
                          kvc(1, 0), kvc(1, 0), kvc(2, 0), kvc(2, 0),
                          [OG + own[0] * 3 + 1, OG + own[0] * 3 + 2, OG + own[1] * 3 + 1, OG + own[1] * 3 + 2]]).astype(np.int64)
    tm2 = np.concatenate([kvc(1, 1), kvc(2, 1), [OG + own[0] * 3, OG + own[1] * 3]]).astype(np.int64)
    fm3 = np.concatenate([fx(0, own[0]), fx(0, own[1]), fx(1, own[0]), fx(1, own[1])])
    tm3 = np.concatenate([fx(2, own[0]), fx(2, own[1]), [OF + own[0], OF + own[1]]]).astype(np.int64)

    def fmb(cols, nch):
        bb = np.zeros((128, nch), np.float32)
        v = b_in[cols]
        for c in range(nch):
            seg = v[c * 128:(c + 1) * 128]
            bb[:len(seg), c] = seg
        return bb
    c32 = np.ascontiguousarray
    win = POOL_WINDOWS[j]
    cwv = np.zeros((128, 4), np.float32); cwv[:, j] = 1.0 / win
    fixv = np.tile((win / np.minimum(np.arange(16) + 1, win)).astype(np.float32)[None, :], (128, 1))
    cw1 = inp['cmp_w1'][l]
    cw1r = np.concatenate([cw1[kv].reshape(32, 64, 256).transpose(1, 0, 2).reshape(64, 32 * 256) for kv in range(2)], axis=0)
    posT = np.concatenate([inp['cmp_pos'][l][kv].T for kv in range(2)], axis=0)
    cb1 = inp['cmp_b1'][l].reshape(2, 2, 128).transpose(2, 0, 1).reshape(128, 4)
    w2 = inp['cmp_w2'][l]
    cw2k = np.concatenate([np.concatenate([w2[0][hf * 128:(hf + 1) * 128], w2[0][hf * 128:(hf + 1) * 128]], axis=1) for hf in range(2)], axis=1)
    cb2k = np.concatenate([inp['cmp_b2'][l][0], inp['cmp_b2'][l][0]])[:, None]
    cw2v = np.concatenate([w2[1][hf * 128:(hf + 1) * 128] for hf in range(2)], axis=1)
    cb2v = np.tile(inp['cmp_b2'][l][1][None, :], (128, 1))
    return {
        "xT": x_b,
        "wfm1": c32(w_in[:, fm1]), "bfm1": fmb(fm1, 2),
        "wfm2": c32(w_in[:, fm2]), "bfm2": fmb(fm2, 5),
        "wtm2": c32(w_in[:, tm2]), "btm2": c32(np.tile(b_in[tm2][None, :], (128, 1))),
        "wfm3": c32(w_in[:, fm3]), "bfm3": fmb(fm3, 2),
        "wtm3": c32(w_in[:, tm3]), "btm3": c32(np.tile(b_in[tm3][None, :], (128, 1))),
        "pw": c32(inp['pool_w'][l][j]), "pbs": c32(np.stack([inp['pool_b'][l][j], inp['pool_scale'][l][128 * j:128 * j + 128]], axis=1)),
        "cw": cwv, "fix": c32(fixv),
        "cw1": c32(cw1r), "posT": c32(posT), "cb1": c32(cb1), "cw2k": c32(cw2k), "cb2k": c32(cb2k.astype(np.float32)),
        "cw2v": c32(cw2v), "cb2v": c32(cb2v),
    }


_NC = {}


def run_A(inp, l, x):
    if 'A' not in _NC:
        _NC['A'] = build_A()
    xTs = [np.ascontiguousarray(x[b].T) for b in range(2)]
    maps = [_prep_A(inp, l, c // 4, c % 4, xTs[c // 4]) for c in range(8)]
    res = run_bass_kernel_spmd(_NC['A'], maps, core_ids=list(range(8)))
    ys = np.empty((2, T, 1536), np.float32)
    for c in range(8):
        b, j = c // 4, c % 4
        yt = res.results[c]["yT"]
        for n in range(3):
            ys[b, :, n * 512 + 128 * j: n * 512 + 128 * j + 128] = yt[n * 128:(n + 1) * 128, :].T
    return ys


NTB = 2048
NE = 32
CAP = 384
U32 = mybir.dt.uint32


def _layernorm(P, nc, R, rk, g_t, b_t, out, outk, ST, MV, tag):
    for c in range(2):
        P.I('dve', lambda e, c=c: e.bn_stats(out=ST[:, c, :], in_=R[:, c * 512:(c + 1) * 512]), r=[rk], w=['ST' + tag])
    P.I('dve', lambda e: e.bn_aggr(out=MV[:, 0:2], in_=ST[:].rearrange("p c s -> p (c s)")), r=['ST' + tag], w=['MV' + tag])
    P.I('dve', lambda e: e.tensor_scalar(out=MV[:, 2:3], in0=MV[:, 1:2], scalar1=LN_EPS, scalar2=None, op0=OP.add), r=['MV' + tag], w=['MV' + tag])
    P.I('act', lambda e: e.activation(out=MV[:, 2:3], in_=MV[:, 2:3], func=AF.Sqrt), r=['MV' + tag], w=['MV' + tag])
    P.I('dve', lambda e: e.reciprocal(out=MV[:, 2:3], in_=MV[:, 2:3]), r=['MV' + tag], w=['MV' + tag])
    P.I('dve', lambda e: e.tensor_scalar(out=out, in0=R[:], scalar1=MV[:, 0:1], scalar2=MV[:, 2:3], op0=OP.subtract, op1=OP.mult), r=[rk, 'MV' + tag], w=[outk])
    P.I('dve', lambda e: e.tensor_tensor(out=out, in0=out, in1=g_t[:], op=OP.mult), r=[outk, 'lng' + tag], w=[outk])
    P.I('dve', lambda e: e.tensor_tensor(out=out, in0=out, in1=b_t[:], op=OP.add), r=[outk, 'lnb' + tag], w=[outk])


def build_B():
    nc = bass.Bass("TRN2", target_bir_lowering=False)
    dt = lambda n, s, k="ExternalInput": nc.dram_tensor(n, s, F32, kind=k).ap()
    xT = dt("xT", [D, NTB]); xtok = dt("xtok", [NTB, D]); yT = dt("yT", [1536, NTB])
    wg_d = dt("wg", [D, 3072]); bg_d = dt("bg", [128, 24]); wup_d = dt("wup", [1536, D]); wo_d = dt("wo", [D, D])
    l1g_d = dt("l1g", [128, D]); l1b_d = dt("l1b", [128, D]); l2g_d = dt("l2g", [128, D]); l2b_d = dt("l2b", [128, D])
    rw_d = dt("rw", [D, NE]); rb_d = dt("rb", [128, NE])
    w1_d = dt("w1", [NE, D, 2048]); b1_d = dt("b1", [128, NE * 16]); w2_d = dt("w2", [NE, D, D]); b2_d = dt("b2", [NE, D])
    xo = dt("xo", [NTB, D], "ExternalOutput")
    x1s = dt("x1s", [NTB, D], "Internal")
    xg_d = nc.dram_tensor("xg", [NE * CAP, D], BF16, kind="Internal").ap()
    yg_d = dt("yg", [NE * CAP, D], "Internal")
    NTT = NTB // 128

    with ExitStack() as es:
        P = Prog(nc, es)
        sbg = lambda n, s, d: es.enter_context(nc.sbuf_tensor(n, s, d))
        PA = [es.enter_context(nc.psum_tensor(f"pa{i}", [128, 512], F32)) for i in range(4)]
        pak = [f"pa{i}" for i in range(4)]
        PH = es.enter_context(nc.psum_tensor("ph", [128, 1024], F32))
        PSB = es.enter_context(nc.psum_tensor("psb", [128, 1024], BF16))
        PR = es.enter_context(nc.psum_tensor("pr", [128, 512], F32))
        C = _consts(P, nc, sbg)
        identf = sbg("identf", [128, 128], F32)
        P.I('dve', lambda e: e.tensor_copy(out=identf[:], in_=C['ident'][:]), r=['c_ident'], w=['identf'])
        DSTI = sbg("DSTI", [128, NTT, 4], U32)
        GR = sbg("GR", [128, NTT, 4], F32)
        GT = sbg("GT", [128, NTT, NE], F32)
        ST = sbg("ST", [128, 2, 6], F32); MV = sbg("MV", [128, 4], F32)

        with ExitStack() as e1:
            sb = lambda n, s, d: e1.enter_context(nc.sbuf_tensor(n, s, d))
            WG = sb("WG", [128, 8, 3072], BF16); WU = sb("WU", [128, 12, D], BF16); WO = sb("WO", [128, 8, D], BF16)
            for k in range(8):
                P.D('pool', lambda e, k=k: e.dma_start(out=WG[:, k, :], in_=wg_d[k * 128:(k + 1) * 128, :]), w=['WG'])
                P.D('pool', lambda e, k=k: e.dma_start(out=WO[:, k, :], in_=wo_d[k * 128:(k + 1) * 128, :]), w=['WO'])
            for k in range(12):
                P.D('pool', lambda e, k=k: e.dma_start(out=WU[:, k, :], in_=wup_d[k * 128:(k + 1) * 128, :]), w=['WU'])
            bg = sb("bg_s", [128, 24], F32); l1g = sb("l1g_s", [128, D], F32); l1b = sb("l1b_s", [128, D], F32)
            RW = sb("RW", [128, 8, NE], F32); rb = sb("rb_s", [128, NE], F32)
            P.D('sp', lambda e: e.dma_start(out=bg[:], in_=bg_d), w=['bg'])
            P.D('sp', lambda e: e.dma_start(out=l1g[:], in_=l1g_d), w=['lng1'])
            P.D('sp', lambda e: e.dma_start(out=l1b[:], in_=l1b_d), w=['lnb1'])
            P.D('sp', lambda e: e.dma_start(out=RW[:], in_=rw_d.rearrange("(k p) n -> p k n", p=128)), w=['RW'])
            P.D('sp', lambda e: e.dma_start(out=rb[:], in_=rb_d), w=['rb'])
            XB = [sb(f"XB{i}", [128, 8, 512], BF16) for i in range(2)]
            YB = [sb(f"YB{i}", [128, 12, 512], BF16) for i in range(1)]
            GS = sb("GS", [128, 512], F32); TM = sb("TM", [128, 512], F32); MA = sb("MA", [128, 512], F32)
            MT = sb("MT", [128, 8, 512], BF16)
            XT_ = [sb(f"XTK{i}", [128, D], F32) for i in range(2)]
            RR_ = sb("RRb", [128, D], F32); X1 = sb("X1", [128, D], F32); X1B = sb("X1B", [128, D], BF16)
            X1T32 = sb("X1T32", [128, 8, 128], F32); LG = sb("LG", [128, NE], F32); M8 = sb("M8b", [128, 8], F32)
            EXg = sb("EXg", [128, NE], F32); MK = sb("MK", [128, NE], F32); SM = sb("SM", [128, 2], F32)
            TRIS = sb("TRIS", [128, 128], BF16); ONESB = sb("ONESB", [128, 128], BF16); RUNE = sb("RUNE", [128, NE], F32)
            P.I('dve', lambda e: e.tensor_scalar(out=TRIS[:], in0=C['trif'][:], scalar1=0.0, scalar2=None, op0=OP.add), r=['c_trif'], w=['TRIS'])
            P.I('dve', lambda e: e.tensor_tensor(out=TRIS[:], in0=TRIS[:], in1=C['ident'][:], op=OP.subtract), r=['TRIS', 'c_ident'], w=['TRIS'])
            P.I('dve', lambda e: e.memset(ONESB[:], 1.0), w=['ONESB'])
            P.I('pool', lambda e: e.iota(RUNE[:], pattern=[[CAP, NE]], base=0, channel_multiplier=0, allow_small_or_imprecise_dtypes=True), w=['RUNE'])
            MKB = sb("MKB", [128, NE], BF16); DESTF = sb("DESTF", [128, NE], F32); TMPR = sb("TMPR", [128, NE], F32); DSTF = sb("DSTF", [128, 4], F32)
            X1Bs = [X1B, sb("X1B2", [128, D], BF16)]
            ZR = sb("ZR", [128, D], BF16)
            P.I('dve', lambda e: e.memset(ZR[:], 0.0), w=['ZR'])
            for zb in range(NE * CAP // 128):
                P.D('sp', lambda e, zb=zb: e.dma_start(out=xg_d[zb * 128:(zb + 1) * 128, :], in_=ZR[:]), r=['ZR'], w=['xg'])
            xv = xT.rearrange("(k p) t -> p k t", p=128)
            yv = yT.rearrange("(k p) t -> p k t", p=128)
            pi = 0
            for tc in range(NTB // 512):
                X = XB[tc % 2]; Y = YB[0]; xk = f"XB{tc % 2}"; yk = "YB0"
                for k2 in range(2):
                    P.D('pool', lambda e, X=X, tc=tc, k2=k2: e.dma_start(out=X[:, 4 * k2:4 * k2 + 4, :], in_=xv[:, 4 * k2:4 * k2 + 4, tc * 512:(tc + 1) * 512]), w=[xk])
                for k3 in range(3):
                    P.D('pool', lambda e, Y=Y, tc=tc, k3=k3: e.dma_start(out=Y[:, 4 * k3:4 * k3 + 4, :], in_=yv[:, 4 * k3:4 * k3 + 4, tc * 512:(tc + 1) * 512]), w=[yk])
                for dc in range(8):
                    for n in range(3):
                        pg, pgk = PA[pi % 4], pak[pi % 4]; pi += 1
                        pu, puk = PA[pi % 4], pak[pi % 4]; pi += 1
                        col = n * 1024 + dc * 128
                        for k in range(8):
                            _mm(P, pg[:, 0:512], WG[:, k, col:col + 128], X[:, k, :], k == 0, k == 7, ['WG', xk], [pgk])
                        for k in range(4):
                            _mm(P, pu[:, 0:512], WU[:, n * 4 + k, dc * 128:(dc + 1) * 128], Y[:, n * 4 + k, :], k == 0, k == 3, ['WU', yk], [puk])
                        bc_ = n * 8 + dc
                        P.I('act', lambda e, pg=pg, bc_=bc_: e.activation(out=GS[:], in_=pg[:, 0:512], func=AF.Sigmoid, bias=bg[:, bc_:bc_ + 1], scale=1.0), r=[pgk, 'bg'], w=['GS'])
                        if n == 0:
                            P.I('dve', lambda e, pu=pu: e.tensor_tensor(out=MA[:], in0=pu[:, 0:512], in1=GS[:], op=OP.mult), r=[puk, 'GS'], w=['MA'])
                        else:
                            P.I('dve', lambda e, pu=pu: e.tensor_tensor(out=TM[:], in0=pu[:, 0:512], in1=GS[:], op=OP.mult), r=[puk, 'GS'], w=['TM'])
                            if n == 1:
                                P.I('dve', lambda e: e.tensor_tensor(out=MA[:], in0=MA[:], in1=TM[:], op=OP.add), r=['MA', 'TM'], w=['MA'])
                            else:
                                P.I('dve', lambda e, dc=dc: e.tensor_tensor(out=MT[:, dc, :], in0=MA[:], in1=TM[:], op=OP.add), r=['MA', 'TM'], w=['MT'])
                for tt in range(4):
                    ti = tc * 4 + tt
                    xt = XT_[ti % 2]; xtk = f"XTK{ti % 2}"
                    P.D('sp', lambda e, xt=xt, ti=ti: e.dma_start(out=xt[:], in_=xtok[ti * 128:(ti + 1) * 128, :]), w=[xtk])
                    for hf in range(2):
                        for k in range(8):
                            _mm(P, PH[:, hf * 512:(hf + 1) * 512], MT[:, k, tt * 128:(tt + 1) * 128], WO[:, k, hf * 512:(hf + 1) * 512], k == 0, k == 7, ['MT', 'WO'], ['ph'])
                    P.I('dve', lambda e, xt=xt: e.scalar_tensor_tensor(out=RR_[:], in0=xt[:], scalar=ALPHA, in1=PH[:, :], op0=OP.mult, op1=OP.add), r=[xtk, 'ph'], w=['RRb'])
                    _layernorm(P, nc, RR_, 'RRb', l1g, l1b, X1[:], 'X1', ST, MV, '1')
                    P.D('sp', lambda e, ti=ti: e.dma_start(out=x1s[ti * 128:(ti + 1) * 128, :], in_=X1[:]), r=['X1'], w=['x1s'])
                    x1b = X1Bs[ti % 2]; x1bk = f"X1B{ti % 2}"
                    P.I('act', lambda e, x1b=x1b: e.copy(out=x1b[:], in_=X1[:]), r=['X1'], w=[x1bk])
                    for k in range(8):
                        P.I('pe', lambda e, k=k: e.transpose(out=PH[:, k * 128:(k + 1) * 128], in_=X1[:, k * 128:(k + 1) * 128], identity=identf[:]), r=['X1', 'identf'], w=['ph'])
                    P.I('act', lambda e: e.copy(out=X1T32[:].rearrange("p k t -> p (k t)"), in_=PH[:, :]), r=['ph'], w=['X1T32'])
                    for k in range(8):
                        _mm(P, PR[:, 0:NE], X1T32[:, k, :], RW[:, k, :], k == 0, k == 7, ['X1T32', 'RW'], ['pr'])
                    P.I('dve', lambda e: e.tensor_tensor(out=LG[:], in0=PR[:, 0:NE], in1=rb[:], op=OP.add), r=['pr', 'rb'], w=['LG'])
                    P.I('dve', lambda e: e.max(out=M8[:], in_=LG[:]), r=['LG'], w=['M8b'])
                    P.I('dve', lambda e: e.tensor_scalar(out=SM[:, 0:1], in0=M8[:, 0:1], scalar1=-1.0, scalar2=None, op0=OP.mult), r=['M8b'], w=['SM'])
                    P.I('act', lambda e: e.activation(out=EXg[:], in_=LG[:], func=AF.Exp, bias=SM[:, 0:1], scale=1.0), r=['LG', 'SM'], w=['EXg'])
                    P.I('dve', lambda e: e.scalar_tensor_tensor(out=MK[:], in0=LG[:], scalar=M8[:, 3:4], in1=EXg[:], op0=OP.is_ge, op1=OP.mult), r=['LG', 'M8b', 'EXg'], w=['MK'])
                    P.I('dve', lambda e: e.reduce_sum(out=SM[:, 1:2], in_=MK[:], axis=AX.X), r=['MK'], w=['SM'])
                    P.I('dve', lambda e: e.reciprocal(out=SM[:, 1:2], in_=SM[:, 1:2]), r=['SM'], w=['SM'])
                    P.I('dve', lambda e, ti=ti: e.tensor_scalar(out=GT[:, ti, :], in0=MK[:], scalar1=SM[:, 1:2], scalar2=None, op0=OP.mult), r=['MK', 'SM'], w=['GT'])
                    P.I('dve', lambda e: e.tensor_scalar(out=MKB[:], in0=LG[:], scalar1=M8[:, 3:4], scalar2=None, op0=OP.is_ge), r=['LG', 'M8b'], w=['MKB'])
                    _mm(P, PR[:, 32:64], TRIS[:], MKB[:], True, True, ['TRIS', 'MKB'], ['pr'])
                    _mm(P, PR[:, 64:96], ONESB[:], MKB[:], True, True, ['ONESB', 'MKB'], ['pr'])
                    P.I('dve', lambda e: e.tensor_tensor(out=DESTF[:], in0=PR[:, 32:64], in1=RUNE[:], op=OP.add), r=['pr', 'RUNE'], w=['DESTF'])
                    P.I('dve', lambda e: e.tensor_tensor(out=RUNE[:], in0=PR[:, 64:96], in1=RUNE[:], op=OP.add), r=['pr', 'RUNE'], w=['RUNE'])
                    for r_ in range(4):
                        P.I('dve', lambda e, r_=r_: e.scalar_tensor_tensor(out=TMPR[:], in0=LG[:], scalar=M8[:, r_:r_ + 1], in1=DESTF[:], op0=OP.is_equal, op1=OP.mult), r=['LG', 'M8b', 'DESTF'], w=['TMPR'])
                        P.I('dve', lambda e, r_=r_: e.reduce_sum(out=DSTF[:, r_:r_ + 1], in_=TMPR[:], axis=AX.X), r=['TMPR'], w=['DSTF'])
                        P.I('dve', lambda e, r_=r_, ti=ti: e.scalar_tensor_tensor(out=TMPR[:], in0=LG[:], scalar=M8[:, r_:r_ + 1], in1=GT[:, ti, :], op0=OP.is_equal, op1=OP.mult), r=['LG', 'M8b', 'GT'], w=['TMPR'])
                        P.I('dve', lambda e, r_=r_, ti=ti: e.reduce_sum(out=GR[:, ti, r_:r_ + 1], in_=TMPR[:], axis=AX.X), r=['TMPR'], w=['GR'])
                    P.I('dve', lambda e, ti=ti: e.tensor_copy(out=DSTI[:, ti, :], in_=DSTF[:]), r=['DSTF'], w=['DSTI'])
                    for r_ in range(4):
                        P.D('pool', lambda e, r_=r_, ti=ti, x1b=x1b: e.indirect_dma_start(out=xg_d, out_offset=bass.IndirectOffsetOnAxis(ap=DSTI[:, ti, r_:r_ + 1], axis=0), in_=x1b[:], in_offset=None),
                            r=[x1bk, 'DSTI'], w=['xg'])
            P.barrier()

        NCT = CAP // 128
        with ExitStack() as e2:
            sb = lambda n, s, d: e2.enter_context(nc.sbuf_tensor(n, s, d))
            W1 = [sb(f"W1_{i}", [128, 8, 2048], BF16) for i in range(2)]
            W2 = [sb(f"W2_{i}", [128, 8, D], BF16) for i in range(1)]
            B1 = sb("B1", [128, NE * 16], F32)
            P.D('sp', lambda e: e.dma_start(out=B1[:], in_=b1_d), w=['B1'])
            ACC = sb("ACC", [128, NTT, D], F32)
            B2 = sb("B2", [NE, D], F32); GTT = sb("GTT", [NE, 128], F32)
            P.D('sp', lambda e: e.dma_start(out=B2[:], in_=b2_d), w=['B2'])
            XG = [sb(f"XG{i}", [128, NCT, D], BF16) for i in range(2)]
            XGT = [sb(f"XGT{i}", [128, 8, CAP], BF16) for i in range(2)]
            AT = sb("AT", [128, 8, CAP], BF16)
            GEN = [sb(f"GEN{i}", [128, D], F32) for i in range(3)]
            YRS = [sb(f"YRS{i}", [128, D], F32) for i in range(2)]
            GG = GEN[0][:, 0:CAP]; SG = GEN[0][:, 512:512 + CAP]; UU = GEN[1][:, 0:CAP]
            YO = [GEN[1], GEN[2]]

            def load_w(ex):
                s_ = ex % 2
                P.D('pool', lambda e: e.dma_start(out=W1[s_][:], in_=w1_d[ex].rearrange("(k p) n -> p k n", p=128)), w=[f'W1_{s_}'])

            def load_w2(ex):
                P.D('pool', lambda e: e.dma_start(out=W2[0][:], in_=w2_d[ex].rearrange("(k p) n -> p k n", p=128)), w=['W2_0'])

            def load_x(ex):
                s_ = ex % 2
                P.D('sp', lambda e: e.dma_start(out=XG[s_][:], in_=xg_d[ex * CAP:(ex + 1) * CAP, :].rearrange("(t p) d -> p t d", p=128)), r=['xg'], w=[f'XG{s_}'])

            load_w(0)
            load_w2(0)
            load_x(0)
            for ti in range(NTT):
                P.D('sp', lambda e, ti=ti: e.dma_start(out=ACC[:, ti, :], in_=x1s[ti * 128:(ti + 1) * 128, :]), w=[f'ACC{ti}'])
                P.I('act', lambda e, ti=ti: e.activation(out=ACC[:, ti, :], in_=ACC[:, ti, :], func=AF.Copy, scale=ALPHA), r=[f'ACC{ti}'], w=[f'ACC{ti}'])
                P.I('pe', lambda e, ti=ti: e.transpose(out=PR[0:NE, 128:256], in_=GT[:, ti, :], identity=identf[:]), r=['GT', 'identf'], w=['pr'])
                P.I('act', lambda e: e.copy(out=GTT[:], in_=PR[0:NE, 128:256]), r=['pr'], w=['GTT'])
                for hf in range(2):
                    _mm(P, PH[:, hf * 512:(hf + 1) * 512], GTT[:], B2[:, hf * 512:(hf + 1) * 512], True, True, ['GTT', 'B2'], ['ph'])
                P.I('dve', lambda e, ti=ti: e.tensor_tensor(out=ACC[:, ti, :], in0=ACC[:, ti, :], in1=PH[:, :], op=OP.add), r=[f'ACC{ti}', 'ph'], w=[f'ACC{ti}'])
            pi = 0
            yoc = 0
            for ex in range(NE):
                if ex + 1 < NE:
                    load_w(ex + 1)
                    load_x(ex + 1)
                s_ = ex % 2
                w1k, w2k, xgk, xgtk = f'W1_{s_}', 'W2_0', f'XG{s_}', f'XGT{s_}'
                for tt in range(NCT):
                    for k in range(8):
                        P.I('pe', lambda e, k=k, tt=tt: e.transpose(out=PSB[:, k * 128:(k + 1) * 128], in_=XG[s_][:, tt, k * 128:(k + 1) * 128], identity=C['ident'][:]), r=[xgk, 'c_ident'], w=['psb'])
                    P.I('act', lambda e, tt=tt: e.copy(out=XGT[s_][:, :, tt * 128:(tt + 1) * 128], in_=PSB[:, :].rearrange("p (k t) -> p k t", t=128)), r=['psb'], w=[xgtk])
                for f in range(8):
                    pg, pgk = PA[pi % 4], pak[pi % 4]; pi += 1
                    pu, puk = PA[pi % 4], pak[pi % 4]; pi += 1
                    for k in range(8):
                        _mm(P, pg[:, 0:CAP], W1[s_][:, k, f * 128:(f + 1) * 128], XGT[s_][:, k, :], k == 0, k == 7, [w1k, xgtk], [pgk])
                    for k in range(8):
                        _mm(P, pu[:, 0:CAP], W1[s_][:, k, 1024 + f * 128:1024 + (f + 1) * 128], XGT[s_][:, k, :], k == 0, k == 7, [w1k, xgtk], [puk])
                    cg = ex * 16 + f; cu = ex * 16 + 8 + f
                    P.I('dve', lambda e, pg=pg, cg=cg: e.tensor_scalar(out=GG, in0=pg[:, 0:CAP], scalar1=B1[:, cg:cg + 1], scalar2=7.0, op0=OP.add, op1=OP.min), r=[pgk, 'B1'], w=['GEN0'])
                    P.I('act', lambda e: e.activation(out=SG, in_=GG, func=AF.Sigmoid, scale=1.702), r=['GEN0'], w=['GEN0'])
                    P.I('dve', lambda e, pu=pu, cu=cu: e.tensor_scalar(out=UU, in0=pu[:, 0:CAP], scalar1=B1[:, cu:cu + 1], scalar2=7.0, op0=OP.add, op1=OP.min), r=[puk, 'B1'], w=['GEN1'])
                    P.I('pool', lambda e: e.tensor_scalar(out=UU, in0=UU, scalar1=-7.0, scalar2=1.0, op0=OP.max, op1=OP.add), r=['GEN1'], w=['GEN1'])
                    P.I('pool', lambda e: e.tensor_tensor(out=GG, in0=GG, in1=UU, op=OP.mult), r=['GEN0', 'GEN1'], w=['GEN0'])
                    P.I('dve', lambda e, f=f: e.tensor_tensor(out=AT[:, f, :], in0=GG, in1=SG, op=OP.mult), r=['GEN0'], w=['AT'])
                for tt in range(NCT):
                    for hf in range(2):
                        for f in range(8):
                            _mm(P, PH[:, hf * 512:(hf + 1) * 512], AT[:, f, tt * 128:(tt + 1) * 128], W2[0][:, f, hf * 512:(hf + 1) * 512], f == 0, f == 7, ['AT', w2k], ['ph'])
                    yo = GEN[2]; yok = 'GEN2'
                    P.I('act', lambda e, yo=yo: e.copy(out=yo[:], in_=PH[:, :]), r=['ph'], w=[yok])
                    row = ex * CAP + tt * 128
                    P.D('sp', lambda e, yo=yo, row=row: e.dma_start(out=yg_d[row:row + 128, :], in_=yo[:]), r=[yok], w=['yg'])
                if ex + 1 < NE:
                    load_w2(ex + 1)
            P.barrier()
            l2g = GEN[0]; l2b = GEN[1]
            P.D('sp', lambda e: e.dma_start(out=l2g[:], in_=l2g_d), w=['lng2'])
            P.D('sp', lambda e: e.dma_start(out=l2b[:], in_=l2b_d), w=['lnb2'])
            gi = 0
            for ti in range(NTT):
                for r_ in range(4):
                    yr = YRS[gi % 2]; yrk = f"YRS{gi % 2}"; gi += 1
                    P.D('pool', lambda e, yr=yr, ti=ti, r_=r_: e.indirect_dma_start(out=yr[:], out_offset=None, in_=yg_d, in_offset=bass.IndirectOffsetOnAxis(ap=DSTI[:, ti, r_:r_ + 1], axis=0)),
                        r=['yg', 'DSTI'], w=[yrk])
                    P.I('dve', lambda e, yr=yr, ti=ti, r_=r_: e.scalar_tensor_tensor(out=ACC[:, ti, :], in0=yr[:], scalar=GR[:, ti, r_:r_ + 1], in1=ACC[:, ti, :], op0=OP.mult, op1=OP.add),
                        r=[yrk, 'GR', f'ACC{ti}'], w=[f'ACC{ti}'])
                o = GEN[2]; ok = "GEN2"
                _layernorm(P, nc, ACC[:, ti, :], f'ACC{ti}', l2g, l2b, o[:], ok, ST, MV, '2')
                P.D('sp', lambda e, o=o, ti=ti: e.dma_start(out=xo[ti * 128:(ti + 1) * 128, :], in_=o[:]), r=[ok])
        P.finish()
    return nc


def _prep_B(inp, l, b, r, x, ys):
    tok = slice(NTB * r, NTB * (r + 1))
    c32 = lambda a: np.ascontiguousarray(a, dtype=np.float32)
    w_in = inp['w_in'][l]; b_in = inp['b_in'][l]
    bc = lambda v: c32(np.tile(v[None, :], (128, 1)))
    return {
        "xT": c32(x[b, tok].T), "xtok": c32(x[b, tok]), "yT": c32(ys[b, tok].T),
        "wg": c32(w_in[:, 3360:6432]), "bg": c32(b_in[3360:6432].reshape(24, 128).T),
        "wup": c32(inp['w_up'][l].reshape(1536, D)), "wo": c32(inp['w_o'][l]),
        "l1g": bc(inp['ln1_g'][l]), "l1b": bc(inp['ln1_b'][l]), "l2g": bc(inp['ln2_g'][l]), "l2b": bc(inp['ln2_b'][l]),
        "rw": c32(inp['router_w'][l]), "rb": bc(inp['router_b'][l]),
        "w1": inp['moe_w1'][l], "b1": c32(inp['moe_b1'][l].reshape(NE * 16, 128).T), "w2": inp['moe_w2'][l], "b2": c32(inp['moe_b2'][l]),
    }


def run_B(inp, l, x, ys):
    if 'B' not in _NC:
        _NC['B'] = build_B()
    maps = [_prep_B(inp, l, c // 4, c % 4, x, ys) for c in range(8)]
    res = run_bass_kernel_spmd(_NC['B'], maps, core_ids=list(range(8)))
    out = np.empty((2, T, D), np.float32)
    for c in range(8):
        out[c // 4, NTB * (c % 4):NTB * (c % 4 + 1)] = res.results[c]["xo"]
    return out


def kernel(**inputs):
    inp = {k: np.asarray(v) for k, v in inputs.items()}
    x = np.ascontiguousarray(inp['x'], dtype=np.float32)
    for l in range(2):
        ys = run_A(inp, l, x)
        x = run_B(inp, l, x, ys)
    return x
```

```python
import numpy as np
from contextlib import ExitStack
import concourse.bass as bass
import concourse.mybir as mybir
from concourse.bass_utils import run_bass_kernel_spmd

F32 = mybir.dt.float32
BF16 = mybir.dt.bfloat16
AF = mybir.ActivationFunctionType
OP = mybir.AluOpType
AX = mybir.AxisListType

T = 8192
D = 1024
NT = T // 128
SCALE = 0.125
ALPHA = 4.0 ** 0.25
POOL_WINDOWS = (2, 4, 8, 16)
LN_EPS = 1e-5


class Prog:
    NDMA = 12

    def __init__(self, nc, es):
        self.nc = nc
        self.eng = {'pe': nc.tensor, 'act': nc.scalar, 'dve': nc.vector, 'pool': nc.gpsimd, 'sp': nc.sync}
        self.sem = {n: es.enter_context(nc.semaphore("s_" + n)) for n in self.eng}
        self.cnt = {n: 0 for n in self.eng}
        self.dsem = [es.enter_context(nc.semaphore(f"d{i}")) for i in range(self.NDMA)]
        self.dval = [0] * self.NDMA
        self.dnext = 0
        self.seen = {n: {} for n in self.eng}
        self.lastw = {}
        self.lastw_isread = {}
        self.readers = {}

    def _wait(self, en, tok):
        if tok is None:
            return
        kind, a, v = tok
        if kind == 'e' and a == en and en == 'pe':
            return
        src = (kind, a)
        if self.seen[en].get(src, 0) >= v:
            return
        s = self.sem[a] if kind == 'e' else self.dsem[a]
        self.eng[en].wait_ge(s, v)
        self.seen[en][src] = v

    def _deps(self, en, r, w):
        for k in r:
            self._wait(en, self.lastw.get(k))
        for k in w:
            self._wait(en, self.lastw.get(k))
            for t in self.readers.get(k, ()):
                self._wait(en, t)

    def _commit(self, tok, r, w):
        for k in r:
            self.readers.setdefault(k, []).append(tok)
        for k in w:
            self.lastw[k] = tok
            self.readers[k] = []

    @staticmethod
    def _psum_excl(r, w):
        pr = [k for k in r if k.startswith(('ps', 'pa', 'ph', 'pr'))]
        if pr:
            r = [k for k in r if k not in pr]
            w = list(w) + pr
        w = ['psb' if k in ('psb0', 'psb1') else k for k in w]
        return r, w

    def I(self, en, fn, r=(), w=()):
        pr = [k for k in r if k.startswith(('ps', 'pa', 'ph', 'pr'))]
        r, w = self._psum_excl(r, w)
        skip = [k for k in pr if self.lastw_isread.get(k) == en]
        saved = {k: self.lastw[k] for k in skip}
        for k in skip:
            del self.lastw[k]
        self._deps(en, r, w)
        for k in skip:
            self.lastw[k] = saved[k]
        ins = fn(self.eng[en])
        self.cnt[en] += 1
        ins.then_inc(self.sem[en], 1)
        self._commit(('e', en, self.cnt[en]), r, w)
        for k in w:
            self.lastw_isread[k] = en if k in pr else None
        return ins

    def D(self, en, fn, r=(), w=()):
        i = self.dnext
        self.dnext = (self.dnext + 1) % self.NDMA
        if self.dval[i] > 0:
            self._wait(en, ('d', i, self.dval[i]))
        self._deps(en, r, w)
        ins = fn(self.eng[en])
        self.dval[i] += 16
        ins.then_inc(self.dsem[i], 16)
        self._commit(('d', i, self.dval[i]), r, w)
        return ins

    def barrier(self):
        for en in self.eng:
            for o in self.eng:
                if o != en and self.cnt[o] > 0:
                    self._wait(en, ('e', o, self.cnt[o]))
            for i in range(self.NDMA):
                if self.dval[i] > 0:
                    self._wait(en, ('d', i, self.dval[i]))
        self.lastw.clear()
        self.lastw_isread.clear()
        self.readers.clear()

    def finish(self):
        for i in range(self.NDMA):
            if self.dval[i] > 0:
                self._wait('sp', ('d', i, self.dval[i]))
        for o in self.eng:
            if o != 'sp' and self.cnt[o] > 0:
                self._wait('sp', ('e', o, self.cnt[o]))


def _mm(P, out, lhsT, rhs, start, stop, r, w):
    P.I('pe', lambda e: e.matmul(out, lhsT=lhsT, rhs=rhs, start=start, stop=stop), r=r, w=w)


def _consts(P, nc, sb):
    c = {}
    tf = sb("c_tf", [128, 128], F32)
    P.I('pool', lambda e: e.iota(tf[:], pattern=[[1, 128]], base=0, channel_multiplier=-1,
                                 allow_small_or_imprecise_dtypes=True), w=['c_tf'])
    c['ident'] = sb("c_ident", [128, 128], BF16)
    c['tri'] = sb("c_tri", [128, 128], BF16)
    c['atri'] = sb("c_atri", [128, 128], BF16)
    c['trif'] = sb("c_trif", [128, 128], F32)
    c['onesf'] = sb("c_onesf", [128, 128], F32)
    P.I('dve', lambda e: e.tensor_scalar(out=c['ident'][:], in0=tf[:], scalar1=0.0, scalar2=None, op0=OP.is_equal), r=['c_tf'], w=['c_ident'])
    P.I('dve', lambda e: e.tensor_scalar(out=c['tri'][:], in0=tf[:], scalar1=0.0, scalar2=None, op0=OP.is_ge), r=['c_tf'], w=['c_tri'])
    P.I('dve', lambda e: e.tensor_scalar(out=c['atri'][:], in0=tf[:], scalar1=0.0, scalar2=None, op0=OP.is_lt), r=['c_tf'], w=['c_atri'])
    P.I('dve', lambda e: e.tensor_scalar(out=c['trif'][:], in0=tf[:], scalar1=0.0, scalar2=None, op0=OP.is_ge), r=['c_tf'], w=['c_trif'])
    P.I('dve', lambda e: e.memset(c['onesf'][:], 1.0), w=['c_onesf'])
    return c


def _inproj(P, nc, es_outer, xT, wfm_d, bfm, fm_list, wtm_d, ntm, tm_evac, pss, tag, n_tok=T):
    with ExitStack() as es:
        sb = lambda n, s, d: es.enter_context(nc.sbuf_tensor(tag + n, s, d))
        nfm = int(wfm_d.shape[1])
        W = sb("W", [128, 8, nfm], BF16)
        for k in range(8):
            P.D('pool', lambda e, k=k: e.dma_start(out=W[:, k, :], in_=wfm_d[k * 128:(k + 1) * 128, :]), w=[tag + 'W'])
        if ntm:
            WT = sb("WT", [128, 8, ntm], BF16)
            for k in range(8):
                P.D('pool', lambda e, k=k: e.dma_start(out=WT[:, k, :], in_=wtm_d[k * 128:(k + 1) * 128, :]), w=[tag + 'WT'])
        xb = [sb(f"xb{i}", [128, 8, 512], BF16) for i in range(2)]
        xv = xT.rearrange("(k p) t -> p k t", p=128)
        nch = n_tok // 512
        pi = 0
        for tc in range(nch):
            X = xb[tc % 2]
            xk = f"{tag}xb{tc % 2}"
            for k2 in range(2):
                P.D('pool', lambda e, X=X, tc=tc, k2=k2: e.dma_start(out=X[:, 4 * k2:4 * k2 + 4, :], in_=xv[:, 4 * k2:4 * k2 + 4, tc * 512:(tc + 1) * 512]), w=[xk])
            for (c0, M, evac) in fm_list:
                ps, pk = pss[pi % len(pss)]
                pi += 1
                for k in range(8):
                    _mm(P, ps[0:M, 0:512], W[:, k, c0:c0 + M], X[:, k, :], k == 0, k == 7, [tag + 'W', xk], [pk])
                evac(tc, ps, pk)
            if ntm:
                for tt in range(4):
                    ps, pk = pss[pi % len(pss)]
                    pi += 1
                    for k in range(8):
                        _mm(P, ps[:, 0:ntm], X[:, k, tt * 128:(tt + 1) * 128], WT[:, k, :], k == 0, k == 7, [tag + 'WT', xk], [pk])
                    tm_evac(tc * 4 + tt, ps, pk)
    P.barrier()


def build_A():
    nc = bass.Bass("TRN2", target_bir_lowering=False)
    dt = lambda n, s, k="ExternalInput": nc.dram_tensor(n, s, F32, kind=k).ap()
    xT = dt("xT", [D, T])
    wfm1 = dt("wfm1", [D, 256]); bfm1 = dt("bfm1", [128, 2])
    wfm2 = dt("wfm2", [D, 516]); bfm2 = dt("bfm2", [128, 5])
    wtm2 = dt("wtm2", [D, 130]); btm2 = dt("btm2", [128, 130])
    wfm3 = dt("wfm3", [D, 256]); bfm3 = dt("bfm3", [128, 2])
    wtm3 = dt("wtm3", [D, 130]); btm3 = dt("btm3", [128, 130])
    pw_d = dt("pw", [128, 128]); pbs_d = dt("pbs", [128, 2]); cw_d = dt("cw", [128, 4]); fix_d = dt("fix", [128, 16])
    cw1_d = dt("cw1", [128, 32 * 256]); posT_d = dt("posT", [128, 32]); cb1_d = dt("cb1", [128, 4])
    cw2k_d = dt("cw2k", [128, 256]); cb2k_d = dt("cb2k", [128, 1]); cw2v_d = dt("cw2v", [128, 128]); cb2v_d = dt("cb2v", [128, 64])
    yT = dt("yT", [384, T], "ExternalOutput")

    with ExitStack() as es:
        P = Prog(nc, es)
        sbg = lambda n, s, d: es.enter_context(nc.sbuf_tensor(n, s, d))
        PS = [es.enter_context(nc.psum_tensor(f"ps{i}", [128, 512], F32)) for i in range(7)]
        PSB = es.enter_context(nc.psum_tensor("psb", [128, 1024], BF16))
        pk = [f"ps{i}" for i in range(7)]
        C = _consts(P, nc, sbg)
        KCT = sbg("KCT", [128, 512], BF16)
        VC = sbg("VC", [128, 4, 64], BF16)

        with ExitStack() as e1:
            sb = lambda n, s, d: e1.enter_context(nc.sbuf_tensor(n, s, d))
            bf1 = sb("bf1", [128, 2], F32); pwf = sb("pwf", [128, 128], BF16); pbs = sb("pbs_s", [128, 2], F32)
            cw = sb("cw_s", [128, 4], F32); fix = sb("fix_s", [128, 16], F32)
            P.D('sp', lambda e: e.dma_start(out=bf1[:], in_=bfm1), w=['bf1'])
            P.D('pool', lambda e: e.dma_start(out=pwf[:], in_=pw_d), w=['pwf'])
            P.D('sp', lambda e: e.dma_start(out=pbs[:], in_=pbs_d), w=['pbs'])
            P.D('sp', lambda e: e.dma_start(out=cw[:], in_=cw_d), w=['cw'])
            P.D('sp', lambda e: e.dma_start(out=fix[:], in_=fix_d), w=['fix'])
            KVC = sb("KVC", [128, T], BF16)
            UC = [sb(f"UC{i}", [128, 528], F32) for i in range(2)]
            S2 = sb("S2", [128, 528], F32); S4 = sb("S4", [128, 528], F32); S8 = sb("S8", [128, 528], F32); S16 = sb("S16", [128, 528], F32)
            ACC = sb("ACC", [128, 512], F32); DT_ = [sb(f"DTb{i}", [128, 512], BF16) for i in range(2)]
            YP = [sb(f"YP{i}", [128, 512], F32) for i in range(2)]
            P.I('dve', lambda e: e.memset(UC[1][:], 0.0), w=['UC1'])

            def evac_u(tc, ps, pkk):
                U = UC[tc % 2]; Up = UC[(tc + 1) % 2]
                uk, upk = f"UC{tc % 2}", f"UC{(tc + 1) % 2}"
                P.I('act', lambda e: e.activation(out=U[:, 16:528], in_=ps[:, 0:512], func=AF.Identity, bias=bf1[:, 0:1], scale=1.0), r=[pkk, 'bf1'], w=[uk])
                P.I('dve', lambda e: e.tensor_copy(out=U[:, 0:16], in_=Up[:, 512:528]), r=[upk], w=[uk])
                P.I('dve', lambda e: e.tensor_tensor(out=S2[:, 1:528], in0=U[:, 1:528], in1=U[:, 0:527], op=OP.add), r=[uk], w=['S2'])
                P.I('dve', lambda e: e.tensor_tensor(out=S4[:, 3:528], in0=S2[:, 3:528], in1=S2[:, 1:526], op=OP.add), r=['S2'], w=['S4'])
                P.I('dve', lambda e: e.tensor_tensor(out=S8[:, 7:528], in0=S4[:, 7:528], in1=S4[:, 3:524], op=OP.add), r=['S4'], w=['S8'])
                P.I('dve', lambda e: e.tensor_tensor(out=S16[:, 15:528], in0=S8[:, 15:528], in1=S8[:, 7:520], op=OP.add), r=['S8'], w=['S16'])
                P.I('dve', lambda e: e.tensor_scalar(out=ACC[:], in0=S2[:, 16:528], scalar1=cw[:, 0:1], scalar2=None, op0=OP.mult), r=['S2', 'cw'], w=['ACC'])
                for wi, S in enumerate((S4, S8, S16)):
                    P.I('dve', lambda e, S=S, wi=wi: e.scalar_tensor_tensor(out=ACC[:], in0=S[:, 16:528], scalar=cw[:, wi + 1:wi + 2], in1=ACC[:], op0=OP.mult, op1=OP.add),
                        r=['S4', 'S8', 'S16', 'cw', 'ACC'], w=['ACC'])
                if tc == 0:
                    P.I('dve', lambda e: e.tensor_tensor(out=ACC[:, 0:16], in0=ACC[:, 0:16], in1=fix[:], op=OP.mult), r=['ACC', 'fix'], w=['ACC'])
                Dt = DT_[tc % 2]; dk = f"DT{tc % 2}"
                P.I('dve', lambda e: e.tensor_tensor(out=Dt[:], in0=ACC[:], in1=U[:, 16:528], op=OP.subtract), r=['ACC', uk], w=[dk])
                ps2, pk2 = PS[4 + tc % 2], pk[4 + tc % 2]
                _mm(P, ps2[:, 0:512], pwf[:], Dt[:], True, True, ['pwf', dk], [pk2])
                Y = YP[tc % 2]; yk = f"YP{tc % 2}"
                P.I('dve', lambda e: e.tensor_scalar(out=Y[:], in0=ps2[:, 0:512], scalar1=pbs[:, 0:1], scalar2=pbs[:, 1:2], op0=OP.add, op1=OP.mult), r=[pk2, 'pbs'], w=[yk])
                P.D('sp', lambda e: e.dma_start(out=yT[0:128, tc * 512:(tc + 1) * 512], in_=Y[:]), r=[yk])

            def evac_kvc(tc, ps, pkk):
                P.I('act', lambda e: e.activation(out=KVC[:, tc * 512:(tc + 1) * 512], in_=ps[:, 0:512], func=AF.Identity, bias=bf1[:, 1:2], scale=1.0), r=[pkk, 'bf1'], w=['KVC'])

            _inproj(P, nc, e1, xT, wfm1, bf1, [(0, 128, evac_u), (128, 128, evac_kvc)], None, 0, None,
                    [(PS[0], pk[0]), (PS[1], pk[1]), (PS[2], pk[2]), (PS[3], pk[3])], "p1")

            W1 = sb("W1", [128, 32 * 256], BF16)
            for q4 in range(4):
                P.D('pool', lambda e, q4=q4: e.dma_start(out=W1[:, q4 * 2048:(q4 + 1) * 2048], in_=cw1_d[:, q4 * 2048:(q4 + 1) * 2048]), w=['W1'])
            posT = sb("posT_s", [128, 32], BF16); cb1 = sb("cb1_s", [128, 4], F32)
            w2k = sb("w2k", [128, 256], BF16); b2k = sb("b2k", [128, 1], F32); w2v = sb("w2v", [128, 128], BF16); b2v = sb("b2v", [128, 64], F32)
            P.D('pool', lambda e: e.dma_start(out=posT[:], in_=posT_d), w=['posT'])
            P.D('sp', lambda e: e.dma_start(out=cb1[:], in_=cb1_d), w=['cb1'])
            P.D('pool', lambda e: e.dma_start(out=w2k[:], in_=cw2k_d), w=['w2k'])
            P.D('sp', lambda e: e.dma_start(out=b2k[:], in_=cb2k_d), w=['b2k'])
            P.D('pool', lambda e: e.dma_start(out=w2v[:], in_=cw2v_d), w=['w2v'])
            P.D('sp', lambda e: e.dma_start(out=b2v[:], in_=cb2v_d), w=['b2v'])
            HT = [[sb(f"HT{kv}{hf}", [128, 512], BF16) for hf in range(2)] for kv in range(2)]
            XH = sb("XH", [128, 512], F32); T1 = sb("T1c", [128, 512], F32); T2 = sb("T2c", [128, 512], F32); cbt = sb("cbt", [128, 4], F32)
            P.I('dve', lambda e: e.memset(VC[:], 0.0), w=['VC'])
            for kv in range(2):
                po = 64 * kv
                for hf in range(2):
                    ci = kv * 2 + hf
                    ps, pkk = PS[ci % 4], pk[ci % 4]
                    psc, pkc = PS[4 + ci % 2], pk[4 + ci % 2]
                    for l in range(32):
                        wsl = W1[po:po + 64, l * 256 + hf * 128: l * 256 + hf * 128 + 128]
                        _mm(P, psc[:, 0:1], wsl, posT[po:po + 64, l:l + 1], l == 0, l == 31, ['W1', 'posT'], [pkc])
                    P.I('dve', lambda e, ci=ci, psc=psc: e.tensor_tensor(out=cbt[:, ci:ci + 1], in0=psc[:, 0:1], in1=cb1[:, ci:ci + 1], op=OP.add), r=[pkc, 'cb1'], w=['cbt'])
                    for l in range(32):
                        wsl = W1[po:po + 64, l * 256 + hf * 128: l * 256 + hf * 128 + 128]
                        _mm(P, ps[:, 0:511], wsl, KVC[po:po + 64, l:l + 16 * 510 + 1:16], l == 0, l == 31, ['W1', 'KVC'], [pkk])
                    P.I('act', lambda e, ci=ci, ps=ps: e.activation(out=XH[:, 0:511], in_=ps[:, 0:511], func=AF.Identity, bias=cbt[:, ci:ci + 1], scale=1.0), r=[pkk, 'cbt'], w=['XH'])
                    P.I('dve', lambda e: e.tensor_tensor(out=T1[:, 0:511], in0=XH[:, 0:511], in1=XH[:, 0:511], op=OP.mult), r=['XH'], w=['T1'])
                    P.I('dve', lambda e: e.tensor_scalar(out=T1[:, 0:511], in0=T1[:, 0:511], scalar1=0.044715, scalar2=1.0, op0=OP.mult, op1=OP.add), r=['T1'], w=['T1'])
                    P.I('dve', lambda e: e.tensor_tensor(out=T1[:, 0:511], in0=T1[:, 0:511], in1=XH[:, 0:511], op=OP.mult), r=['T1', 'XH'], w=['T1'])
                    P.I('act', lambda e: e.activation(out=T2[:, 0:511], in_=T1[:, 0:511], func=AF.Sigmoid, scale=1.5957691216057308), r=['T1'], w=['T2'])
                    H = HT[kv][hf]
                    P.I('dve', lambda e, H=H: e.memset(H[:, 511:512], 0.0), w=[f'HT{kv}{hf}'])
                    P.I('dve', lambda e, H=H: e.tensor_tensor(out=H[:, 0:511], in0=T2[:, 0:511], in1=XH[:, 0:511], op=OP.mult), r=['T2', 'XH'], w=[f'HT{kv}{hf}'])
            for hf in range(2):
                _mm(P, PS[0][:, 0:512], w2k[:, hf * 128:(hf + 1) * 128], HT[0][hf][:], hf == 0, hf == 1, ['w2k', f'HT0{hf}'], [pk[0]])
            P.I('act', lambda e: e.activation(out=KCT[:], in_=PS[0][:, 0:512], func=AF.Identity, bias=b2k[:, 0:1], scale=1.0), r=[pk[0], 'b2k'], w=['KCT'])
            for ct in range(4):
                m = 128 if ct < 3 else 127
                for hf in range(2):
                    _mm(P, PS[1 + ct % 2][0:m, 0:64], HT[1][hf][:, ct * 128:ct * 128 + m], w2v[:, hf * 64:(hf + 1) * 64], hf == 0, hf == 1, ['w2v', f'HT1{hf}'], [pk[1 + ct % 2]])
                P.I('dve', lambda e, ct=ct, m=m: e.tensor_tensor(out=VC[0:m, ct, :], in0=PS[1 + ct % 2][0:m, 0:64], in1=b2v[0:m, :], op=OP.add), r=[pk[1 + ct % 2], 'b2v'], w=['VC'])
            P.barrier()

        with ExitStack() as e2:
            sb = lambda n, s, d: e2.enter_context(nc.sbuf_tensor(n, s, d))
            bf2 = sb("bf2", [128, 5], F32); bt2 = sb("bt2", [128, 130], F32)
            P.D('sp', lambda e: e.dma_start(out=bf2[:], in_=bfm2), w=['bf2'])
            P.D('sp', lambda e: e.dma_start(out=bt2[:], in_=btm2), w=['bt2'])
            NQ = sb("NQ", [128, T], BF16); NQo = sb("NQo", [128, T], BF16); KS = sb("KS", [128, T], BF16); KW = sb("KW", [128, T], BF16)
            G4 = sb("G4", [4, T], BF16)
            VS = sb("VS", [128, NT, 65], BF16); VW = sb("VW", [128, NT, 65], BF16); GC = sb("GC", [128, NT, 2], F32)
            P.I('pool', lambda e: e.memset(VS[:, :, 64:65], 1.0), w=['VS'])
            P.I('pool', lambda e: e.memset(VW[:, :, 64:65], 1.0), w=['VW'])

            def ev(dst, key, col, func=AF.Identity, M=128):
                def f(tc, ps, pkk):
                    P.I('act', lambda e: e.activation(out=dst[0:M, tc * 512:(tc + 1) * 512], in_=ps[0:M, 0:512], func=func, bias=bf2[0:M, col:col + 1], scale=1.0), r=[pkk, 'bf2'], w=[key])
                return f

            TMB = sb("TMB", [128, 130], F32)

            def tm2(ti, ps, pkk):
                P.I('dve', lambda e: e.tensor_tensor(out=VS[:, ti, 0:64], in0=ps[:, 0:64], in1=bt2[:, 0:64], op=OP.add), r=[pkk, 'bt2'], w=['VS'])
                P.I('dve', lambda e: e.tensor_tensor(out=VW[:, ti, 0:64], in0=ps[:, 64:128], in1=bt2[:, 64:128], op=OP.add), r=[pkk, 'bt2'], w=['VW'])
                P.I('dve', lambda e: e.tensor_tensor(out=TMB[:, 128:130], in0=ps[:, 128:130], in1=bt2[:, 128:130], op=OP.add), r=[pkk, 'bt2'], w=['TMB'])
                P.I('act', lambda e: e.activation(out=GC[:, ti, :], in_=TMB[:, 128:130], func=AF.Sigmoid), r=['TMB'], w=['GC'])

            _inproj(P, nc, e2, xT, wfm2, bf2,
                    [(0, 128, ev(NQ, 'NQ', 0)), (128, 128, ev(NQo, 'NQo', 1)), (256, 128, ev(KS, 'KS', 2)), (384, 128, ev(KW, 'KW', 3)),
                     (512, 4, ev(G4, 'G4', 4, AF.Sigmoid, 4))],
                    wtm2, 130, tm2, [(PS[0], pk[0]), (PS[1], pk[1]), (PS[2], pk[2]), (PS[3], pk[3])], "p2")

            EJ = sb("EJ", [128, NT * 128], BF16); ejf = sb("ejf", [128, 2048], F32)
            for q4 in range(4):
                P.I('pool', lambda e, q4=q4: e.iota(ejf[:], pattern=[[-2, 16], [-1, 2], [0, 64]], base=-32 * q4, channel_multiplier=1,
                                             allow_small_or_imprecise_dtypes=True), w=['ejf'])
                P.I('dve', lambda e, q4=q4: e.tensor_scalar(out=EJ[:, q4 * 2048:(q4 + 1) * 2048], in0=ejf[:], scalar1=0.0, scalar2=None, op0=OP.is_equal), r=['ejf'], w=['EJ'])
            REL = sb("REL", [128, 512], F32)
            P.I('pool', lambda e: e.iota(REL[:], pattern=[[16, 512]], base=31, channel_multiplier=-1, allow_small_or_imprecise_dtypes=True), w=['REL'])
            VV = sb("VV", [128, 254], F32); HP = sb("HP", [128, 1], F32)
            P.I('pool', lambda e: e.iota(VV[:], pattern=[[1, 254]], base=-126, channel_multiplier=0, allow_small_or_imprecise_dtypes=True), w=['VV'])
            P.I('pool', lambda e: e.iota(HP[:], pattern=[[0, 1]], base=0, channel_multiplier=1, allow_small_or_imprecise_dtypes=True), w=['HP'])
            P.I('dve', lambda e: e.tensor_scalar(out=HP[:], in0=HP[:], scalar1=64.0, scalar2=None, op0=OP.is_ge), r=['HP'], w=['HP'])
            P.I('dve', lambda e: e.tensor_scalar(out=VV[:], in0=VV[:], scalar1=HP[:, 0:1], scalar2=None, op0=OP.subtract), r=['VV', 'HP'], w=['VV'])
            KEEP = sb("KEEP", [128, 254], F32); NF = sb("NF", [128, 254], F32); ADD = sb("ADD", [128, 254], F32); TA = sb("TA", [128, 254], F32)
            P.I('dve', lambda e: e.tensor_scalar(out=KEEP[:], in0=VV[:], scalar1=-2.0, scalar2=None, op0=OP.is_le), r=['VV'], w=['KEEP'])
            P.I('dve', lambda e: e.tensor_scalar(out=NF[:], in0=VV[:], scalar1=0.0, scalar2=None, op0=OP.is_le), r=['VV'], w=['NF'])
            P.I('dve', lambda e: e.tensor_scalar(out=ADD[:], in0=VV[:], scalar1=-1.0, scalar2=1.0e6, op0=OP.is_ge, op1=OP.mult), r=['VV'], w=['ADD'])
            P.I('dve', lambda e: e.tensor_scalar(out=TA[:], in0=VV[:], scalar1=0.0, scalar2=-1000001.0, op0=OP.is_gt, op1=OP.mult), r=['VV'], w=['TA'])
            P.I('dve', lambda e: e.tensor_tensor(out=ADD[:], in0=ADD[:], in1=TA[:], op=OP.add), r=['ADD', 'TA'], w=['ADD'])
            SEL = sb("SEL", [4, 256], BF16); self_ = sb("self_", [4, 256], F32)
            P.I('pool', lambda e: e.iota(self_[:], pattern=[[1, 4], [0, 64]], base=0, channel_multiplier=-1, allow_small_or_imprecise_dtypes=True), w=['self_'])
            P.I('dve', lambda e: e.tensor_scalar(out=SEL[:], in0=self_[:], scalar1=0.0, scalar2=None, op0=OP.is_equal), r=['self_'], w=['SEL'])

            WMASK = sb("WMASK", [128, 8 * 512], BF16)
            P.I('dve', lambda e: e.memset(WMASK[:], 0.0), w=['WMASK'])
            for a in range(-4, 4):
                for b in range(4):
                    dst = WMASK[:, (a + 4) * 512 + b * 128:(a + 4) * 512 + (b + 1) * 128]
                    if b == a:
                        P.I('dve', lambda e, dst=dst: e.tensor_copy(out=dst, in_=C['tri'][:]), r=['c_tri'], w=['WMASK'])
                    elif b == a + 4:
                        P.I('dve', lambda e, dst=dst: e.tensor_copy(out=dst, in_=C['atri'][:]), r=['c_atri'], w=['WMASK'])
                    elif a < b < a + 4:
                        P.I('dve', lambda e, dst=dst: e.memset(dst, 1.0), w=['WMASK'])
            EX = [sb(f"EX{i}", [128, 512], F32) for i in range(2)]
            EM = [sb(f"EM{i}", [128, 512], F32) for i in range(2)]
            RS = sb("RS", [128, 8], F32)
            PSP = sb("PSP", [128, 520], F32)
            P.I('dve', lambda e: e.memset(PSP[:], 0.0), w=['PSP'])
            PN = [sb(f"PN{i}", [128, 512], BF16) for i in range(2)]
            PNT = [sb(f"PNT{i}", [128, 512], BF16) for i in range(2)]
            IMP = sb("IMP", [128, 128], F32); SC = sb("SC", [128, 128], F32); SC2 = sb("SC2", [128, 128], F32); M8 = sb("M8", [128, 16], F32)
            SELM = sb("SELM", [128, 128], F32); MB = sb("MB", [128, 128], BF16)
            MBT = [sb(f"MBT{i}", [128, 512], BF16) for i in range(2)]
            OCc = [[sb(f"OC{i}{h}", [64, 512], F32) for h in range(2)] for i in range(2)]
            PT = [sb(f"PT{i}", [128, 512], BF16) for i in range(3)]
            RR = sb("RR", [65, 512], F32); BCS = sb("BCS", [64, 512], F32); BGS = sb("BGS", [64, 512], F32)
            TY = sb("TY", [64, 512], F32); YN = [sb(f"YN{i}", [64, 512], F32) for i in range(2)]
            ptc = 0

            for qc in range(T // 512):
                par = qc % 2
                mbk = f"MBT{par}"
                for b in range(4):
                    i = 4 * qc + b
                    for hh in range(4):
                        Q = NQ if hh < 2 else NQo
                        qk_ = 'NQ' if hh < 2 else 'NQo'
                        po = 64 * (hh % 2)
                        ps, pkk = PS[hh % 2], pk[hh % 2]
                        _mm(P, ps[:, 0:512], Q[po:po + 64, i * 128:(i + 1) * 128], KCT[po:po + 64, :], True, True, [qk_, 'KCT'], [pkk])
                        ex, exk = EX[hh % 2], f"EX{hh % 2}"
                        em, emk = EM[hh % 2], f"EM{hh % 2}"
                        P.I('act', lambda e, ex=ex, ps=ps: e.activation(out=ex[:], in_=ps[:, 0:512], func=AF.Exp, scale=SCALE), r=[pkk], w=[exk])
                        P.I('dve', lambda e, ex=ex, em=em, i=i, hh=hh: e.scalar_tensor_tensor(out=em[:], in0=REL[:], scalar=float(128 * i), in1=ex[:], op0=OP.is_le, op1=OP.mult),
                            r=['REL', exk], w=[emk])
                        P.I('dve', lambda e, em=em, hh=hh: e.reduce_sum(out=RS[:, hh:hh + 1], in_=em[:], axis=AX.X), r=[emk], w=['RS'])
                        P.I('dve', lambda e, hh=hh: e.tensor_scalar(out=RS[:, hh:hh + 1], in0=RS[:, hh:hh + 1], scalar1=1e-30, scalar2=None, op0=OP.max), r=['RS'], w=['RS'])
                        P.I('dve', lambda e, hh=hh: e.reciprocal(out=RS[:, hh:hh + 1], in_=RS[:, hh:hh + 1]), r=['RS'], w=['RS'])
                        if hh == 0:
                            P.I('dve', lambda e, em=em, hh=hh: e.tensor_scalar(out=PSP[:, 1:513], in0=em[:], scalar1=RS[:, hh:hh + 1], scalar2=None, op0=OP.mult), r=[emk, 'RS'], w=['PSP'])
                        else:
                            P.I('dve', lambda e, em=em, hh=hh: e.scalar_tensor_tensor(out=PSP[:, 1:513], in0=em[:], scalar=RS[:, hh:hh + 1], in1=PSP[:, 1:513], op0=OP.mult, op1=OP.add),
                                r=[emk, 'RS', 'PSP'], w=['PSP'])
                        if hh < 2:
                            P.I('dve', lambda e, hh=hh, i=i: e.tensor_tensor(out=RS[:, 4 + hh:5 + hh], in0=RS[:, hh:hh + 1], in1=GC[:, i, hh:hh + 1], op=OP.mult), r=['RS', 'GC'], w=['RS'])
                            pn, pnk = PN[hh], f"PN{hh}"
                            P.I('pool', lambda e, pn=pn, em=em, hh=hh: e.tensor_scalar(out=pn[:], in0=em[:], scalar1=RS[:, 4 + hh:5 + hh], scalar2=None, op0=OP.mult), r=[emk, 'RS'], w=[pnk])
                            for ct in range(4):
                                P.I('pe', lambda e, pn=pn, ct=ct, hh=hh: e.transpose(out=PSB[:, hh * 512 + ct * 128: hh * 512 + (ct + 1) * 128], in_=pn[:, ct * 128:(ct + 1) * 128], identity=C['ident'][:]),
                                    r=[pnk, 'c_ident'], w=[f'psb{hh}'])
                            pnt, pntk = PNT[hh], f"PNT{hh}"
                            P.I('act', lambda e, pnt=pnt, hh=hh: e.copy(out=pnt[:], in_=PSB[:, hh * 512:(hh + 1) * 512]), r=[f'psb{hh}'], w=[pntk])
                            po_, pok = PS[2 + hh], pk[2 + hh]
                            for ct in range(4):
                                _mm(P, po_[0:64, b * 128:(b + 1) * 128], VC[:, ct, :], pnt[:, ct * 128:(ct + 1) * 128], ct == 0, ct == 3, ['VC', pntk], [pok])
                    P.I('dve', lambda e: e.tensor_reduce(out=IMP[:], in_=PSP[:, 0:512].rearrange("p (s m) -> p s m", m=4), axis=AX.X, op=OP.add), r=['PSP'], w=['IMP'])
                    P.I('dve', lambda e: e.tensor_tensor(out=IMP[:], in0=IMP[:], in1=PSP[:, 4:516:4], op=OP.add), r=['PSP', 'IMP'], w=['IMP'])
                    x0 = 126 - 2 * i
                    P.I('dve', lambda e, x0=x0: e.tensor_tensor(out=SC[:], in0=IMP[:], in1=KEEP[:, x0:x0 + 128], op=OP.mult), r=['IMP', 'KEEP'], w=['SC'])
                    P.I('dve', lambda e, x0=x0: e.tensor_tensor(out=SC[:], in0=SC[:], in1=ADD[:, x0:x0 + 128], op=OP.add), r=['SC', 'ADD'], w=['SC'])
                    P.I('dve', lambda e: e.memset(SC[:, 0:1], 1.0e6), r=['SC'], w=['SC'])
                    P.I('dve', lambda e: e.max(out=M8[:, 0:8], in_=SC[:]), r=['SC'], w=['M8'])
                    P.I('dve', lambda e: e.match_replace(out=SC2[:], in_to_replace=M8[:, 0:8], in_values=SC[:], imm_value=-2.0), r=['SC', 'M8'], w=['SC2'])
                    P.I('dve', lambda e: e.max(out=M8[:, 8:16], in_=SC2[:]), r=['SC2'], w=['M8'])
                    P.I('dve', lambda e, x0=x0: e.scalar_tensor_tensor(out=SELM[:], in0=SC[:], scalar=M8[:, 15:16], in1=NF[:, x0:x0 + 128], op0=OP.is_ge, op1=OP.mult), r=['SC', 'M8', 'NF'], w=['SELM'])
                    P.I('dve', lambda e: e.tensor_scalar(out=MB[:], in0=SELM[:], scalar1=-1.0, scalar2=30000.0, op0=OP.add, op1=OP.mult), r=['SELM'], w=['MB'])
                    P.I('pe', lambda e: e.transpose(out=PSB[:, 0:128], in_=MB[:], identity=C['ident'][:]), r=['MB', 'c_ident'], w=['psb0'])
                    P.I('act', lambda e, b=b, par=par: e.copy(out=MBT[par][:, b * 128:(b + 1) * 128], in_=PSB[:, 0:128]), r=['psb0'], w=[mbk])
                for h in range(2):
                    P.I('act', lambda e, h=h, par=par: e.copy(out=OCc[par][h][:], in_=PS[2 + h][0:64, 0:512]), r=[pk[2 + h]], w=[f'OC{par}{h}'])

                for h in range(2):
                    po = 64 * h
                    pso, psok = PS[4], pk[4]
                    psw, pswk = PS[5], pk[5]
                    nj = 4 * qc + 4
                    jfirst = max(4 * qc - 4, 0)
                    steps = [('s', j) for j in range(nj)] + [('w', 4 * qc + a) for a in range(-4, 4) if 4 * qc + a >= 0]

                    def qk(si):
                        kind, j = steps[si]
                        pst, pstk = PS[si % 2], pk[si % 2]
                        a = j - 4 * qc
                        if kind == 's':
                            c0 = 128 * max(a, 0)
                            _mm(P, pst[:, c0:512], KS[po:po + 64, j * 128:(j + 1) * 128], NQ[po:po + 64, qc * 512 + c0:(qc + 1) * 512], True, False, ['KS', 'NQ'], [pstk])
                            _mm(P, pst[:, c0:512], EJ[:, j * 128:(j + 1) * 128], MBT[par][:, c0:512], False, True, ['EJ', mbk], [pstk])
                        else:
                            _mm(P, pst[:, 0:512], KW[po:po + 64, j * 128:(j + 1) * 128], NQ[po:po + 64, qc * 512:(qc + 1) * 512], True, True, ['KW', 'NQ'], [pstk])

                    def rest(si):
                        nonlocal ptc
                        kind, j = steps[si]
                        pst, pstk = PS[si % 2], pk[si % 2]
                        a = j - 4 * qc
                        pt, ptk = PT[ptc % 3], f"PT{ptc % 3}"
                        ptc += 1
                        if kind == 's':
                            c0 = 128 * max(a, 0)
                            P.I('act', lambda e: e.activation(out=pt[:, c0:512], in_=pst[:, c0:512], func=AF.Exp, scale=SCALE), r=[pstk], w=[ptk])
                            if a >= 0:
                                P.I('dve', lambda e: e.tensor_tensor(out=pt[:, c0:c0 + 128], in0=pt[:, c0:c0 + 128], in1=C['tri'][:], op=OP.mult), r=[ptk, 'c_tri'], w=[ptk])
                            _mm(P, pso[0:65, c0:512], VS[:, j, :], pt[:, c0:512], j == 0, j == nj - 1, ['VS', ptk], [psok])
                        else:
                            P.I('act', lambda e: e.activation(out=pt[:, 0:512], in_=pst[:, 0:512], func=AF.Exp, scale=SCALE), r=[pstk], w=[ptk])
                            P.I('dve', lambda e: e.tensor_tensor(out=pt[:, 0:512], in0=pt[:, 0:512], in1=WMASK[:, (a + 4) * 512:(a + 5) * 512], op=OP.mult), r=[ptk, 'WMASK'], w=[ptk])
                            _mm(P, psw[0:65, 0:512], VW[:, j, :], pt[:, 0:512], j == jfirst, a == 3, ['VW', ptk], [pswk])

                    qk(0)
                    for si in range(len(steps)):
                        if si + 1 < len(steps):
                            qk(si + 1)
                        rest(si)
                    Y = YN[h]; yk = f"YN{h}"
                    for br, (pacc, pacck) in enumerate(((pso, psok), (psw, pswk))):
                        P.I('dve', lambda e, pacc=pacc: e.reciprocal(out=RR[64:65, :], in_=pacc[64:65, 0:512]), r=[pacck], w=['RR'])
                        _mm(P, PS[6][0:64, 0:512], C['onesf'][64:65, 0:64], RR[64:65, :], True, True, ['c_onesf', 'RR'], [pk[6]])
                        P.I('act', lambda e: e.copy(out=BCS[:], in_=PS[6][0:64, 0:512]), r=[pk[6]], w=['BCS'])
                        gi = 2 * h + br
                        _mm(P, PS[6][0:64, 0:512], SEL[0:4, gi * 64:(gi + 1) * 64], G4[0:4, qc * 512:(qc + 1) * 512], True, True, ['SEL', 'G4'], [pk[6]])
                        P.I('act', lambda e: e.copy(out=BGS[:], in_=PS[6][0:64, 0:512]), r=[pk[6]], w=['BGS'])
                        P.I('dve', lambda e, pacc=pacc: e.tensor_tensor(out=TY[:], in0=pacc[0:64, 0:512], in1=BCS[:], op=OP.mult), r=[pacck, 'BCS'], w=['TY'])
                        P.I('dve', lambda e: e.tensor_tensor(out=TY[:], in0=TY[:], in1=BGS[:], op=OP.mult), r=['TY', 'BGS'], w=['TY'])
                        src = OCc[par][h] if br == 0 else Y
                        srck = f'OC{par}{h}' if br == 0 else yk
                        P.I('dve', lambda e, src=src, Y=Y: e.tensor_tensor(out=Y[:], in0=TY[:], in1=src[:], op=OP.add), r=['TY', srck], w=[yk])
                    P.D('sp', lambda e, Y=Y, h=h, qc=qc: e.dma_start(out=yT[128 + 64 * h:192 + 64 * h, qc * 512:(qc + 1) * 512], in_=Y[:]), r=[yk])
            P.barrier()

        with ExitStack() as e3:
            sb = lambda n, s, d: e3.enter_context(nc.sbuf_tensor(n, s, d))
            bf3 = sb("bf3", [128, 2], F32); bt3 = sb("bt3", [128, 130], F32)
            P.D('sp', lambda e: e.dma_start(out=bf3[:], in_=bfm3), w=['bf3'])
            P.D('sp', lambda e: e.dma_start(out=bt3[:], in_=btm3), w=['bt3'])
            FQ = sb("FQ", [128, T], BF16); FK = sb("FK", [128, T], BF16)
            FV = sb("FV", [128, NT, 2, 65], BF16); LF = sb("LF", [128, NT, 2], F32)
            P.I('pool', lambda e: e.memset(FV[:, :, :, 64:65], 1.0), w=['FV'])

            def ev3(dst, key, col):
                def f(tc, ps, pkk):
                    P.I('act', lambda e: e.activation(out=dst[:, tc * 512:(tc + 1) * 512], in_=ps[:, 0:512], func=AF.Identity, bias=bf3[:, col:col + 1], scale=1.0), r=[pkk, 'bf3'], w=[key])
                return f

            def tm3(ti, ps, pkk):
                P.I('dve', lambda e: e.tensor_tensor(out=FV[:, ti, :, 0:64], in0=ps[:, 0:128].rearrange("p (h d) -> p h d", d=64), in1=bt3[:, 0:128].rearrange("p (h d) -> p h d", d=64), op=OP.add),
                    r=[pkk, 'bt3'], w=['FV'])
                P.I('dve', lambda e: e.tensor_tensor(out=LF[:, ti, :], in0=ps[:, 128:130], in1=bt3[:, 128:130], op=OP.add), r=[pkk, 'bt3'], w=['LF'])

            _inproj(P, nc, e3, xT, wfm3, bf3, [(0, 128, ev3(FQ, 'FQ', 0)), (128, 128, ev3(FK, 'FK', 1))],
                    wtm3, 130, tm3, [(PS[0], pk[0]), (PS[1], pk[1]), (PS[2], pk[2]), (PS[3], pk[3])], "p3")
            LFv = LF[:].rearrange("p j h -> p (j h)")
            P.I('act', lambda e: e.activation(out=LFv, in_=LFv, func=AF.Exp, scale=-1.0), r=['LF'], w=['LF'])
            P.I('dve', lambda e: e.tensor_scalar(out=LFv, in0=LFv, scalar1=1.0, scalar2=None, op0=OP.add), r=['LF'], w=['LF'])
            P.I('act', lambda e: e.activation(out=LFv, in_=LFv, func=AF.Ln), r=['LF'], w=['LF'])
            P.I('dve', lambda e: e.tensor_scalar(out=LFv, in0=LFv, scalar1=-1.0, scalar2=None, op0=OP.mult), r=['LF'], w=['LF'])
            _mm(P, PS[0][:, 0:128], C['trif'][:], LFv, True, True, ['c_trif', 'LF'], [pk[0]])
            _mm(P, PS[1][:, 0:128], C['onesf'][:], LFv, True, True, ['c_onesf', 'LF'], [pk[1]])
            TOT = sb("TOT", [128, NT, 2], F32); INCL = sb("INCL", [128, NT, 2], F32); CUM = sb("CUM", [128, NT, 2], F32); ONE64 = sb("ONE64", [128, NT], F32)
            P.I('dve', lambda e: e.memset(ONE64[:], 1.0), w=['ONE64'])
            P.I('act', lambda e: e.copy(out=TOT[:].rearrange("p j h -> p (j h)"), in_=PS[1][:, 0:128]), r=[pk[1]], w=['TOT'])
            for h in range(2):
                P.I('dve', lambda e, h=h: e.tensor_tensor_scan(out=INCL[:, :, h], data0=ONE64[:], data1=TOT[:, :, h], initial=0.0, op0=OP.mult, op1=OP.add), r=['TOT', 'ONE64'], w=['INCL'])
            P.I('dve', lambda e: e.tensor_tensor(out=CUM[:].rearrange("p j h -> p (j h)"), in0=PS[0][:, 0:128], in1=INCL[:].rearrange("p j h -> p (j h)"), op=OP.add), r=[pk[0], 'INCL'], w=['CUM'])
            P.I('dve', lambda e: e.tensor_tensor(out=CUM[:], in0=CUM[:], in1=TOT[:], op=OP.subtract), r=['CUM', 'TOT'], w=['CUM'])

            BI = [sb(f"BI{i}", [128, 4, NT], F32) for i in range(2)]
            PT = [sb(f"FPT{i}", [128, 512], BF16) for i in range(3)]
            RR = sb("FRR", [65, 512], F32); BCS = sb("FBCS", [64, 512], F32); YF = [sb(f"YF{i}", [64, 512], F32) for i in range(2)]
            ptc = 0
            it = 0
            for h in range(2):
                po = 64 * h
                for qc in range(T // 512):
                    bi, bik = BI[it % 2], f"BI{it % 2}"
                    pso, psok = PS[4 + it % 2], pk[4 + it % 2]
                    for b in range(4):
                        i = 4 * qc + b
                        P.I('dve', lambda e, bi=bi, b=b, i=i, h=h: e.tensor_scalar(out=bi[:, b, 0:i + 1], in0=CUM[:, 0:i + 1, h], scalar1=-1.0, scalar2=INCL[:, i, h:h + 1], op0=OP.mult, op1=OP.add),
                            r=['CUM', 'INCL'], w=[bik])
                    nj = 4 * qc + 4

                    def fqk(j):
                        c0 = 128 * max(j - 4 * qc, 0)
                        pst, pstk = PS[j % 4], pk[j % 4]
                        _mm(P, pst[:, c0:512], FK[po:po + 64, j * 128:(j + 1) * 128], FQ[po:po + 64, qc * 512 + c0:(qc + 1) * 512], True, True, ['FK', 'FQ'], [pstk])

                    def frest(j):
                        nonlocal ptc
                        a = j - 4 * qc
                        b0 = max(a, 0)
                        c0 = 128 * b0
                        pst, pstk = PS[j % 4], pk[j % 4]
                        pt, ptk = PT[ptc % 3], f"FPT{ptc % 3}"
                        ptc += 1
                        for b in range(b0, 4):
                            P.I('act', lambda e, b=b: e.activation(out=pt[:, b * 128:(b + 1) * 128], in_=pst[:, b * 128:(b + 1) * 128], func=AF.Exp, bias=bi[:, b, j:j + 1], scale=SCALE),
                                r=[pstk, bik], w=[f"{ptk}_{b}"])
                        if a >= 0:
                            P.I('dve', lambda e: e.tensor_tensor(out=pt[:, c0:c0 + 128], in0=pt[:, c0:c0 + 128], in1=C['tri'][:], op=OP.mult), r=[f"{ptk}_{b0}", 'c_tri'], w=[f"{ptk}_{b0}"])
                        _mm(P, pso[0:65, c0:512], FV[:, j, h, :], pt[:, c0:512], j == 0, j == nj - 1, ['FV'] + [f"{ptk}_{b}" for b in range(b0, 4)], [psok])

                    fqk(0)
                    for j in range(nj):
                        if j + 1 < nj:
                            fqk(j + 1)
                        frest(j)
                    P.I('dve', lambda e, pso=pso: e.reciprocal(out=RR[64:65, :], in_=pso[64:65, 0:512]), r=[psok], w=['FRR'])
                    _mm(P, PS[6][0:64, 0:512], C['onesf'][64:65, 0:64], RR[64:65, :], True, True, ['c_onesf', 'FRR'], [pk[6]])
                    P.I('act', lambda e: e.copy(out=BCS[:], in_=PS[6][0:64, 0:512]), r=[pk[6]], w=['FBCS'])
                    Y = YF[it % 2]; yk = f"YF{it % 2}"
                    P.I('dve', lambda e, pso=pso, Y=Y: e.tensor_tensor(out=Y[:], in0=pso[0:64, 0:512], in1=BCS[:], op=OP.mult), r=[psok, 'FBCS'], w=[yk])
                    P.D('sp', lambda e, Y=Y, h=h, qc=qc: e.dma_start(out=yT[256 + 64 * h:320 + 64 * h, qc * 512:(qc + 1) * 512], in_=Y[:]), r=[yk])
                    it += 1
        P.finish()
    return nc


def _prep_A(inp, l, b, j, x_b):
    g = j // 2
    own = [2 * j, 2 * j + 1]
    oth = [2 * j + 2, 2 * j + 3] if j % 2 == 0 else [2 * j - 2, 2 * j - 1]
    w_in = inp['w_in'][l]; b_in = inp['b_in'][l]
    OQ, OKV, OG, OFX, OF = 512, 1024, 1792, 1816, 3352
    r64 = np.arange(64)
    kvc = lambda n, kvi: OKV + ((n * 2 + kvi) * 2 + g) * 64 + r64
    fx = lambda q, h: OFX + (q * 8 + h) * 64 + r64
    fm1 = np.concatenate([128 * j + np.arange(128), kvc(0, 0), kvc(0, 1)])
    fm2 = np.concatenate([OQ + 64 * own[0] + r64, OQ + 64 * own[1] + r64, OQ + 64 * oth[0] + r64, OQ + 64 * oth[1] + r64,
                          kvc(1, 0), kvc(1, 0), kvc(2, 0), kvc(2, 0),
                          [OG + own[0] * 3 + 1, OG + own[0] * 3 + 2, OG + own[1] * 3 + 1, OG + own[1] * 3 + 2]]).astype(np.int64)
    tm2 = np.concatenate([kvc(1, 1), kvc(2, 1), [OG + own[0] * 3, OG + own[1] * 3]]).astype(np.int64)
    fm3 = np.concatenate([fx(0, own[0]), fx(0, own[1]), fx(1, own[0]), fx(1, own[1])])
    tm3 = np.concatenate([fx(2, own[0]), fx(2, own[1]), [OF + own[0], OF + own[1]]]).astype(np.int64)

    def fmb(cols, nch):
        bb = np.zeros((128, nch), np.float32)
        v = b_in[cols]
        for c in range(nch):
            seg = v[c * 128:(c + 1) * 128]
            bb[:len(seg), c] = seg
        return bb
    c32 = np.ascontiguousarray
    win = POOL_WINDOWS[j]
    cwv = np.zeros((128, 4), np.float32); cwv[:, j] = 1.0 / win
    fixv = np.tile((win / np.minimum(np.arange(16) + 1, win)).astype(np.float32)[None, :], (128, 1))
    cw1 = inp['cmp_w1'][l]
    cw1r = np.concatenate([cw1[kv].reshape(32, 64, 256).transpose(1, 0, 2).reshape(64, 32 * 256) for kv in range(2)], axis=0)
    posT = np.concatenate([inp['cmp_pos'][l][kv].T for kv in range(2)], axis=0)
    cb1 = inp['cmp_b1'][l].reshape(2, 2, 128).transpose(2, 0, 1).reshape(128, 4)
    w2 = inp['cmp_w2'][l]
    cw2k = np.concatenate([np.concatenate([w2[0][hf * 128:(hf + 1) * 128], w2[0][hf * 128:(hf + 1) * 128]], axis=1) for hf in range(2)], axis=1)
    cb2k = np.concatenate([inp['cmp_b2'][l][0], inp['cmp_b2'][l][0]])[:, None]
    cw2v = np.concatenate([w2[1][hf * 128:(hf + 1) * 128] for hf in range(2)], axis=1)
    cb2v = np.tile(inp['cmp_b2'][l][1][None, :], (128, 1))
    return {
        "xT": x_b,
        "wfm1": c32(w_in[:, fm1]), "bfm1": fmb(fm1, 2),
        "wfm2": c32(w_in[:, fm2]), "bfm2": fmb(fm2, 5),
        "wtm2": c32(w_in[:, tm2]), "btm2": c32(np.tile(b_in[tm2][None, :], (128, 1))),
        "wfm3": c32(w_in[:, fm3]), "bfm3": fmb(fm3, 2),
        "wtm3": c32(w_in[:, tm3]), "btm3": c32(np.tile(b_in[tm3][None, :], (128, 1))),
        "pw": c32(inp['pool_w'][l][j]), "pbs": c32(np.stack([inp['pool_b'][l][j], inp['pool_scale'][l][128 * j:128 * j + 128]], axis=1)),
        "cw": cwv, "fix": c32(fixv),
        "cw1": c32(cw1r), "posT": c32(posT), "cb1": c32(cb1), "cw2k": c32(cw2k), "cb2k": c32(cb2k.astype(np.float32)),
        "cw2v": c32(cw2v), "cb2v": c32(cb2v),
    }


_NC = {}


def run_A(inp, l, x):
    if 'A' not in _NC:
        _NC['A'] = build_A()
    xTs = [np.ascontiguousarray(x[b].T) for b in range(2)]
    maps = [_prep_A(inp, l, c // 4, c % 4, xTs[c // 4]) for c in range(8)]
    res = run_bass_kernel_spmd(_NC['A'], maps, core_ids=list(range(8)))
    ys = np.empty((2, T, 1536), np.float32)
    for c in range(8):
        b, j = c // 4, c % 4
        yt = res.results[c]["yT"]
        for n in range(3):
            ys[b, :, n * 512 + 128 * j: n * 512 + 128 * j + 128] = yt[n * 128:(n + 1) * 128, :].T
    return ys


NTB = 2048
NE = 32
CAP = 384
U32 = mybir.dt.uint32


def _layernorm(P, nc, R, rk, g_t, b_t, out, outk, ST, MV, tag):
    for c in range(2):
        P.I('dve', lambda e, c=c: e.bn_stats(out=ST[:, c, :], in_=R[:, c * 512:(c + 1) * 512]), r=[rk], w=['ST' + tag])
    P.I('dve', lambda e: e.bn_aggr(out=MV[:, 0:2], in_=ST[:].rearrange("p c s -> p (c s)")), r=['ST' + tag], w=['MV' + tag])
    P.I('dve', lambda e: e.tensor_scalar(out=MV[:, 2:3], in0=MV[:, 1:2], scalar1=LN_EPS, scalar2=None, op0=OP.add), r=['MV' + tag], w=['MV' + tag])
    P.I('act', lambda e: e.activation(out=MV[:, 2:3], in_=MV[:, 2:3], func=AF.Sqrt), r=['MV' + tag], w=['MV' + tag])
    P.I('dve', lambda e: e.reciprocal(out=MV[:, 2:3], in_=MV[:, 2:3]), r=['MV' + tag], w=['MV' + tag])
    P.I('dve', lambda e: e.tensor_scalar(out=out, in0=R[:], scalar1=MV[:, 0:1], scalar2=MV[:, 2:3], op0=OP.subtract, op1=OP.mult), r=[rk, 'MV' + tag], w=[outk])
    P.I('dve', lambda e: e.tensor_tensor(out=out, in0=out, in1=g_t[:], op=OP.mult), r=[outk, 'lng' + tag], w=[outk])
    P.I('dve', lambda e: e.tensor_tensor(out=out, in0=out, in1=b_t[:], op=OP.add), r=[outk, 'lnb' + tag], w=[outk])


def build_B():
    nc = bass.Bass("TRN2", target_bir_lowering=False)
    dt = lambda n, s, k="ExternalInput": nc.dram_tensor(n, s, F32, kind=k).ap()
    xT = dt("xT", [D, NTB]); xtok = dt("xtok", [NTB, D]); yT = dt("yT", [1536, NTB])
    wg_d = dt("wg", [D, 3072]); bg_d = dt("bg", [128, 24]); wup_d = dt("wup", [1536, D]); wo_d = dt("wo", [D, D])
    l1g_d = dt("l1g", [128, D]); l1b_d = dt("l1b", [128, D]); l2g_d = dt("l2g", [128, D]); l2b_d = dt("l2b", [128, D])
    rw_d = dt("rw", [D, NE]); rb_d = dt("rb", [128, NE])
    w1_d = dt("w1", [NE, D, 2048]); b1_d = dt("b1", [128, NE * 16]); w2_d = dt("w2", [NE, D, D]); b2_d = dt("b2", [NE, D])
    xo = dt("xo", [NTB, D], "ExternalOutput")
    x1s = dt("x1s", [NTB, D], "Internal")
    xg_d = nc.dram_tensor("xg", [NE * CAP, D], BF16, kind="Internal").ap()
    yg_d = dt("yg", [NE * CAP, D], "Internal")
    NTT = NTB // 128

    with ExitStack() as es:
        P = Prog(nc, es)
        sbg = lambda n, s, d: es.enter_context(nc.sbuf_tensor(n, s, d))
        PA = [es.enter_context(nc.psum_tensor(f"pa{i}", [128, 512], F32)) for i in range(4)]
        pak = [f"pa{i}" for i in range(4)]
        PH = es.enter_context(nc.psum_tensor("ph", [128, 1024], F32))
        PSB = es.enter_context(nc.psum_tensor("psb", [128, 1024], BF16))
        PR = es.enter_context(nc.psum_tensor("pr", [128, 512], F32))
        C = _consts(P, nc, sbg)
        identf = sbg("identf", [128, 128], F32)
        P.I('dve', lambda e: e.tensor_copy(out=identf[:], in_=C['ident'][:]), r=['c_ident'], w=['identf'])
        DSTI = sbg("DSTI", [128, NTT, 4], U32)
        GR = sbg("GR", [128, NTT, 4], F32)
        GT = sbg("GT", [128, NTT, NE], F32)
        ST = sbg("ST", [128, 2, 6], F32); MV = sbg("MV", [128, 4], F32)

        with ExitStack() as e1:
            sb = lambda n, s, d: e1.enter_context(nc.sbuf_tensor(n, s, d))
            WG = sb("WG", [128, 8, 3072], BF16); WU = sb("WU", [128, 12, D], BF16); WO = sb("WO", [128, 8, D], BF16)
            for k in range(8):
                P.D('pool', lambda e, k=k: e.dma_start(out=WG[:, k, :], in_=wg_d[k * 128:(k + 1) * 128, :]), w=['WG'])
                P.D('pool', lambda e, k=k: e.dma_start(out=WO[:, k, :], in_=wo_d[k * 128:(k + 1) * 128, :]), w=['WO'])
            for k in range(12):
                P.D('pool', lambda e, k=k: e.dma_start(out=WU[:, k, :], in_=wup_d[k * 128:(k + 1) * 128, :]), w=['WU'])
            bg = sb("bg_s", [128, 24], F32); l1g = sb("l1g_s", [128, D], F32); l1b = sb("l1b_s", [128, D], F32)
            RW = sb("RW", [128, 8, NE], F32); rb = sb("rb_s", [128, NE], F32)
            P.D('sp', lambda e: e.dma_start(out=bg[:], in_=bg_d), w=['bg'])
            P.D('sp', lambda e: e.dma_start(out=l1g[:], in_=l1g_d), w=['lng1'])
            P.D('sp', lambda e: e.dma_start(out=l1b[:], in_=l1b_d), w=['lnb1'])
            P.D('sp', lambda e: e.dma_start(out=RW[:], in_=rw_d.rearrange("(k p) n -> p k n", p=128)), w=['RW'])
            P.D('sp', lambda e: e.dma_start(out=rb[:], in_=rb_d), w=['rb'])
            XB = [sb(f"XB{i}", [128, 8, 512], BF16) for i in range(2)]
            YB = [sb(f"YB{i}", [128, 12, 512], BF16) for i in range(1)]
            GS = sb("GS", [128, 512], F32); TM = sb("TM", [128, 512], F32); MA = sb("MA", [128, 512], F32)
            MT = sb("MT", [128, 8, 512], BF16)
            XT_ = [sb(f"XTK{i}", [128, D], F32) for i in range(2)]
            RR_ = sb("RRb", [128, D], F32); X1 = sb("X1", [128, D], F32); X1B = sb("X1B", [128, D], BF16)
            X1T32 = sb("X1T32", [128, 8, 128], F32); LG = sb("LG", [128, NE], F32); M8 = sb("M8b", [128, 8], F32)
            EXg = sb("EXg", [128, NE], F32); MK = sb("MK", [128, NE], F32); SM = sb("SM", [128, 2], F32)
            TRIS = sb("TRIS", [128, 128], BF16); ONESB = sb("ONESB", [128, 128], BF16); RUNE = sb("RUNE", [128, NE], F32)
            P.I('dve', lambda e: e.tensor_scalar(out=TRIS[:], in0=C['trif'][:], scalar1=0.0, scalar2=None, op0=OP.add), r=['c_trif'], w=['TRIS'])
            P.I('dve', lambda e: e.tensor_tensor(out=TRIS[:], in0=TRIS[:], in1=C['ident'][:], op=OP.subtract), r=['TRIS', 'c_ident'], w=['TRIS'])
            P.I('dve', lambda e: e.memset(ONESB[:], 1.0), w=['ONESB'])
            P.I('pool', lambda e: e.iota(RUNE[:], pattern=[[CAP, NE]], base=0, channel_multiplier=0, allow_small_or_imprecise_dtypes=True), w=['RUNE'])
            MKB = sb("MKB", [128, NE], BF16); DESTF = sb("DESTF", [128, NE], F32); TMPR = sb("TMPR", [128, NE], F32); DSTF = sb("DSTF", [128, 4], F32)
            X1Bs = [X1B, sb("X1B2", [128, D], BF16)]
            ZR = sb("ZR", [128, D], BF16)
            P.I('dve', lambda e: e.memset(ZR[:], 0.0), w=['ZR'])
            for zb in range(NE * CAP // 128):
                P.D('sp', lambda e, zb=zb: e.dma_start(out=xg_d[zb * 128:(zb + 1) * 128, :], in_=ZR[:]), r=['ZR'], w=['xg'])
            xv = xT.rearrange("(k p) t -> p k t", p=128)
            yv = yT.rearrange("(k p) t -> p k t", p=128)
            pi = 0
            for tc in range(NTB // 512):
                X = XB[tc % 2]; Y = YB[0]; xk = f"XB{tc % 2}"; yk = "YB0"
                for k2 in range(2):
                    P.D('pool', lambda e, X=X, tc=tc, k2=k2: e.dma_start(out=X[:, 4 * k2:4 * k2 + 4, :], in_=xv[:, 4 * k2:4 * k2 + 4, tc * 512:(tc + 1) * 512]), w=[xk])
                for k3 in range(3):
                    P.D('pool', lambda e, Y=Y, tc=tc, k3=k3: e.dma_start(out=Y[:, 4 * k3:4 * k3 + 4, :], in_=yv[:, 4 * k3:4 * k3 + 4, tc * 512:(tc + 1) * 512]), w=[yk])
                for dc in range(8):
                    for n in range(3):
                        pg, pgk = PA[pi % 4], pak[pi % 4]; pi += 1
                        pu, puk = PA[pi % 4], pak[pi % 4]; pi += 1
                        col = n * 1024 + dc * 128
                        for k in range(8):
                            _mm(P, pg[:, 0:512], WG[:, k, col:col + 128], X[:, k, :], k == 0, k == 7, ['WG', xk], [pgk])
                        for k in range(4):
                            _mm(P, pu[:, 0:512], WU[:, n * 4 + k, dc * 128:(dc + 1) * 128], Y[:, n * 4 + k, :], k == 0, k == 3, ['WU', yk], [puk])
                        bc_ = n * 8 + dc
                        P.I('act', lambda e, pg=pg, bc_=bc_: e.activation(out=GS[:], in_=pg[:, 0:512], func=AF.Sigmoid, bias=bg[:, bc_:bc_ + 1], scale=1.0), r=[pgk, 'bg'], w=['GS'])
                        if n == 0:
                            P.I('dve', lambda e, pu=pu: e.tensor_tensor(out=MA[:], in0=pu[:, 0:512], in1=GS[:], op=OP.mult), r=[puk, 'GS'], w=['MA'])
                        else:
                            P.I('dve', lambda e, pu=pu: e.tensor_tensor(out=TM[:], in0=pu[:, 0:512], in1=GS[:], op=OP.mult), r=[puk, 'GS'], w=['TM'])
                            if n == 1:
                                P.I('dve', lambda e: e.tensor_tensor(out=MA[:], in0=MA[:], in1=TM[:], op=OP.add), r=['MA', 'TM'], w=['MA'])
                            else:
                                P.I('dve', lambda e, dc=dc: e.tensor_tensor(out=MT[:, dc, :], in0=MA[:], in1=TM[:], op=OP.add), r=['MA', 'TM'], w=['MT'])
                for tt in range(4):
                    ti = tc * 4 + tt
                    xt = XT_[ti % 2]; xtk = f"XTK{ti % 2}"
                    P.D('sp', lambda e, xt=xt, ti=ti: e.dma_start(out=xt[:], in_=xtok[ti * 128:(ti + 1) * 128, :]), w=[xtk])
                    for hf in range(2):
                        for k in range(8):
                            _mm(P, PH[:, hf * 512:(hf + 1) * 512], MT[:, k, tt * 128:(tt + 1) * 128], WO[:, k, hf * 512:(hf + 1) * 512], k == 0, k == 7, ['MT', 'WO'], ['ph'])
                    P.I('dve', lambda e, xt=xt: e.scalar_tensor_tensor(out=RR_[:], in0=xt[:], scalar=ALPHA, in1=PH[:, :], op0=OP.mult, op1=OP.add), r=[xtk, 'ph'], w=['RRb'])
                    _layernorm(P, nc, RR_, 'RRb', l1g, l1b, X1[:], 'X1', ST, MV, '1')
                    P.D('sp', lambda e, ti=ti: e.dma_start(out=x1s[ti * 128:(ti + 1) * 128, :], in_=X1[:]), r=['X1'], w=['x1s'])
                    x1b = X1Bs[ti % 2]; x1bk = f"X1B{ti % 2}"
                    P.I('act', lambda e, x1b=x1b: e.copy(out=x1b[:], in_=X1[:]), r=['X1'], w=[x1bk])
                    for k in range(8):
                        P.I('pe', lambda e, k=k: e.transpose(out=PH[:, k * 128:(k + 1) * 128], in_=X1[:, k * 128:(k + 1) * 128], identity=identf[:]), r=['X1', 'identf'], w=['ph'])
                    P.I('act', lambda e: e.copy(out=X1T32[:].rearrange("p k t -> p (k t)"), in_=PH[:, :]), r=['ph'], w=['X1T32'])
                    for k in range(8):
                        _mm(P, PR[:, 0:NE], X1T32[:, k, :], RW[:, k, :], k == 0, k == 7, ['X1T32', 'RW'], ['pr'])
                    P.I('dve', lambda e: e.tensor_tensor(out=LG[:], in0=PR[:, 0:NE], in1=rb[:], op=OP.add), r=['pr', 'rb'], w=['LG'])
                    P.I('dve', lambda e: e.max(out=M8[:], in_=LG[:]), r=['LG'], w=['M8b'])
                    P.I('dve', lambda e: e.tensor_scalar(out=SM[:, 0:1], in0=M8[:, 0:1], scalar1=-1.0, scalar2=None, op0=OP.mult), r=['M8b'], w=['SM'])
                    P.I('act', lambda e: e.activation(out=EXg[:], in_=LG[:], func=AF.Exp, bias=SM[:, 0:1], scale=1.0), r=['LG', 'SM'], w=['EXg'])
                    P.I('dve', lambda e: e.scalar_tensor_tensor(out=MK[:], in0=LG[:], scalar=M8[:, 3:4], in1=EXg[:], op0=OP.is_ge, op1=OP.mult), r=['LG', 'M8b', 'EXg'], w=['MK'])
                    P.I('dve', lambda e: e.reduce_sum(out=SM[:, 1:2], in_=MK[:], axis=AX.X), r=['MK'], w=['SM'])
                    P.I('dve', lambda e: e.reciprocal(out=SM[:, 1:2], in_=SM[:, 1:2]), r=['SM'], w=['SM'])
                    P.I('dve', lambda e, ti=ti: e.tensor_scalar(out=GT[:, ti, :], in0=MK[:], scalar1=SM[:, 1:2], scalar2=None, op0=OP.mult), r=['MK', 'SM'], w=['GT'])
                    P.I('dve', lambda e: e.tensor_scalar(out=MKB[:], in0=LG[:], scalar1=M8[:, 3:4], scalar2=None, op0=OP.is_ge), r=['LG', 'M8b'], w=['MKB'])
                    _mm(P, PR[:, 32:64], TRIS[:], MKB[:], True, True, ['TRIS', 'MKB'], ['pr'])
                    _mm(P, PR[:, 64:96], ONESB[:], MKB[:], True, True, ['ONESB', 'MKB'], ['pr'])
                    P.I('dve', lambda e: e.tensor_tensor(out=DESTF[:], in0=PR[:, 32:64], in1=RUNE[:], op=OP.add), r=['pr', 'RUNE'], w=['DESTF'])
                    P.I('dve', lambda e: e.tensor_tensor(out=RUNE[:], in0=PR[:, 64:96], in1=RUNE[:], op=OP.add), r=['pr', 'RUNE'], w=['RUNE'])
                    for r_ in range(4):
                        P.I('dve', lambda e, r_=r_: e.scalar_tensor_tensor(out=TMPR[:], in0=LG[:], scalar=M8[:, r_:r_ + 1], in1=DESTF[:], op0=OP.is_equal, op1=OP.mult), r=['LG', 'M8b', 'DESTF'], w=['TMPR'])
                        P.I('dve', lambda e, r_=r_: e.reduce_sum(out=DSTF[:, r_:r_ + 1], in_=TMPR[:], axis=AX.X), r=['TMPR'], w=['DSTF'])
                        P.I('dve', lambda e, r_=r_, ti=ti: e.scalar_tensor_tensor(out=TMPR[:], in0=LG[:], scalar=M8[:, r_:r_ + 1], in1=GT[:, ti, :], op0=OP.is_equal, op1=OP.mult), r=['LG', 'M8b', 'GT'], w=['TMPR'])
                        P.I('dve', lambda e, r_=r_, ti=ti: e.reduce_sum(out=GR[:, ti, r_:r_ + 1], in_=TMPR[:], axis=AX.X), r=['TMPR'], w=['GR'])
                    P.I('dve', lambda e, ti=ti: e.tensor_copy(out=DSTI[:, ti, :], in_=DSTF[:]), r=['DSTF'], w=['DSTI'])
                    for r_ in range(4):
                        P.D('pool', lambda e, r_=r_, ti=ti, x1b=x1b: e.indirect_dma_start(out=xg_d, out_offset=bass.IndirectOffsetOnAxis(ap=DSTI[:, ti, r_:r_ + 1], axis=0), in_=x1b[:], in_offset=None),
                            r=[x1bk, 'DSTI'], w=['xg'])
            P.barrier()

        NCT = CAP // 128
        with ExitStack() as e2:
            sb = lambda n, s, d: e2.enter_context(nc.sbuf_tensor(n, s, d))
            W1 = [sb(f"W1_{i}", [128, 8, 2048], BF16) for i in range(2)]
            W2 = [sb(f"W2_{i}", [128, 8, D], BF16) for i in range(1)]
            B1 = sb("B1", [128, NE * 16], F32)
            P.D('sp', lambda e: e.dma_start(out=B1[:], in_=b1_d), w=['B1'])
            ACC = sb("ACC", [128, NTT, D], F32)
            B2 = sb("B2", [NE, D], F32); GTT = sb("GTT", [NE, 128], F32)
            P.D('sp', lambda e: e.dma_start(out=B2[:], in_=b2_d), w=['B2'])
            XG = [sb(f"XG{i}", [128, NCT, D], BF16) for i in range(2)]
            XGT = [sb(f"XGT{i}", [128, 8, CAP], BF16) for i in range(2)]
            AT = sb("AT", [128, 8, CAP], BF16)
            GEN = [sb(f"GEN{i}", [128, D], F32) for i in range(3)]
            YRS = [sb(f"YRS{i}", [128, D], F32) for i in range(2)]
            GG = GEN[0][:, 0:CAP]; SG = GEN[0][:, 512:512 + CAP]; UU = GEN[1][:, 0:CAP]
            YO = [GEN[1], GEN[2]]

            def load_w(ex):
                s_ = ex % 2
                P.D('pool', lambda e: e.dma_start(out=W1[s_][:], in_=w1_d[ex].rearrange("(k p) n -> p k n", p=128)), w=[f'W1_{s_}'])

            def load_w2(ex):
                P.D('pool', lambda e: e.dma_start(out=W2[0][:], in_=w2_d[ex].rearrange("(k p) n -> p k n", p=128)), w=['W2_0'])

            def load_x(ex):
                s_ = ex % 2
                P.D('sp', lambda e: e.dma_start(out=XG[s_][:], in_=xg_d[ex * CAP:(ex + 1) * CAP, :].rearrange("(t p) d -> p t d", p=128)), r=['xg'], w=[f'XG{s_}'])

            load_w(0)
            load_w2(0)
            load_x(0)
            for ti in range(NTT):
                P.D('sp', lambda e, ti=ti: e.dma_start(out=ACC[:, ti, :], in_=x1s[ti * 128:(ti + 1) * 128, :]), w=[f'ACC{ti}'])
                P.I('act', lambda e, ti=ti: e.activation(out=ACC[:, ti, :], in_=ACC[:, ti, :], func=AF.Copy, scale=ALPHA), r=[f'ACC{ti}'], w=[f'ACC{ti}'])
                P.I('pe', lambda e, ti=ti: e.transpose(out=PR[0:NE, 128:256], in_=GT[:, ti, :], identity=identf[:]), r=['GT', 'identf'], w=['pr'])
                P.I('act', lambda e: e.copy(out=GTT[:], in_=PR[0:NE, 128:256]), r=['pr'], w=['GTT'])
                for hf in range(2):
                    _mm(P, PH[:, hf * 512:(hf + 1) * 512], GTT[:], B2[:, hf * 512:(hf + 1) * 512], True, True, ['GTT', 'B2'], ['ph'])
                P.I('dve', lambda e, ti=ti: e.tensor_tensor(out=ACC[:, ti, :], in0=ACC[:, ti, :], in1=PH[:, :], op=OP.add), r=[f'ACC{ti}', 'ph'], w=[f'ACC{ti}'])
            pi = 0
            yoc = 0
            for ex in range(NE):
                if ex + 1 < NE:
                    load_w(ex + 1)
                    load_x(ex + 1)
                s_ = ex % 2
                w1k, w2k, xgk, xgtk = f'W1_{s_}', 'W2_0', f'XG{s_}', f'XGT{s_}'
                for tt in range(NCT):
                    for k in range(8):
                        P.I('pe', lambda e, k=k, tt=tt: e.transpose(out=PSB[:, k * 128:(k + 1) * 128], in_=XG[s_][:, tt, k * 128:(k + 1) * 128], identity=C['ident'][:]), r=[xgk, 'c_ident'], w=['psb'])
                    P.I('act', lambda e, tt=tt: e.copy(out=XGT[s_][:, :, tt * 128:(tt + 1) * 128], in_=PSB[:, :].rearrange("p (k t) -> p k t", t=128)), r=['psb'], w=[xgtk])
                for f in range(8):
                    pg, pgk = PA[pi % 4], pak[pi % 4]; pi += 1
                    pu, puk = PA[pi % 4], pak[pi % 4]; pi += 1
                    for k in range(8):
                        _mm(P, pg[:, 0:CAP], W1[s_][:, k, f * 128:(f + 1) * 128], XGT[s_][:, k, :], k == 0, k == 7, [w1k, xgtk], [pgk])
                    for k in range(8):
                        _mm(P, pu[:, 0:CAP], W1[s_][:, k, 1024 + f * 128:1024 + (f + 1) * 128], XGT[s_][:, k, :], k == 0, k == 7, [w1k, xgtk], [puk])
                    cg = ex * 16 + f; cu = ex * 16 + 8 + f
                    P.I('dve', lambda e, pg=pg, cg=cg: e.tensor_scalar(out=GG, in0=pg[:, 0:CAP], scalar1=B1[:, cg:cg + 1], scalar2=7.0, op0=OP.add, op1=OP.min), r=[pgk, 'B1'], w=['GEN0'])
                    P.I('act', lambda e: e.activation(out=SG, in_=GG, func=AF.Sigmoid, scale=1.702), r=['GEN0'], w=['GEN0'])
                    P.I('dve', lambda e, pu=pu, cu=cu: e.tensor_scalar(out=UU, in0=pu[:, 0:CAP], scalar1=B1[:, cu:cu + 1], scalar2=7.0, op0=OP.add, op1=OP.min), r=[puk, 'B1'], w=['GEN1'])
                    P.I('pool', lambda e: e.tensor_scalar(out=UU, in0=UU, scalar1=-7.0, scalar2=1.0, op0=OP.max, op1=OP.add), r=['GEN1'], w=['GEN1'])
                    P.I('pool', lambda e: e.tensor_tensor(out=GG, in0=GG, in1=UU, op=OP.mult), r=['GEN0', 'GEN1'], w=['GEN0'])
                    P.I('dve', lambda e, f=f: e.tensor_tensor(out=AT[:, f, :], in0=GG, in1=SG, op=OP.mult), r=['GEN0'], w=['AT'])
                for tt in range(NCT):
                    for hf in range(2):
                        for f in range(8):
                            _mm(P, PH[:, hf * 512:(hf + 1) * 512], AT[:, f, tt * 128:(tt + 1) * 128], W2[0][:, f, hf * 512:(hf + 1) * 512], f == 0, f == 7, ['AT', w2k], ['ph'])
                    yo = GEN[2]; yok = 'GEN2'
                    P.I('act', lambda e, yo=yo: e.copy(out=yo[:], in_=PH[:, :]), r=['ph'], w=[yok])
                    row = ex * CAP + tt * 128
                    P.D('sp', lambda e, yo=yo, row=row: e.dma_start(out=yg_d[row:row + 128, :], in_=yo[:]), r=[yok], w=['yg'])
                if ex + 1 < NE:
                    load_w2(ex + 1)
            P.barrier()
            l2g = GEN[0]; l2b = GEN[1]
            P.D('sp', lambda e: e.dma_start(out=l2g[:], in_=l2g_d), w=['lng2'])
            P.D('sp', lambda e: e.dma_start(out=l2b[:], in_=l2b_d), w=['lnb2'])
            gi = 0
            for ti in range(NTT):
                for r_ in range(4):
                    yr = YRS[gi % 2]; yrk = f"YRS{gi % 2}"; gi += 1
                    P.D('pool', lambda e, yr=yr, ti=ti, r_=r_: e.indirect_dma_start(out=yr[:], out_offset=None, in_=yg_d, in_offset=bass.IndirectOffsetOnAxis(ap=DSTI[:, ti, r_:r_ + 1], axis=0)),
                        r=['yg', 'DSTI'], w=[yrk])
                    P.I('dve', lambda e, yr=yr, ti=ti, r_=r_: e.scalar_tensor_tensor(out=ACC[:, ti, :], in0=yr[:], scalar=GR[:, ti, r_:r_ + 1], in1=ACC[:, ti, :], op0=OP.mult, op1=OP.add),
                        r=[yrk, 'GR', f'ACC{ti}'], w=[f'ACC{ti}'])
                o = GEN[2]; ok = "GEN2"
                _layernorm(P, nc, ACC[:, ti, :], f'ACC{ti}', l2g, l2b, o[:], ok, ST, MV, '2')
                P.D('sp', lambda e, o=o, ti=ti: e.dma_start(out=xo[ti * 128:(ti + 1) * 128, :], in_=o[:]), r=[ok])
        P.finish()
    return nc


def _prep_B(inp, l, b, r, x, ys):
    tok = slice(NTB * r, NTB * (r + 1))
    c32 = lambda a: np.ascontiguousarray(a, dtype=np.float32)
    w_in = inp['w_in'][l]; b_in = inp['b_in'][l]
    bc = lambda v: c32(np.tile(v[None, :], (128, 1)))
    return {
        "xT": c32(x[b, tok].T), "xtok": c32(x[b, tok]), "yT": c32(ys[b, tok].T),
        "wg": c32(w_in[:, 3360:6432]), "bg": c32(b_in[3360:6432].reshape(24, 128).T),
        "wup": c32(inp['w_up'][l].reshape(1536, D)), "wo": c32(inp['w_o'][l]),
        "l1g": bc(inp['ln1_g'][l]), "l1b": bc(inp['ln1_b'][l]), "l2g": bc(inp['ln2_g'][l]), "l2b": bc(inp['ln2_b'][l]),
        "rw": c32(inp['router_w'][l]), "rb": bc(inp['router_b'][l]),
        "w1": inp['moe_w1'][l], "b1": c32(inp['moe_b1'][l].reshape(NE * 16, 128).T), "w2": inp['moe_w2'][l], "b2": c32(inp['moe_b2'][l]),
    }


def run_B(inp, l, x, ys):
    if 'B' not in _NC:
        _NC['B'] = build_B()
    maps = [_prep_B(inp, l, c // 4, c % 4, x, ys) for c in range(8)]
    res = run_bass_kernel_spmd(_NC['B'], maps, core_ids=list(range(8)))
    out = np.empty((2, T, D), np.float32)
    for c in range(8):
        out[c // 4, NTB * (c % 4):NTB * (c % 4 + 1)] = res.results[c]["xo"]
    return out


def kernel(**inputs):
    inp = {k: np.asarray(v) for k, v in inputs.items()}
    x = np.ascontiguousarray(inp['x'], dtype=np.float32)
    for l in range(2):
        ys = run_A(inp, l, x)
        x = run_B(inp, l, x, ys)
    return x
```

```python
import numpy as np
from contextlib import ExitStack
import concourse.bass as bass
import concourse.mybir as mybir
from concourse.bass_utils import run_bass_kernel_spmd

F32 = mybir.dt.float32
BF16 = mybir.dt.bfloat16
AF = mybir.ActivationFunctionType
OP = mybir.AluOpType
AX = mybir.AxisListType

T = 8192
D = 1024
NT = T // 128
SCALE = 0.125
ALPHA = 4.0 ** 0.25
POOL_WINDOWS = (2, 4, 8, 16)
LN_EPS = 1e-5


class Prog:
    NDMA = 12

    def __init__(self, nc, es):
        self.nc = nc
        self.eng = {'pe': nc.tensor, 'act': nc.scalar, 'dve': nc.vector, 'pool': nc.gpsimd, 'sp': nc.sync}
        self.sem = {n: es.enter_context(nc.semaphore("s_" + n)) for n in self.eng}
        self.cnt = {n: 0 for n in self.eng}
        self.dsem = [es.enter_context(nc.semaphore(f"d{i}")) for i in range(self.NDMA)]
        self.dval = [0] * self.NDMA
        self.dnext = 0
        self.seen = {n: {} for n in self.eng}
        self.lastw = {}
        self.lastw_isread = {}
        self.readers = {}

    def _wait(self, en, tok):
        if tok is None:
            return
        kind, a, v = tok
        if kind == 'e' and a == en and en == 'pe':
            return
        src = (kind, a)
        if self.seen[en].get(src, 0) >= v:
            return
        s = self.sem[a] if kind == 'e' else self.dsem[a]
        self.eng[en].wait_ge(s, v)
        self.seen[en][src] = v

    def _deps(self, en, r, w):
        for k in r:
            self._wait(en, self.lastw.get(k))
        for k in w:
            self._wait(en, self.lastw.get(k))
            for t in self.readers.get(k, ()):
                self._wait(en, t)

    def _commit(self, tok, r, w):
        for k in r:
            self.readers.setdefault(k, []).append(tok)
        for k in w:
            self.lastw[k] = tok
            self.readers[k] = []

    @staticmethod
    def _psum_excl(r, w):
        pr = [k for k in r if k.startswith(('ps', 'pa', 'ph', 'pr'))]
        if pr:
            r = [k for k in r if k not in pr]
            w = list(w) + pr
        w = ['psb' if k in ('psb0', 'psb1') else k for k in w]
        return r, w

    def I(self, en, fn, r=(), w=()):
        pr = [k for k in r if k.startswith(('ps', 'pa', 'ph', 'pr'))]
        r, w = self._psum_excl(r, w)
        skip = [k for k in pr if self.lastw_isread.get(k) == en]
        saved = {k: self.lastw[k] for k in skip}
        for k in skip:
            del self.lastw[k]
        self._deps(en, r, w)
        for k in skip:
            self.lastw[k] = saved[k]
        ins = fn(self.eng[en])
        self.cnt[en] += 1
        ins.then_inc(self.sem[en], 1)
        self._commit(('e', en, self.cnt[en]), r, w)
        for k in w:
            self.lastw_isread[k] = en if k in pr else None
        return ins

    def D(self, en, fn, r=(), w=()):
        i = self.dnext
        self.dnext = (self.dnext + 1) % self.NDMA
        if self.dval[i] > 0:
            self._wait(en, ('d', i, self.dval[i]))
        self._deps(en, r, w)
        ins = fn(self.eng[en])
        self.dval[i] += 16
        ins.then_inc(self.dsem[i], 16)
        self._commit(('d', i, self.dval[i]), r, w)
        return ins

    def barrier(self):
        for en in self.eng:
            for o in self.eng:
                if o != en and self.cnt[o] > 0:
                    self._wait(en, ('e', o, self.cnt[o]))
            for i in range(self.NDMA):
                if self.dval[i] > 0:
                    self._wait(en, ('d', i, self.dval[i]))
        self.lastw.clear()
        self.lastw_isread.clear()
        self.readers.clear()

    def finish(self):
        for i in range(self.NDMA):
            if self.dval[i] > 0:
                self._wait('sp', ('d', i, self.dval[i]))
        for o in self.eng:
            if o != 'sp' and self.cnt[o] > 0:
                self._wait('sp', ('e', o, self.cnt[o]))


def _mm(P, out, lhsT, rhs, start, stop, r, w):
    P.I('pe', lambda e: e.matmul(out, lhsT=lhsT, rhs=rhs, start=start, stop=stop), r=r, w=w)


def _consts(P, nc, sb):
    c = {}
    tf = sb("c_tf", [128, 128], F32)
    P.I('pool', lambda e: e.iota(tf[:], pattern=[[1, 128]], base=0, channel_multiplier=-1,
                                 allow_small_or_imprecise_dtypes=True), w=['c_tf'])
    c['ident'] = sb("c_ident", [128, 128], BF16)
    c['tri'] = sb("c_tri", [128, 128], BF16)
    c['atri'] = sb("c_atri", [128, 128], BF16)
    c['trif'] = sb("c_trif", [128, 128], F32)
    c['onesf'] = sb("c_onesf", [128, 128], F32)
    P.I('dve', lambda e: e.tensor_scalar(out=c['ident'][:], in0=tf[:], scalar1=0.0, scalar2=None, op0=OP.is_equal), r=['c_tf'], w=['c_ident'])
    P.I('dve', lambda e: e.tensor_scalar(out=c['tri'][:], in0=tf[:], scalar1=0.0, scalar2=None, op0=OP.is_ge), r=['c_tf'], w=['c_tri'])
    P.I('dve', lambda e: e.tensor_scalar(out=c['atri'][:], in0=tf[:], scalar1=0.0, scalar2=None, op0=OP.is_lt), r=['c_tf'], w=['c_atri'])
    P.I('dve', lambda e: e.tensor_scalar(out=c['trif'][:], in0=tf[:], scalar1=0.0, scalar2=None, op0=OP.is_ge), r=['c_tf'], w=['c_trif'])
    P.I('dve', lambda e: e.memset(c['onesf'][:], 1.0), w=['c_onesf'])
    return c


def _inproj(P, nc, es_outer, xT, wfm_d, bfm, fm_list, wtm_d, ntm, tm_evac, pss, tag, n_tok=T):
    with ExitStack() as es:
        sb = lambda n, s, d: es.enter_context(nc.sbuf_tensor(tag + n, s, d))
        nfm = int(wfm_d.shape[1])
        W = sb("W", [128, 8, nfm], BF16)
        for k in range(8):
            P.D('pool', lambda e, k=k: e.dma_start(out=W[:, k, :], in_=wfm_d[k * 128:(k + 1) * 128, :]), w=[tag + 'W'])
        if ntm:
            WT = sb("WT", [128, 8, ntm], BF16)
            for k in range(8):
                P.D('pool', lambda e, k=k: e.dma_start(out=WT[:, k, :], in_=wtm_d[k * 128:(k + 1) * 128, :]), w=[tag + 'WT'])
        xb = [sb(f"xb{i}", [128, 8, 512], BF16) for i in range(2)]
        xv = xT.rearrange("(k p) t -> p k t", p=128)
        nch = n_tok // 512
        pi = 0
        for tc in range(nch):
            X = xb[tc % 2]
            xk = f"{tag}xb{tc % 2}"
            for k2 in range(2):
                P.D('pool', lambda e, X=X, tc=tc, k2=k2: e.dma_start(out=X[:, 4 * k2:4 * k2 + 4, :], in_=xv[:, 4 * k2:4 * k2 + 4, tc * 512:(tc + 1) * 512]), w=[xk])
            for (c0, M, evac) in fm_list:
                ps, pk = pss[pi % len(pss)]
                pi += 1
                for k in range(8):
                    _mm(P, ps[0:M, 0:512], W[:, k, c0:c0 + M], X[:, k, :], k == 0, k == 7, [tag + 'W', xk], [pk])
                evac(tc, ps, pk)
            if ntm:
                for tt in range(4):
                    ps, pk = pss[pi % len(pss)]
                    pi += 1
                    for k in range(8):
                        _mm(P, ps[:, 0:ntm], X[:, k, tt * 128:(tt + 1) * 128], WT[:, k, :], k == 0, k == 7, [tag + 'WT', xk], [pk])
                    tm_evac(tc * 4 + tt, ps, pk)
    P.barrier()


def build_A():
    nc = bass.Bass("TRN2", target_bir_lowering=False)
    dt = lambda n, s, k="ExternalInput": nc.dram_tensor(n, s, F32, kind=k).ap()
    xT = dt("xT", [D, T])
    wfm1 = dt("wfm1", [D, 256]); bfm1 = dt("bfm1", [128, 2])
    wfm2 = dt("wfm2", [D, 516]); bfm2 = dt("bfm2", [128, 5])
    wtm2 = dt("wtm2", [D, 130]); btm2 = dt("btm2", [128, 130])
    wfm3 = dt("wfm3", [D, 256]); bfm3 = dt("bfm3", [128, 2])
    wtm3 = dt("wtm3", [D, 130]); btm3 = dt("btm3", [128, 130])
    pw_d = dt("pw", [128, 128]); pbs_d = dt("pbs", [128, 2]); cw_d = dt("cw", [128, 4]); fix_d = dt("fix", [128, 16])
    cw1_d = dt("cw1", [128, 32 * 256]); posT_d = dt("posT", [128, 32]); cb1_d = dt("cb1", [128, 4])
    cw2k_d = dt("cw2k", [128, 256]); cb2k_d = dt("cb2k", [128, 1]); cw2v_d = dt("cw2v", [128, 128]); cb2v_d = dt("cb2v", [128, 64])
    yT = dt("yT", [384, T], "ExternalOutput")

    with ExitStack() as es:
        P = Prog(nc, es)
        sbg = lambda n, s, d: es.enter_context(nc.sbuf_tensor(n, s, d))
        PS = [es.enter_context(nc.psum_tensor(f"ps{i}", [128, 512], F32)) for i in range(7)]
        PSB = es.enter_context(nc.psum_tensor("psb", [128, 1024], BF16))
        pk = [f"ps{i}" for i in range(7)]
        C = _consts(P, nc, sbg)
        KCT = sbg("KCT", [128, 512], BF16)
        VC = sbg("VC", [128, 4, 64], BF16)

        with ExitStack() as e1:
            sb = lambda n, s, d: e1.enter_context(nc.sbuf_tensor(n, s, d))
            bf1 = sb("bf1", [128, 2], F32); pwf = sb("pwf", [128, 128], BF16); pbs = sb("pbs_s", [128, 2], F32)
            cw = sb("cw_s", [128, 4], F32); fix = sb("fix_s", [128, 16], F32)
            P.D('sp', lambda e: e.dma_start(out=bf1[:], in_=bfm1), w=['bf1'])
            P.D('pool', lambda e: e.dma_start(out=pwf[:], in_=pw_d), w=['pwf'])
            P.D('sp', lambda e: e.dma_start(out=pbs[:], in_=pbs_d), w=['pbs'])
            P.D('sp', lambda e: e.dma_start(out=cw[:], in_=cw_d), w=['cw'])
            P.D('sp', lambda e: e.dma_start(out=fix[:], in_=fix_d), w=['fix'])
            KVC = sb("KVC", [128, T], BF16)
            UC = [sb(f"UC{i}", [128, 528], F32) for i in range(2)]
            S2 = sb("S2", [128, 528], F32); S4 = sb("S4", [128, 528], F32); S8 = sb("S8", [128, 528], F32); S16 = sb("S16", [128, 528], F32)
            ACC = sb("ACC", [128, 512], F32); DT_ = [sb(f"DTb{i}", [128, 512], BF16) for i in range(2)]
            YP = [sb(f"YP{i}", [128, 512], F32) for i in range(2)]
            P.I('dve', lambda e: e.memset(UC[1][:], 0.0), w=['UC1'])

            def evac_u(tc, ps, pkk):
                U = UC[tc % 2]; Up = UC[(tc + 1) % 2]
                uk, upk = f"UC{tc % 2}", f"UC{(tc + 1) % 2}"
                P.I('act', lambda e: e.activation(out=U[:, 16:528], in_=ps[:, 0:512], func=AF.Identity, bias=bf1[:, 0:1], scale=1.0), r=[pkk, 'bf1'], w=[uk])
                P.I('dve', lambda e: e.tensor_copy(out=U[:, 0:16], in_=Up[:, 512:528]), r=[upk], w=[uk])
                P.I('dve', lambda e: e.tensor_tensor(out=S2[:, 1:528], in0=U[:, 1:528], in1=U[:, 0:527], op=OP.add), r=[uk], w=['S2'])
                P.I('dve', lambda e: e.tensor_tensor(out=S4[:, 3:528], in0=S2[:, 3:528], in1=S2[:, 1:526], op=OP.add), r=['S2'], w=['S4'])
                P.I('dve', lambda e: e.tensor_tensor(out=S8[:, 7:528], in0=S4[:, 7:528], in1=S4[:, 3:524], op=OP.add), r=['S4'], w=['S8'])
                P.I('dve', lambda e: e.tensor_tensor(out=S16[:, 15:528], in0=S8[:, 15:528], in1=S8[:, 7:520], op=OP.add), r=['S8'], w=['S16'])
                P.I('dve', lambda e: e.tensor_scalar(out=ACC[:], in0=S2[:, 16:528], scalar1=cw[:, 0:1], scalar2=None, op0=OP.mult), r=['S2', 'cw'], w=['ACC'])
                for wi, S in enumerate((S4, S8, S16)):
                    P.I('dve', lambda e, S=S, wi=wi: e.scalar_tensor_tensor(out=ACC[:], in0=S[:, 16:528], scalar=cw[:, wi + 1:wi + 2], in1=ACC[:], op0=OP.mult, op1=OP.add),
                        r=['S4', 'S8', 'S16', 'cw', 'ACC'], w=['ACC'])
                if tc == 0:
                    P.I('dve', lambda e: e.tensor_tensor(out=ACC[:, 0:16], in0=ACC[:, 0:16], in1=fix[:], op=OP.mult), r=['ACC', 'fix'], w=['ACC'])
                Dt = DT_[tc % 2]; dk = f"DT{tc % 2}"
                P.I('dve', lambda e: e.tensor_tensor(out=Dt[:], in0=ACC[:], in1=U[:, 16:528], op=OP.subtract), r=['ACC', uk], w=[dk])
                ps2, pk2 = PS[4 + tc % 2], pk[4 + tc % 2]
                _mm(P, ps2[:, 0:512], pwf[:], Dt[:], True, True, ['pwf', dk], [pk2])
                Y = YP[tc % 2]; yk = f"YP{tc % 2}"
                P.I('dve', lambda e: e.tensor_scalar(out=Y[:], in0=ps2[:, 0:512], scalar1=pbs[:, 0:1], scalar2=pbs[:, 1:2], op0=OP.add, op1=OP.mult), r=[pk2, 'pbs'], w=[yk])
                P.D('sp', lambda e: e.dma_start(out=yT[0:128, tc * 512:(tc + 1) * 512], in_=Y[:]), r=[yk])

            def evac_kvc(tc, ps, pkk):
                P.I('act', lambda e: e.activation(out=KVC[:, tc * 512:(tc + 1) * 512], in_=ps[:, 0:512], func=AF.Identity, bias=bf1[:, 1:2], scale=1.0), r=[pkk, 'bf1'], w=['KVC'])

            _inproj(P, nc, e1, xT, wfm1, bf1, [(0, 128, evac_u), (128, 128, evac_kvc)], None, 0, None,
                    [(PS[0], pk[0]), (PS[1], pk[1]), (PS[2], pk[2]), (PS[3], pk[3])], "p1")

            W1 = sb("W1", [128, 32 * 256], BF16)
            for q4 in range(4):
                P.D('pool', lambda e, q4=q4: e.dma_start(out=W1[:, q4 * 2048:(q4 + 1) * 2048], in_=cw1_d[:, q4 * 2048:(q4 + 1) * 2048]), w=['W1'])
            posT = sb("posT_s", [128, 32], BF16); cb1 = sb("cb1_s", [128, 4], F32)
            w2k = sb("w2k", [128, 256], BF16); b2k = sb("b2k", [128, 1], F32); w2v = sb("w2v", [128, 128], BF16); b2v = sb("b2v", [128, 64], F32)
            P.D('pool', lambda e: e.dma_start(out=posT[:], in_=posT_d), w=['posT'])
            P.D('sp', lambda e: e.dma_start(out=cb1[:], in_=cb1_d), w=['cb1'])
            P.D('pool', lambda e: e.dma_start(out=w2k[:], in_=cw2k_d), w=['w2k'])
            P.D('sp', lambda e: e.dma_start(out=b2k[:], in_=cb2k_d), w=['b2k'])
            P.D('pool', lambda e: e.dma_start(out=w2v[:], in_=cw2v_d), w=['w2v'])
            P.D('sp', lambda e: e.dma_start(out=b2v[:], in_=cb2v_d), w=['b2v'])
            HT = [[sb(f"HT{kv}{hf}", [128, 512], BF16) for hf in range(2)] for kv in range(2)]
            XH = sb("XH", [128, 512], F32); T1 = sb("T1c", [128, 512], F32); T2 = sb("T2c", [128, 512], F32); cbt = sb("cbt", [128, 4], F32)
            P.I('dve', lambda e: e.memset(VC[:], 0.0), w=['VC'])
            for kv in range(2):
                po = 64 * kv
                for hf in range(2):
                    ci = kv * 2 + hf
                    ps, pkk = PS[ci % 4], pk[ci % 4]
                    psc, pkc = PS[4 + ci % 2], pk[4 + ci % 2]
                    for l in range(32):
                        wsl = W1[po:po + 64, l * 256 + hf * 128: l * 256 + hf * 128 + 128]
                        _mm(P, psc[:, 0:1], wsl, posT[po:po + 64, l:l + 1], l == 0, l == 31, ['W1', 'posT'], [pkc])
                    P.I('dve', lambda e, ci=ci, psc=psc: e.tensor_tensor(out=cbt[:, ci:ci + 1], in0=psc[:, 0:1], in1=cb1[:, ci:ci + 1], op=OP.add), r=[pkc, 'cb1'], w=['cbt'])
                    for l in range(32):
                        wsl = W1[po:po + 64, l * 256 + hf * 128: l * 256 + hf * 128 + 128]
                        _mm(P, ps[:, 0:511], wsl, KVC[po:po + 64, l:l + 16 * 510 + 1:16], l == 0, l == 31, ['W1', 'KVC'], [pkk])
                    P.I('act', lambda e, ci=ci, ps=ps: e.activation(out=XH[:, 0:511], in_=ps[:, 0:511], func=AF.Identity, bias=cbt[:, ci:ci + 1], scale=1.0), r=[pkk, 'cbt'], w=['XH'])
                    P.I('dve', lambda e: e.tensor_tensor(out=T1[:, 0:511], in0=XH[:, 0:511], in1=XH[:, 0:511], op=OP.mult), r=['XH'], w=['T1'])
                    P.I('dve', lambda e: e.tensor_scalar(out=T1[:, 0:511], in0=T1[:, 0:511], scalar1=0.044715, scalar2=1.0, op0=OP.mult, op1=OP.add), r=['T1'], w=['T1'])
                    P.I('dve', lambda e: e.tensor_tensor(out=T1[:, 0:511], in0=T1[:, 0:511], in1=XH[:, 0:511], op=OP.mult), r=['T1', 'XH'], w=['T1'])
                    P.I('act', lambda e: e.activation(out=T2[:, 0:511], in_=T1[:, 0:511], func=AF.Sigmoid, scale=1.5957691216057308), r=['T1'], w=['T2'])
                    H = HT[kv][hf]
                    P.I('dve', lambda e, H=H: e.memset(H[:, 511:512], 0.0), w=[f'HT{kv}{hf}'])
                    P.I('dve', lambda e, H=H: e.tensor_tensor(out=H[:, 0:511], in0=T2[:, 0:511], in1=XH[:, 0:511], op=OP.mult), r=['T2', 'XH'], w=[f'HT{kv}{hf}'])
            for hf in range(2):
                _mm(P, PS[0][:, 0:512], w2k[:, hf * 128:(hf + 1) * 128], HT[0][hf][:], hf == 0, hf == 1, ['w2k', f'HT0{hf}'], [pk[0]])
            P.I('act', lambda e: e.activation(out=KCT[:], in_=PS[0][:, 0:512], func=AF.Identity, bias=b2k[:, 0:1], scale=1.0), r=[pk[0], 'b2k'], w=['KCT'])
            for ct in range(4):
                m = 128 if ct < 3 else 127
                for hf in range(2):
                    _mm(P, PS[1 + ct % 2][0:m, 0:64], HT[1][hf][:, ct * 128:ct * 128 + m], w2v[:, hf * 64:(hf + 1) * 64], hf == 0, hf == 1, ['w2v', f'HT1{hf}'], [pk[1 + ct % 2]])
                P.I('dve', lambda e, ct=ct, m=m: e.tensor_tensor(out=VC[0:m, ct, :], in0=PS[1 + ct % 2][0:m, 0:64], in1=b2v[0:m, :], op=OP.add), r=[pk[1 + ct % 2], 'b2v'], w=['VC'])
            P.barrier()

        with ExitStack() as e2:
            sb = lambda n, s, d: e2.enter_context(nc.sbuf_tensor(n, s, d))
            bf2 = sb("bf2", [128, 5], F32); bt2 = sb("bt2", [128, 130], F32)
            P.D('sp', lambda e: e.dma_start(out=bf2[:], in_=bfm2), w=['bf2'])
            P.D('sp', lambda e: e.dma_start(out=bt2[:], in_=btm2), w=['bt2'])
            NQ = sb("NQ", [128, T], BF16); NQo = sb("NQo", [128, T], BF16); KS = sb("KS", [128, T], BF16); KW = sb("KW", [128, T], BF16)
            G4 = sb("G4", [4, T], BF16)
            VS = sb("VS", [128, NT, 65], BF16); VW = sb("VW", [128, NT, 65], BF16); GC = sb("GC", [128, NT, 2], F32)
            P.I('pool', lambda e: e.memset(VS[:, :, 64:65], 1.0), w=['VS'])
            P.I('pool', lambda e: e.memset(VW[:, :, 64:65], 1.0), w=['VW'])

            def ev(dst, key, col, func=AF.Identity, M=128):
                def f(tc, ps, pkk):
                    P.I('act', lambda e: e.activation(out=dst[0:M, tc * 512:(tc + 1) * 512], in_=ps[0:M, 0:512], func=func, bias=bf2[0:M, col:col + 1], scale=1.0), r=[pkk, 'bf2'], w=[key])
                return f

            TMB = sb("TMB", [128, 130], F32)

            def tm2(ti, ps, pkk):
                P.I('dve', lambda e: e.tensor_tensor(out=VS[:, ti, 0:64], in0=ps[:, 0:64], in1=bt2[:, 0:64], op=OP.add), r=[pkk, 'bt2'], w=['VS'])
                P.I('dve', lambda e: e.tensor_tensor(out=VW[:, ti, 0:64], in0=ps[:, 64:128], in1=bt2[:, 64:128], op=OP.add), r=[pkk, 'bt2'], w=['VW'])
                P.I('dve', lambda e: e.tensor_tensor(out=TMB[:, 128:130], in0=ps[:, 128:130], in1=bt2[:, 128:130], op=OP.add), r=[pkk, 'bt2'], w=['TMB'])
                P.I('act', lambda e: e.activation(out=GC[:, ti, :], in_=TMB[:, 128:130], func=AF.Sigmoid), r=['TMB'], w=['GC'])

            _inproj(P, nc, e2, xT, wfm2, bf2,
                    [(0, 128, ev(NQ, 'NQ', 0)), (128, 128, ev(NQo, 'NQo', 1)), (256, 128, ev(KS, 'KS', 2)), (384, 128, ev(KW, 'KW', 3)),
                     (512, 4, ev(G4, 'G4', 4, AF.Sigmoid, 4))],
                    wtm2, 130, tm2, [(PS[0], pk[0]), (PS[1], pk[1]), (PS[2], pk[2]), (PS[3], pk[3])], "p2")

            EJ = sb("EJ", [128, NT * 128], BF16); ejf = sb("ejf", [128, 2048], F32)
            for q4 in range(4):
                P.I('pool', lambda e, q4=q4: e.iota(ejf[:], pattern=[[-2, 16], [-1, 2], [0, 64]], base=-32 * q4, channel_multiplier=1,
                                             allow_small_or_imprecise_dtypes=True), w=['ejf'])
                P.I('dve', lambda e, q4=q4: e.tensor_scalar(out=EJ[:, q4 * 2048:(q4 + 1) * 2048], in0=ejf[:], scalar1=0.0, scalar2=None, op0=OP.is_equal), r=['ejf'], w=['EJ'])
            REL = sb("REL", [128, 512], F32)
            P.I('pool', lambda e: e.iota(REL[:], pattern=[[16, 512]], base=31, channel_multiplier=-1, allow_small_or_imprecise_dtypes=True), w=['REL'])
            VV = sb("VV", [128, 254], F32); HP = sb("HP", [128, 1], F32)
            P.I('pool', lambda e: e.iota(VV[:], pattern=[[1, 254]], base=-126, channel_multiplier=0, allow_small_or_imprecise_dtypes=True), w=['VV'])
            P.I('pool', lambda e: e.iota(HP[:], pattern=[[0, 1]], base=0, channel_multiplier=1, allow_small_or_imprecise_dtypes=True), w=['HP'])
            P.I('dve', lambda e: e.tensor_scalar(out=HP[:], in0=HP[:], scalar1=64.0, scalar2=None, op0=OP.is_ge), r=['HP'], w=['HP'])
            P.I('dve', lambda e: e.tensor_scalar(out=VV[:], in0=VV[:], scalar1=HP[:, 0:1], scalar2=None, op0=OP.subtract), r=['VV', 'HP'], w=['VV'])
            KEEP = sb("KEEP", [128, 254], F32); NF = sb("NF", [128, 254], F32); ADD = sb("ADD", [128, 254], F32); TA = sb("TA", [128, 254], F32)
            P.I('dve', lambda e: e.tensor_scalar(out=KEEP[:], in0=VV[:], scalar1=-2.0, scalar2=None, op0=OP.is_le), r=['VV'], w=['KEEP'])
            P.I('dve', lambda e: e.tensor_scalar(out=NF[:], in0=VV[:], scalar1=0.0, scalar2=None, op0=OP.is_le), r=['VV'], w=['NF'])
            P.I('dve', lambda e: e.tensor_scalar(out=ADD[:], in0=VV[:], scalar1=-1.0, scalar2=1.0e6, op0=OP.is_ge, op1=OP.mult), r=['VV'], w=['ADD'])
            P.I('dve', lambda e: e.tensor_scalar(out=TA[:], in0=VV[:], scalar1=0.0, scalar2=-1000001.0, op0=OP.is_gt, op1=OP.mult), r=['VV'], w=['TA'])
            P.I('dve', lambda e: e.tensor_tensor(out=ADD[:], in0=ADD[:], in1=TA[:], op=OP.add), r=['ADD', 'TA'], w=['ADD'])
            SEL = sb("SEL", [4, 256], BF16); self_ = sb("self_", [4, 256], F32)
            P.I('pool', lambda e: e.iota(self_[:], pattern=[[1, 4], [0, 64]], base=0, channel_multiplier=-1, allow_small_or_imprecise_dtypes=True), w=['self_'])
            P.I('dve', lambda e: e.tensor_scalar(out=SEL[:], in0=self_[:], scalar1=0.0, scalar2=None, op0=OP.is_equal), r=['self_'], w=['SEL'])

            WMASK = sb("WMASK", [128, 8 * 512], BF16)
            P.I('dve', lambda e: e.memset(WMASK[:], 0.0), w=['WMASK'])
            for a in range(-4, 4):
                for b in range(4):
                    dst = WMASK[:, (a + 4) * 512 + b * 128:(a + 4) * 512 + (b + 1) * 128]
                    if b == a:
                        P.I('dve', lambda e, dst=dst: e.tensor_copy(out=dst, in_=C['tri'][:]), r=['c_tri'], w=['WMASK'])
                    elif b == a + 4:
                        P.I('dve', lambda e, dst=dst: e.tensor_copy(out=dst, in_=C['atri'][:]), r=['c_atri'], w=['WMASK'])
                    elif a < b < a + 4:
                        P.I('dve', lambda e, dst=dst: e.memset(dst, 1.0), w=['WMASK'])
            EX = [sb(f"EX{i}", [128, 512], F32) for i in range(2)]
            EM = [sb(f"EM{i}", [128, 512], F32) for i in range(2)]
            RS = sb("RS", [128, 8], F32)
            PSP = sb("PSP", [128, 520], F32)
            P.I('dve', lambda e: e.memset(PSP[:], 0.0), w=['PSP'])
            PN2 = [[sb(f"PN{i}{h}", [128, 512], BF16) for h in range(2)] for i in range(2)]
            MB2 = [sb(f"MB{i}", [128, 128], BF16) for i in range(2)]
            PNT = [sb(f"PNT{i}", [128, 512], BF16) for i in range(2)]
            IMP = sb("IMP", [128, 128], F32); SC = sb("SC", [128, 128], F32); SC2 = sb("SC2", [128, 128], F32); M8 = sb("M8", [128, 16], F32)
            SELM = sb("SELM", [128, 128], F32); MB = sb("MB", [128, 128], BF16)
            MBT = [sb(f"MBT{i}", [128, 512], BF16) for i in range(2)]
            OCc = [[sb(f"OC{i}{h}", [64, 512], F32) for h in range(2)] for i in range(2)]
            PT = [sb(f"PT{i}", [128, 512], BF16) for i in range(3)]
            RR = sb("RR", [65, 512], F32); BCS = sb("BCS", [64, 512], F32); BGS = sb("BGS", [64, 512], F32)
            TY = sb("TY", [64, 512], F32); YN = [sb(f"YN{i}", [64, 512], F32) for i in range(2)]
            ptc = 0

            for qc in range(T // 512):
                par = qc % 2
                mbk = f"MBT{par}"
                def cmp_stage1(b):
                    i = 4 * qc + b
                    for hh in range(4):
                        Q = NQ if hh < 2 else NQo
                        qk_ = 'NQ' if hh < 2 else 'NQo'
                        po = 64 * (hh % 2)
                        ps, pkk = PS[hh % 2], pk[hh % 2]
                        _mm(P, ps[:, 0:512], Q[po:po + 64, i * 128:(i + 1) * 128], KCT[po:po + 64, :], True, True, [qk_, 'KCT'], [pkk])
                        ex, exk = EX[hh % 2], f"EX{hh % 2}"
                        em, emk = EM[hh % 2], f"EM{hh % 2}"
                        P.I('act', lambda e: e.activation(out=ex[:], in_=ps[:, 0:512], func=AF.Exp, scale=SCALE), r=[pkk], w=[exk])
                        P.I('dve', lambda e: e.scalar_tensor_tensor(out=em[:], in0=REL[:], scalar=float(128 * i), in1=ex[:], op0=OP.is_le, op1=OP.mult),
                            r=['REL', exk], w=[emk])
                        P.I('dve', lambda e: e.reduce_sum(out=RS[:, hh:hh + 1], in_=em[:], axis=AX.X), r=[emk], w=['RS'])
                        P.I('dve', lambda e: e.tensor_scalar(out=RS[:, hh:hh + 1], in0=RS[:, hh:hh + 1], scalar1=1e-30, scalar2=None, op0=OP.max), r=['RS'], w=['RS'])
                        P.I('dve', lambda e: e.reciprocal(out=RS[:, hh:hh + 1], in_=RS[:, hh:hh + 1]), r=['RS'], w=['RS'])
                        if hh == 0:
                            P.I('dve', lambda e: e.tensor_scalar(out=PSP[:, 1:513], in0=em[:], scalar1=RS[:, hh:hh + 1], scalar2=None, op0=OP.mult), r=[emk, 'RS'], w=['PSP'])
                        else:
                            P.I('dve', lambda e: e.scalar_tensor_tensor(out=PSP[:, 1:513], in0=em[:], scalar=RS[:, hh:hh + 1], in1=PSP[:, 1:513], op0=OP.mult, op1=OP.add),
                                r=[emk, 'RS', 'PSP'], w=['PSP'])
                        if hh < 2:
                            P.I('dve', lambda e: e.tensor_tensor(out=RS[:, 4 + hh:5 + hh], in0=RS[:, hh:hh + 1], in1=GC[:, i, hh:hh + 1], op=OP.mult), r=['RS', 'GC'], w=['RS'])
                            pn, pnk = PN2[b % 2][hh], f"PN{b % 2}{hh}"
                            P.I('pool', lambda e: e.tensor_scalar(out=pn[:], in0=em[:], scalar1=RS[:, 4 + hh:5 + hh], scalar2=None, op0=OP.mult), r=[emk, 'RS'], w=[pnk])
                    mb, mbk_ = MB2[b % 2], f"MB{b % 2}"
                    P.I('dve', lambda e: e.tensor_reduce(out=IMP[:], in_=PSP[:, 0:512].rearrange("p (s m) -> p s m", m=4), axis=AX.X, op=OP.add), r=['PSP'], w=['IMP'])
                    P.I('dve', lambda e: e.tensor_tensor(out=IMP[:], in0=IMP[:], in1=PSP[:, 4:516:4], op=OP.add), r=['PSP', 'IMP'], w=['IMP'])
                    x0 = 126 - 2 * i
                    P.I('dve', lambda e: e.tensor_tensor(out=SC[:], in0=IMP[:], in1=KEEP[:, x0:x0 + 128], op=OP.mult), r=['IMP', 'KEEP'], w=['SC'])
                    P.I('dve', lambda e: e.tensor_tensor(out=SC[:], in0=SC[:], in1=ADD[:, x0:x0 + 128], op=OP.add), r=['SC', 'ADD'], w=['SC'])
                    P.I('dve', lambda e: e.memset(SC[:, 0:1], 1.0e6), r=['SC'], w=['SC'])
                    P.I('dve', lambda e: e.max(out=M8[:, 0:8], in_=SC[:]), r=['SC'], w=['M8'])
                    P.I('dve', lambda e: e.match_replace(out=SC2[:], in_to_replace=M8[:, 0:8], in_values=SC[:], imm_value=-2.0), r=['SC', 'M8'], w=['SC2'])
                    P.I('dve', lambda e: e.max(out=M8[:, 8:16], in_=SC2[:]), r=['SC2'], w=['M8'])
                    P.I('dve', lambda e: e.scalar_tensor_tensor(out=SELM[:], in0=SC[:], scalar=M8[:, 15:16], in1=NF[:, x0:x0 + 128], op0=OP.is_ge, op1=OP.mult), r=['SC', 'M8', 'NF'], w=['SELM'])
                    P.I('dve', lambda e: e.tensor_scalar(out=mb[:], in0=SELM[:], scalar1=-1.0, scalar2=30000.0, op0=OP.add, op1=OP.mult), r=['SELM'], w=[mbk_])

                def cmp_stage2(b):
                    for hh in range(2):
                        pn, pnk = PN2[b % 2][hh], f"PN{b % 2}{hh}"
                        for ct in range(4):
                            P.I('pe', lambda e, ct=ct: e.transpose(out=PSB[:, hh * 512 + ct * 128: hh * 512 + (ct + 1) * 128], in_=pn[:, ct * 128:(ct + 1) * 128], identity=C['ident'][:]),
                                r=[pnk, 'c_ident'], w=[f'psb{hh}'])
                        pnt, pntk = PNT[hh], f"PNT{hh}"
                        P.I('act', lambda e: e.copy(out=pnt[:], in_=PSB[:, hh * 512:(hh + 1) * 512]), r=[f'psb{hh}'], w=[pntk])
                        po_, pok = PS[2 + hh], pk[2 + hh]
                        for ct in range(4):
                            _mm(P, po_[0:64, b * 128:(b + 1) * 128], VC[:, ct, :], pnt[:, ct * 128:(ct + 1) * 128], ct == 0, ct == 3, ['VC', pntk], [pok])
                    mb, mbk_ = MB2[b % 2], f"MB{b % 2}"
                    P.I('pe', lambda e: e.transpose(out=PSB[:, 0:128], in_=mb[:], identity=C['ident'][:]), r=[mbk_, 'c_ident'], w=['psb0'])
                    P.I('act', lambda e: e.copy(out=MBT[par][:, b * 128:(b + 1) * 128], in_=PSB[:, 0:128]), r=['psb0'], w=[mbk])

                for b in range(4):
                    cmp_stage1(b)
                    if b >= 1:
                        cmp_stage2(b - 1)
                cmp_stage2(3)
                for h in range(2):
                    P.I('act', lambda e, h=h, par=par: e.copy(out=OCc[par][h][:], in_=PS[2 + h][0:64, 0:512]), r=[pk[2 + h]], w=[f'OC{par}{h}'])

                for h in range(2):
                    po = 64 * h
                    pso, psok = PS[4], pk[4]
                    psw, pswk = PS[5], pk[5]
                    nj = 4 * qc + 4
                    jfirst = max(4 * qc - 4, 0)
                    steps = [('s', j) for j in range(nj)] + [('w', 4 * qc + a) for a in range(-4, 4) if 4 * qc + a >= 0]

                    def qk(si):
                        kind, j = steps[si]
                        pst, pstk = PS[si % 2], pk[si % 2]
                        a = j - 4 * qc
                        if kind == 's':
                            c0 = 128 * max(a, 0)
                            _mm(P, pst[:, c0:512], KS[po:po + 64, j * 128:(j + 1) * 128], NQ[po:po + 64, qc * 512 + c0:(qc + 1) * 512], True, False, ['KS', 'NQ'], [pstk])
                            _mm(P, pst[:, c0:512], EJ[:, j * 128:(j + 1) * 128], MBT[par][:, c0:512], False, True, ['EJ', mbk], [pstk])
                        else:
                            _mm(P, pst[:, 0:512], KW[po:po + 64, j * 128:(j + 1) * 128], NQ[po:po + 64, qc * 512:(qc + 1) * 512], True, True, ['KW', 'NQ'], [pstk])

                    def rest(si):
                        nonlocal ptc
                        kind, j = steps[si]
                        pst, pstk = PS[si % 2], pk[si % 2]
                        a = j - 4 * qc
                        pt, ptk = PT[ptc % 3], f"PT{ptc % 3}"
                        ptc += 1
                        if kind == 's':
                            c0 = 128 * max(a, 0)
                            P.I('act', lambda e: e.activation(out=pt[:, c0:512], in_=pst[:, c0:512], func=AF.Exp, scale=SCALE), r=[pstk], w=[ptk])
                            if a >= 0:
                                P.I('dve', lambda e: e.tensor_tensor(out=pt[:, c0:c0 + 128], in0=pt[:, c0:c0 + 128], in1=C['tri'][:], op=OP.mult), r=[ptk, 'c_tri'], w=[ptk])
                            _mm(P, pso[0:65, c0:512], VS[:, j, :], pt[:, c0:512], j == 0, j == nj - 1, ['VS', ptk], [psok])
                        else:
                            P.I('act', lambda e: e.activation(out=pt[:, 0:512], in_=pst[:, 0:512], func=AF.Exp, scale=SCALE), r=[pstk], w=[ptk])
                            P.I('dve', lambda e: e.tensor_tensor(out=pt[:, 0:512], in0=pt[:, 0:512], in1=WMASK[:, (a + 4) * 512:(a + 5) * 512], op=OP.mult), r=[ptk, 'WMASK'], w=[ptk])
                            _mm(P, psw[0:65, 0:512], VW[:, j, :], pt[:, 0:512], j == jfirst, a == 3, ['VW', ptk], [pswk])

                    qk(0)
                    for si in range(len(steps)):
                        if si + 1 < len(steps):
                            qk(si + 1)
                        rest(si)
                    Y = YN[h]; yk = f"YN{h}"
                    for br, (pacc, pacck) in enumerate(((pso, psok), (psw, pswk))):
                        P.I('dve', lambda e, pacc=pacc: e.reciprocal(out=RR[64:65, :], in_=pacc[64:65, 0:512]), r=[pacck], w=['RR'])
                        _mm(P, PS[6][0:64, 0:512], C['onesf'][64:65, 0:64], RR[64:65, :], True, True, ['c_onesf', 'RR'], [pk[6]])
                        P.I('act', lambda e: e.copy(out=BCS[:], in_=PS[6][0:64, 0:512]), r=[pk[6]], w=['BCS'])
                        gi = 2 * h + br
                        _mm(P, PS[6][0:64, 0:512], SEL[0:4, gi * 64:(gi + 1) * 64], G4[0:4, qc * 512:(qc + 1) * 512], True, True, ['SEL', 'G4'], [pk[6]])
                        P.I('act', lambda e: e.copy(out=BGS[:], in_=PS[6][0:64, 0:512]), r=[pk[6]], w=['BGS'])
                        P.I('dve', lambda e, pacc=pacc: e.tensor_tensor(out=TY[:], in0=pacc[0:64, 0:512], in1=BCS[:], op=OP.mult), r=[pacck, 'BCS'], w=['TY'])
                        P.I('dve', lambda e: e.tensor_tensor(out=TY[:], in0=TY[:], in1=BGS[:], op=OP.mult), r=['TY', 'BGS'], w=['TY'])
                        src = OCc[par][h] if br == 0 else Y
                        srck = f'OC{par}{h}' if br == 0 else yk
                        P.I('dve', lambda e, src=src, Y=Y: e.tensor_tensor(out=Y[:], in0=TY[:], in1=src[:], op=OP.add), r=['TY', srck], w=[yk])
                    P.D('sp', lambda e, Y=Y, h=h, qc=qc: e.dma_start(out=yT[128 + 64 * h:192 + 64 * h, qc * 512:(qc + 1) * 512], in_=Y[:]), r=[yk])
            P.barrier()

        with ExitStack() as e3:
            sb = lambda n, s, d: e3.enter_context(nc.sbuf_tensor(n, s, d))
            bf3 = sb("bf3", [128, 2], F32); bt3 = sb("bt3", [128, 130], F32)
            P.D('sp', lambda e: e.dma_start(out=bf3[:], in_=bfm3), w=['bf3'])
            P.D('sp', lambda e: e.dma_start(out=bt3[:], in_=btm3), w=['bt3'])
            FQ = sb("FQ", [128, T], BF16); FK = sb("FK", [128, T], BF16)
            FV = sb("FV", [128, NT, 2, 65], BF16); LF = sb("LF", [128, NT, 2], F32)
            P.I('pool', lambda e: e.memset(FV[:, :, :, 64:65], 1.0), w=['FV'])

            def ev3(dst, key, col):
                def f(tc, ps, pkk):
                    P.I('act', lambda e: e.activation(out=dst[:, tc * 512:(tc + 1) * 512], in_=ps[:, 0:512], func=AF.Identity, bias=bf3[:, col:col + 1], scale=1.0), r=[pkk, 'bf3'], w=[key])
                return f

            def tm3(ti, ps, pkk):
                P.I('dve', lambda e: e.tensor_tensor(out=FV[:, ti, :, 0:64], in0=ps[:, 0:128].rearrange("p (h d) -> p h d", d=64), in1=bt3[:, 0:128].rearrange("p (h d) -> p h d", d=64), op=OP.add),
                    r=[pkk, 'bt3'], w=['FV'])
                P.I('dve', lambda e: e.tensor_tensor(out=LF[:, ti, :], in0=ps[:, 128:130], in1=bt3[:, 128:130], op=OP.add), r=[pkk, 'bt3'], w=['LF'])

            _inproj(P, nc, e3, xT, wfm3, bf3, [(0, 128, ev3(FQ, 'FQ', 0)), (128, 128, ev3(FK, 'FK', 1))],
                    wtm3, 130, tm3, [(PS[0], pk[0]), (PS[1], pk[1]), (PS[2], pk[2]), (PS[3], pk[3])], "p3")
            LFv = LF[:].rearrange("p j h -> p (j h)")
            P.I('act', lambda e: e.activation(out=LFv, in_=LFv, func=AF.Exp, scale=-1.0), r=['LF'], w=['LF'])
            P.I('dve', lambda e: e.tensor_scalar(out=LFv, in0=LFv, scalar1=1.0, scalar2=None, op0=OP.add), r=['LF'], w=['LF'])
            P.I('act', lambda e: e.activation(out=LFv, in_=LFv, func=AF.Ln), r=['LF'], w=['LF'])
            P.I('dve', lambda e: e.tensor_scalar(out=LFv, in0=LFv, scalar1=-1.0, scalar2=None, op0=OP.mult), r=['LF'], w=['LF'])
            _mm(P, PS[0][:, 0:128], C['trif'][:], LFv, True, True, ['c_trif', 'LF'], [pk[0]])
            _mm(P, PS[1][:, 0:128], C['onesf'][:], LFv, True, True, ['c_onesf', 'LF'], [pk[1]])
            TOT = sb("TOT", [128, NT, 2], F32); INCL = sb("INCL", [128, NT, 2], F32); CUM = sb("CUM", [128, NT, 2], F32); ONE64 = sb("ONE64", [128, NT], F32)
            P.I('dve', lambda e: e.memset(ONE64[:], 1.0), w=['ONE64'])
            P.I('act', lambda e: e.copy(out=TOT[:].rearrange("p j h -> p (j h)"), in_=PS[1][:, 0:128]), r=[pk[1]], w=['TOT'])
            for h in range(2):
                P.I('dve', lambda e, h=h: e.tensor_tensor_scan(out=INCL[:, :, h], data0=ONE64[:], data1=TOT[:, :, h], initial=0.0, op0=OP.mult, op1=OP.add), r=['TOT', 'ONE64'], w=['INCL'])
            P.I('dve', lambda e: e.tensor_tensor(out=CUM[:].rearrange("p j h -> p (j h)"), in0=PS[0][:, 0:128], in1=INCL[:].rearrange("p j h -> p (j h)"), op=OP.add), r=[pk[0], 'INCL'], w=['CUM'])
            P.I('dve', lambda e: e.tensor_tensor(out=CUM[:], in0=CUM[:], in1=TOT[:], op=OP.subtract), r=['CUM', 'TOT'], w=['CUM'])

            BI = [sb(f"BI{i}", [128, 4, NT], F32) for i in range(2)]
            PT = [sb(f"FPT{i}", [128, 512], BF16) for i in range(3)]
            RR = sb("FRR", [65, 512], F32); BCS = sb("FBCS", [64, 512], F32); YF = [sb(f"YF{i}", [64, 512], F32) for i in range(2)]
            ptc = 0
            it = 0
            for h in range(2):
                po = 64 * h
                for qc in range(T // 512):
                    bi, bik = BI[it % 2], f"BI{it % 2}"
                    pso, psok = PS[4 + it % 2], pk[4 + it % 2]
                    for b in range(4):
                        i = 4 * qc + b
                        P.I('dve', lambda e, bi=bi, b=b, i=i, h=h: e.tensor_scalar(out=bi[:, b, 0:i + 1], in0=CUM[:, 0:i + 1, h], scalar1=-1.0, scalar2=INCL[:, i, h:h + 1], op0=OP.mult, op1=OP.add),
                            r=['CUM', 'INCL'], w=[bik])
                    nj = 4 * qc + 4

                    def fqk(j):
                        c0 = 128 * max(j - 4 * qc, 0)
                        pst, pstk = PS[j % 4], pk[j % 4]
                        _mm(P, pst[:, c0:512], FK[po:po + 64, j * 128:(j + 1) * 128], FQ[po:po + 64, qc * 512 + c0:(qc + 1) * 512], True, True, ['FK', 'FQ'], [pstk])

                    def frest(j):
                        nonlocal ptc
                        a = j - 4 * qc
                        b0 = max(a, 0)
                        c0 = 128 * b0
                        pst, pstk = PS[j % 4], pk[j % 4]
                        pt, ptk = PT[ptc % 3], f"FPT{ptc % 3}"
                        ptc += 1
                        for b in range(b0, 4):
                            P.I('act', lambda e, b=b: e.activation(out=pt[:, b * 128:(b + 1) * 128], in_=pst[:, b * 128:(b + 1) * 128], func=AF.Exp, bias=bi[:, b, j:j + 1], scale=SCALE),
                                r=[pstk, bik], w=[f"{ptk}_{b}"])
                        if a >= 0:
                            P.I('dve', lambda e: e.tensor_tensor(out=pt[:, c0:c0 + 128], in0=pt[:, c0:c0 + 128], in1=C['tri'][:], op=OP.mult), r=[f"{ptk}_{b0}", 'c_tri'], w=[f"{ptk}_{b0}"])
                        _mm(P, pso[0:65, c0:512], FV[:, j, h, :], pt[:, c0:512], j == 0, j == nj - 1, ['FV'] + [f"{ptk}_{b}" for b in range(b0, 4)], [psok])

                    fqk(0)
                    for j in range(nj):
                        if j + 1 < nj:
                            fqk(j + 1)
                        frest(j)
                    P.I('dve', lambda e, pso=pso: e.reciprocal(out=RR[64:65, :], in_=pso[64:65, 0:512]), r=[psok], w=['FRR'])
                    _mm(P, PS[6][0:64, 0:512], C['onesf'][64:65, 0:64], RR[64:65, :], True, True, ['c_onesf', 'FRR'], [pk[6]])
                    P.I('act', lambda e: e.copy(out=BCS[:], in_=PS[6][0:64, 0:512]), r=[pk[6]], w=['FBCS'])
                    Y = YF[it % 2]; yk = f"YF{it % 2}"
                    P.I('dve', lambda e, pso=pso, Y=Y: e.tensor_tensor(out=Y[:], in0=pso[0:64, 0:512], in1=BCS[:], op=OP.mult), r=[psok, 'FBCS'], w=[yk])
                    P.D('sp', lambda e, Y=Y, h=h, qc=qc: e.dma_start(out=yT[256 + 64 * h:320 + 64 * h, qc * 512:(qc + 1) * 512], in_=Y[:]), r=[yk])
                    it += 1
        P.finish()
    return nc


def _prep_A(inp, l, b, j, x_b):
    g = j // 2
    own = [2 * j, 2 * j + 1]
    oth = [2 * j + 2, 2 * j + 3] if j % 2 == 0 else [2 * j - 2, 2 * j - 1]
    w_in = inp['w_in'][l]; b_in = inp['b_in'][l]
    OQ, OKV, OG, OFX, OF = 512, 1024, 1792, 1816, 3352
    r64 = np.arange(64)
    kvc = lambda n, kvi: OKV + ((n * 2 + kvi) * 2 + g) * 64 + r64
    fx = lambda q, h: OFX + (q * 8 + h) * 64 + r64
    fm1 = np.concatenate([128 * j + np.arange(128), kvc(0, 0), kvc(0, 1)])
    fm2 = np.concatenate([OQ + 64 * own[0] + r64, OQ + 64 * own[1] + r64, OQ + 64 * oth[0] + r64, OQ + 64 * oth[1] + r64,
                          kvc(1, 0), kvc(1, 0), kvc(2, 0), kvc(2, 0),
                          [OG + own[0] * 3 + 1, OG + own[0] * 3 + 2, OG + own[1] * 3 + 1, OG + own[1] * 3 + 2]]).astype(np.int64)
    tm2 = np.concatenate([kvc(1, 1), kvc(2, 1), [OG + own[0] * 3, OG + own[1] * 3]]).astype(np.int64)
    fm3 = np.concatenate([fx(0, own[0]), fx(0, own[1]), fx(1, own[0]), fx(1, own[1])])
    tm3 = np.concatenate([fx(2, own[0]), fx(2, own[1]), [OF + own[0], OF + own[1]]]).astype(np.int64)

    def fmb(cols, nch):
        bb = np.zeros((128, nch), np.float32)
        v = b_in[cols]
        for c in range(nch):
            seg = v[c * 128:(c + 1) * 128]
            bb[:len(seg), c] = seg
        return bb
    c32 = np.ascontiguousarray
    win = POOL_WINDOWS[j]
    cwv = np.zeros((128, 4), np.float32); cwv[:, j] = 1.0 / win
    fixv = np.tile((win / np.minimum(np.arange(16) + 1, win)).astype(np.float32)[None, :], (128, 1))
    cw1 = inp['cmp_w1'][l]
    cw1r = np.concatenate([cw1[kv].reshape(32, 64, 256).transpose(1, 0, 2).reshape(64, 32 * 256) for kv in range(2)], axis=0)
    posT = np.concatenate([inp['cmp_pos'][l][kv].T for kv in range(2)], axis=0)
    cb1 = inp['cmp_b1'][l].reshape(2, 2, 128).transpose(2, 0, 1).reshape(128, 4)
    w2 = inp['cmp_w2'][l]
    cw2k = np.concatenate([np.concatenate([w2[0][hf * 128:(hf + 1) * 128], w2[0][hf * 128:(hf + 1) * 128]], axis=1) for hf in range(2)], axis=1)
    cb2k = np.concatenate([inp['cmp_b2'][l][0], inp['cmp_b2'][l][0]])[:, None]
    cw2v = np.concatenate([w2[1][hf * 128:(hf + 1) * 128] for hf in range(2)], axis=1)
    cb2v = np.tile(inp['cmp_b2'][l][1][None, :], (128, 1))
    return {
        "xT": x_b,
        "wfm1": c32(w_in[:, fm1]), "bfm1": fmb(fm1, 2),
        "wfm2": c32(w_in[:, fm2]), "bfm2": fmb(fm2, 5),
        "wtm2": c32(w_in[:, tm2]), "btm2": c32(np.tile(b_in[tm2][None, :], (128, 1))),
        "wfm3": c32(w_in[:, fm3]), "bfm3": fmb(fm3, 2),
        "wtm3": c32(w_in[:, tm3]), "btm3": c32(np.tile(b_in[tm3][None, :], (128, 1))),
        "pw": c32(inp['pool_w'][l][j]), "pbs": c32(np.stack([inp['pool_b'][l][j], inp['pool_scale'][l][128 * j:128 * j + 128]], axis=1)),
        "cw": cwv, "fix": c32(fixv),
        "cw1": c32(cw1r), "posT": c32(posT), "cb1": c32(cb1), "cw2k": c32(cw2k), "cb2k": c32(cb2k.astype(np.float32)),
        "cw2v": c32(cw2v), "cb2v": c32(cb2v),
    }


_NC = {}


def run_A(inp, l, x):
    if 'A' not in _NC:
        _NC['A'] = build_A()
    xTs = [np.ascontiguousarray(x[b].T) for b in range(2)]
    maps = [_prep_A(inp, l, c // 4, c % 4, xTs[c // 4]) for c in range(8)]
    res = run_bass_kernel_spmd(_NC['A'], maps, core_ids=list(range(8)))
    ys = np.empty((2, T, 1536), np.float32)
    for c in range(8):
        b, j = c // 4, c % 4
        yt = res.results[c]["yT"]
        for n in range(3):
            ys[b, :, n * 512 + 128 * j: n * 512 + 128 * j + 128] = yt[n * 128:(n + 1) * 128, :].T
    return ys


NTB = 2048
NE = 32
CAP = 384
U32 = mybir.dt.uint32


def _layernorm(P, nc, R, rk, g_t, b_t, out, outk, ST, MV, tag):
    for c in range(2):
        P.I('dve', lambda e, c=c: e.bn_stats(out=ST[:, c, :], in_=R[:, c * 512:(c + 1) * 512]), r=[rk], w=['ST' + tag])
    P.I('dve', lambda e: e.bn_aggr(out=MV[:, 0:2], in_=ST[:].rearrange("p c s -> p (c s)")), r=['ST' + tag], w=['MV' + tag])
    P.I('dve', lambda e: e.tensor_scalar(out=MV[:, 2:3], in0=MV[:, 1:2], scalar1=LN_EPS, scalar2=None, op0=OP.add), r=['MV' + tag], w=['MV' + tag])
    P.I('act', lambda e: e.activation(out=MV[:, 2:3], in_=MV[:, 2:3], func=AF.Sqrt), r=['MV' + tag], w=['MV' + tag])
    P.I('dve', lambda e: e.reciprocal(out=MV[:, 2:3], in_=MV[:, 2:3]), r=['MV' + tag], w=['MV' + tag])
    P.I('dve', lambda e: e.tensor_scalar(out=out, in0=R[:], scalar1=MV[:, 0:1], scalar2=MV[:, 2:3], op0=OP.subtract, op1=OP.mult), r=[rk, 'MV' + tag], w=[outk])
    P.I('dve', lambda e: e.tensor_tensor(out=out, in0=out, in1=g_t[:], op=OP.mult), r=[outk, 'lng' + tag], w=[outk])
    P.I('dve', lambda e: e.tensor_tensor(out=out, in0=out, in1=b_t[:], op=OP.add), r=[outk, 'lnb' + tag], w=[outk])


def build_B():
    nc = bass.Bass("TRN2", target_bir_lowering=False)
    dt = lambda n, s, k="ExternalInput": nc.dram_tensor(n, s, F32, kind=k).ap()
    xT = dt("xT", [D, NTB]); xtok = dt("xtok", [NTB, D]); yT = dt("yT", [1536, NTB])
    wg_d = dt("wg", [D, 3072]); bg_d = dt("bg", [128, 24]); wup_d = dt("wup", [1536, D]); wo_d = dt("wo", [D, D])
    l1g_d = dt("l1g", [128, D]); l1b_d = dt("l1b", [128, D]); l2g_d = dt("l2g", [128, D]); l2b_d = dt("l2b", [128, D])
    rw_d = dt("rw", [D, NE]); rb_d = dt("rb", [128, NE])
    w1_d = dt("w1", [NE, D, 2048]); b1_d = dt("b1", [128, NE * 16]); w2_d = dt("w2", [NE, D, D]); b2_d = dt("b2", [NE, D])
    xo = dt("xo", [NTB, D], "ExternalOutput")
    x1s = dt("x1s", [NTB, D], "Internal")
    xg_d = nc.dram_tensor("xg", [NE * CAP, D], BF16, kind="Internal").ap()
    yg_d = dt("yg", [NE * CAP, D], "Internal")
    NTT = NTB // 128

    with ExitStack() as es:
        P = Prog(nc, es)
        sbg = lambda n, s, d: es.enter_context(nc.sbuf_tensor(n, s, d))
        PA = [es.enter_context(nc.psum_tensor(f"pa{i}", [128, 512], F32)) for i in range(4)]
        pak = [f"pa{i}" for i in range(4)]
        PH = es.enter_context(nc.psum_tensor("ph", [128, 1024], F32))
        PSB = es.enter_context(nc.psum_tensor("psb", [128, 1024], BF16))
        PR = es.enter_context(nc.psum_tensor("pr", [128, 512], F32))
        C = _consts(P, nc, sbg)
        identf = sbg("identf", [128, 128], F32)
        P.I('dve', lambda e: e.tensor_copy(out=identf[:], in_=C['ident'][:]), r=['c_ident'], w=['identf'])
        DSTI = sbg("DSTI", [128, NTT, 4], U32)
        GR = sbg("GR", [128, NTT, 4], F32)
        GT = sbg("GT", [128, NTT, NE], F32)
        ST = sbg("ST", [128, 2, 6], F32); MV = sbg("MV", [128, 4], F32)

        with ExitStack() as e1:
            sb = lambda n, s, d: e1.enter_context(nc.sbuf_tensor(n, s, d))
            WG = sb("WG", [128, 8, 3072], BF16); WU = sb("WU", [128, 12, D], BF16); WO = sb("WO", [128, 8, D], BF16)
            for k in range(8):
                P.D('pool', lambda e, k=k: e.dma_start(out=WG[:, k, :], in_=wg_d[k * 128:(k + 1) * 128, :]), w=['WG'])
                P.D('pool', lambda e, k=k: e.dma_start(out=WO[:, k, :], in_=wo_d[k * 128:(k + 1) * 128, :]), w=['WO'])
            for k in range(12):
                P.D('pool', lambda e, k=k: e.dma_start(out=WU[:, k, :], in_=wup_d[k * 128:(k + 1) * 128, :]), w=['WU'])
            bg = sb("bg_s", [128, 24], F32); l1g = sb("l1g_s", [128, D], F32); l1b = sb("l1b_s", [128, D], F32)
            RW = sb("RW", [128, 8, NE], F32); rb = sb("rb_s", [128, NE], F32)
            P.D('sp', lambda e: e.dma_start(out=bg[:], in_=bg_d), w=['bg'])
            P.D('sp', lambda e: e.dma_start(out=l1g[:], in_=l1g_d), w=['lng1'])
            P.D('sp', lambda e: e.dma_start(out=l1b[:], in_=l1b_d), w=['lnb1'])
            P.D('sp', lambda e: e.dma_start(out=RW[:], in_=rw_d.rearrange("(k p) n -> p k n", p=128)), w=['RW'])
            P.D('sp', lambda e: e.dma_start(out=rb[:], in_=rb_d), w=['rb'])
            XB = [sb(f"XB{i}", [128, 8, 512], BF16) for i in range(2)]
            YB = [sb(f"YB{i}", [128, 12, 512], BF16) for i in range(1)]
            GS = sb("GS", [128, 512], F32); TM = sb("TM", [128, 512], F32); MA = sb("MA", [128, 512], F32)
            MT = sb("MT", [128, 8, 512], BF16)
            XT_ = [sb(f"XTK{i}", [128, D], F32) for i in range(2)]
            RR_ = sb("RRb", [128, D], F32); X1 = sb("X1", [128, D], F32); X1B = sb("X1B", [128, D], BF16)
            X1T32 = sb("X1T32", [128, 8, 128], F32); LG = sb("LG", [128, NE], F32); M8 = sb("M8b", [128, 8], F32)
            EXg = sb("EXg", [128, NE], F32); MK = sb("MK", [128, NE], F32); SM = sb("SM", [128, 2], F32)
            TRIS = sb("TRIS", [128, 128], BF16); ONESB = sb("ONESB", [128, 128], BF16); RUNE = sb("RUNE", [128, NE], F32)
            P.I('dve', lambda e: e.tensor_scalar(out=TRIS[:], in0=C['trif'][:], scalar1=0.0, scalar2=None, op0=OP.add), r=['c_trif'], w=['TRIS'])
            P.I('dve', lambda e: e.tensor_tensor(out=TRIS[:], in0=TRIS[:], in1=C['ident'][:], op=OP.subtract), r=['TRIS', 'c_ident'], w=['TRIS'])
            P.I('dve', lambda e: e.memset(ONESB[:], 1.0), w=['ONESB'])
            P.I('pool', lambda e: e.iota(RUNE[:], pattern=[[CAP, NE]], base=0, channel_multiplier=0, allow_small_or_imprecise_dtypes=True), w=['RUNE'])
            MKB = sb("MKB", [128, NE], BF16); DESTF = sb("DESTF", [128, NE], F32); TMPR = sb("TMPR", [128, NE], F32); DSTF = sb("DSTF", [128, 4], F32)
            X1Bs = [X1B, sb("X1B2", [128, D], BF16)]
            ZR = sb("ZR", [128, D], BF16)
            P.I('dve', lambda e: e.memset(ZR[:], 0.0), w=['ZR'])
            for zb in range(NE * CAP // 128):
                P.D('sp', lambda e, zb=zb: e.dma_start(out=xg_d[zb * 128:(zb + 1) * 128, :], in_=ZR[:]), r=['ZR'], w=['xg'])
            xv = xT.rearrange("(k p) t -> p k t", p=128)
            yv = yT.rearrange("(k p) t -> p k t", p=128)
            pi = 0
            for tc in range(NTB // 512):
                X = XB[tc % 2]; Y = YB[0]; xk = f"XB{tc % 2}"; yk = "YB0"
                for k2 in range(2):
                    P.D('pool', lambda e, X=X, tc=tc, k2=k2: e.dma_start(out=X[:, 4 * k2:4 * k2 + 4, :], in_=xv[:, 4 * k2:4 * k2 + 4, tc * 512:(tc + 1) * 512]), w=[xk])
                for k3 in range(3):
                    P.D('pool', lambda e, Y=Y, tc=tc, k3=k3: e.dma_start(out=Y[:, 4 * k3:4 * k3 + 4, :], in_=yv[:, 4 * k3:4 * k3 + 4, tc * 512:(tc + 1) * 512]), w=[yk])
                for dc in range(8):
                    for n in range(3):
                        pg, pgk = PA[pi % 4], pak[pi % 4]; pi += 1
                        pu, puk = PA[pi % 4], pak[pi % 4]; pi += 1
                        col = n * 1024 + dc * 128
                        for k in range(8):
                            _mm(P, pg[:, 0:512], WG[:, k, col:col + 128], X[:, k, :], k == 0, k == 7, ['WG', xk], [pgk])
                        for k in range(4):
                            _mm(P, pu[:, 0:512], WU[:, n * 4 + k, dc * 128:(dc + 1) * 128], Y[:, n * 4 + k, :], k == 0, k == 3, ['WU', yk], [puk])
                        bc_ = n * 8 + dc
                        P.I('act', lambda e, pg=pg, bc_=bc_: e.activation(out=GS[:], in_=pg[:, 0:512], func=AF.Sigmoid, bias=bg[:, bc_:bc_ + 1], scale=1.0), r=[pgk, 'bg'], w=['GS'])
                        if n == 0:
                            P.I('dve', lambda e, pu=pu: e.tensor_tensor(out=MA[:], in0=pu[:, 0:512], in1=GS[:], op=OP.mult), r=[puk, 'GS'], w=['MA'])
                        else:
                            P.I('dve', lambda e, pu=pu: e.tensor_tensor(out=TM[:], in0=pu[:, 0:512], in1=GS[:], op=OP.mult), r=[puk, 'GS'], w=['TM'])
                            if n == 1:
                                P.I('dve', lambda e: e.tensor_tensor(out=MA[:], in0=MA[:], in1=TM[:], op=OP.add), r=['MA', 'TM'], w=['MA'])
                            else:
                                P.I('dve', lambda e, dc=dc: e.tensor_tensor(out=MT[:, dc, :], in0=MA[:], in1=TM[:], op=OP.add), r=['MA', 'TM'], w=['MT'])
                for tt in range(4):
                    ti = tc * 4 + tt
                    xt = XT_[ti % 2]; xtk = f"XTK{ti % 2}"
                    P.D('sp', lambda e, xt=xt, ti=ti: e.dma_start(out=xt[:], in_=xtok[ti * 128:(ti + 1) * 128, :]), w=[xtk])
                    for hf in range(2):
                        for k in range(8):
                            _mm(P, PH[:, hf * 512:(hf + 1) * 512], MT[:, k, tt * 128:(tt + 1) * 128], WO[:, k, hf * 512:(hf + 1) * 512], k == 0, k == 7, ['MT', 'WO'], ['ph'])
                    P.I('dve', lambda e, xt=xt: e.scalar_tensor_tensor(out=RR_[:], in0=xt[:], scalar=ALPHA, in1=PH[:, :], op0=OP.mult, op1=OP.add), r=[xtk, 'ph'], w=['RRb'])
                    _layernorm(P, nc, RR_, 'RRb', l1g, l1b, X1[:], 'X1', ST, MV, '1')
                    P.D('sp', lambda e, ti=ti: e.dma_start(out=x1s[ti * 128:(ti + 1) * 128, :], in_=X1[:]), r=['X1'], w=['x1s'])
                    x1b = X1Bs[ti % 2]; x1bk = f"X1B{ti % 2}"
                    P.I('act', lambda e, x1b=x1b: e.copy(out=x1b[:], in_=X1[:]), r=['X1'], w=[x1bk])
                    for k in range(8):
                        P.I('pe', lambda e, k=k: e.transpose(out=PH[:, k * 128:(k + 1) * 128], in_=X1[:, k * 128:(k + 1) * 128], identity=identf[:]), r=['X1', 'identf'], w=['ph'])
                    P.I('act', lambda e: e.copy(out=X1T32[:].rearrange("p k t -> p (k t)"), in_=PH[:, :]), r=['ph'], w=['X1T32'])
                    for k in range(8):
                        _mm(P, PR[:, 0:NE], X1T32[:, k, :], RW[:, k, :], k == 0, k == 7, ['X1T32', 'RW'], ['pr'])
                    P.I('dve', lambda e: e.tensor_tensor(out=LG[:], in0=PR[:, 0:NE], in1=rb[:], op=OP.add), r=['pr', 'rb'], w=['LG'])
                    P.I('dve', lambda e: e.max(out=M8[:], in_=LG[:]), r=['LG'], w=['M8b'])
                    P.I('dve', lambda e: e.tensor_scalar(out=SM[:, 0:1], in0=M8[:, 0:1], scalar1=-1.0, scalar2=None, op0=OP.mult), r=['M8b'], w=['SM'])
                    P.I('act', lambda e: e.activation(out=EXg[:], in_=LG[:], func=AF.Exp, bias=SM[:, 0:1], scale=1.0), r=['LG', 'SM'], w=['EXg'])
                    P.I('dve', lambda e: e.scalar_tensor_tensor(out=MK[:], in0=LG[:], scalar=M8[:, 3:4], in1=EXg[:], op0=OP.is_ge, op1=OP.mult), r=['LG', 'M8b', 'EXg'], w=['MK'])
                    P.I('dve', lambda e: e.reduce_sum(out=SM[:, 1:2], in_=MK[:], axis=AX.X), r=['MK'], w=['SM'])
                    P.I('dve', lambda e: e.reciprocal(out=SM[:, 1:2], in_=SM[:, 1:2]), r=['SM'], w=['SM'])
                    P.I('dve', lambda e, ti=ti: e.tensor_scalar(out=GT[:, ti, :], in0=MK[:], scalar1=SM[:, 1:2], scalar2=None, op0=OP.mult), r=['MK', 'SM'], w=['GT'])
                    P.I('dve', lambda e: e.tensor_scalar(out=MKB[:], in0=LG[:], scalar1=M8[:, 3:4], scalar2=None, op0=OP.is_ge), r=['LG', 'M8b'], w=['MKB'])
                    _mm(P, PR[:, 32:64], TRIS[:], MKB[:], True, True, ['TRIS', 'MKB'], ['pr'])
                    _mm(P, PR[:, 64:96], ONESB[:], MKB[:], True, True, ['ONESB', 'MKB'], ['pr'])
                    P.I('dve', lambda e: e.tensor_tensor(out=DESTF[:], in0=PR[:, 32:64], in1=RUNE[:], op=OP.add), r=['pr', 'RUNE'], w=['DESTF'])
                    P.I('dve', lambda e: e.tensor_tensor(out=RUNE[:], in0=PR[:, 64:96], in1=RUNE[:], op=OP.add), r=['pr', 'RUNE'], w=['RUNE'])
                    for r_ in range(4):
                        P.I('dve', lambda e, r_=r_: e.scalar_tensor_tensor(out=TMPR[:], in0=LG[:], scalar=M8[:, r_:r_ + 1], in1=DESTF[:], op0=OP.is_equal, op1=OP.mult), r=['LG', 'M8b', 'DESTF'], w=['TMPR'])
                        P.I('dve', lambda e, r_=r_: e.reduce_sum(out=DSTF[:, r_:r_ + 1], in_=TMPR[:], axis=AX.X), r=['TMPR'], w=['DSTF'])
                        P.I('dve', lambda e, r_=r_, ti=ti: e.scalar_tensor_tensor(out=TMPR[:], in0=LG[:], scalar=M8[:, r_:r_ + 1], in1=GT[:, ti, :], op0=OP.is_equal, op1=OP.mult), r=['LG', 'M8b', 'GT'], w=['TMPR'])
                        P.I('dve', lambda e, r_=r_, ti=ti: e.reduce_sum(out=GR[:, ti, r_:r_ + 1], in_=TMPR[:], axis=AX.X), r=['TMPR'], w=['GR'])
                    P.I('dve', lambda e, ti=ti: e.tensor_copy(out=DSTI[:, ti, :], in_=DSTF[:]), r=['DSTF'], w=['DSTI'])
                    for r_ in range(4):
                        P.D('pool', lambda e, r_=r_, ti=ti, x1b=x1b: e.indirect_dma_start(out=xg_d, out_offset=bass.IndirectOffsetOnAxis(ap=DSTI[:, ti, r_:r_ + 1], axis=0), in_=x1b[:], in_offset=None),
                            r=[x1bk, 'DSTI'], w=['xg'])
            P.barrier()

        NCT = CAP // 128
        with ExitStack() as e2:
            sb = lambda n, s, d: e2.enter_context(nc.sbuf_tensor(n, s, d))
            W1 = [sb(f"W1_{i}", [128, 8, 2048], BF16) for i in range(2)]
            W2 = [sb(f"W2_{i}", [128, 8, D], BF16) for i in range(1)]
            B1 = sb("B1", [128, NE * 16], F32)
            P.D('sp', lambda e: e.dma_start(out=B1[:], in_=b1_d), w=['B1'])
            ACC = sb("ACC", [128, NTT, D], F32)
            B2 = sb("B2", [NE, D], F32); GTT = sb("GTT", [NE, 128], F32)
            P.D('sp', lambda e: e.dma_start(out=B2[:], in_=b2_d), w=['B2'])
            XG = [sb(f"XG{i}", [128, NCT, D], BF16) for i in range(2)]
            XGT = [sb(f"XGT{i}", [128, 8, CAP], BF16) for i in range(2)]
            AT = sb("AT", [128, 8, CAP], BF16)
            GEN = [sb(f"GEN{i}", [128, D], F32) for i in range(3)]
            YRS = [sb(f"YRS{i}", [128, D], F32) for i in range(2)]
            GG = GEN[0][:, 0:CAP]; SG = GEN[0][:, 512:512 + CAP]; UU = GEN[1][:, 0:CAP]
            YO = [GEN[1], GEN[2]]

            def load_w(ex):
                s_ = ex % 2
                P.D('pool', lambda e: e.dma_start(out=W1[s_][:], in_=w1_d[ex].rearrange("(k p) n -> p k n", p=128)), w=[f'W1_{s_}'])

            def load_w2(ex):
                P.D('pool', lambda e: e.dma_start(out=W2[0][:], in_=w2_d[ex].rearrange("(k p) n -> p k n", p=128)), w=['W2_0'])

            def load_x(ex):
                s_ = ex % 2
                P.D('sp', lambda e: e.dma_start(out=XG[s_][:], in_=xg_d[ex * CAP:(ex + 1) * CAP, :].rearrange("(t p) d -> p t d", p=128)), r=['xg'], w=[f'XG{s_}'])

            load_w(0)
            load_w2(0)
            load_x(0)
            for ti in range(NTT):
                P.D('sp', lambda e, ti=ti: e.dma_start(out=ACC[:, ti, :], in_=x1s[ti * 128:(ti + 1) * 128, :]), w=[f'ACC{ti}'])
                P.I('act', lambda e, ti=ti: e.activation(out=ACC[:, ti, :], in_=ACC[:, ti, :], func=AF.Copy, scale=ALPHA), r=[f'ACC{ti}'], w=[f'ACC{ti}'])
                P.I('pe', lambda e, ti=ti: e.transpose(out=PR[0:NE, 128:256], in_=GT[:, ti, :], identity=identf[:]), r=['GT', 'identf'], w=['pr'])
                P.I('act', lambda e: e.copy(out=GTT[:], in_=PR[0:NE, 128:256]), r=['pr'], w=['GTT'])
                for hf in range(2):
                    _mm(P, PH[:, hf * 512:(hf + 1) * 512], GTT[:], B2[:, hf * 512:(hf + 1) * 512], True, True, ['GTT', 'B2'], ['ph'])
                P.I('dve', lambda e, ti=ti: e.tensor_tensor(out=ACC[:, ti, :], in0=ACC[:, ti, :], in1=PH[:, :], op=OP.add), r=[f'ACC{ti}', 'ph'], w=[f'ACC{ti}'])
            pi = 0
            yoc = 0
            for ex in range(NE):
                if ex + 1 < NE:
                    load_w(ex + 1)
                    load_x(ex + 1)
                s_ = ex % 2
                w1k, w2k, xgk, xgtk = f'W1_{s_}', 'W2_0', f'XG{s_}', f'XGT{s_}'
                for tt in range(NCT):
                    for k in range(8):
                        P.I('pe', lambda e, k=k, tt=tt: e.transpose(out=PSB[:, k * 128:(k + 1) * 128], in_=XG[s_][:, tt, k * 128:(k + 1) * 128], identity=C['ident'][:]), r=[xgk, 'c_ident'], w=['psb'])
                    P.I('act', lambda e, tt=tt: e.copy(out=XGT[s_][:, :, tt * 128:(tt + 1) * 128], in_=PSB[:, :].rearrange("p (k t) -> p k t", t=128)), r=['psb'], w=[xgtk])
                for f in range(8):
                    pg, pgk = PA[pi % 4], pak[pi % 4]; pi += 1
                    pu, puk = PA[pi % 4], pak[pi % 4]; pi += 1
                    for k in range(8):
                        _mm(P, pg[:, 0:CAP], W1[s_][:, k, f * 128:(f + 1) * 128], XGT[s_][:, k, :], k == 0, k == 7, [w1k, xgtk], [pgk])
                    for k in range(8):
                        _mm(P, pu[:, 0:CAP], W1[s_][:, k, 1024 + f * 128:1024 + (f + 1) * 128], XGT[s_][:, k, :], k == 0, k == 7, [w1k, xgtk], [puk])
                    cg = ex * 16 + f; cu = ex * 16 + 8 + f
                    P.I('dve', lambda e, pg=pg, cg=cg: e.tensor_scalar(out=GG, in0=pg[:, 0:CAP], scalar1=B1[:, cg:cg + 1], scalar2=7.0, op0=OP.add, op1=OP.min), r=[pgk, 'B1'], w=['GEN0'])
                    P.I('act', lambda e: e.activation(out=SG, in_=GG, func=AF.Sigmoid, scale=1.702), r=['GEN0'], w=['GEN0'])
                    P.I('dve', lambda e, pu=pu, cu=cu: e.tensor_scalar(out=UU, in0=pu[:, 0:CAP], scalar1=B1[:, cu:cu + 1], scalar2=7.0, op0=OP.add, op1=OP.min), r=[puk, 'B1'], w=['GEN1'])
                    P.I('pool', lambda e: e.tensor_scalar(out=UU, in0=UU, scalar1=-7.0, scalar2=1.0, op0=OP.max, op1=OP.add), r=['GEN1'], w=['GEN1'])
                    P.I('pool', lambda e: e.tensor_tensor(out=GG, in0=GG, in1=UU, op=OP.mult), r=['GEN0', 'GEN1'], w=['GEN0'])
                    P.I('dve', lambda e, f=f: e.tensor_tensor(out=AT[:, f, :], in0=GG, in1=SG, op=OP.mult), r=['GEN0'], w=['AT'])
                for tt in range(NCT):
                    for hf in range(2):
                        for f in range(8):
                            _mm(P, PH[:, hf * 512:(hf + 1) * 512], AT[:, f, tt * 128:(tt + 1) * 128], W2[0][:, f, hf * 512:(hf + 1) * 512], f == 0, f == 7, ['AT', w2k], ['ph'])
                    yo = GEN[2]; yok = 'GEN2'
                    P.I('act', lambda e, yo=yo: e.copy(out=yo[:], in_=PH[:, :]), r=['ph'], w=[yok])
                    row = ex * CAP + tt * 128
                    P.D('sp', lambda e, yo=yo, row=row: e.dma_start(out=yg_d[row:row + 128, :], in_=yo[:]), r=[yok], w=['yg'])
                if ex + 1 < NE:
                    load_w2(ex + 1)
            P.barrier()
            l2g = GEN[0]; l2b = GEN[1]
            P.D('sp', lambda e: e.dma_start(out=l2g[:], in_=l2g_d), w=['lng2'])
            P.D('sp', lambda e: e.dma_start(out=l2b[:], in_=l2b_d), w=['lnb2'])
            gi = 0
            for ti in range(NTT):
                for r_ in range(4):
                    yr = YRS[gi % 2]; yrk = f"YRS{gi % 2}"; gi += 1
                    P.D('pool', lambda e, yr=yr, ti=ti, r_=r_: e.indirect_dma_start(out=yr[:], out_offset=None, in_=yg_d, in_offset=bass.IndirectOffsetOnAxis(ap=DSTI[:, ti, r_:r_ + 1], axis=0)),
                        r=['yg', 'DSTI'], w=[yrk])
                    P.I('dve', lambda e, yr=yr, ti=ti, r_=r_: e.scalar_tensor_tensor(out=ACC[:, ti, :], in0=yr[:], scalar=GR[:, ti, r_:r_ + 1], in1=ACC[:, ti, :], op0=OP.mult, op1=OP.add),
                        r=[yrk, 'GR', f'ACC{ti}'], w=[f'ACC{ti}'])
                o = GEN[2]; ok = "GEN2"
                _layernorm(P, nc, ACC[:, ti, :], f'ACC{ti}', l2g, l2b, o[:], ok, ST, MV, '2')
                P.D('sp', lambda e, o=o, ti=ti: e.dma_start(out=xo[ti * 128:(ti + 1) * 128, :], in_=o[:]), r=[ok])
        P.finish()
    return nc


def _prep_B(inp, l, b, r, x, ys):
    tok = slice(NTB * r, NTB * (r + 1))
    c32 = lambda a: np.ascontiguousarray(a, dtype=np.float32)
    w_in = inp['w_in'][l]; b_in = inp['b_in'][l]
    bc = lambda v: c32(np.tile(v[None, :], (128, 1)))
    return {
        "xT": c32(x[b, tok].T), "xtok": c32(x[b, tok]), "yT": c32(ys[b, tok].T),
        "wg": c32(w_in[:, 3360:6432]), "bg": c32(b_in[3360:6432].reshape(24, 128).T),
        "wup": c32(inp['w_up'][l].reshape(1536, D)), "wo": c32(inp['w_o'][l]),
        "l1g": bc(inp['ln1_g'][l]), "l1b": bc(inp['ln1_b'][l]), "l2g": bc(inp['ln2_g'][l]), "l2b": bc(inp['ln2_b'][l]),
        "rw": c32(inp['router_w'][l]), "rb": bc(inp['router_b'][l]),
        "w1": inp['moe_w1'][l], "b1": c32(inp['moe_b1'][l].reshape(NE * 16, 128).T), "w2": inp['moe_w2'][l], "b2": c32(inp['moe_b2'][l]),
    }


def run_B(inp, l, x, ys):
    if 'B' not in _NC:
        _NC['B'] = build_B()
    maps = [_prep_B(inp, l, c // 4, c % 4, x, ys) for c in range(8)]
    res = run_bass_kernel_spmd(_NC['B'], maps, core_ids=list(range(8)))
    out = np.empty((2, T, D), np.float32)
    for c in range(8):
        out[c // 4, NTB * (c % 4):NTB * (c % 4 + 1)] = res.results[c]["xo"]
    return out


def kernel(**inputs):
    inp = {k: np.asarray(v) for k, v in inputs.items()}
    x = np.ascontiguousarray(inp['x'], dtype=np.float32)
    for l in range(2):
        ys = run_A(inp, l, x)
        x = run_B(inp, l, x, ys)
    return x
```

```python
import numpy as np
from contextlib import ExitStack
import concourse.bass as bass
import concourse.mybir as mybir
from concourse.bass_utils import run_bass_kernel_spmd

F32 = mybir.dt.float32
BF16 = mybir.dt.bfloat16
AF = mybir.ActivationFunctionType
OP = mybir.AluOpType
AX = mybir.AxisListType

T = 8192
D = 1024
NT = T // 128
SCALE = 0.125
ALPHA = 4.0 ** 0.25
POOL_WINDOWS = (2, 4, 8, 16)
LN_EPS = 1e-5


class Prog:
    NDMA = 12

    def __init__(self, nc, es):
        self.nc = nc
        self.eng = {'pe': nc.tensor, 'act': nc.scalar, 'dve': nc.vector, 'pool': nc.gpsimd, 'sp': nc.sync}
        self.sem = {n: es.enter_context(nc.semaphore("s_" + n)) for n in self.eng}
        self.cnt = {n: 0 for n in self.eng}
        self.dsem = [es.enter_context(nc.semaphore(f"d{i}")) for i in range(self.NDMA)]
        self.dval = [0] * self.NDMA
        self.dnext = 0
        self.seen = {n: {} for n in self.eng}
        self.lastw = {}
        self.lastw_isread = {}
        self.readers = {}

    def _wait(self, en, tok):
        if tok is None:
            return
        kind, a, v = tok
        if kind == 'e' and a == en and en == 'pe':
            return
        src = (kind, a)
        if self.seen[en].get(src, 0) >= v:
            return
        s = self.sem[a] if kind == 'e' else self.dsem[a]
        self.eng[en].wait_ge(s, v)
        self.seen[en][src] = v

    def _deps(self, en, r, w):
        for k in r:
            self._wait(en, self.lastw.get(k))
        for k in w:
            self._wait(en, self.lastw.get(k))
            for t in self.readers.get(k, ()):
                self._wait(en, t)

    def _commit(self, tok, r, w):
        for k in r:
            self.readers.setdefault(k, []).append(tok)
        for k in w:
            self.lastw[k] = tok
            self.readers[k] = []

    @staticmethod
    def _psum_excl(r, w):
        pr = [k for k in r if k.startswith(('ps', 'pa', 'ph', 'pr'))]
        if pr:
            r = [k for k in r if k not in pr]
            w = list(w) + pr
        w = ['psb' if k in ('psb0', 'psb1') else k for k in w]
        return r, w

    def I(self, en, fn, r=(), w=()):
        pr = [k for k in r if k.startswith(('ps', 'pa', 'ph', 'pr'))]
        r, w = self._psum_excl(r, w)
        skip = [k for k in pr if self.lastw_isread.get(k) == en]
        saved = {k: self.lastw[k] for k in skip}
        for k in skip:
            del self.lastw[k]
        self._deps(en, r, w)
        for k in skip:
            self.lastw[k] = saved[k]
        ins = fn(self.eng[en])
        self.cnt[en] += 1
        ins.then_inc(self.sem[en], 1)
        self._commit(('e', en, self.cnt[en]), r, w)
        for k in w:
            self.lastw_isread[k] = en if k in pr else None
        return ins

    def D(self, en, fn, r=(), w=()):
        i = self.dnext
        self.dnext = (self.dnext + 1) % self.NDMA
        if self.dval[i] > 0:
            self._wait(en, ('d', i, self.dval[i]))
        self._deps(en, r, w)
        ins = fn(self.eng[en])
        self.dval[i] += 16
        ins.then_inc(self.dsem[i], 16)
        self._commit(('d', i, self.dval[i]), r, w)
        return ins

    def barrier(self):
        for en in self.eng:
            for o in self.eng:
                if o != en and self.cnt[o] > 0:
                    self._wait(en, ('e', o, self.cnt[o]))
            for i in range(self.NDMA):
                if self.dval[i] > 0:
                    self._wait(en, ('d', i, self.dval[i]))
        self.lastw.clear()
        self.lastw_isread.clear()
        self.readers.clear()

    def finish(self):
        for i in range(self.NDMA):
            if self.dval[i] > 0:
                self._wait('sp', ('d', i, self.dval[i]))
        for o in self.eng:
            if o != 'sp' and self.cnt[o] > 0:
                self._wait('sp', ('e', o, self.cnt[o]))


def _mm(P, out, lhsT, rhs, start, stop, r, w):
    P.I('pe', lambda e: e.matmul(out, lhsT=lhsT, rhs=rhs, start=start, stop=stop), r=r, w=w)


def _consts(P, nc, sb):
    c = {}
    tf = sb("c_tf", [128, 128], F32)
    P.I('pool', lambda e: e.iota(tf[:], pattern=[[1, 128]], base=0, channel_multiplier=-1,
                                 allow_small_or_imprecise_dtypes=True), w=['c_tf'])
    c['ident'] = sb("c_ident", [128, 128], BF16)
    c['tri'] = sb("c_tri", [128, 128], BF16)
    c['atri'] = sb("c_atri", [128, 128], BF16)
    c['trif'] = sb("c_trif", [128, 128], F32)
    c['onesf'] = sb("c_onesf", [128, 128], F32)
    P.I('dve', lambda e: e.tensor_scalar(out=c['ident'][:], in0=tf[:], scalar1=0.0, scalar2=None, op0=OP.is_equal), r=['c_tf'], w=['c_ident'])
    P.I('dve', lambda e: e.tensor_scalar(out=c['tri'][:], in0=tf[:], scalar1=0.0, scalar2=None, op0=OP.is_ge), r=['c_tf'], w=['c_tri'])
    P.I('dve', lambda e: e.tensor_scalar(out=c['atri'][:], in0=tf[:], scalar1=0.0, scalar2=None, op0=OP.is_lt), r=['c_tf'], w=['c_atri'])
    P.I('dve', lambda e: e.tensor_scalar(out=c['trif'][:], in0=tf[:], scalar1=0.0, scalar2=None, op0=OP.is_ge), r=['c_tf'], w=['c_trif'])
    P.I('dve', lambda e: e.memset(c['onesf'][:], 1.0), w=['c_onesf'])
    return c


def _inproj(P, nc, es_outer, xT, wfm_d, bfm, fm_list, wtm_d, ntm, tm_evac, pss, tag, n_tok=T):
    with ExitStack() as es:
        sb = lambda n, s, d: es.enter_context(nc.sbuf_tensor(tag + n, s, d))
        nfm = int(wfm_d.shape[1])
        W = sb("W", [128, 8, nfm], BF16)
        for k in range(8):
            P.D('pool', lambda e, k=k: e.dma_start(out=W[:, k, :], in_=wfm_d[k * 128:(k + 1) * 128, :]), w=[tag + 'W'])
        if ntm:
            WT = sb("WT", [128, 8, ntm], BF16)
            for k in range(8):
                P.D('pool', lambda e, k=k: e.dma_start(out=WT[:, k, :], in_=wtm_d[k * 128:(k + 1) * 128, :]), w=[tag + 'WT'])
        xb = [sb(f"xb{i}", [128, 8, 512], BF16) for i in range(2)]
        xv = xT.rearrange("(k p) t -> p k t", p=128)
        nch = n_tok // 512
        pi = 0
        for tc in range(nch):
            X = xb[tc % 2]
            xk = f"{tag}xb{tc % 2}"
            for k2 in range(2):
                P.D('pool', lambda e, X=X, tc=tc, k2=k2: e.dma_start(out=X[:, 4 * k2:4 * k2 + 4, :], in_=xv[:, 4 * k2:4 * k2 + 4, tc * 512:(tc + 1) * 512]), w=[xk])
            for (c0, M, evac) in fm_list:
                ps, pk = pss[pi % len(pss)]
                pi += 1
                for k in range(8):
                    _mm(P, ps[0:M, 0:512], W[:, k, c0:c0 + M], X[:, k, :], k == 0, k == 7, [tag + 'W', xk], [pk])
                evac(tc, ps, pk)
            if ntm:
                for tt in range(4):
                    ps, pk = pss[pi % len(pss)]
                    pi += 1
                    for k in range(8):
                        _mm(P, ps[:, 0:ntm], X[:, k, tt * 128:(tt + 1) * 128], WT[:, k, :], k == 0, k == 7, [tag + 'WT', xk], [pk])
                    tm_evac(tc * 4 + tt, ps, pk)
    P.barrier()


def build_A():
    nc = bass.Bass("TRN2", target_bir_lowering=False)
    dt = lambda n, s, k="ExternalInput": nc.dram_tensor(n, s, F32, kind=k).ap()
    xT = dt("xT", [D, T])
    wfm1 = dt("wfm1", [D, 256]); bfm1 = dt("bfm1", [128, 2])
    wfm2 = dt("wfm2", [D, 516]); bfm2 = dt("bfm2", [128, 5])
    wtm2 = dt("wtm2", [D, 130]); btm2 = dt("btm2", [128, 130])
    wfm3 = dt("wfm3", [D, 256]); bfm3 = dt("bfm3", [128, 2])
    wtm3 = dt("wtm3", [D, 130]); btm3 = dt("btm3", [128, 130])
    pw_d = dt("pw", [128, 128]); pbs_d = dt("pbs", [128, 2]); cw_d = dt("cw", [128, 4]); fix_d = dt("fix", [128, 16])
    cw1_d = dt("cw1", [128, 32 * 256]); posT_d = dt("posT", [128, 32]); cb1_d = dt("cb1", [128, 4])
    cw2k_d = dt("cw2k", [128, 256]); cb2k_d = dt("cb2k", [128, 1]); cw2v_d = dt("cw2v", [128, 128]); cb2v_d = dt("cb2v", [128, 64])
    yT = dt("yT", [384, T], "ExternalOutput")

    with ExitStack() as es:
        P = Prog(nc, es)
        sbg = lambda n, s, d: es.enter_context(nc.sbuf_tensor(n, s, d))
        PS = [es.enter_context(nc.psum_tensor(f"ps{i}", [128, 512], F32)) for i in range(7)]
        PSB = es.enter_context(nc.psum_tensor("psb", [128, 1024], BF16))
        pk = [f"ps{i}" for i in range(7)]
        C = _consts(P, nc, sbg)
        KCT = sbg("KCT", [128, 512], BF16)
        VC = sbg("VC", [128, 4, 64], BF16)

        with ExitStack() as e1:
            sb = lambda n, s, d: e1.enter_context(nc.sbuf_tensor(n, s, d))
            bf1 = sb("bf1", [128, 2], F32); pwf = sb("pwf", [128, 128], BF16); pbs = sb("pbs_s", [128, 2], F32)
            cw = sb("cw_s", [128, 4], F32); fix = sb("fix_s", [128, 16], F32)
            P.D('sp', lambda e: e.dma_start(out=bf1[:], in_=bfm1), w=['bf1'])
            P.D('pool', lambda e: e.dma_start(out=pwf[:], in_=pw_d), w=['pwf'])
            P.D('sp', lambda e: e.dma_start(out=pbs[:], in_=pbs_d), w=['pbs'])
            P.D('sp', lambda e: e.dma_start(out=cw[:], in_=cw_d), w=['cw'])
            P.D('sp', lambda e: e.dma_start(out=fix[:], in_=fix_d), w=['fix'])
            KVC = sb("KVC", [128, T], BF16)
            UC = [sb(f"UC{i}", [128, 528], F32) for i in range(2)]
            S2 = sb("S2", [128, 528], F32); S4 = sb("S4", [128, 528], F32); S8 = sb("S8", [128, 528], F32); S16 = sb("S16", [128, 528], F32)
            ACC = sb("ACC", [128, 512], F32); DT_ = [sb(f"DTb{i}", [128, 512], BF16) for i in range(2)]
            YP = [sb(f"YP{i}", [128, 512], F32) for i in range(2)]
            P.I('dve', lambda e: e.memset(UC[1][:], 0.0), w=['UC1'])

            def evac_u(tc, ps, pkk):
                U = UC[tc % 2]; Up = UC[(tc + 1) % 2]
                uk, upk = f"UC{tc % 2}", f"UC{(tc + 1) % 2}"
                P.I('act', lambda e: e.activation(out=U[:, 16:528], in_=ps[:, 0:512], func=AF.Identity, bias=bf1[:, 0:1], scale=1.0), r=[pkk, 'bf1'], w=[uk])
                P.I('dve', lambda e: e.tensor_copy(out=U[:, 0:16], in_=Up[:, 512:528]), r=[upk], w=[uk])
                P.I('dve', lambda e: e.tensor_tensor(out=S2[:, 1:528], in0=U[:, 1:528], in1=U[:, 0:527], op=OP.add), r=[uk], w=['S2'])
                P.I('dve', lambda e: e.tensor_tensor(out=S4[:, 3:528], in0=S2[:, 3:528], in1=S2[:, 1:526], op=OP.add), r=['S2'], w=['S4'])
                P.I('dve', lambda e: e.tensor_tensor(out=S8[:, 7:528], in0=S4[:, 7:528], in1=S4[:, 3:524], op=OP.add), r=['S4'], w=['S8'])
                P.I('dve', lambda e: e.tensor_tensor(out=S16[:, 15:528], in0=S8[:, 15:528], in1=S8[:, 7:520], op=OP.add), r=['S8'], w=['S16'])
                P.I('dve', lambda e: e.tensor_scalar(out=ACC[:], in0=S2[:, 16:528], scalar1=cw[:, 0:1], scalar2=None, op0=OP.mult), r=['S2', 'cw'], w=['ACC'])
                for wi, S in enumerate((S4, S8, S16)):
                    P.I('dve', lambda e, S=S, wi=wi: e.scalar_tensor_tensor(out=ACC[:], in0=S[:, 16:528], scalar=cw[:, wi + 1:wi + 2], in1=ACC[:], op0=OP.mult, op1=OP.add),
                        r=['S4', 'S8', 'S16', 'cw', 'ACC'], w=['ACC'])
                if tc == 0:
                    P.I('dve', lambda e: e.tensor_tensor(out=ACC[:, 0:16], in0=ACC[:, 0:16], in1=fix[:], op=OP.mult), r=['ACC', 'fix'], w=['ACC'])
                Dt = DT_[tc % 2]; dk = f"DT{tc % 2}"
                P.I('dve', lambda e: e.tensor_tensor(out=Dt[:], in0=ACC[:], in1=U[:, 16:528], op=OP.subtract), r=['ACC', uk], w=[dk])
                ps2, pk2 = PS[4 + tc % 2], pk[4 + tc % 2]
                _mm(P, ps2[:, 0:512], pwf[:], Dt[:], True, True, ['pwf', dk], [pk2])
                Y = YP[tc % 2]; yk = f"YP{tc % 2}"
                P.I('dve', lambda e: e.tensor_scalar(out=Y[:], in0=ps2[:, 0:512], scalar1=pbs[:, 0:1], scalar2=pbs[:, 1:2], op0=OP.add, op1=OP.mult), r=[pk2, 'pbs'], w=[yk])
                P.D('sp', lambda e: e.dma_start(out=yT[0:128, tc * 512:(tc + 1) * 512], in_=Y[:]), r=[yk])

            def evac_kvc(tc, ps, pkk):
                P.I('act', lambda e: e.activation(out=KVC[:, tc * 512:(tc + 1) * 512], in_=ps[:, 0:512], func=AF.Identity, bias=bf1[:, 1:2], scale=1.0), r=[pkk, 'bf1'], w=['KVC'])

            _inproj(P, nc, e1, xT, wfm1, bf1, [(0, 128, evac_u), (128, 128, evac_kvc)], None, 0, None,
                    [(PS[0], pk[0]), (PS[1], pk[1]), (PS[2], pk[2]), (PS[3], pk[3])], "p1")

            W1 = sb("W1", [128, 32 * 256], BF16)
            for q4 in range(4):
                P.D('pool', lambda e, q4=q4: e.dma_start(out=W1[:, q4 * 2048:(q4 + 1) * 2048], in_=cw1_d[:, q4 * 2048:(q4 + 1) * 2048]), w=['W1'])
            posT = sb("posT_s", [128, 32], BF16); cb1 = sb("cb1_s", [128, 4], F32)
            w2k = sb("w2k", [128, 256], BF16); b2k = sb("b2k", [128, 1], F32); w2v = sb("w2v", [128, 128], BF16); b2v = sb("b2v", [128, 64], F32)
            P.D('pool', lambda e: e.dma_start(out=posT[:], in_=posT_d), w=['posT'])
            P.D('sp', lambda e: e.dma_start(out=cb1[:], in_=cb1_d), w=['cb1'])
            P.D('pool', lambda e: e.dma_start(out=w2k[:], in_=cw2k_d), w=['w2k'])
            P.D('sp', lambda e: e.dma_start(out=b2k[:], in_=cb2k_d), w=['b2k'])
            P.D('pool', lambda e: e.dma_start(out=w2v[:], in_=cw2v_d), w=['w2v'])
            P.D('sp', lambda e: e.dma_start(out=b2v[:], in_=cb2v_d), w=['b2v'])
            HT = [[sb(f"HT{kv}{hf}", [128, 512], BF16) for hf in range(2)] for kv in range(2)]
            XH = sb("XH", [128, 512], F32); T1 = sb("T1c", [128, 512], F32); T2 = sb("T2c", [128, 512], F32); cbt = sb("cbt", [128, 4], F32)
            P.I('dve', lambda e: e.memset(VC[:], 0.0), w=['VC'])
            for kv in range(2):
                po = 64 * kv
                for hf in range(2):
                    ci = kv * 2 + hf
                    ps, pkk = PS[ci % 4], pk[ci % 4]
                    psc, pkc = PS[4 + ci % 2], pk[4 + ci % 2]
                    for l in range(32):
                        wsl = W1[po:po + 64, l * 256 + hf * 128: l * 256 + hf * 128 + 128]
                        _mm(P, psc[:, 0:1], wsl, posT[po:po + 64, l:l + 1], l == 0, l == 31, ['W1', 'posT'], [pkc])
                    P.I('dve', lambda e, ci=ci, psc=psc: e.tensor_tensor(out=cbt[:, ci:ci + 1], in0=psc[:, 0:1], in1=cb1[:, ci:ci + 1], op=OP.add), r=[pkc, 'cb1'], w=['cbt'])
                    for l in range(32):
                        wsl = W1[po:po + 64, l * 256 + hf * 128: l * 256 + hf * 128 + 128]
                        _mm(P, ps[:, 0:511], wsl, KVC[po:po + 64, l:l + 16 * 510 + 1:16], l == 0, l == 31, ['W1', 'KVC'], [pkk])
                    P.I('act', lambda e, ci=ci, ps=ps: e.activation(out=XH[:, 0:511], in_=ps[:, 0:511], func=AF.Identity, bias=cbt[:, ci:ci + 1], scale=1.0), r=[pkk, 'cbt'], w=['XH'])
                    P.I('dve', lambda e: e.tensor_tensor(out=T1[:, 0:511], in0=XH[:, 0:511], in1=XH[:, 0:511], op=OP.mult), r=['XH'], w=['T1'])
                    P.I('dve', lambda e: e.tensor_scalar(out=T1[:, 0:511], in0=T1[:, 0:511], scalar1=0.044715, scalar2=1.0, op0=OP.mult, op1=OP.add), r=['T1'], w=['T1'])
                    P.I('dve', lambda e: e.tensor_tensor(out=T1[:, 0:511], in0=T1[:, 0:511], in1=XH[:, 0:511], op=OP.mult), r=['T1', 'XH'], w=['T1'])
                    P.I('act', lambda e: e.activation(out=T2[:, 0:511], in_=T1[:, 0:511], func=AF.Sigmoid, scale=1.5957691216057308), r=['T1'], w=['T2'])
                    H = HT[kv][hf]
                    P.I('dve', lambda e, H=H: e.memset(H[:, 511:512], 0.0), w=[f'HT{kv}{hf}'])
                    P.I('dve', lambda e, H=H: e.tensor_tensor(out=H[:, 0:511], in0=T2[:, 0:511], in1=XH[:, 0:511], op=OP.mult), r=['T2', 'XH'], w=[f'HT{kv}{hf}'])
            for hf in range(2):
                _mm(P, PS[0][:, 0:512], w2k[:, hf * 128:(hf + 1) * 128], HT[0][hf][:], hf == 0, hf == 1, ['w2k', f'HT0{hf}'], [pk[0]])
            P.I('act', lambda e: e.activation(out=KCT[:], in_=PS[0][:, 0:512], func=AF.Identity, bias=b2k[:, 0:1], scale=1.0), r=[pk[0], 'b2k'], w=['KCT'])
            for ct in range(4):
                m = 128 if ct < 3 else 127
                for hf in range(2):
                    _mm(P, PS[1 + ct % 2][0:m, 0:64], HT[1][hf][:, ct * 128:ct * 128 + m], w2v[:, hf * 64:(hf + 1) * 64], hf == 0, hf == 1, ['w2v', f'HT1{hf}'], [pk[1 + ct % 2]])
                P.I('dve', lambda e, ct=ct, m=m: e.tensor_tensor(out=VC[0:m, ct, :], in0=PS[1 + ct % 2][0:m, 0:64], in1=b2v[0:m, :], op=OP.add), r=[pk[1 + ct % 2], 'b2v'], w=['VC'])
            P.barrier()

        with ExitStack() as e2:
            sb = lambda n, s, d: e2.enter_context(nc.sbuf_tensor(n, s, d))
            bf2 = sb("bf2", [128, 5], F32); bt2 = sb("bt2", [128, 130], F32)
            P.D('sp', lambda e: e.dma_start(out=bf2[:], in_=bfm2), w=['bf2'])
            P.D('sp', lambda e: e.dma_start(out=bt2[:], in_=btm2), w=['bt2'])
            NQ = sb("NQ", [128, T], BF16); NQo = sb("NQo", [128, T], BF16); KS = sb("KS", [128, T], BF16); KW = sb("KW", [128, T], BF16)
            G4 = sb("G4", [4, T], BF16)
            VS = sb("VS", [128, NT, 65], BF16); VW = sb("VW", [128, NT, 65], BF16); GC = sb("GC", [128, NT, 2], F32)
            P.I('pool', lambda e: e.memset(VS[:, :, 64:65], 1.0), w=['VS'])
            P.I('pool', lambda e: e.memset(VW[:, :, 64:65], 1.0), w=['VW'])

            def ev(dst, key, col, func=AF.Identity, M=128):
                def f(tc, ps, pkk):
                    P.I('act', lambda e: e.activation(out=dst[0:M, tc * 512:(tc + 1) * 512], in_=ps[0:M, 0:512], func=func, bias=bf2[0:M, col:col + 1], scale=1.0), r=[pkk, 'bf2'], w=[key])
                return f

            TMB = sb("TMB", [128, 130], F32)

            def tm2(ti, ps, pkk):
                P.I('dve', lambda e: e.tensor_tensor(out=VS[:, ti, 0:64], in0=ps[:, 0:64], in1=bt2[:, 0:64], op=OP.add), r=[pkk, 'bt2'], w=['VS'])
                P.I('dve', lambda e: e.tensor_tensor(out=VW[:, ti, 0:64], in0=ps[:, 64:128], in1=bt2[:, 64:128], op=OP.add), r=[pkk, 'bt2'], w=['VW'])
                P.I('dve', lambda e: e.tensor_tensor(out=TMB[:, 128:130], in0=ps[:, 128:130], in1=bt2[:, 128:130], op=OP.add), r=[pkk, 'bt2'], w=['TMB'])
                P.I('act', lambda e: e.activation(out=GC[:, ti, :], in_=TMB[:, 128:130], func=AF.Sigmoid), r=['TMB'], w=['GC'])

            _inproj(P, nc, e2, xT, wfm2, bf2,
                    [(0, 128, ev(NQ, 'NQ', 0)), (128, 128, ev(NQo, 'NQo', 1)), (256, 128, ev(KS, 'KS', 2)), (384, 128, ev(KW, 'KW', 3)),
                     (512, 4, ev(G4, 'G4', 4, AF.Sigmoid, 4))],
                    wtm2, 130, tm2, [(PS[0], pk[0]), (PS[1], pk[1]), (PS[2], pk[2]), (PS[3], pk[3])], "p2")

            EJ = sb("EJ", [128, NT * 128], BF16); ejf = sb("ejf", [128, 2048], F32)
            for q4 in range(4):
                P.I('pool', lambda e, q4=q4: e.iota(ejf[:], pattern=[[-2, 16], [-1, 2], [0, 64]], base=-32 * q4, channel_multiplier=1,
                                             allow_small_or_imprecise_dtypes=True), w=['ejf'])
                P.I('dve', lambda e, q4=q4: e.tensor_scalar(out=EJ[:, q4 * 2048:(q4 + 1) * 2048], in0=ejf[:], scalar1=0.0, scalar2=None, op0=OP.is_equal), r=['ejf'], w=['EJ'])
            REL = sb("REL", [128, 512], F32)
            P.I('pool', lambda e: e.iota(REL[:], pattern=[[16, 512]], base=31, channel_multiplier=-1, allow_small_or_imprecise_dtypes=True), w=['REL'])
            VV = sb("VV", [128, 254], F32); HP = sb("HP", [128, 1], F32)
            P.I('pool', lambda e: e.iota(VV[:], pattern=[[1, 254]], base=-126, channel_multiplier=0, allow_small_or_imprecise_dtypes=True), w=['VV'])
            P.I('pool', lambda e: e.iota(HP[:], pattern=[[0, 1]], base=0, channel_multiplier=1, allow_small_or_imprecise_dtypes=True), w=['HP'])
            P.I('dve', lambda e: e.tensor_scalar(out=HP[:], in0=HP[:], scalar1=64.0, scalar2=None, op0=OP.is_ge), r=['HP'], w=['HP'])
            P.I('dve', lambda e: e.tensor_scalar(out=VV[:], in0=VV[:], scalar1=HP[:, 0:1], scalar2=None, op0=OP.subtract), r=['VV', 'HP'], w=['VV'])
            KEEP = sb("KEEP", [128, 254], F32); NF = sb("NF", [128, 254], F32); ADD = sb("ADD", [128, 254], F32); TA = sb("TA", [128, 254], F32)
            P.I('dve', lambda e: e.tensor_scalar(out=KEEP[:], in0=VV[:], scalar1=-2.0, scalar2=None, op0=OP.is_le), r=['VV'], w=['KEEP'])
            P.I('dve', lambda e: e.tensor_scalar(out=NF[:], in0=VV[:], scalar1=0.0, scalar2=None, op0=OP.is_le), r=['VV'], w=['NF'])
            P.I('dve', lambda e: e.tensor_scalar(out=ADD[:], in0=VV[:], scalar1=-1.0, scalar2=1.0e6, op0=OP.is_ge, op1=OP.mult), r=['VV'], w=['ADD'])
            P.I('dve', lambda e: e.tensor_scalar(out=TA[:], in0=VV[:], scalar1=0.0, scalar2=-1000001.0, op0=OP.is_gt, op1=OP.mult), r=['VV'], w=['TA'])
            P.I('dve', lambda e: e.tensor_tensor(out=ADD[:], in0=ADD[:], in1=TA[:], op=OP.add), r=['ADD', 'TA'], w=['ADD'])
            SEL = sb("SEL", [4, 256], BF16); self_ = sb("self_", [4, 256], F32)
            P.I('pool', lambda e: e.iota(self_[:], pattern=[[1, 4], [0, 64]], base=0, channel_multiplier=-1, allow_small_or_imprecise_dtypes=True), w=['self_'])
            P.I('dve', lambda e: e.tensor_scalar(out=SEL[:], in0=self_[:], scalar1=0.0, scalar2=None, op0=OP.is_equal), r=['self_'], w=['SEL'])

            WMASK = sb("WMASK", [128, 8 * 512], BF16)
            P.I('dve', lambda e: e.memset(WMASK[:], 0.0), w=['WMASK'])
            for a in range(-4, 4):
                for b in range(4):
                    dst = WMASK[:, (a + 4) * 512 + b * 128:(a + 4) * 512 + (b + 1) * 128]
                    if b == a:
                        P.I('dve', lambda e, dst=dst: e.tensor_copy(out=dst, in_=C['tri'][:]), r=['c_tri'], w=['WMASK'])
                    elif b == a + 4:
                        P.I('dve', lambda e, dst=dst: e.tensor_copy(out=dst, in_=C['atri'][:]), r=['c_atri'], w=['WMASK'])
                    elif a < b < a + 4:
                        P.I('dve', lambda e, dst=dst: e.memset(dst, 1.0), w=['WMASK'])
            EX = [sb(f"EX{i}", [128, 512], F32) for i in range(2)]
            EM = [sb(f"EM{i}", [128, 512], F32) for i in range(2)]
            RS = sb("RS", [128, 8], F32)
            PSP = sb("PSP", [128, 520], F32)
            P.I('dve', lambda e: e.memset(PSP[:], 0.0), w=['PSP'])
            PN2 = [[sb(f"PN{i}{h}", [128, 512], BF16) for h in range(2)] for i in range(2)]
            MB2 = [sb(f"MB{i}", [128, 128], BF16) for i in range(2)]
            PNT = [sb(f"PNT{i}", [128, 512], BF16) for i in range(2)]
            IMP = sb("IMP", [128, 128], F32); SC = sb("SC", [128, 128], F32); SC2 = sb("SC2", [128, 128], F32); M8 = sb("M8", [128, 16], F32)
            SELM = sb("SELM", [128, 128], F32); MB = sb("MB", [128, 128], BF16)
            MBT = [sb(f"MBT{i}", [128, 512], BF16) for i in range(2)]
            OCc = [[sb(f"OC{i}{h}", [64, 512], F32) for h in range(2)] for i in range(2)]
            PT = [sb(f"PT{i}", [128, 512], BF16) for i in range(3)]
            RR = sb("RR", [65, 512], F32); BCS = sb("BCS", [64, 512], F32); BGS = sb("BGS", [64, 512], F32)
            TY = sb("TY", [64, 512], F32); YN = [sb(f"YN{i}", [64, 512], F32) for i in range(2)]
            ptc = 0

            def make_cmp(qc):
                par = qc % 2
                mbk = f"MBT{par}"
                def cmp_stage1(b):
                    i = 4 * qc + b
                    for hh in range(4):
                        Q = NQ if hh < 2 else NQo
                        qk_ = 'NQ' if hh < 2 else 'NQo'
                        po = 64 * (hh % 2)
                        ps, pkk = PS[6], pk[6]
                        _mm(P, ps[:, 0:512], Q[po:po + 64, i * 128:(i + 1) * 128], KCT[po:po + 64, :], True, True, [qk_, 'KCT'], [pkk])
                        ex, exk = EX[hh % 2], f"EX{hh % 2}"
                        em, emk = EM[hh % 2], f"EM{hh % 2}"
                        P.I('act', lambda e: e.activation(out=ex[:], in_=ps[:, 0:512], func=AF.Exp, scale=SCALE), r=[pkk], w=[exk])
                        P.I('dve', lambda e: e.scalar_tensor_tensor(out=em[:], in0=REL[:], scalar=float(128 * i), in1=ex[:], op0=OP.is_le, op1=OP.mult),
                            r=['REL', exk], w=[emk])
                        P.I('dve', lambda e: e.reduce_sum(out=RS[:, hh:hh + 1], in_=em[:], axis=AX.X), r=[emk], w=['RS'])
                        P.I('dve', lambda e: e.tensor_scalar(out=RS[:, hh:hh + 1], in0=RS[:, hh:hh + 1], scalar1=1e-30, scalar2=None, op0=OP.max), r=['RS'], w=['RS'])
                        P.I('dve', lambda e: e.reciprocal(out=RS[:, hh:hh + 1], in_=RS[:, hh:hh + 1]), r=['RS'], w=['RS'])
                        if hh == 0:
                            P.I('dve', lambda e: e.tensor_scalar(out=PSP[:, 1:513], in0=em[:], scalar1=RS[:, hh:hh + 1], scalar2=None, op0=OP.mult), r=[emk, 'RS'], w=['PSP'])
                        else:
                            P.I('dve', lambda e: e.scalar_tensor_tensor(out=PSP[:, 1:513], in0=em[:], scalar=RS[:, hh:hh + 1], in1=PSP[:, 1:513], op0=OP.mult, op1=OP.add),
                                r=[emk, 'RS', 'PSP'], w=['PSP'])
                        if hh < 2:
                            P.I('dve', lambda e: e.tensor_tensor(out=RS[:, 4 + hh:5 + hh], in0=RS[:, hh:hh + 1], in1=GC[:, i, hh:hh + 1], op=OP.mult), r=['RS', 'GC'], w=['RS'])
                            pn, pnk = PN2[b % 2][hh], f"PN{b % 2}{hh}"
                            P.I('pool', lambda e: e.tensor_scalar(out=pn[:], in0=em[:], scalar1=RS[:, 4 + hh:5 + hh], scalar2=None, op0=OP.mult), r=[emk, 'RS'], w=[pnk])
                    mb, mbk_ = MB2[b % 2], f"MB{b % 2}"
                    P.I('dve', lambda e: e.tensor_reduce(out=IMP[:], in_=PSP[:, 0:512].rearrange("p (s m) -> p s m", m=4), axis=AX.X, op=OP.add), r=['PSP'], w=['IMP'])
                    P.I('dve', lambda e: e.tensor_tensor(out=IMP[:], in0=IMP[:], in1=PSP[:, 4:516:4], op=OP.add), r=['PSP', 'IMP'], w=['IMP'])
                    x0 = 126 - 2 * i
                    P.I('dve', lambda e: e.tensor_tensor(out=SC[:], in0=IMP[:], in1=KEEP[:, x0:x0 + 128], op=OP.mult), r=['IMP', 'KEEP'], w=['SC'])
                    P.I('dve', lambda e: e.tensor_tensor(out=SC[:], in0=SC[:], in1=ADD[:, x0:x0 + 128], op=OP.add), r=['SC', 'ADD'], w=['SC'])
                    P.I('dve', lambda e: e.memset(SC[:, 0:1], 1.0e6), r=['SC'], w=['SC'])
                    P.I('dve', lambda e: e.max(out=M8[:, 0:8], in_=SC[:]), r=['SC'], w=['M8'])
                    P.I('dve', lambda e: e.match_replace(out=SC2[:], in_to_replace=M8[:, 0:8], in_values=SC[:], imm_value=-2.0), r=['SC', 'M8'], w=['SC2'])
                    P.I('dve', lambda e: e.max(out=M8[:, 8:16], in_=SC2[:]), r=['SC2'], w=['M8'])
                    P.I('dve', lambda e: e.scalar_tensor_tensor(out=SELM[:], in0=SC[:], scalar=M8[:, 15:16], in1=NF[:, x0:x0 + 128], op0=OP.is_ge, op1=OP.mult), r=['SC', 'M8', 'NF'], w=['SELM'])
                    P.I('dve', lambda e: e.tensor_scalar(out=mb[:], in0=SELM[:], scalar1=-1.0, scalar2=30000.0, op0=OP.add, op1=OP.mult), r=['SELM'], w=[mbk_])

                def cmp_stage2(b):
                    for hh in range(2):
                        pn, pnk = PN2[b % 2][hh], f"PN{b % 2}{hh}"
                        for ct in range(4):
                            P.I('pe', lambda e, ct=ct: e.transpose(out=PSB[:, hh * 512 + ct * 128: hh * 512 + (ct + 1) * 128], in_=pn[:, ct * 128:(ct + 1) * 128], identity=C['ident'][:]),
                                r=[pnk, 'c_ident'], w=[f'psb{hh}'])
                        pnt, pntk = PNT[hh], f"PNT{hh}"
                        P.I('act', lambda e: e.copy(out=pnt[:], in_=PSB[:, hh * 512:(hh + 1) * 512]), r=[f'psb{hh}'], w=[pntk])
                        po_, pok = PS[2 + hh], pk[2 + hh]
                        for ct in range(4):
                            _mm(P, po_[0:64, b * 128:(b + 1) * 128], VC[:, ct, :], pnt[:, ct * 128:(ct + 1) * 128], ct == 0, ct == 3, ['VC', pntk], [pok])
                    mb, mbk_ = MB2[b % 2], f"MB{b % 2}"
                    P.I('pe', lambda e: e.transpose(out=PSB[:, 0:128], in_=mb[:], identity=C['ident'][:]), r=[mbk_, 'c_ident'], w=['psb0'])
                    P.I('act', lambda e: e.copy(out=MBT[par][:, b * 128:(b + 1) * 128], in_=PSB[:, 0:128]), r=['psb0'], w=[mbk])

                def occ():
                    for h in range(2):
                        P.I('act', lambda e, h=h: e.copy(out=OCc[par][h][:], in_=PS[2 + h][0:64, 0:512]), r=[pk[2 + h]], w=[f'OC{par}{h}'])
                return [lambda: cmp_stage1(0), lambda: cmp_stage1(1), lambda: cmp_stage2(0), lambda: cmp_stage1(2), lambda: cmp_stage2(1),
                        lambda: cmp_stage1(3), lambda: cmp_stage2(2), lambda: cmp_stage2(3), occ]

            pend = make_cmp(0)
            for fn in pend:
                fn()
            for qc in range(T // 512):
                par = qc % 2
                mbk = f"MBT{par}"
                pend = make_cmp(qc + 1) if qc + 1 < T // 512 else []
                nst = 2 * (4 * qc + 4 + min(8, 4 * qc + 4))
                intv = max(1, nst // (len(pend) + 1))
                stc = 0
                for h in range(2):
                    po = 64 * h
                    pso, psok = PS[4], pk[4]
                    psw, pswk = PS[5], pk[5]
                    nj = 4 * qc + 4
                    jfirst = max(4 * qc - 4, 0)
                    steps = [('s', j) for j in range(nj)] + [('w', 4 * qc + a) for a in range(-4, 4) if 4 * qc + a >= 0]

                    def qk(si):
                        kind, j = steps[si]
                        pst, pstk = PS[si % 2], pk[si % 2]
                        a = j - 4 * qc
                        if kind == 's':
                            c0 = 128 * max(a, 0)
                            _mm(P, pst[:, c0:512], KS[po:po + 64, j * 128:(j + 1) * 128], NQ[po:po + 64, qc * 512 + c0:(qc + 1) * 512], True, False, ['KS', 'NQ'], [pstk])
                            _mm(P, pst[:, c0:512], EJ[:, j * 128:(j + 1) * 128], MBT[par][:, c0:512], False, True, ['EJ', mbk], [pstk])
                        else:
                            _mm(P, pst[:, 0:512], KW[po:po + 64, j * 128:(j + 1) * 128], NQ[po:po + 64, qc * 512:(qc + 1) * 512], True, True, ['KW', 'NQ'], [pstk])

                    def rest(si):
                        nonlocal ptc
                        kind, j = steps[si]
                        pst, pstk = PS[si % 2], pk[si % 2]
                        a = j - 4 * qc
                        pt, ptk = PT[ptc % 3], f"PT{ptc % 3}"
                        ptc += 1
                        if kind == 's':
                            c0 = 128 * max(a, 0)
                            P.I('act', lambda e: e.activation(out=pt[:, c0:512], in_=pst[:, c0:512], func=AF.Exp, scale=SCALE), r=[pstk], w=[ptk])
                            if a >= 0:
                                P.I('dve', lambda e: e.tensor_tensor(out=pt[:, c0:c0 + 128], in0=pt[:, c0:c0 + 128], in1=C['tri'][:], op=OP.mult), r=[ptk, 'c_tri'], w=[ptk])
                            _mm(P, pso[0:65, c0:512], VS[:, j, :], pt[:, c0:512], j == 0, j == nj - 1, ['VS', ptk], [psok])
                        else:
                            P.I('act', lambda e: e.activation(out=pt[:, 0:512], in_=pst[:, 0:512], func=AF.Exp, scale=SCALE), r=[pstk], w=[ptk])
                            P.I('dve', lambda e: e.tensor_tensor(out=pt[:, 0:512], in0=pt[:, 0:512], in1=WMASK[:, (a + 4) * 512:(a + 5) * 512], op=OP.mult), r=[ptk, 'WMASK'], w=[ptk])
                            _mm(P, psw[0:65, 0:512], VW[:, j, :], pt[:, 0:512], j == jfirst, a == 3, ['VW', ptk], [pswk])

                    qk(0)
                    for si in range(len(steps)):
                        if si + 1 < len(steps):
                            qk(si + 1)
                        rest(si)
                        stc += 1
                        if pend and stc % intv == 0:
                            pend.pop(0)()
                    Y = YN[h]; yk = f"YN{h}"
                    for br, (pacc, pacck) in enumerate(((pso, psok), (psw, pswk))):
                        P.I('dve', lambda e, pacc=pacc: e.reciprocal(out=RR[64:65, :], in_=pacc[64:65, 0:512]), r=[pacck], w=['RR'])
                        _mm(P, PS[6][0:64, 0:512], C['onesf'][64:65, 0:64], RR[64:65, :], True, True, ['c_onesf', 'RR'], [pk[6]])
                        P.I('act', lambda e: e.copy(out=BCS[:], in_=PS[6][0:64, 0:512]), r=[pk[6]], w=['BCS'])
                        gi = 2 * h + br
                        _mm(P, PS[6][0:64, 0:512], SEL[0:4, gi * 64:(gi + 1) * 64], G4[0:4, qc * 512:(qc + 1) * 512], True, True, ['SEL', 'G4'], [pk[6]])
                        P.I('act', lambda e: e.copy(out=BGS[:], in_=PS[6][0:64, 0:512]), r=[pk[6]], w=['BGS'])
                        P.I('dve', lambda e, pacc=pacc: e.tensor_tensor(out=TY[:], in0=pacc[0:64, 0:512], in1=BCS[:], op=OP.mult), r=[pacck, 'BCS'], w=['TY'])
                        P.I('dve', lambda e: e.tensor_tensor(out=TY[:], in0=TY[:], in1=BGS[:], op=OP.mult), r=['TY', 'BGS'], w=['TY'])
                        src = OCc[par][h] if br == 0 else Y
                        srck = f'OC{par}{h}' if br == 0 else yk
                        P.I('dve', lambda e, src=src, Y=Y: e.tensor_tensor(out=Y[:], in0=TY[:], in1=src[:], op=OP.add), r=['TY', srck], w=[yk])
                    P.D('sp', lambda e, Y=Y, h=h, qc=qc: e.dma_start(out=yT[128 + 64 * h:192 + 64 * h, qc * 512:(qc + 1) * 512], in_=Y[:]), r=[yk])
                while pend:
                    pend.pop(0)()
            P.barrier()

        with ExitStack() as e3:
            sb = lambda n, s, d: e3.enter_context(nc.sbuf_tensor(n, s, d))
            bf3 = sb("bf3", [128, 2], F32); bt3 = sb("bt3", [128, 130], F32)
            P.D('sp', lambda e: e.dma_start(out=bf3[:], in_=bfm3), w=['bf3'])
            P.D('sp', lambda e: e.dma_start(out=bt3[:], in_=btm3), w=['bt3'])
            FQ = sb("FQ", [128, T], BF16); FK = sb("FK", [128, T], BF16)
            FV = sb("FV", [128, NT, 2, 65], BF16); LF = sb("LF", [128, NT, 2], F32)
            P.I('pool', lambda e: e.memset(FV[:, :, :, 64:65], 1.0), w=['FV'])

            def ev3(dst, key, col):
                def f(tc, ps, pkk):
                    P.I('act', lambda e: e.activation(out=dst[:, tc * 512:(tc + 1) * 512], in_=ps[:, 0:512], func=AF.Identity, bias=bf3[:, col:col + 1], scale=1.0), r=[pkk, 'bf3'], w=[key])
                return f

            def tm3(ti, ps, pkk):
                P.I('dve', lambda e: e.tensor_tensor(out=FV[:, ti, :, 0:64], in0=ps[:, 0:128].rearrange("p (h d) -> p h d", d=64), in1=bt3[:, 0:128].rearrange("p (h d) -> p h d", d=64), op=OP.add),
                    r=[pkk, 'bt3'], w=['FV'])
                P.I('dve', lambda e: e.tensor_tensor(out=LF[:, ti, :], in0=ps[:, 128:130], in1=bt3[:, 128:130], op=OP.add), r=[pkk, 'bt3'], w=['LF'])

            _inproj(P, nc, e3, xT, wfm3, bf3, [(0, 128, ev3(FQ, 'FQ', 0)), (128, 128, ev3(FK, 'FK', 1))],
                    wtm3, 130, tm3, [(PS[0], pk[0]), (PS[1], pk[1]), (PS[2], pk[2]), (PS[3], pk[3])], "p3")
            LFv = LF[:].rearrange("p j h -> p (j h)")
            P.I('act', lambda e: e.activation(out=LFv, in_=LFv, func=AF.Exp, scale=-1.0), r=['LF'], w=['LF'])
            P.I('dve', lambda e: e.tensor_scalar(out=LFv, in0=LFv, scalar1=1.0, scalar2=None, op0=OP.add), r=['LF'], w=['LF'])
            P.I('act', lambda e: e.activation(out=LFv, in_=LFv, func=AF.Ln), r=['LF'], w=['LF'])
            P.I('dve', lambda e: e.tensor_scalar(out=LFv, in0=LFv, scalar1=-1.0, scalar2=None, op0=OP.mult), r=['LF'], w=['LF'])
            _mm(P, PS[0][:, 0:128], C['trif'][:], LFv, True, True, ['c_trif', 'LF'], [pk[0]])
            _mm(P, PS[1][:, 0:128], C['onesf'][:], LFv, True, True, ['c_onesf', 'LF'], [pk[1]])
            TOT = sb("TOT", [128, NT, 2], F32); INCL = sb("INCL", [128, NT, 2], F32); CUM = sb("CUM", [128, NT, 2], F32); ONE64 = sb("ONE64", [128, NT], F32)
            P.I('dve', lambda e: e.memset(ONE64[:], 1.0), w=['ONE64'])
            P.I('act', lambda e: e.copy(out=TOT[:].rearrange("p j h -> p (j h)"), in_=PS[1][:, 0:128]), r=[pk[1]], w=['TOT'])
            for h in range(2):
                P.I('dve', lambda e, h=h: e.tensor_tensor_scan(out=INCL[:, :, h], data0=ONE64[:], data1=TOT[:, :, h], initial=0.0, op0=OP.mult, op1=OP.add), r=['TOT', 'ONE64'], w=['INCL'])
            P.I('dve', lambda e: e.tensor_tensor(out=CUM[:].rearrange("p j h -> p (j h)"), in0=PS[0][:, 0:128], in1=INCL[:].rearrange("p j h -> p (j h)"), op=OP.add), r=[pk[0], 'INCL'], w=['CUM'])
            P.I('dve', lambda e: e.tensor_tensor(out=CUM[:], in0=CUM[:], in1=TOT[:], op=OP.subtract), r=['CUM', 'TOT'], w=['CUM'])

            BI = [sb(f"BI{i}", [128, 4, NT], F32) for i in range(2)]
            PT = [sb(f"FPT{i}", [128, 512], BF16) for i in range(3)]
            RR = sb("FRR", [65, 512], F32); BCS = sb("FBCS", [64, 512], F32); YF = [sb(f"YF{i}", [64, 512], F32) for i in range(2)]
            ptc = 0
            it = 0
            for h in range(2):
                po = 64 * h
                for qc in range(T // 512):
                    bi, bik = BI[it % 2], f"BI{it % 2}"
                    pso, psok = PS[4 + it % 2], pk[4 + it % 2]
                    for b in range(4):
                        i = 4 * qc + b
                        P.I('dve', lambda e, bi=bi, b=b, i=i, h=h: e.tensor_scalar(out=bi[:, b, 0:i + 1], in0=CUM[:, 0:i + 1, h], scalar1=-1.0, scalar2=INCL[:, i, h:h + 1], op0=OP.mult, op1=OP.add),
                            r=['CUM', 'INCL'], w=[bik])
                    nj = 4 * qc + 4

                    def fqk(j):
                        c0 = 128 * max(j - 4 * qc, 0)
                        pst, pstk = PS[j % 4], pk[j % 4]
                        _mm(P, pst[:, c0:512], FK[po:po + 64, j * 128:(j + 1) * 128], FQ[po:po + 64, qc * 512 + c0:(qc + 1) * 512], True, True, ['FK', 'FQ'], [pstk])

                    def frest(j):
                        nonlocal ptc
                        a = j - 4 * qc
                        b0 = max(a, 0)
                        c0 = 128 * b0
                        pst, pstk = PS[j % 4], pk[j % 4]
                        pt, ptk = PT[ptc % 3], f"FPT{ptc % 3}"
                        ptc += 1
                        for b in range(b0, 4):
                            P.I('act', lambda e, b=b: e.activation(out=pt[:, b * 128:(b + 1) * 128], in_=pst[:, b * 128:(b + 1) * 128], func=AF.Exp, bias=bi[:, b, j:j + 1], scale=SCALE),
                                r=[pstk, bik], w=[f"{ptk}_{b}"])
                        if a >= 0:
                            P.I('dve', lambda e: e.tensor_tensor(out=pt[:, c0:c0 + 128], in0=pt[:, c0:c0 + 128], in1=C['tri'][:], op=OP.mult), r=[f"{ptk}_{b0}", 'c_tri'], w=[f"{ptk}_{b0}"])
                        _mm(P, pso[0:65, c0:512], FV[:, j, h, :], pt[:, c0:512], j == 0, j == nj - 1, ['FV'] + [f"{ptk}_{b}" for b in range(b0, 4)], [psok])

                    fqk(0)
                    for j in range(nj):
                        if j + 1 < nj:
                            fqk(j + 1)
                        frest(j)
                    P.I('dve', lambda e, pso=pso: e.reciprocal(out=RR[64:65, :], in_=pso[64:65, 0:512]), r=[psok], w=['FRR'])
                    _mm(P, PS[6][0:64, 0:512], C['onesf'][64:65, 0:64], RR[64:65, :], True, True, ['c_onesf', 'FRR'], [pk[6]])
                    P.I('act', lambda e: e.copy(out=BCS[:], in_=PS[6][0:64, 0:512]), r=[pk[6]], w=['FBCS'])
                    Y = YF[it % 2]; yk = f"YF{it % 2}"
                    P.I('dve', lambda e, pso=pso, Y=Y: e.tensor_tensor(out=Y[:], in0=pso[0:64, 0:512], in1=BCS[:], op=OP.mult), r=[psok, 'FBCS'], w=[yk])
                    P.D('sp', lambda e, Y=Y, h=h, qc=qc: e.dma_start(out=yT[256 + 64 * h:320 + 64 * h, qc * 512:(qc + 1) * 512], in_=Y[:]), r=[yk])
                    it += 1
        P.finish()
    return nc


def _prep_A(inp, l, b, j, x_b):
    g = j // 2
    own = [2 * j, 2 * j + 1]
    oth = [2 * j + 2, 2 * j + 3] if j % 2 == 0 else [2 * j - 2, 2 * j - 1]
    w_in = inp['w_in'][l]; b_in = inp['b_in'][l]
    OQ, OKV, OG, OFX, OF = 512, 1024, 1792, 1816, 3352
    r64 = np.arange(64)
    kvc = lambda n, kvi: OKV + ((n * 2 + kvi) * 2 + g) * 64 + r64
    fx = lambda q, h: OFX + (q * 8 + h) * 64 + r64
    fm1 = np.concatenate([128 * j + np.arange(128), kvc(0, 0), kvc(0, 1)])
    fm2 = np.concatenate([OQ + 64 * own[0] + r64, OQ + 64 * own[1] + r64, OQ + 64 * oth[0] + r64, OQ + 64 * oth[1] + r64,
                          kvc(1, 0), kvc(1, 0), kvc(2, 0), kvc(2, 0),
                          [OG + own[0] * 3 + 1, OG + own[0] * 3 + 2, OG + own[1] * 3 + 1, OG + own[1] * 3 + 2]]).astype(np.int64)
    tm2 = np.concatenate([kvc(1, 1), kvc(2, 1), [OG + own[0] * 3, OG + own[1] * 3]]).astype(np.int64)
    fm3 = np.concatenate([fx(0, own[0]), fx(0, own[1]), fx(1, own[0]), fx(1, own[1])])
    tm3 = np.concatenate([fx(2, own[0]), fx(2, own[1]), [OF + own[0], OF + own[1]]]).astype(np.int64)

    def fmb(cols, nch):
        bb = np.zeros((128, nch), np.float32)
        v = b_in[cols]
        for c in range(nch):
            seg = v[c * 128:(c + 1) * 128]
            bb[:len(seg), c] = seg
        return bb
    c32 = np.ascontiguousarray
    win = POOL_WINDOWS[j]
    cwv = np.zeros((128, 4), np.float32); cwv[:, j] = 1.0 / win
    fixv = np.tile((win / np.minimum(np.arange(16) + 1, win)).astype(np.float32)[None, :], (128, 1))
    cw1 = inp['cmp_w1'][l]
    cw1r = np.concatenate([cw1[kv].reshape(32, 64, 256).transpose(1, 0, 2).reshape(64, 32 * 256) for kv in range(2)], axis=0)
    posT = np.concatenate([inp['cmp_pos'][l][kv].T for kv in range(2)], axis=0)
    cb1 = inp['cmp_b1'][l].reshape(2, 2, 128).transpose(2, 0, 1).reshape(128, 4)
    w2 = inp['cmp_w2'][l]
    cw2k = np.concatenate([np.concatenate([w2[0][hf * 128:(hf + 1) * 128], w2[0][hf * 128:(hf + 1) * 128]], axis=1) for hf in range(2)], axis=1)
    cb2k = np.concatenate([inp['cmp_b2'][l][0], inp['cmp_b2'][l][0]])[:, None]
    cw2v = np.concatenate([w2[1][hf * 128:(hf + 1) * 128] for hf in range(2)], axis=1)
    cb2v = np.tile(inp['cmp_b2'][l][1][None, :], (128, 1))
    return {
        "xT": x_b,
        "wfm1": c32(w_in[:, fm1]), "bfm1": fmb(fm1, 2),
        "wfm2": c32(w_in[:, fm2]), "bfm2": fmb(fm2, 5),
        "wtm2": c32(w_in[:, tm2]), "btm2": c32(np.tile(b_in[tm2][None, :], (128, 1))),
        "wfm3": c32(w_in[:, fm3]), "bfm3": fmb(fm3, 2),
        "wtm3": c32(w_in[:, tm3]), "btm3": c32(np.tile(b_in[tm3][None, :], (128, 1))),
        "pw": c32(inp['pool_w'][l][j]), "pbs": c32(np.stack([inp['pool_b'][l][j], inp['pool_scale'][l][128 * j:128 * j + 128]], axis=1)),
        "cw": cwv, "fix": c32(fixv),
        "cw1": c32(cw1r), "posT": c32(posT), "cb1": c32(cb1), "cw2k": c32(cw2k), "cb2k": c32(cb2k.astype(np.float32)),
        "cw2v": c32(cw2v), "cb2v": c32(cb2v),
    }


_NC = {}


def run_A(inp, l, x):
    if 'A' not in _NC:
        _NC['A'] = build_A()
    xTs = [np.ascontiguousarray(x[b].T) for b in range(2)]
    maps = [_prep_A(inp, l, c // 4, c % 4, xTs[c // 4]) for c in range(8)]
    res = run_bass_kernel_spmd(_NC['A'], maps, core_ids=list(range(8)))
    ys = np.empty((2, T, 1536), np.float32)
    for c in range(8):
        b, j = c // 4, c % 4
        yt = res.results[c]["yT"]
        for n in range(3):
            ys[b, :, n * 512 + 128 * j: n * 512 + 128 * j + 128] = yt[n * 128:(n + 1) * 128, :].T
    return ys


NTB = 2048
NE = 32
CAP = 384
U32 = mybir.dt.uint32


def _layernorm(P, nc, R, rk, g_t, b_t, out, outk, ST, MV, tag):
    for c in range(2):
        P.I('dve', lambda e, c=c: e.bn_stats(out=ST[:, c, :], in_=R[:, c * 512:(c + 1) * 512]), r=[rk], w=['ST' + tag])
    P.I('dve', lambda e: e.bn_aggr(out=MV[:, 0:2], in_=ST[:].rearrange("p c s -> p (c s)")), r=['ST' + tag], w=['MV' + tag])
    P.I('dve', lambda e: e.tensor_scalar(out=MV[:, 2:3], in0=MV[:, 1:2], scalar1=LN_EPS, scalar2=None, op0=OP.add), r=['MV' + tag], w=['MV' + tag])
    P.I('act', lambda e: e.activation(out=MV[:, 2:3], in_=MV[:, 2:3], func=AF.Sqrt), r=['MV' + tag], w=['MV' + tag])
    P.I('dve', lambda e: e.reciprocal(out=MV[:, 2:3], in_=MV[:, 2:3]), r=['MV' + tag], w=['MV' + tag])
    P.I('dve', lambda e: e.tensor_scalar(out=out, in0=R[:], scalar1=MV[:, 0:1], scalar2=MV[:, 2:3], op0=OP.subtract, op1=OP.mult), r=[rk, 'MV' + tag], w=[outk])
    P.I('dve', lambda e: e.tensor_tensor(out=out, in0=out, in1=g_t[:], op=OP.mult), r=[outk, 'lng' + tag], w=[outk])
    P.I('dve', lambda e: e.tensor_tensor(out=out, in0=out, in1=b_t[:], op=OP.add), r=[outk, 'lnb' + tag], w=[outk])


def build_B():
    nc = bass.Bass("TRN2", target_bir_lowering=False)
    dt = lambda n, s, k="ExternalInput": nc.dram_tensor(n, s, F32, kind=k).ap()
    xT = dt("xT", [D, NTB]); xtok = dt("xtok", [NTB, D]); yT = dt("yT", [1536, NTB])
    wg_d = dt("wg", [D, 3072]); bg_d = dt("bg", [128, 24]); wup_d = dt("wup", [1536, D]); wo_d = dt("wo", [D, D])
    l1g_d = dt("l1g", [128, D]); l1b_d = dt("l1b", [128, D]); l2g_d = dt("l2g", [128, D]); l2b_d = dt("l2b", [128, D])
    rw_d = dt("rw", [D, NE]); rb_d = dt("rb", [128, NE])
    w1_d = dt("w1", [NE, D, 2048]); b1_d = dt("b1", [128, NE * 16]); w2_d = dt("w2", [NE, D, D]); b2_d = dt("b2", [NE, D])
    xo = dt("xo", [NTB, D], "ExternalOutput")
    x1s = dt("x1s", [NTB, D], "Internal")
    xg_d = nc.dram_tensor("xg", [NE * CAP, D], BF16, kind="Internal").ap()
    yg_d = dt("yg", [NE * CAP, D], "Internal")
    NTT = NTB // 128

    with ExitStack() as es:
        P = Prog(nc, es)
        sbg = lambda n, s, d: es.enter_context(nc.sbuf_tensor(n, s, d))
        PA = [es.enter_context(nc.psum_tensor(f"pa{i}", [128, 512], F32)) for i in range(4)]
        pak = [f"pa{i}" for i in range(4)]
        PH = es.enter_context(nc.psum_tensor("ph", [128, 1024], F32))
        PSB = es.enter_context(nc.psum_tensor("psb", [128, 1024], BF16))
        PR = es.enter_context(nc.psum_tensor("pr", [128, 512], F32))
        C = _consts(P, nc, sbg)
        identf = sbg("identf", [128, 128], F32)
        P.I('dve', lambda e: e.tensor_copy(out=identf[:], in_=C['ident'][:]), r=['c_ident'], w=['identf'])
        DSTI = sbg("DSTI", [128, NTT, 4], U32)
        GR = sbg("GR", [128, NTT, 4], F32)
        GT = sbg("GT", [128, NTT, NE], F32)
        ST = sbg("ST", [128, 2, 6], F32); MV = sbg("MV", [128, 4], F32)

        with ExitStack() as e1:
            sb = lambda n, s, d: e1.enter_context(nc.sbuf_tensor(n, s, d))
            WG = sb("WG", [128, 8, 3072], BF16); WU = sb("WU", [128, 12, D], BF16); WO = sb("WO", [128, 8, D], BF16)
            for k in range(8):
                P.D('pool', lambda e, k=k: e.dma_start(out=WG[:, k, :], in_=wg_d[k * 128:(k + 1) * 128, :]), w=['WG'])
                P.D('pool', lambda e, k=k: e.dma_start(out=WO[:, k, :], in_=wo_d[k * 128:(k + 1) * 128, :]), w=['WO'])
            for k in range(12):
                P.D('pool', lambda e, k=k: e.dma_start(out=WU[:, k, :], in_=wup_d[k * 128:(k + 1) * 128, :]), w=['WU'])
            bg = sb("bg_s", [128, 24], F32); l1g = sb("l1g_s", [128, D], F32); l1b = sb("l1b_s", [128, D], F32)
            RW = sb("RW", [128, 8, NE], F32); rb = sb("rb_s", [128, NE], F32)
            P.D('sp', lambda e: e.dma_start(out=bg[:], in_=bg_d), w=['bg'])
            P.D('sp', lambda e: e.dma_start(out=l1g[:], in_=l1g_d), w=['lng1'])
            P.D('sp', lambda e: e.dma_start(out=l1b[:], in_=l1b_d), w=['lnb1'])
            P.D('sp', lambda e: e.dma_start(out=RW[:], in_=rw_d.rearrange("(k p) n -> p k n", p=128)), w=['RW'])
            P.D('sp', lambda e: e.dma_start(out=rb[:], in_=rb_d), w=['rb'])
            XB = [sb(f"XB{i}", [128, 8, 512], BF16) for i in range(2)]
            YB = [sb(f"YB{i}", [128, 12, 512], BF16) for i in range(1)]
            GS = sb("GS", [128, 512], F32); TM = sb("TM", [128, 512], F32); MA = sb("MA", [128, 512], F32)
            MT = sb("MT", [128, 8, 512], BF16)
            XT_ = [sb(f"XTK{i}", [128, D], F32) for i in range(2)]
            RR_ = sb("RRb", [128, D], F32); X1 = sb("X1", [128, D], F32); X1B = sb("X1B", [128, D], BF16)
            X1T32 = sb("X1T32", [128, 8, 128], F32); LG = sb("LG", [128, NE], F32); M8 = sb("M8b", [128, 8], F32)
            EXg = sb("EXg", [128, NE], F32); MK = sb("MK", [128, NE], F32); SM = sb("SM", [128, 2], F32)
            TRIS = sb("TRIS", [128, 128], BF16); ONESB = sb("ONESB", [128, 128], BF16); RUNE = sb("RUNE", [128, NE], F32)
            P.I('dve', lambda e: e.tensor_scalar(out=TRIS[:], in0=C['trif'][:], scalar1=0.0, scalar2=None, op0=OP.add), r=['c_trif'], w=['TRIS'])
            P.I('dve', lambda e: e.tensor_tensor(out=TRIS[:], in0=TRIS[:], in1=C['ident'][:], op=OP.subtract), r=['TRIS', 'c_ident'], w=['TRIS'])
            P.I('dve', lambda e: e.memset(ONESB[:], 1.0), w=['ONESB'])
            P.I('pool', lambda e: e.iota(RUNE[:], pattern=[[CAP, NE]], base=0, channel_multiplier=0, allow_small_or_imprecise_dtypes=True), w=['RUNE'])
            MKB = sb("MKB", [128, NE], BF16); DESTF = sb("DESTF", [128, NE], F32); TMPR = sb("TMPR", [128, NE], F32); DSTF = sb("DSTF", [128, 4], F32)
            X1Bs = [X1B, sb("X1B2", [128, D], BF16)]
            ZR = sb("ZR", [128, D], BF16)
            P.I('dve', lambda e: e.memset(ZR[:], 0.0), w=['ZR'])
            for zb in range(NE * CAP // 128):
                P.D('sp', lambda e, zb=zb: e.dma_start(out=xg_d[zb * 128:(zb + 1) * 128, :], in_=ZR[:]), r=['ZR'], w=['xg'])
            xv = xT.rearrange("(k p) t -> p k t", p=128)
            yv = yT.rearrange("(k p) t -> p k t", p=128)
            pi = 0
            for tc in range(NTB // 512):
                X = XB[tc % 2]; Y = YB[0]; xk = f"XB{tc % 2}"; yk = "YB0"
                for k2 in range(2):
                    P.D('pool', lambda e, X=X, tc=tc, k2=k2: e.dma_start(out=X[:, 4 * k2:4 * k2 + 4, :], in_=xv[:, 4 * k2:4 * k2 + 4, tc * 512:(tc + 1) * 512]), w=[xk])
                for k3 in range(3):
                    P.D('pool', lambda e, Y=Y, tc=tc, k3=k3: e.dma_start(out=Y[:, 4 * k3:4 * k3 + 4, :], in_=yv[:, 4 * k3:4 * k3 + 4, tc * 512:(tc + 1) * 512]), w=[yk])
                for dc in range(8):
                    for n in range(3):
                        pg, pgk = PA[pi % 4], pak[pi % 4]; pi += 1
                        pu, puk = PA[pi % 4], pak[pi % 4]; pi += 1
                        col = n * 1024 + dc * 128
                        for k in range(8):
                            _mm(P, pg[:, 0:512], WG[:, k, col:col + 128], X[:, k, :], k == 0, k == 7, ['WG', xk], [pgk])
                        for k in range(4):
                            _mm(P, pu[:, 0:512], WU[:, n * 4 + k, dc * 128:(dc + 1) * 128], Y[:, n * 4 + k, :], k == 0, k == 3, ['WU', yk], [puk])
                        bc_ = n * 8 + dc
                        P.I('act', lambda e, pg=pg, bc_=bc_: e.activation(out=GS[:], in_=pg[:, 0:512], func=AF.Sigmoid, bias=bg[:, bc_:bc_ + 1], scale=1.0), r=[pgk, 'bg'], w=['GS'])
                        if n == 0:
                            P.I('dve', lambda e, pu=pu: e.tensor_tensor(out=MA[:], in0=pu[:, 0:512], in1=GS[:], op=OP.mult), r=[puk, 'GS'], w=['MA'])
                        else:
                            P.I('dve', lambda e, pu=pu: e.tensor_tensor(out=TM[:], in0=pu[:, 0:512], in1=GS[:], op=OP.mult), r=[puk, 'GS'], w=['TM'])
                            if n == 1:
                                P.I('dve', lambda e: e.tensor_tensor(out=MA[:], in0=MA[:], in1=TM[:], op=OP.add), r=['MA', 'TM'], w=['MA'])
                            else:
                                P.I('dve', lambda e, dc=dc: e.tensor_tensor(out=MT[:, dc, :], in0=MA[:], in1=TM[:], op=OP.add), r=['MA', 'TM'], w=['MT'])
                for tt in range(4):
                    ti = tc * 4 + tt
                    xt = XT_[ti % 2]; xtk = f"XTK{ti % 2}"
                    P.D('sp', lambda e, xt=xt, ti=ti: e.dma_start(out=xt[:], in_=xtok[ti * 128:(ti + 1) * 128, :]), w=[xtk])
                    for hf in range(2):
                        for k in range(8):
                            _mm(P, PH[:, hf * 512:(hf + 1) * 512], MT[:, k, tt * 128:(tt + 1) * 128], WO[:, k, hf * 512:(hf + 1) * 512], k == 0, k == 7, ['MT', 'WO'], ['ph'])
                    P.I('dve', lambda e, xt=xt: e.scalar_tensor_tensor(out=RR_[:], in0=xt[:], scalar=ALPHA, in1=PH[:, :], op0=OP.mult, op1=OP.add), r=[xtk, 'ph'], w=['RRb'])
                    _layernorm(P, nc, RR_, 'RRb', l1g, l1b, X1[:], 'X1', ST, MV, '1')
                    P.D('sp', lambda e, ti=ti: e.dma_start(out=x1s[ti * 128:(ti + 1) * 128, :], in_=X1[:]), r=['X1'], w=['x1s'])
                    x1b = X1Bs[ti % 2]; x1bk = f"X1B{ti % 2}"
                    P.I('act', lambda e, x1b=x1b: e.copy(out=x1b[:], in_=X1[:]), r=['X1'], w=[x1bk])
                    for k in range(8):
                        P.I('pe', lambda e, k=k: e.transpose(out=PH[:, k * 128:(k + 1) * 128], in_=X1[:, k * 128:(k + 1) * 128], identity=identf[:]), r=['X1', 'identf'], w=['ph'])
                    P.I('act', lambda e: e.copy(out=X1T32[:].rearrange("p k t -> p (k t)"), in_=PH[:, :]), r=['ph'], w=['X1T32'])
                    for k in range(8):
                        _mm(P, PR[:, 0:NE], X1T32[:, k, :], RW[:, k, :], k == 0, k == 7, ['X1T32', 'RW'], ['pr'])
                    P.I('dve', lambda e: e.tensor_tensor(out=LG[:], in0=PR[:, 0:NE], in1=rb[:], op=OP.add), r=['pr', 'rb'], w=['LG'])
                    P.I('dve', lambda e: e.max(out=M8[:], in_=LG[:]), r=['LG'], w=['M8b'])
                    P.I('dve', lambda e: e.tensor_scalar(out=SM[:, 0:1], in0=M8[:, 0:1], scalar1=-1.0, scalar2=None, op0=OP.mult), r=['M8b'], w=['SM'])
                    P.I('act', lambda e: e.activation(out=EXg[:], in_=LG[:], func=AF.Exp, bias=SM[:, 0:1], scale=1.0), r=['LG', 'SM'], w=['EXg'])
                    P.I('dve', lambda e: e.scalar_tensor_tensor(out=MK[:], in0=LG[:], scalar=M8[:, 3:4], in1=EXg[:], op0=OP.is_ge, op1=OP.mult), r=['LG', 'M8b', 'EXg'], w=['MK'])
                    P.I('dve', lambda e: e.reduce_sum(out=SM[:, 1:2], in_=MK[:], axis=AX.X), r=['MK'], w=['SM'])
                    P.I('dve', lambda e: e.reciprocal(out=SM[:, 1:2], in_=SM[:, 1:2]), r=['SM'], w=['SM'])
                    P.I('dve', lambda e, ti=ti: e.tensor_scalar(out=GT[:, ti, :], in0=MK[:], scalar1=SM[:, 1:2], scalar2=None, op0=OP.mult), r=['MK', 'SM'], w=['GT'])
                    P.I('dve', lambda e: e.tensor_scalar(out=MKB[:], in0=LG[:], scalar1=M8[:, 3:4], scalar2=None, op0=OP.is_ge), r=['LG', 'M8b'], w=['MKB'])
                    _mm(P, PR[:, 32:64], TRIS[:], MKB[:], True, True, ['TRIS', 'MKB'], ['pr'])
                    _mm(P, PR[:, 64:96], ONESB[:], MKB[:], True, True, ['ONESB', 'MKB'], ['pr'])
                    P.I('dve', lambda e: e.tensor_tensor(out=DESTF[:], in0=PR[:, 32:64], in1=RUNE[:], op=OP.add), r=['pr', 'RUNE'], w=['DESTF'])
                    P.I('dve', lambda e: e.tensor_tensor(out=RUNE[:], in0=PR[:, 64:96], in1=RUNE[:], op=OP.add), r=['pr', 'RUNE'], w=['RUNE'])
                    for r_ in range(4):
                        P.I('dve', lambda e, r_=r_: e.scalar_tensor_tensor(out=TMPR[:], in0=LG[:], scalar=M8[:, r_:r_ + 1], in1=DESTF[:], op0=OP.is_equal, op1=OP.mult), r=['LG', 'M8b', 'DESTF'], w=['TMPR'])
                        P.I('dve', lambda e, r_=r_: e.reduce_sum(out=DSTF[:, r_:r_ + 1], in_=TMPR[:], axis=AX.X), r=['TMPR'], w=['DSTF'])
                        P.I('dve', lambda e, r_=r_, ti=ti: e.scalar_tensor_tensor(out=TMPR[:], in0=LG[:], scalar=M8[:, r_:r_ + 1], in1=GT[:, ti, :], op0=OP.is_equal, op1=OP.mult), r=['LG', 'M8b', 'GT'], w=['TMPR'])
                        P.I('dve', lambda e, r_=r_, ti=ti: e.reduce_sum(out=GR[:, ti, r_:r_ + 1], in_=TMPR[:], axis=AX.X), r=['TMPR'], w=['GR'])
                    P.I('dve', lambda e, ti=ti: e.tensor_copy(out=DSTI[:, ti, :], in_=DSTF[:]), r=['DSTF'], w=['DSTI'])
                    for r_ in range(4):
                        P.D('pool', lambda e, r_=r_, ti=ti, x1b=x1b: e.indirect_dma_start(out=xg_d, out_offset=bass.IndirectOffsetOnAxis(ap=DSTI[:, ti, r_:r_ + 1], axis=0), in_=x1b[:], in_offset=None),
                            r=[x1bk, 'DSTI'], w=['xg'])
            P.barrier()

        NCT = CAP // 128
        with ExitStack() as e2:
            sb = lambda n, s, d: e2.enter_context(nc.sbuf_tensor(n, s, d))
            W1 = [sb(f"W1_{i}", [128, 8, 2048], BF16) for i in range(2)]
            W2 = [sb(f"W2_{i}", [128, 8, D], BF16) for i in range(1)]
            B1 = sb("B1", [128, NE * 16], F32)
            P.D('sp', lambda e: e.dma_start(out=B1[:], in_=b1_d), w=['B1'])
            ACC = sb("ACC", [128, NTT, D], F32)
            B2 = sb("B2", [NE, D], F32); GTT = sb("GTT", [NE, 128], F32)
            P.D('sp', lambda e: e.dma_start(out=B2[:], in_=b2_d), w=['B2'])
            XG = [sb(f"XG{i}", [128, NCT, D], BF16) for i in range(2)]
            XGT = [sb(f"XGT{i}", [128, 8, CAP], BF16) for i in range(2)]
            AT = sb("AT", [128, 8, CAP], BF16)
            GEN = [sb(f"GEN{i}", [128, D], F32) for i in range(3)]
            YRS = [sb(f"YRS{i}", [128, D], F32) for i in range(2)]
            GG = GEN[0][:, 0:CAP]; SG = GEN[0][:, 512:512 + CAP]; UU = GEN[1][:, 0:CAP]
            YO = [GEN[1], GEN[2]]

            def load_w(ex):
                s_ = ex % 2
                P.D('pool', lambda e: e.dma_start(out=W1[s_][:], in_=w1_d[ex].rearrange("(k p) n -> p k n", p=128)), w=[f'W1_{s_}'])

            def load_w2(ex):
                P.D('pool', lambda e: e.dma_start(out=W2[0][:], in_=w2_d[ex].rearrange("(k p) n -> p k n", p=128)), w=['W2_0'])

            def load_x(ex):
                s_ = ex % 2
                P.D('sp', lambda e: e.dma_start(out=XG[s_][:], in_=xg_d[ex * CAP:(ex + 1) * CAP, :].rearrange("(t p) d -> p t d", p=128)), r=['xg'], w=[f'XG{s_}'])

            load_w(0)
            load_w2(0)
            load_x(0)
            for ti in range(NTT):
                P.D('sp', lambda e, ti=ti: e.dma_start(out=ACC[:, ti, :], in_=x1s[ti * 128:(ti + 1) * 128, :]), w=[f'ACC{ti}'])
                P.I('act', lambda e, ti=ti: e.activation(out=ACC[:, ti, :], in_=ACC[:, ti, :], func=AF.Copy, scale=ALPHA), r=[f'ACC{ti}'], w=[f'ACC{ti}'])
                P.I('pe', lambda e, ti=ti: e.transpose(out=PR[0:NE, 128:256], in_=GT[:, ti, :], identity=identf[:]), r=['GT', 'identf'], w=['pr'])
                P.I('act', lambda e: e.copy(out=GTT[:], in_=PR[0:NE, 128:256]), r=['pr'], w=['GTT'])
                for hf in range(2):
                    _mm(P, PH[:, hf * 512:(hf + 1) * 512], GTT[:], B2[:, hf * 512:(hf + 1) * 512], True, True, ['GTT', 'B2'], ['ph'])
                P.I('dve', lambda e, ti=ti: e.tensor_tensor(out=ACC[:, ti, :], in0=ACC[:, ti, :], in1=PH[:, :], op=OP.add), r=[f'ACC{ti}', 'ph'], w=[f'ACC{ti}'])
            pi = 0
            yoc = 0
            for ex in range(NE):
                if ex + 1 < NE:
                    load_w(ex + 1)
                    load_x(ex + 1)
                s_ = ex % 2
                w1k, w2k, xgk, xgtk = f'W1_{s_}', 'W2_0', f'XG{s_}', f'XGT{s_}'
                for tt in range(NCT):
                    for k in range(8):
                        P.I('pe', lambda e, k=k, tt=tt: e.transpose(out=PSB[:, k * 128:(k + 1) * 128], in_=XG[s_][:, tt, k * 128:(k + 1) * 128], identity=C['ident'][:]), r=[xgk, 'c_ident'], w=['psb'])
                    P.I('act', lambda e, tt=tt: e.copy(out=XGT[s_][:, :, tt * 128:(tt + 1) * 128], in_=PSB[:, :].rearrange("p (k t) -> p k t", t=128)), r=['psb'], w=[xgtk])
                for f in range(8):
                    pg, pgk = PA[pi % 4], pak[pi % 4]; pi += 1
                    pu, puk = PA[pi % 4], pak[pi % 4]; pi += 1
                    for k in range(8):
                        _mm(P, pg[:, 0:CAP], W1[s_][:, k, f * 128:(f + 1) * 128], XGT[s_][:, k, :], k == 0, k == 7, [w1k, xgtk], [pgk])
                    for k in range(8):
                        _mm(P, pu[:, 0:CAP], W1[s_][:, k, 1024 + f * 128:1024 + (f + 1) * 128], XGT[s_][:, k, :], k == 0, k == 7, [w1k, xgtk], [puk])
                    cg = ex * 16 + f; cu = ex * 16 + 8 + f
                    P.I('dve', lambda e, pg=pg, cg=cg: e.tensor_scalar(out=GG, in0=pg[:, 0:CAP], scalar1=B1[:, cg:cg + 1], scalar2=7.0, op0=OP.add, op1=OP.min), r=[pgk, 'B1'], w=['GEN0'])
                    P.I('act', lambda e: e.activation(out=SG, in_=GG, func=AF.Sigmoid, scale=1.702), r=['GEN0'], w=['GEN0'])
                    P.I('dve', lambda e, pu=pu, cu=cu: e.tensor_scalar(out=UU, in0=pu[:, 0:CAP], scalar1=B1[:, cu:cu + 1], scalar2=7.0, op0=OP.add, op1=OP.min), r=[puk, 'B1'], w=['GEN1'])
                    P.I('pool', lambda e: e.tensor_scalar(out=UU, in0=UU, scalar1=-7.0, scalar2=1.0, op0=OP.max, op1=OP.add), r=['GEN1'], w=['GEN1'])
                    P.I('pool', lambda e: e.tensor_tensor(out=GG, in0=GG, in1=UU, op=OP.mult), r=['GEN0', 'GEN1'], w=['GEN0'])
                    P.I('dve', lambda e, f=f: e.tensor_tensor(out=AT[:, f, :], in0=GG, in1=SG, op=OP.mult), r=['GEN0'], w=['AT'])
                for tt in range(NCT):
                    for hf in range(2):
                        for f in range(8):
                            _mm(P, PH[:, hf * 512:(hf + 1) * 512], AT[:, f, tt * 128:(tt + 1) * 128], W2[0][:, f, hf * 512:(hf + 1) * 512], f == 0, f == 7, ['AT', w2k], ['ph'])
                    yo = GEN[2]; yok = 'GEN2'
                    P.I('act', lambda e, yo=yo: e.copy(out=yo[:], in_=PH[:, :]), r=['ph'], w=[yok])
                    row = ex * CAP + tt * 128
                    P.D('sp', lambda e, yo=yo, row=row: e.dma_start(out=yg_d[row:row + 128, :], in_=yo[:]), r=[yok], w=['yg'])
                if ex + 1 < NE:
                    load_w2(ex + 1)
            P.barrier()
            l2g = GEN[0]; l2b = GEN[1]
            P.D('sp', lambda e: e.dma_start(out=l2g[:], in_=l2g_d), w=['lng2'])
            P.D('sp', lambda e: e.dma_start(out=l2b[:], in_=l2b_d), w=['lnb2'])
            gi = 0
            for ti in range(NTT):
                for r_ in range(4):
                    yr = YRS[gi % 2]; yrk = f"YRS{gi % 2}"; gi += 1
                    P.D('pool', lambda e, yr=yr, ti=ti, r_=r_: e.indirect_dma_start(out=yr[:], out_offset=None, in_=yg_d, in_offset=bass.IndirectOffsetOnAxis(ap=DSTI[:, ti, r_:r_ + 1], axis=0)),
                        r=['yg', 'DSTI'], w=[yrk])
                    P.I('dve', lambda e, yr=yr, ti=ti, r_=r_: e.scalar_tensor_tensor(out=ACC[:, ti, :], in0=yr[:], scalar=GR[:, ti, r_:r_ + 1], in1=ACC[:, ti, :], op0=OP.mult, op1=OP.add),
                        r=[yrk, 'GR', f'ACC{ti}'], w=[f'ACC{ti}'])
                o = GEN[2]; ok = "GEN2"
                _layernorm(P, nc, ACC[:, ti, :], f'ACC{ti}', l2g, l2b, o[:], ok, ST, MV, '2')
                P.D('sp', lambda e, o=o, ti=ti: e.dma_start(out=xo[ti * 128:(ti + 1) * 128, :], in_=o[:]), r=[ok])
        P.finish()
    return nc


def _prep_B(inp, l, b, r, x, ys):
    tok = slice(NTB * r, NTB * (r + 1))
    c32 = lambda a: np.ascontiguousarray(a, dtype=np.float32)
    w_in = inp['w_in'][l]; b_in = inp['b_in'][l]
    bc = lambda v: c32(np.tile(v[None, :], (128, 1)))
    return {
        "xT": c32(x[b, tok].T), "xtok": c32(x[b, tok]), "yT": c32(ys[b, tok].T),
        "wg": c32(w_in[:, 3360:6432]), "bg": c32(b_in[3360:6432].reshape(24, 128).T),
        "wup": c32(inp['w_up'][l].reshape(1536, D)), "wo": c32(inp['w_o'][l]),
        "l1g": bc(inp['ln1_g'][l]), "l1b": bc(inp['ln1_b'][l]), "l2g": bc(inp['ln2_g'][l]), "l2b": bc(inp['ln2_b'][l]),
        "rw": c32(inp['router_w'][l]), "rb": bc(inp['router_b'][l]),
        "w1": inp['moe_w1'][l], "b1": c32(inp['moe_b1'][l].reshape(NE * 16, 128).T), "w2": inp['moe_w2'][l], "b2": c32(inp['moe_b2'][l]),
    }


def run_B(inp, l, x, ys):
    if 'B' not in _NC:
        _NC['B'] = build_B()
    maps = [_prep_B(inp, l, c // 4, c % 4, x, ys) for c in range(8)]
    res = run_bass_kernel_spmd(_NC['B'], maps, core_ids=list(range(8)))
    out = np.empty((2, T, D), np.float32)
    for c in range(8):
        out[c // 4, NTB * (c % 4):NTB * (c % 4 + 1)] = res.results[c]["xo"]
    return out


def kernel(**inputs):
    inp = {k: np.asarray(v) for k, v in inputs.items()}
    x = np.ascontiguousarray(inp['x'], dtype=np.float32)
    for l in range(2):
        ys = run_A(inp, l, x)
        x = run_B(inp, l, x, ys)
    return x
```
